# Optimizing a Trainium2 kernel written in Bass

```python
import math
import jax, jax.numpy as jnp
from jax import lax
import numpy as np

D_MODEL = 2048
BATCH = 16
SEQ = 256
DEPTH = 2
DEC_BATCH = 8
DEC_SEQ = 2048
PAST_LEN = 512

GRID_W = 64
BLK = 128
EPS = 1e-6
ROPE_BASE = 10000.0
NEG_INF = -1e30
N_HEADS_A = 16
N_KV_A = 2
HD_A = 64
WINDOW = 128
A_W = N_HEADS_A * HD_A
KV_W = N_KV_A * HD_A
N_HEADS_B = 8
Q_LORA = 512
KV_LORA = 256
QK_NOPE = 128
QK_ROPE = 64
V_HD = 128
B_W = N_HEADS_B * V_HD
POOL_WINDOWS = (2, 4, 8, 16)
POOL_GROUPS = 4
POOL_GW = 256
C_W = POOL_GROUPS * POOL_GW
N_BRANCH = 3
OFF_K = A_W
OFF_V = OFF_K + KV_W
OFF_CQ = OFF_V + KV_W
OFF_CKV = OFF_CQ + Q_LORA
OFF_KR = OFF_CKV + KV_LORA
OFF_POOL = OFF_KR + QK_ROPE
OFF_GATE = OFF_POOL + C_W
IN_COLS = OFF_GATE + N_BRANCH * D_MODEL
SPLITS = (OFF_K, OFF_V, OFF_CQ, OFF_CKV, OFF_KR, OFF_POOL, OFF_GATE)
D_FF = 5632
N_EXPERTS = 8
TOP_K = 2
D_FF_E = 5632

kernel_name = "hybrid_diffusion_prefix_trunk_step"


def rmsnorm(x, g):
    xf = x.astype(jnp.float32)
    y = xf * lax.rsqrt(jnp.mean(xf * xf, axis=-1, keepdims=True) + EPS)
    return (y * g.astype(jnp.float32)).astype(x.dtype)


def adaln(cond, w, b):
    m = jnp.dot(jax.nn.silu(cond), w) + b
    return jnp.split(m[:, None, :], 6, axis=-1)


def modulate(h, shift, scale):
    return h * (1 + scale) + shift


def axial_angles(rows, dim):
    quarter = dim // 4
    inv = ROPE_BASE ** (-jnp.arange(quarter, dtype=jnp.float32) / quarter)
    r = jnp.repeat(jnp.arange(rows, dtype=jnp.float32), GRID_W)
    col = jnp.tile(jnp.arange(GRID_W, dtype=jnp.float32), rows)
    return r[:, None] * inv, col[:, None] * inv


def _rot_half(x, ang):
    x1, x2 = jnp.split(x, 2, axis=-1)
    cos = jnp.cos(ang)[:, None, :]
    sin = jnp.sin(ang)[:, None, :]
    return jnp.concatenate([x1 * cos - x2 * sin, x1 * sin + x2 * cos], axis=-1)


def rope2d(x, ang):
    ang_r, ang_c = ang
    xf = x.astype(jnp.float32)
    h = x.shape[-1] // 2
    y = jnp.concatenate([_rot_half(xf[..., :h], ang_r), _rot_half(xf[..., h:], ang_c)], axis=-1)
    return y.astype(x.dtype)


def ctx_attn(q, k, v, sink):
    B_, n, H_, d_ = q.shape
    G_ = k.shape[2]
    R_ = H_ // G_
    nb = n // BLK
    qb = jnp.moveaxis(q.reshape(B_, nb, BLK, G_, R_, d_), 1, 0)
    sk = sink.astype(jnp.float32).reshape(1, G_, R_, 1, 1)
    scale = d_ ** -0.5
    vf = v.astype(jnp.float32)

    def blk(qi):
        s = jnp.einsum('bqgrd,bkgd->bgrqk', qi, k, preferred_element_type=jnp.float32) * scale
        m = jnp.maximum(s.max(-1, keepdims=True), sk)
        p = jnp.exp(s - m)
        denom = p.sum(-1, keepdims=True) + jnp.exp(sk - m)
        return jnp.einsum('bgrqk,bkgd->bqgrd', p / denom, vf).astype(q.dtype)

    o = lax.map(blk, qb)
    return jnp.moveaxis(o, 0, 1).reshape(B_, n, H_ * d_)


def _band_blocks(t, nb):
    B_, N_, G_, d_ = t.shape
    tp = jnp.pad(t, ((0, 0), (BLK, BLK), (0, 0), (0, 0))).reshape(B_, nb + 2, BLK, G_, d_)
    band = jnp.concatenate([tp[:, :-2], tp[:, 1:-1], tp[:, 2:]], axis=2)
    return jnp.moveaxis(band, 1, 0)


def local_ctx_attn(q, k, v, kc, vc, sink):
    B_, N_, H_, d_ = q.shape
    G_ = k.shape[2]
    R_ = H_ // G_
    nb = N_ // BLK
    qb = jnp.moveaxis(q.reshape(B_, nb, BLK, G_, R_, d_), 1, 0)
    kb = _band_blocks(k, nb)
    vb = _band_blocks(v, nb)
    qpos = jnp.arange(N_).reshape(nb, BLK)
    kpos = (jnp.arange(nb)[:, None] - 1) * BLK + jnp.arange(3 * BLK)[None, :]
    valid = ((kpos[:, None, :] >= 0) & (kpos[:, None, :] < N_)
             & (jnp.abs(qpos[:, :, None] - kpos[:, None, :]) <= WINDOW))
    sk = sink.astype(jnp.float32).reshape(1, G_, R_, 1, 1)
    scale = d_ ** -0.5
    vcf = vc.astype(jnp.float32)

    def blk(args):
        qi, ki, vi, mi = args
        s_loc = jnp.einsum('bqgrd,bkgd->bgrqk', qi, ki, preferred_element_type=jnp.float32) * scale
        s_loc = jnp.where(mi[None, None, None], s_loc, NEG_INF)
        s_ctx = jnp.einsum('bqgrd,bkgd->bgrqk', qi, kc, preferred_element_type=jnp.float32) * scale
        m = jnp.maximum(jnp.maximum(s_loc.max(-1, keepdims=True), s_ctx.max(-1, keepdims=True)), sk)
        p_loc = jnp.exp(s_loc - m)
        p_ctx = jnp.exp(s_ctx - m)
        denom = p_loc.sum(-1, keepdims=True) + p_ctx.sum(-1, keepdims=True) + jnp.exp(sk - m)
        o = (jnp.einsum('bgrqk,bkgd->bqgrd', p_loc / denom, vi.astype(jnp.float32))
             + jnp.einsum('bgrqk,bkgd->bqgrd', p_ctx / denom, vcf))
        return o.astype(q.dtype)

    o = lax.map(blk, (qb, kb, vb, valid))
    return jnp.moveaxis(o, 0, 1).reshape(B_, N_, H_ * d_)


def mla_attn(q_nope, q_rope, k_nope, k_rope, v):
    B_, Nq, H_, dn = q_nope.shape
    dr = q_rope.shape[-1]
    nb = Nq // BLK
    qn = jnp.moveaxis(q_nope.reshape(B_, nb, BLK, H_, dn), 1, 0)
    qr = jnp.moveaxis(q_rope.reshape(B_, nb, BLK, H_, dr), 1, 0)
    scale = (dn + dr) ** -0.5
    vf = v.astype(jnp.float32)

    def blk(args):
        qn_i, qr_i = args
        s = (jnp.einsum('bqhd,bkhd->bhqk', qn_i, k_nope, preferred_element_type=jnp.float32)
             + jnp.einsum('bqhd,bkd->bhqk', qr_i, k_rope, preferred_element_type=jnp.float32)) * scale
        p = jax.nn.softmax(s, axis=-1)
        return jnp.einsum('bhqk,bkhd->bqhd', p, vf).astype(q_nope.dtype)

    o = lax.map(blk, (qn, qr))
    return jnp.moveaxis(o, 0, 1).reshape(B_, Nq, H_ * v.shape[-1])


def pool_mixer(u, w, scale):
    B_, n, _ = u.shape
    ug = u.reshape(B_, n, POOL_GROUPS, POOL_GW).astype(jnp.float32)
    cs = jnp.pad(jnp.cumsum(ug, axis=1), ((0, 0), (1, 0), (0, 0), (0, 0)))
    t = jnp.arange(n)
    outs = []
    for g, win in enumerate(POOL_WINDOWS):
        left = win // 2
        right = win - left - 1
        lo = jnp.maximum(t - left, 0)
        hi = jnp.minimum(t + right, n - 1) + 1
        cg = cs[:, :, g]
        cnt = (hi - lo).astype(jnp.float32)[None, :, None]
        outs.append((cg[:, hi] - cg[:, lo]) / cnt - ug[:, :, g])
    d = jnp.stack(outs, axis=2)
    y = jnp.einsum('bngc,gcd->bngd', d, w.astype(jnp.float32)).reshape(B_, n, C_W)
    return (y * scale.astype(jnp.float32)).astype(u.dtype)


def project(h, w_in):
    z = jnp.einsum('bnd,dc->bnc', h, w_in)
    return jnp.split(z, SPLITS, axis=-1)


def mla_query_latent(cq, ckv_raw, lw):
    B_, n, _ = cq.shape
    q = jnp.dot(rmsnorm(cq, lw['q_norm_g']), lw['w_uq']).reshape(B_, n, N_HEADS_B, QK_NOPE + QK_ROPE)
    ckv = rmsnorm(ckv_raw, lw['kv_norm_g'])
    return q[..., :QK_NOPE], q[..., QK_NOPE:], ckv


def mla_expand(ckv, lw):
    B_, n, _ = ckv.shape
    kv = jnp.dot(ckv, lw['w_ukv']).reshape(B_, n, N_HEADS_B, QK_NOPE + V_HD)
    return kv[..., :QK_NOPE], kv[..., QK_NOPE:]


def merge(oa, ob, oc, gates, lw):
    ga, gb, gc = jnp.split(gates, N_BRANCH, axis=-1)
    y = (jax.nn.sigmoid(ga) * jnp.dot(oa, lw['wpa'])
         + jax.nn.sigmoid(gb) * jnp.dot(ob, lw['wpb'])
         + jax.nn.sigmoid(gc) * jnp.dot(oc, lw['wpc']))
    return jnp.dot(y, lw['w_out'])


def mixer_context(h, lw):
    B_, n, _ = h.shape
    qa, ka, va, cq, ckv_raw, kr, u, gates = project(h, lw['w_in'])
    qa = qa.reshape(B_, n, N_HEADS_A, HD_A)
    ka = ka.reshape(B_, n, N_KV_A, HD_A)
    va = va.reshape(B_, n, N_KV_A, HD_A)
    oa = ctx_attn(qa, ka, va, lw['sink'])
    q_nope, q_rope, ckv = mla_query_latent(cq, ckv_raw, lw)
    k_nope, vb = mla_expand(ckv, lw)
    ob = mla_attn(q_nope, q_rope, k_nope, kr, vb)
    oc = pool_mixer(u, lw['pool_w'], lw['pool_scale'])
    return merge(oa, ob, oc, gates, lw), (ka, va, ckv, kr)


def mixer_latent(h, lw, ctx_k, ctx_v, ctx_ckv, ctx_kr, ang_a, ang_b):
    B_, n, _ = h.shape
    qa, ka, va, cq, ckv_raw, kr, u, gates = project(h, lw['w_in'])
    qa = rope2d(qa.reshape(B_, n, N_HEADS_A, HD_A), ang_a)
    ka = rope2d(ka.reshape(B_, n, N_KV_A, HD_A), ang_a)
    va = va.reshape(B_, n, N_KV_A, HD_A)
    oa = local_ctx_attn(qa, ka, va, ctx_k, ctx_v, lw['sink'])
    q_nope, q_rope, ckv = mla_query_latent(cq, ckv_raw, lw)
    q_rope = rope2d(q_rope, ang_b)
    kr = rope2d(kr[:, :, None, :], ang_b)[:, :, 0, :]
    k_nope, vb = mla_expand(jnp.concatenate([ckv, ctx_ckv.astype(ckv.dtype)], axis=1), lw)
    kr_all = jnp.concatenate([kr, ctx_kr.astype(kr.dtype)], axis=1)
    ob = mla_attn(q_nope, q_rope, k_nope, kr_all, vb)
    oc = pool_mixer(u, lw['pool_w'], lw['pool_scale'])
    return merge(oa, ob, oc, gates, lw)


def swiglu(h, wg, wu, wd):
    return jnp.dot(jax.nn.silu(jnp.dot(h, wg)) * jnp.dot(h, wu), wd)


def moe_ffn(h, router_w, wg, wu, wd):
    B_, n, D_ = h.shape
    t = h.reshape(B_ * n, D_)
    logits = jnp.dot(t, router_w, preferred_element_type=jnp.float32)
    top_v, top_i = lax.top_k(logits, TOP_K)
    top_w = jax.nn.softmax(top_v, axis=-1)
    gate = jnp.sum(jax.nn.one_hot(top_i, N_EXPERTS, dtype=jnp.float32) * top_w[..., None], axis=1)
    y = jnp.zeros(t.shape, jnp.float32)
    for e in range(N_EXPERTS):
        y = y + gate[:, e:e + 1] * swiglu(t, wg[e], wu[e], wd[e]).astype(jnp.float32)
    return y.astype(h.dtype).reshape(B_, n, D_)


def setup_inputs(seed: int = 0) -> dict:
    key = jax.random.key(seed)
    ks = iter(jax.random.split(key, 40))

    def nrm(shape, s):
        return jax.random.normal(next(ks), shape, jnp.float32) * s

    n_dense = (DEPTH + 1) // 2
    n_moe = DEPTH // 2
    return {
        'x_prompt': nrm((BATCH, SEQ, D_MODEL), 1.0),
        'x_sample': nrm((DEC_BATCH, DEC_SEQ, D_MODEL), 1.0),
        'cache_attn_k': nrm((DEC_BATCH, DEPTH, PAST_LEN, N_KV_A, HD_A), 1.0),
        'cache_attn_v': nrm((DEC_BATCH, DEPTH, PAST_LEN, N_KV_A, HD_A), 1.0),
        'cache_mla_ckv': nrm((DEC_BATCH, DEPTH, PAST_LEN, KV_LORA), 1.0),
        'cache_mla_krope': nrm((DEC_BATCH, DEPTH, PAST_LEN, QK_ROPE), 1.0),
        'c': nrm((DEC_BATCH, D_MODEL), 1.0),
        'c_ctx': nrm((D_MODEL,), 1.0),
        'ln1_g': 1.0 + nrm((DEPTH, D_MODEL), 0.02),
        'ln2_g': 1.0 + nrm((DEPTH, D_MODEL), 0.02),
        'w_ada': nrm((DEPTH, D_MODEL, 6 * D_MODEL), D_MODEL ** -0.5),
        'b_ada': nrm((DEPTH, 6 * D_MODEL), 0.01),
        'w_in': nrm((DEPTH, D_MODEL, IN_COLS), D_MODEL ** -0.5),
        'attn_sink': nrm((DEPTH, N_HEADS_A), 0.5),
        'mla_q_norm_g': 1.0 + nrm((DEPTH, Q_LORA), 0.02),
        'w_uq': nrm((DEPTH, Q_LORA, N_HEADS_B * (QK_NOPE + QK_ROPE)), Q_LORA ** -0.5),
        'mla_kv_norm_g': 1.0 + nrm((DEPTH, KV_LORA), 0.02),
        'w_ukv': nrm((DEPTH, KV_LORA, N_HEADS_B * (QK_NOPE + V_HD)), KV_LORA ** -0.5),
        'pool_w': nrm((DEPTH, POOL_GROUPS, POOL_GW, POOL_GW), POOL_GW ** -0.5),
        'pool_scale': 1.0 + nrm((DEPTH, C_W), 0.1),
        'w_branch_a': nrm((DEPTH, A_W, D_MODEL), A_W ** -0.5),
        'w_branch_b': nrm((DEPTH, B_W, D_MODEL), B_W ** -0.5),
        'w_branch_c': nrm((DEPTH, C_W, D_MODEL), C_W ** -0.5),
        'w_out': nrm((DEPTH, D_MODEL, D_MODEL), D_MODEL ** -0.5),
        'ffn_w_gate': nrm((n_dense, D_MODEL, D_FF), D_MODEL ** -0.5),
        'ffn_w_up': nrm((n_dense, D_MODEL, D_FF), D_MODEL ** -0.5),
        'ffn_w_down': nrm((n_dense, D_FF, D_MODEL), D_FF ** -0.5),
        'router_w': nrm((n_moe, D_MODEL, N_EXPERTS), D_MODEL ** -0.5),
        'moe_w_gate': nrm((n_moe, N_EXPERTS, D_MODEL, D_FF_E), D_MODEL ** -0.5),
        'moe_w_up': nrm((n_moe, N_EXPERTS, D_MODEL, D_FF_E), D_MODEL ** -0.5),
        'moe_w_down': nrm((n_moe, N_EXPERTS, D_FF_E, D_MODEL), D_FF_E ** -0.5),
        'final_g': 1.0 + nrm((D_MODEL,), 0.02),
    }


def reference(x_prompt, x_sample, cache_attn_k, cache_attn_v, cache_mla_ckv, cache_mla_krope,
              c, c_ctx, ln1_g, ln2_g, w_ada, b_ada, w_in, attn_sink, mla_q_norm_g, w_uq,
              mla_kv_norm_g, w_ukv, pool_w, pool_scale, w_branch_a, w_branch_b, w_branch_c,
              w_out, ffn_w_gate, ffn_w_up, ffn_w_down, router_w, moe_w_gate, moe_w_up,
              moe_w_down, final_g):
    n_lat = x_sample.shape[1]
    rows = n_lat // GRID_W
    ang_a = axial_angles(rows, HD_A)
    ang_b = axial_angles(rows, QK_ROPE)
    xp, xs = x_prompt, x_sample
    st_k, st_v, st_ckv, st_kr = [], [], [], []
    for l in range(DEPTH):
        lw = {'w_in': w_in[l], 'sink': attn_sink[l], 'q_norm_g': mla_q_norm_g[l], 'w_uq': w_uq[l],
              'kv_norm_g': mla_kv_norm_g[l], 'w_ukv': w_ukv[l], 'pool_w': pool_w[l],
              'pool_scale': pool_scale[l], 'wpa': w_branch_a[l], 'wpb': w_branch_b[l],
              'wpc': w_branch_c[l], 'w_out': w_out[l]}
        sh1p, sc1p, g1p, sh2p, sc2p, g2p = adaln(c_ctx[None, :], w_ada[l], b_ada[l])
        sh1s, sc1s, g1s, sh2s, sc2s, g2s = adaln(c, w_ada[l], b_ada[l])
        hp = modulate(rmsnorm(xp, ln1_g[l]), sh1p, sc1p)
        op, (ka, va, ckv, kr) = mixer_context(hp, lw)
        xp = xp + g1p * op
        hs = modulate(rmsnorm(xs, ln1_g[l]), sh1s, sc1s)
        osm = mixer_latent(hs, lw, cache_attn_k[:, l], cache_attn_v[:, l],
                           cache_mla_ckv[:, l], cache_mla_krope[:, l], ang_a, ang_b)
        xs = xs + g1s * osm
        st_k.append(ka)
        st_v.append(va)
        st_ckv.append(ckv)
        st_kr.append(kr)
        hp = modulate(rmsnorm(xp, ln2_g[l]), sh2p, sc2p)
        hs = modulate(rmsnorm(xs, ln2_g[l]), sh2s, sc2s)
        i = l // 2
        if l % 2 == 0:
            fp = swiglu(hp, ffn_w_gate[i], ffn_w_up[i], ffn_w_down[i])
            fs = swiglu(hs, ffn_w_gate[i], ffn_w_up[i], ffn_w_down[i])
        else:
            fp = moe_ffn(hp, router_w[i], moe_w_gate[i], moe_w_up[i], moe_w_down[i])
            fs = moe_ffn(hs, router_w[i], moe_w_gate[i], moe_w_up[i], moe_w_down[i])
        xp = xp + g2p * fp
        xs = xs + g2s * fs
    y_prompt = rmsnorm(xp, final_g)
    y_sample = rmsnorm(xs, final_g)
    state_attn_k = jnp.stack(st_k, axis=1)
    state_attn_v = jnp.stack(st_v, axis=1)
    state_mla_ckv = jnp.stack(st_ckv, axis=1)
    state_mla_krope = jnp.stack(st_kr, axis=1)
    return (y_prompt, y_sample, state_attn_k, state_attn_v, state_mla_ckv, state_mla_krope)
```

```python
import contextlib
import numpy as np
import concourse.bass as bass
import concourse.mybir as mybir
from concourse.bass_utils import run_bass_kernel_spmd

F32 = mybir.dt.float32
F32R = mybir.dt.float32r
AF = mybir.ActivationFunctionType
ALU = mybir.AluOpType
AX = mybir.AxisListType

NCORES = 8
D = 2048
NT = 2560
G = 512
NGRP = 5
DEPTH = 2
EPS = 1e-6
NFF = 44
FB = 11
NEXP = 8
DBG_GROUPS = None
SEQS = ((0, 2048, True), (2048, 256, False), (2304, 256, False))
POOL_WINDOWS = (2, 4, 8, 16)

R_C, R_CCTX = 0, 16


def R_LN1(l): return 32 + l * 142
def R_LN2(l): return 32 + l * 142 + 16
def R_BADA(l): return 32 + l * 142 + 32
def R_QG(l): return 32 + l * 142 + 128
def R_KVG(l): return 32 + l * 142 + 132
def R_PSC(l): return 32 + l * 142 + 134


R_FG = 32 + 2 * 142
NROWS = 384


class Buf:
    __slots__ = ("w", "r")

    def __init__(self):
        self.w = None
        self.r = {}


class Eng:
    def __init__(self, name, eng, sem):
        self.name = name
        self.eng = eng
        self.sem = sem
        self.cnt = 0
        self.seen = {}
        self.dsems = []
        self.dvals = []
        self.dn = 0
        self.pend = []
        self.prog = []


class FW:
    def __init__(self, nc, es, ndma_sems=8):
        self.nc = nc
        self.engs = {}
        for name, eng in (("pe", nc.tensor), ("act", nc.scalar), ("dve", nc.vector),
                          ("pool", nc.gpsimd), ("sp", nc.sync)):
            sem = es.enter_context(nc.semaphore("s_" + name))
            self.engs[name] = Eng(name, eng, sem)
        for qn in ("sp", "pool"):
            e = self.engs[qn]
            for i in range(ndma_sems):
                e.dsems.append(es.enter_context(nc.semaphore("d_%s%d" % (qn, i))))
                e.dvals.append(0)
        self.ninst = 0

    def _wait(self, e, ticket):
        sem, val, owner = ticket
        if owner is e:
            if e.name == "pe" or val > e.cnt:
                return
        key = id(sem)
        if e.seen.get(key, 0) >= val:
            return
        e.pend.append((sem, val))
        e.seen[key] = val
        self.ninst += 1

    def _deps(self, e, reads, writes):
        for b in reads:
            if b.w is not None:
                self._wait(e, b.w)
        for b in writes:
            if b.w is not None:
                self._wait(e, b.w)
            for t in b.r.values():
                self._wait(e, t)

    def _mark(self, e, ticket, reads, writes, key=None):
        for b in reads:
            b.r[key or e.name] = ticket
        for b in writes:
            b.w = ticket
            b.r = {}

    def op(self, en, fn, reads=(), writes=(), sync=True):
        e = self.engs[en]
        self._deps(e, reads, writes)
        self.ninst += 1
        waits, e.pend = e.pend, []
        if sync:
            e.cnt += 1
            e.prog.append((waits, fn, e.sem, 1))
            t = (e.sem, e.cnt, e)
        else:
            e.prog.append((waits, fn, None, 0))
            t = (e.sem, e.cnt + 1, e)
        self._mark(e, t, reads, writes)

    def dma(self, qn, out, in_, reads=(), writes=(), **kw):
        e = self.engs[qn]
        slot = e.dn % len(e.dsems)
        e.dn += 1
        sem = e.dsems[slot]
        if e.dvals[slot] > 0:
            self._wait(e, (sem, e.dvals[slot], None))
        self._deps(e, reads, writes)
        e.dvals[slot] += 16
        eng = e.eng
        waits, e.pend = e.pend, []
        e.prog.append((waits, (lambda: eng.dma_start(out=out, in_=in_, **kw)), sem, 16))
        self.ninst += 1
        self._mark(e, (sem, e.dvals[slot], None), reads, writes, key=(e.name, slot))

    def emit(self):
        sp = self.engs["sp"]
        for qn in ("sp", "pool"):
            q = self.engs[qn]
            for s, v in zip(q.dsems, q.dvals):
                if v > 0:
                    self._wait(sp, (s, v, None))
        for en in ("pe", "act", "dve", "pool"):
            e = self.engs[en]
            if e.cnt > 0:
                self._wait(sp, (e.sem, e.cnt, None))

        def replay(e):
            eng = e.eng
            for waits, fn, sem, inc in e.prog:
                for s, v in waits:
                    eng.wait_ge(s, v)
                ins = fn()
                if sem is not None:
                    ins.then_inc(sem, inc)
            for s, v in e.pend:
                eng.wait_ge(s, v)
            e.prog = []
            e.pend = []

        with self.nc.Block() as block:
            @block.sync
            def _(x):
                replay(self.engs["sp"])

            @block.tensor
            def _(x):
                replay(self.engs["pe"])

            @block.scalar
            def _(x):
                replay(self.engs["act"])

            @block.vector
            def _(x):
                replay(self.engs["dve"])

            @block.gpsimd
            def _(x):
                replay(self.engs["pool"])


class Tile:
    _n = [0]

    def __init__(self, nc, es, name, shape, split=False):
        Tile._n[0] += 1
        self.t = es.enter_context(nc.sbuf_tensor("sb%d_%s" % (Tile._n[0], name), list(shape), F32))
        self.split = split
        if split:
            self.bufs = [Buf() for _ in range(shape[1])]
        else:
            self.bufs = [Buf()]

    def __getitem__(self, idx):
        return self.t[idx]

    def r(self, idx):
        return self.t[idx].bitcast(F32R)

    def b(self, i=None):
        if self.split and i is not None:
            return [self.bufs[i]]
        return list(self.bufs)


class Ring:
    def __init__(self, nc, es, name, shape, n):
        self.tiles = [Tile(nc, es, "%s%d" % (name, i), shape) for i in range(n)]
        self.i = 0

    def next(self):
        t = self.tiles[self.i % len(self.tiles)]
        self.i += 1
        return t


class Prog:
    def __init__(self, stop_after=None, moe_experts=NEXP, scratch_in=()):
        self.stop_after = stop_after
        self.moe_experts = moe_experts
        self.nc = nc = bass.Bass("TRN2", target_bir_lowering=False)
        self.es = es = contextlib.ExitStack()
        self.fw = FW(nc, es)
        self.din = {}
        self.evac_i = 0

        self.ishapes = {}

        def inp(name, shape):
            self.ishapes[name] = list(shape)

        def outp(name, shape):
            return nc.dram_tensor(name, list(shape), F32, kind="ExternalOutput").ap()

        def scr(name, shape):
            return nc.dram_tensor(name, list(shape), F32, kind="Internal").ap()

        inp("xinT", [16, 128, NT])
        inp("vecs", [NROWS, 128])
        inp("sink", [DEPTH, 16])
        inp("ident", [128, 128])
        inp("permM", [128, 128])
        inp("ropeC", [128, 2048])
        inp("ropeS", [128, 2048])
        inp("maskA", [128, 384])
        inp("invc_s", [4, 2048])
        inp("invc_p", [4, 256])
        inp("ck2T", [DEPTH, 2, 128, 512])
        inp("cv", [DEPTH, 512, 128])
        inp("cckvT", [DEPTH, 2, 128, 512])
        inp("ckr2T", [DEPTH, 128, 512])
        inp("w_ada", [DEPTH, 96, 128, 16, 128])
        inp("w_inA", [DEPTH, 26, 128, 16, 128])
        inp("w_inG", [DEPTH, 48, 128, 16, 128])
        inp("w_uq", [DEPTH, 12, 128, 4, 128])
        inp("w_ukv", [DEPTH, 16, 128, 2, 128])
        inp("poolw", [DEPTH, 8, 128, 2, 128])
        inp("wbr", [DEPTH, 3, 16, 128, 8, 128])
        inp("wout", [DEPTH, 16, 128, 16, 128])
        inp("ffn_g", [NFF, 128, 16, 128])
        inp("ffn_u", [NFF, 128, 16, 128])
        inp("ffn_d", [4, 16, 128, FB, 128])
        inp("router", [128, 16, 8])
        inp("moe_g", [NEXP, NFF, 128, 16, 128])
        inp("moe_u", [NEXP, NFF, 128, 16, 128])
        inp("moe_d", [NEXP, 4, 16, 128, FB, 128])
        self.o_yT = outp("o_yT", [16, 128, NT])
        self.o_kT = outp("o_kT", [DEPTH, 2, 64, 512])
        self.o_v = outp("o_v", [DEPTH, 4, 128, 128])
        self.o_ckvT = outp("o_ckvT", [DEPTH, 2, 128, 512])
        self.o_krT = outp("o_krT", [DEPTH, 64, 512])
        def mk(name, shape):
            if name in scratch_in:
                return nc.dram_tensor(name, list(shape), F32, kind="ExternalInput").ap()
            return (outp if stop_after is not None else scr)(name, shape)
        self.xT = mk("s_xT", [16, 128, NT])
        self.qaT = mk("s_qaT", [8, 128, NT])
        self.kaT2 = mk("s_kaT2", [2, 128, NT])
        self.vaTok = mk("s_vaTok", [NT // 128, 128, 128])
        self.qnT = mk("s_qnT", [8, 128, NT])
        self.qrT = mk("s_qrT", [4, 128, NT])
        self.ckvT = mk("s_ckvT", [2, 128, NT])
        self.krT2 = mk("s_krT2", [128, NT])
        self.uT = mk("s_uT", [8, 128, NT])
        self.oaT = mk("s_oaT", [8, 128, NT])
        self.obT = mk("s_obT", [8, 128, NT])
        self.ocT = mk("s_ocT", [8, 128, NT])
        self.b_x = [[Buf() for _ in range(16)] for _ in range(NGRP)]
        self.b_scr = {k: Buf() for k in ("qaT", "kaT2", "vaTok", "qnT", "qrT", "ckvT", "krT2", "uT",
                                         "oaT", "obT", "ocT")}
        self.ps = [es.enter_context(nc.psum_tensor("ps%d" % i, [128, 512], F32)) for i in range(8)]
        self.psb = [Buf() for _ in range(8)]

    def I(self, name):
        if name not in self.din:
            self.din[name] = self.nc.dram_tensor(name, self.ishapes[name], F32, kind="ExternalInput").ap()
        return self.din[name]

    def mm(self, out, lhsT, rhs, start, stop, reads, writes, sync=None):
        nc = self.nc
        if sync is None:
            sync = stop
        self.fw.op("pe", lambda: nc.tensor.matmul(out, lhsT=lhsT, rhs=rhs, start=start, stop=stop),
                   reads=reads, writes=writes, sync=sync)

    def tr(self, out, in_, reads, writes, sync=True):
        nc = self.nc
        ident = self.ident[:]
        self.fw.op("pe", lambda: nc.tensor.transpose(out=out, in_=in_, identity=ident),
                   reads=reads + self.ident.b(), writes=writes, sync=sync)

    def copy(self, en, out, in_, reads, writes):
        nc = self.nc
        if en == "act":
            self.fw.op("act", lambda: nc.scalar.copy(out=out, in_=in_), reads=reads, writes=writes)
        elif en == "dve":
            self.fw.op("dve", lambda: nc.vector.tensor_copy(out=out, in_=in_), reads=reads, writes=writes)
        else:
            self.fw.op("pool", lambda: nc.gpsimd.tensor_copy(out=out, in_=in_), reads=reads, writes=writes)

    def evac_eng(self):
        self.evac_i += 1
        return "act" if self.evac_i % 2 else "dve"

    def act(self, out, in_, func, reads, writes, bias=None, scale=None, accum_out=None):
        nc = self.nc
        kw = {}
        if bias is not None:
            kw["bias"] = bias
        if scale is not None:
            kw["scale"] = scale
        if accum_out is not None:
            kw["accum_out"] = accum_out
        self.fw.op("act", lambda: nc.scalar.activation(out=out, in_=in_, func=func, **kw),
                   reads=reads, writes=writes)

    def tt(self, out, in0, in1, op, reads, writes, en="dve"):
        nc = self.nc
        eng = nc.vector if en == "dve" else nc.gpsimd
        self.fw.op(en, lambda: eng.tensor_tensor(out=out, in0=in0, in1=in1, op=op), reads=reads, writes=writes)

    def ts(self, out, in0, s1, s2, op0, op1, reads, writes):
        nc = self.nc
        if op1 is None:
            self.fw.op("dve", lambda: nc.vector.tensor_scalar(out=out, in0=in0, scalar1=s1, scalar2=None, op0=op0),
                       reads=reads, writes=writes)
        else:
            self.fw.op("dve", lambda: nc.vector.tensor_scalar(out=out, in0=in0, scalar1=s1, scalar2=s2,
                                                              op0=op0, op1=op1), reads=reads, writes=writes)

    def stt(self, out, in0, scalar, in1, op0, op1, reads, writes):
        nc = self.nc
        self.fw.op("dve", lambda: nc.vector.scalar_tensor_tensor(out=out, in0=in0, scalar=scalar, in1=in1,
                                                                 op0=op0, op1=op1), reads=reads, writes=writes)

    def wload(self, ring, dram_tile, nk, ncol=128):
        slot = ring.next()
        dst = slot.t[:, 0:nk * ncol].rearrange("p (k m) -> p k m", k=nk)
        self.fw.dma("pool", dst.bitcast(F32R), dram_tile, writes=slot.b())
        return dst.bitcast(F32R), slot

    def alloc_persistent(self):
        nc, es = self.nc, self.es
        self.ident = Tile(nc, es, "ident", [128, 128])
        self.ones = Tile(nc, es, "ones", [128, 128])
        self.identr = Tile(nc, es, "identr", [128, 128])
        self.vT = Tile(nc, es, "vT", [128, NROWS])
        self.modp = [Tile(nc, es, "modp%d" % l, [128, 6, 16, 2]) for l in range(DEPTH)]
        self.sinkb = Tile(nc, es, "sinkb", [128, DEPTH * 16])

    def phase_prologue(self):
        nc, fw = self.nc, self.fw
        with contextlib.ExitStack() as es:
            vrows = Tile(nc, es, "vrows", [128, 3, 128])
            scT = Tile(nc, es, "scT", [128, 32])
            modT = Tile(nc, es, "modT", [128, 96, 2])
            wring = Ring(nc, es, "wada", [128, 2048], 4)
            fw.dma("sp", self.ident[:], self.I("ident"), writes=self.ident.b())
            fw.dma("sp", vrows[:], self.I("vecs").rearrange("(a p) f -> p a f", p=128), writes=vrows.b())
            fw.dma("sp", self.sinkb[:],
                   self.I("sink").rearrange("l h -> (l h)").partition_broadcast(128), writes=self.sinkb.b())
            ones32 = Tile(nc, es, "ones32", [128, 128])
            fw.op("dve", lambda: nc.vector.memset(ones32[:], 1.0), writes=ones32.b())
            self.copy("act", self.ones.r(slice(None)), ones32[:], ones32.b(), self.ones.b())
            self.copy("act", self.identr.r(slice(None)), self.ident[:], self.ident.b(), self.identr.b())
            for a in range(3):
                self.tr(self.ps[0][:, a * 128:(a + 1) * 128], vrows[:, a, :], vrows.b(), [self.psb[0]])
            self.copy("dve", self.vT[:], self.ps[0][:, 0:384], [self.psb[0]], self.vT.b())
            self.act(scT.r(slice(None)), self.vT[:, 0:32], AF.Silu, self.vT.b(), scT.b())
            sc3 = scT.r(slice(None)).rearrange("p (c k) -> p k c", c=2)
            for l in range(DEPTH):
                for j in range(96):
                    w, slot = self.wload(wring, self.I("w_ada")[l, j], 16)
                    bank = self.ps[1 + (j % 2)]
                    bb = self.psb[1 + (j % 2)]
                    for k in range(16):
                        self.mm(bank[:, 0:2], w[:, k, :], sc3[:, k, :], k == 0, k == 15,
                                slot.b() + scT.b(), [bb])
                    col = R_BADA(l) + j
                    self.act(modT[:, j, :], bank[:, 0:2], AF.Identity, [bb] + self.vT.b(), modT.b(),
                             bias=self.vT[:, col:col + 1])
                mp = self.modp[l]
                for s in range(2):
                    lnr = R_LN1(l) if s == 0 else R_LN2(l)
                    sh, sc, gt = 3 * s, 3 * s + 1, 3 * s + 2
                    for cd in range(2):
                        self.stt(mp[:, 3 * s + 0, :, cd], modT[:, sc * 16:(sc + 1) * 16, cd], 1.0,
                                 self.vT[:, lnr:lnr + 16], ALU.add, ALU.mult,
                                 modT.b() + self.vT.b(), mp.b())
                        self.copy("dve", mp[:, 3 * s + 1, :, cd], modT[:, sh * 16:(sh + 1) * 16, cd], modT.b(), mp.b())
                        self.copy("dve", mp[:, 3 * s + 2, :, cd], modT[:, gt * 16:(gt + 1) * 16, cd], modT.b(), mp.b())
            xt = Ring(nc, es, "xcp", [128, 16 * 512], 2)
            for g in range(NGRP):
                t = xt.next()
                v = t.t[:].rearrange("p (c n) -> p c n", c=16)
                fw.dma("sp", v, self.I("xinT")[:, :, g * G:(g + 1) * G].rearrange("c p n -> p c n"), writes=t.b())
                fw.dma("sp", self.xT[:, :, g * G:(g + 1) * G].rearrange("c p n -> p c n"), v,
                       reads=t.b(), writes=self.b_x[g])
            fw.emit()

    def norm_stats(self, x3, xb, nchunk, nfeat, sqring, rstd, bank, bb):
        nc = self.nc
        for c in range(nchunk):
            sq = sqring.next()
            self.act(sq.r(slice(None)), x3[:, c, :], AF.Square, xb, sq.b())
            self.mm(bank[:, :], self.ones.r(slice(None)), sq.r(slice(None)), c == 0, c == nchunk - 1,
                    self.ones.b() + sq.b(), [bb], sync=True)
        self.act(rstd[:], bank[:, :], AF.Sqrt, [bb] + self.epsb.b(), rstd.b(), bias=self.epsb[:, 0:1], scale=1.0 / nfeat)
        self.fw.op("dve", lambda: nc.vector.reciprocal(out=rstd[:], in_=rstd[:]), reads=rstd.b(), writes=rstd.b())

    def norm_mod(self, g, l, s, xg, hT, sqring, tmpring, rstd, bank, bb):
        cd = 0 if g < 4 else 1
        mp = self.modp[l]
        x3 = xg.t[:].rearrange("p (c n) -> p c n", c=16)
        self.norm_stats(x3, xg.b(), 16, D, sqring, rstd, bank, bb)
        for c in range(16):
            tmp = tmpring.next()
            self.tt(tmp[:], x3[:, c, :], rstd[:], ALU.mult, xg.b() + rstd.b(), tmp.b())
            self.act(hT.r((slice(None), c, slice(None))), tmp[:], AF.Identity, tmp.b() + mp.b(), hT.b(c),
                     bias=mp[:, 3 * s + 1, c:c + 1, cd], scale=mp[:, 3 * s + 0, c:c + 1, cd])

    def load_x(self, g, xg):
        v = xg.t[:].rearrange("p (c n) -> p c n", c=16)
        self.fw.dma("sp", v, self.xT[:, :, g * G:(g + 1) * G].rearrange("c p n -> p c n"),
                    reads=self.b_x[g], writes=xg.b())

    def phase_proj(self, l):
        nc, fw = self.nc, self.fw
        with contextlib.ExitStack() as es:
            xg = Tile(nc, es, "xg", [128, 16 * 512])
            hT = Tile(nc, es, "hT", [128, 16, 512], split=True)
            wring = Ring(nc, es, "w1", [128, 2048], 4)
            wuq = Tile(nc, es, "wuq", [128, 12, 512])
            ropeC = Tile(nc, es, "ropeC", [128, 2048])
            ropeS = Tile(nc, es, "ropeS", [128, 2048])
            permM = Tile(nc, es, "permM", [128, 128])
            cqs = Tile(nc, es, "cqs", [128, 4, 512])
            cqn = Tile(nc, es, "cqn", [128, 4, 512])
            ckvs = Tile(nc, es, "ckvs", [128, 2, 512])
            stg = Ring(nc, es, "stg", [128, 512], 4)
            sqring = Ring(nc, es, "sq", [128, 512], 3)
            tmpring = Ring(nc, es, "tmp", [128, 512], 3)
            xsring = Ring(nc, es, "xs", [128, 512], 2)
            t1ring = Ring(nc, es, "t1", [128, 512], 2)
            rstd = Tile(nc, es, "rstd", [128, 512])
            rstd2 = Tile(nc, es, "rstd2", [128, 512])
            vtok = Ring(nc, es, "vtok", [128, 512], 2)
            self.epsb = Tile(nc, es, "epsb", [128, 1])
            fw.op("dve", lambda: nc.vector.memset(self.epsb[:], EPS), writes=self.epsb.b())
            fw.dma("sp", ropeC[:], self.I("ropeC"), writes=ropeC.b())
            fw.dma("sp", ropeS[:], self.I("ropeS"), writes=ropeS.b())
            fw.dma("pool", permM.r(slice(None)), self.I("permM"), writes=permM.b())
            fw.dma("pool", wuq.t[:].rearrange("p a (k m) -> p a k m", k=4).bitcast(F32R),
                   self.I("w_uq")[l].rearrange("a p k m -> p a k m"), writes=wuq.b())
            wuq4 = wuq.t[:].rearrange("p a (k m) -> p a k m", k=4).bitcast(F32R)
            bi = [0]

            def nextbank():
                bi[0] += 1
                i = bi[0] % 4
                return self.ps[i], self.psb[i]

            def finish_chunk(bank, bb, g, rope, dst, dbuf, P=128, dst2=None):
                st = stg.next()
                if rope and g < 4:
                    xs = xsring.next()
                    self.copy("act", xs.r(slice(None))[0:P], bank[0:P, :], [bb], xs.b())
                    pb, pbb = self.ps[4 + (bi[0] % 2)], self.psb[4 + (bi[0] % 2)]
                    self.mm(pb[0:P, :], permM.r(slice(None))[0:P, 0:P], xs.r(slice(None))[0:P], True, True,
                            permM.b() + xs.b(), [pbb])
                    t1 = t1ring.next()
                    self.tt(t1[0:P], xs[0:P], ropeC[0:P, g * G:(g + 1) * G], ALU.mult, xs.b() + ropeC.b(), t1.b())
                    self.tt(st[0:P], pb[0:P, :], ropeS[0:P, g * G:(g + 1) * G], ALU.mult, [pbb] + ropeS.b(), st.b())
                    self.tt(st[0:P], st[0:P], t1[0:P], ALU.add, st.b() + t1.b(), st.b())
                else:
                    self.copy(self.evac_eng(), st[0:P], bank[0:P, :], [bb], st.b())
                fw.dma("sp", dst, st[0:P], reads=st.b(), writes=[dbuf])
                if dst2 is not None:
                    fw.dma("sp", dst2, st[0:dst2.shape[0]], reads=st.b())
                return st

            for g in (DBG_GROUPS or range(NGRP)):
                gs = slice(g * G, (g + 1) * G)
                self.load_x(g, xg)
                self.norm_mod(g, l, 0, xg, hT, sqring, tmpring, rstd, self.ps[7], self.psb[7])
                for ch in range(26):
                    w, slot = self.wload(wring, self.I("w_inA")[l, ch], 16)
                    bank, bb = nextbank()
                    for k in range(16):
                        self.mm(bank[:, :], w[:, k, :], hT.r((slice(None), k, slice(None))), k == 0, k == 15,
                                slot.b() + hT.b(k), [bb])
                    if ch < 8:
                        finish_chunk(bank, bb, g, True, self.qaT[ch, :, gs], self.b_scr["qaT"])
                    elif ch < 10:
                        d2 = self.o_kT[l, ch - 8, :, :] if g == 4 else None
                        finish_chunk(bank, bb, g, True, self.kaT2[ch - 8, :, gs], self.b_scr["kaT2"], dst2=d2)
                    elif ch == 10:
                        st = stg.next()
                        self.copy(self.evac_eng(), st[:], bank[:, :], [bb], st.b())
                        tb, tbb = self.ps[6], self.psb[6]
                        for t in range(4):
                            self.tr(tb[:, t * 128:(t + 1) * 128], st[:, t * 128:(t + 1) * 128], st.b(), [tbb])
                        vt = vtok.next()
                        self.copy(self.evac_eng(), vt[:], tb[:, :], [tbb], vt.b())
                        fw.dma("sp", self.vaTok[g * 4:(g + 1) * 4].rearrange("t p d -> p t d"),
                               vt.t[:].rearrange("p (t d) -> p t d", t=4), reads=vt.b(), writes=[self.b_scr["vaTok"]])
                        if g == 4:
                            fw.dma("sp", self.o_v[l].rearrange("t p d -> p t d"),
                                   vt.t[:].rearrange("p (t d) -> p t d", t=4), reads=vt.b())
                    elif ch < 15:
                        self.copy(self.evac_eng(), cqs[:, ch - 11, :], bank[:, :], [bb], cqs.b())
                        if ch == 14:
                            self.norm_stats(cqs.t[:], cqs.b(), 4, 512, sqring, rstd2, self.ps[7], self.psb[7])
                            for c in range(4):
                                col = R_QG(l) + c
                                self.stt(cqn.r((slice(None), c, slice(None))), cqs[:, c, :], self.vT[:, col:col + 1],
                                         rstd2[:], ALU.mult, ALU.mult, cqs.b() + rstd2.b() + self.vT.b(), cqn.b())
                            for a in range(12):
                                bank2, bb2 = nextbank()
                                for k in range(4):
                                    self.mm(bank2[:, :], wuq4[:, a, k, :], cqn.r((slice(None), k, slice(None))),
                                            k == 0, k == 3, wuq.b() + cqn.b(), [bb2])
                                if a < 8:
                                    finish_chunk(bank2, bb2, g, False, self.qnT[a, :, gs], self.b_scr["qnT"])
                                else:
                                    finish_chunk(bank2, bb2, g, True, self.qrT[a - 8, :, gs], self.b_scr["qrT"])
                    elif ch < 17:
                        self.copy(self.evac_eng(), ckvs[:, ch - 15, :], bank[:, :], [bb], ckvs.b())
                        if ch == 16:
                            self.norm_stats(ckvs.t[:], ckvs.b(), 2, 256, sqring, rstd2, self.ps[7], self.psb[7])
                            for c in range(2):
                                col = R_KVG(l) + c
                                st = stg.next()
                                self.stt(st[:], ckvs[:, c, :], self.vT[:, col:col + 1], rstd2[:], ALU.mult, ALU.mult,
                                         ckvs.b() + rstd2.b() + self.vT.b(), st.b())
                                fw.dma("sp", self.ckvT[c, :, gs], st[:], reads=st.b(), writes=[self.b_scr["ckvT"]])
                                if g == 4:
                                    fw.dma("sp", self.o_ckvT[l, c], st[:], reads=st.b())
                    elif ch == 17:
                        d2 = self.o_krT[l] if g == 4 else None
                        finish_chunk(bank, bb, g, True, self.krT2[:, gs], self.b_scr["krT2"], dst2=d2)
                    else:
                        finish_chunk(bank, bb, g, False, self.uT[ch - 18, :, gs], self.b_scr["uT"])
            fw.emit()

    def rmax(self, out, in_, reads, writes):
        nc = self.nc
        self.fw.op("dve", lambda: nc.vector.reduce_max(out=out, in_=in_, axis=AX.X), reads=reads, writes=writes)

    def rsum(self, out, in_, reads, writes):
        nc = self.nc
        self.fw.op("dve", lambda: nc.vector.reduce_sum(out=out, in_=in_, axis=AX.X), reads=reads, writes=writes)

    def recip(self, out, in_, reads, writes):
        nc = self.nc
        self.fw.op("dve", lambda: nc.vector.reciprocal(out=out, in_=in_), reads=reads, writes=writes)

    def phase_attnA(self, l, seqs=SEQS, qb_limit=None):
        nc, fw = self.nc, self.fw
        with contextlib.ExitStack() as es:
            kT2 = Tile(nc, es, "kT2", [128, 2, 2560])
            Vt = Tile(nc, es, "Vt", [128, 20, 128])
            qring = Ring(nc, es, "qc", [128, 2048], 2)
            maskA = Tile(nc, es, "maskA", [128, 384])
            Pring = Ring(nc, es, "Pa", [128, 896], 2)
            PTring = Ring(nc, es, "PTa", [128, 896], 2)
            slring = Ring(nc, es, "sl", [128, 384], 2)
            small = Ring(nc, es, "sma", [128, 8], 4)
            rdring = Ring(nc, es, "rda", [128, 2], 2)
            Oqring = Ring(nc, es, "Oqa", [128, 128], 2)
            ostg = Ring(nc, es, "ostga", [128, 512], 2)
            fw.dma("sp", maskA[:], self.I("maskA"), writes=maskA.b())
            for (tok0, n, ctx) in seqs:
                nqb = n // 128
                for g2 in range(2):
                    fw.dma("pool", kT2.r((slice(None), g2, slice(0, n))), self.kaT2[g2, :, tok0:tok0 + n],
                           reads=[self.b_scr["kaT2"]], writes=kT2.b())
                    if ctx:
                        fw.dma("pool", kT2.r((slice(None), g2, slice(n, n + 512))), self.I("ck2T")[l, g2],
                               writes=kT2.b())
                fw.dma("pool", Vt.r((slice(None), slice(0, nqb), slice(None))),
                       self.vaTok[tok0 // 128:tok0 // 128 + nqb].rearrange("t p d -> p t d"),
                       reads=[self.b_scr["vaTok"]], writes=Vt.b())
                if ctx:
                    fw.dma("pool", Vt.r((slice(None), slice(nqb, nqb + 4), slice(None))),
                           self.I("cv")[l].rearrange("(t p) d -> p t d", p=128), writes=Vt.b())
                for c in range(8):
                    g2 = c // 4
                    qc = qring.next()
                    fw.dma("pool", qc.r((slice(None), slice(0, n))), self.qaT[c, :, tok0:tok0 + n],
                           reads=[self.b_scr["qaT"]], writes=qc.b())
                    st = None
                    qbs = list(range(nqb)) if qb_limit is None else list(range(min(nqb, qb_limit)))
                    for qb in qbs:
                        if ctx:
                            kb_lo, kb_hi = max(qb - 1, 0), min(qb + 1, nqb - 1)
                        else:
                            kb_lo, kb_hi = 0, nqb - 1
                        nl = (kb_hi - kb_lo + 1) * 128
                        blocks = list(range(kb_lo, kb_hi + 1)) + ([nqb + i for i in range(4)] if ctx else [])
                        nb = len(blocks)
                        rd = rdring.next()
                        obank, obb = self.ps[6], self.psb[6]
                        for hh in range(2):
                            h = 2 * c + hh
                            pb = hh * 64
                            sbank, sbb = self.ps[hh * 2], self.psb[hh * 2]
                            cbank, cbb = self.ps[hh * 2 + 1], self.psb[hh * 2 + 1]
                            lq = qc.r((slice(pb, pb + 64), slice(qb * 128, (qb + 1) * 128)))
                            self.mm(sbank[:, 0:nl], lq, kT2.r((slice(pb, pb + 64), g2, slice(kb_lo * 128, kb_lo * 128 + nl))),
                                    True, True, qc.b() + kT2.b(), [sbb])
                            if ctx:
                                self.mm(cbank[:, :], lq, kT2.r((slice(pb, pb + 64), g2, slice(n, n + 512))),
                                        True, True, qc.b() + kT2.b(), [cbb])
                            sm = small.next()
                            Pt = Pring.next()
                            if ctx:
                                sl = slring.next()
                                mlo = 128 if qb == 0 else 0
                                self.tt(sl[:, 0:nl], sbank[:, 0:nl], maskA[:, mlo:mlo + nl], ALU.add,
                                        [sbb] + maskA.b(), sl.b())
                                self.rmax(sm[:, 0:1], sl[:, 0:nl], sl.b(), sm.b())
                                self.rmax(sm[:, 1:2], cbank[:, :], [cbb], sm.b())
                                self.tt(sm[:, 2:3], sm[:, 0:1], sm[:, 1:2], ALU.max, sm.b(), sm.b())
                                src_loc, src_b = sl[:, 0:nl], sl.b()
                            else:
                                self.rmax(sm[:, 2:3], sbank[:, 0:nl], [sbb], sm.b())
                                src_loc, src_b = sbank[:, 0:nl], [sbb]
                            scol = l * 16 + h
                            self.ts(sm[:, 3:4], sm[:, 2:3], 0.125, self.sinkb[:, scol:scol + 1], ALU.mult, ALU.max,
                                    sm.b() + self.sinkb.b(), sm.b())
                            self.ts(sm[:, 4:5], sm[:, 3:4], -1.0, None, ALU.mult, None, sm.b(), sm.b())
                            self.act(Pt[:, 0:nl], src_loc, AF.Exp, src_b + sm.b(), Pt.b() + sm.b(),
                                     bias=sm[:, 4:5], scale=0.125, accum_out=sm[:, 5:6])
                            if ctx:
                                self.act(Pt[:, nl:nl + 512], cbank[:, :], AF.Exp, [cbb] + sm.b(), Pt.b() + sm.b(),
                                         bias=sm[:, 4:5], scale=0.125, accum_out=sm[:, 6:7])
                            self.act(sm[:, 7:8], self.sinkb[:, scol:scol + 1], AF.Exp, self.sinkb.b() + sm.b(), sm.b(),
                                     bias=sm[:, 4:5], scale=1.0)
                            self.tt(sm[:, 5:6], sm[:, 5:6], sm[:, 7:8], ALU.add, sm.b(), sm.b())
                            if ctx:
                                self.tt(sm[:, 5:6], sm[:, 5:6], sm[:, 6:7], ALU.add, sm.b(), sm.b())
                            self.recip(rd[:, hh:hh + 1], sm[:, 5:6], sm.b(), rd.b())
                            PT = PTring.next()
                            for i0 in range(0, nb, 4):
                                cnt = min(4, nb - i0)
                                tb, tbb = self.ps[4 + (i0 // 4) % 2], self.psb[4 + (i0 // 4) % 2]
                                for i in range(i0, i0 + cnt):
                                    self.tr(tb[:, (i - i0) * 128:(i - i0 + 1) * 128], Pt[:, i * 128:(i + 1) * 128],
                                            Pt.b(), [tbb])
                                self.copy(self.evac_eng(), PT.r((slice(None), slice(i0 * 128, (i0 + cnt) * 128))),
                                          tb[:, 0:cnt * 128], [tbb], PT.b())
                            for i, blk in enumerate(blocks):
                                self.mm(obank[:, hh * 64:(hh + 1) * 64], PT.r((slice(None), slice(i * 128, (i + 1) * 128))),
                                        Vt.r((slice(None), blk, slice(g2 * 64, (g2 + 1) * 64))), i == 0, i == nb - 1,
                                        PT.b() + Vt.b(), [obb], sync=True)
                        Oq = Oqring.next()
                        self.ts(Oq[:, 0:64], obank[:, 0:64], rd[:, 0:1], None, ALU.mult, None, [obb] + rd.b(), Oq.b())
                        self.ts(Oq[:, 64:128], obank[:, 64:128], rd[:, 1:2], None, ALU.mult, None, [obb] + rd.b(), Oq.b())
                        tb, tbb = self.ps[7], self.psb[7]
                        self.tr(tb[:, 0:128], Oq[:, :], Oq.b(), [tbb])
                        if qb % 4 == 0:
                            st = ostg.next()
                        self.copy(self.evac_eng(), st[:, (qb % 4) * 128:(qb % 4 + 1) * 128], tb[:, 0:128], [tbb], st.b())
                        if qb % 4 == 3 or qb == qbs[-1]:
                            q0 = (qb // 4) * 512
                            wd = (qb % 4 + 1) * 128
                            fw.dma("sp", self.oaT[c, :, tok0 + q0:tok0 + q0 + wd], st[:, 0:wd], reads=st.b(),
                                   writes=[self.b_scr["oaT"]])
            fw.emit()

    def phase_mla(self, l, seqs=SEQS, qb_limit=None, heads=range(8)):
        nc, fw = self.nc, self.fw
        scale = float((128 + 64) ** -0.5)
        with contextlib.ExitStack() as es:
            ckvA = Tile(nc, es, "ckvA", [128, 2, 2560])
            krA = Tile(nc, es, "krA", [128, 2560])
            wukv = Tile(nc, es, "wukv", [128, 16, 256])
            knT = Tile(nc, es, "knT", [128, 2560])
            vh = Tile(nc, es, "vh", [128, 20, 128])
            qnr = Ring(nc, es, "qnm", [128, 2048], 2)
            qrr = Ring(nc, es, "qrm", [128, 2048], 2)
            Pring = Ring(nc, es, "Pm", [128, 2560], 2)
            PTring = Ring(nc, es, "PTm", [128, 2560], 2)
            small = Ring(nc, es, "smm", [128, 16], 4)
            Oqring = Ring(nc, es, "Oqm", [128, 128], 2)
            ostg = Ring(nc, es, "ostgm", [128, 512], 2)
            wukv4 = wukv.t[:].rearrange("p a (k m) -> p a k m", k=2).bitcast(F32R)
            fw.dma("pool", wukv4, self.I("w_ukv")[l].rearrange("a p k m -> p a k m"), writes=wukv.b())
            for (tok0, n, ctx) in seqs:
                nqb = n // 128
                nk = n + (512 if ctx else 0)
                nkb = nk // 128
                kgs = [(s, min(512, nk - s)) for s in range(0, nk, 512)]
                ng = len(kgs)
                for k in range(2):
                    fw.dma("pool", ckvA.r((slice(None), k, slice(0, n))), self.ckvT[k, :, tok0:tok0 + n],
                           reads=[self.b_scr["ckvT"]], writes=ckvA.b())
                    if ctx:
                        fw.dma("pool", ckvA.r((slice(None), k, slice(n, n + 512))), self.I("cckvT")[l, k], writes=ckvA.b())
                fw.dma("pool", krA.r((slice(None), slice(0, n))), self.krT2[:, tok0:tok0 + n],
                       reads=[self.b_scr["krT2"]], writes=krA.b())
                if ctx:
                    fw.dma("pool", krA.r((slice(None), slice(n, n + 512))), self.I("ckr2T")[l], writes=krA.b())
                qr, qr_pair = None, -1
                for h in heads:
                    pb = (h % 2) * 64
                    for gi, (s, w) in enumerate(kgs):
                        bank, bb = self.ps[gi % 4], self.psb[gi % 4]
                        for k in range(2):
                            self.mm(bank[:, 0:w], wukv4[:, h, k, :], ckvA.r((slice(None), k, slice(s, s + w))),
                                    k == 0, k == 1, wukv.b() + ckvA.b(), [bb])
                        self.copy(self.evac_eng(), knT.r((slice(None), slice(s, s + w))), bank[:, 0:w], [bb], knT.b())
                    for kb0 in range(0, nkb, 4):
                        cnt = min(4, nkb - kb0)
                        bank, bb = self.ps[4 + (kb0 // 4) % 2], self.psb[4 + (kb0 // 4) % 2]
                        for kb in range(kb0, kb0 + cnt):
                            for k in range(2):
                                self.mm(bank[:, (kb - kb0) * 128:(kb - kb0 + 1) * 128],
                                        ckvA.r((slice(None), k, slice(kb * 128, (kb + 1) * 128))), wukv4[:, 8 + h, k, :],
                                        k == 0, k == 1, wukv.b() + ckvA.b(), [bb], sync=(k == 1 and kb == kb0 + cnt - 1))
                        self.copy(self.evac_eng(), vh.r((slice(None), slice(kb0, kb0 + cnt), slice(None))),
                                  bank[:, 0:cnt * 128].rearrange("p (a d) -> p a d", a=cnt), [bb], vh.b())
                    qn = qnr.next()
                    fw.dma("pool", qn.r((slice(None), slice(0, n))), self.qnT[h, :, tok0:tok0 + n],
                           reads=[self.b_scr["qnT"]], writes=qn.b())
                    if qr_pair != h // 2:
                        qr_pair = h // 2
                        qr = qrr.next()
                        fw.dma("pool", qr.r((slice(None), slice(0, n))), self.qrT[h // 2, :, tok0:tok0 + n],
                               reads=[self.b_scr["qrT"]], writes=qr.b())
                    st = None
                    qbs = list(range(nqb)) if qb_limit is None else list(range(min(nqb, qb_limit)))
                    for qb in qbs:
                        qsl = slice(qb * 128, (qb + 1) * 128)
                        sm = small.next()
                        Pt = Pring.next()
                        for gi, (s, w) in enumerate(kgs):
                            bank, bb = self.ps[gi], self.psb[gi]
                            self.mm(bank[:, 0:w], qn.r((slice(None), qsl)), knT.r((slice(None), slice(s, s + w))),
                                    True, False, qn.b() + knT.b(), [bb], sync=False)
                            self.mm(bank[:, 0:w], qr.r((slice(pb, pb + 64), qsl)), krA.r((slice(pb, pb + 64), slice(s, s + w))),
                                    False, True, qr.b() + krA.b(), [bb], sync=True)
                            self.rmax(sm[:, gi:gi + 1], bank[:, 0:w], [bb], sm.b())
                        self.rmax(sm[:, 8:9], sm[:, 0:ng], sm.b(), sm.b())
                        self.ts(sm[:, 9:10], sm[:, 8:9], -scale, None, ALU.mult, None, sm.b(), sm.b())
                        for gi, (s, w) in enumerate(kgs):
                            bank, bb = self.ps[gi], self.psb[gi]
                            self.act(Pt[:, s:s + w], bank[:, 0:w], AF.Exp, [bb] + sm.b(), Pt.b() + sm.b(),
                                     bias=sm[:, 9:10], scale=scale, accum_out=sm[:, 10 + gi:11 + gi])
                        self.rsum(sm[:, 15:16], sm[:, 10:10 + ng], sm.b(), sm.b())
                        self.recip(sm[:, 15:16], sm[:, 15:16], sm.b(), sm.b())
                        PT = PTring.next()
                        for kb0 in range(0, nkb, 4):
                            cnt = min(4, nkb - kb0)
                            tb, tbb = self.ps[5 + (kb0 // 4) % 2], self.psb[5 + (kb0 // 4) % 2]
                            for kb in range(kb0, kb0 + cnt):
                                self.tr(tb[:, (kb - kb0) * 128:(kb - kb0 + 1) * 128], Pt[:, kb * 128:(kb + 1) * 128],
                                        Pt.b(), [tbb])
                            self.copy(self.evac_eng(), PT.r((slice(None), slice(kb0 * 128, (kb0 + cnt) * 128))),
                                      tb[:, 0:cnt * 128], [tbb], PT.b())
                        obank, obb = self.ps[7], self.psb[7]
                        for kb in range(nkb):
                            self.mm(obank[:, 0:128], PT.r((slice(None), slice(kb * 128, (kb + 1) * 128))),
                                    vh.r((slice(None), kb, slice(None))), kb == 0, kb == nkb - 1, PT.b() + vh.b(), [obb])
                        Oq = Oqring.next()
                        self.ts(Oq[:, :], obank[:, 0:128], sm[:, 15:16], None, ALU.mult, None, [obb] + sm.b(), Oq.b())
                        tb, tbb = self.ps[5], self.psb[5]
                        self.tr(tb[:, 0:128], Oq[:, :], Oq.b(), [tbb])
                        if qb % 4 == 0:
                            st = ostg.next()
                        self.copy(self.evac_eng(), st[:, (qb % 4) * 128:(qb % 4 + 1) * 128], tb[:, 0:128], [tbb], st.b())
                        if qb % 4 == 3 or qb == qbs[-1]:
                            q0 = (qb // 4) * 512
                            wd = (qb % 4 + 1) * 128
                            fw.dma("sp", self.obT[h, :, tok0 + q0:tok0 + q0 + wd], st[:, 0:wd], reads=st.b(),
                                   writes=[self.b_scr["obT"]])
            fw.emit()

    def phase_pool(self, l, seqs=SEQS):
        nc, fw = self.nc, self.fw
        with contextlib.ExitStack() as es:
            invs = Tile(nc, es, "invs", [128, 4 * 2048])
            invp = Tile(nc, es, "invp", [128, 4 * 256])
            pw = Tile(nc, es, "pw", [128, 8, 256])
            upr = Ring(nc, es, "up", [128, 2064], 2)
            Ar = Ring(nc, es, "Apool", [128, 2064], 3)
            dT = [Tile(nc, es, "dT%d" % i, [128, 2048]) for i in range(2)]
            stg = Ring(nc, es, "pstg", [128, 512], 3)
            fw.dma("sp", invs[:], self.I("invc_s").rearrange("a n -> (a n)").partition_broadcast(128), writes=invs.b())
            fw.dma("sp", invp[:], self.I("invc_p").rearrange("a n -> (a n)").partition_broadcast(128), writes=invp.b())
            pw4 = pw.t[:].rearrange("p a (k m) -> p a k m", k=2).bitcast(F32R)
            fw.dma("pool", pw4, self.I("poolw")[l].rearrange("a p k m -> p a k m"), writes=pw.b())
            bi = 0
            for (tok0, n, ctx) in seqs:
                inv = invs if n == 2048 else invp
                for pg in range(4):
                    win = POOL_WINDOWS[pg]
                    left = win // 2
                    for half in range(2):
                        cc = pg * 2 + half
                        u = upr.next()
                        fw.op("dve", lambda u=u: nc.vector.memset(u[:, 0:8], 0.0), writes=u.b())
                        fw.op("dve", lambda u=u, n=n: nc.vector.memset(u[:, 8 + n:16 + n], 0.0), writes=u.b())
                        fw.dma("sp", u[:, 8:8 + n], self.uT[cc, :, tok0:tok0 + n], reads=[self.b_scr["uT"]], writes=u.b())
                        cur, L, step = u, n + 16, 1
                        while step < win:
                            nxt = Ar.next()
                            self.tt(nxt[:, 0:L - step], cur[:, 0:L - step], cur[:, step:L], ALU.add, cur.b(), nxt.b())
                            cur, L, step = nxt, L - step, step * 2
                        tmp = Ar.next()
                        self.tt(tmp[:, 0:n], cur[:, 8 - left:8 - left + n], inv[:, pg * n:(pg + 1) * n], ALU.mult,
                                cur.b() + inv.b(), tmp.b())
                        self.tt(dT[half].r((slice(None), slice(0, n))), tmp[:, 0:n], u[:, 8:8 + n], ALU.subtract,
                                tmp.b() + u.b(), dT[half].b())
                    for mh in range(2):
                        for tg in range(0, n, 512):
                            w = min(512, n - tg)
                            bi += 1
                            bank, bb = self.ps[bi % 4], self.psb[bi % 4]
                            for k in range(2):
                                self.mm(bank[:, 0:w], pw4[:, pg * 2 + mh, k, :], dT[k].r((slice(None), slice(tg, tg + w))),
                                        k == 0, k == 1, pw.b() + dT[k].b(), [bb])
                            st = stg.next()
                            col = R_PSC(l) + pg * 2 + mh
                            self.act(st[:, 0:w], bank[:, 0:w], AF.Copy, [bb] + self.vT.b(), st.b(),
                                     scale=self.vT[:, col:col + 1])
                            fw.dma("sp", self.ocT[pg * 2 + mh, :, tok0 + tg:tok0 + tg + w], st[:, 0:w], reads=st.b(),
                                   writes=[self.b_scr["ocT"]])
            fw.emit()

    def phase_merge(self, l, groups=None):
        nc, fw = self.nc, self.fw
        with contextlib.ExitStack() as es:
            xg = Tile(nc, es, "xg3", [128, 16 * 512])
            hT = Tile(nc, es, "hT3", [128, 16, 512], split=True)
            o3 = [Tile(nc, es, "o3_%d" % i, [128, 8, 512]) for i in range(3)]
            wring = Ring(nc, es, "w3", [128, 2048], 3)
            sgr = Ring(nc, es, "sg3", [128, 512], 2)
            tmr = Ring(nc, es, "tm3", [128, 512], 2)
            sqring = Ring(nc, es, "sq3", [128, 512], 2)
            tmpring = Ring(nc, es, "tmp3", [128, 512], 2)
            rstd = Tile(nc, es, "rstd3", [128, 512])
            xring = Ring(nc, es, "xr3", [128, 512], 3)
            self.epsb = Tile(nc, es, "epsb3", [128, 1])
            fw.op("dve", lambda: nc.vector.memset(self.epsb[:], EPS), writes=self.epsb.b())
            y3 = xg.t[:].rearrange("p (c n) -> p c n", c=16)
            srcs = [(self.oaT, "oaT"), (self.obT, "obT"), (self.ocT, "ocT")]
            mp = self.modp[l]
            it = 0
            for g in (groups or range(NGRP)):
                cd = 0 if g < 4 else 1
                gs = slice(g * G, (g + 1) * G)
                self.load_x(g, xg)
                self.norm_mod(g, l, 0, xg, hT, sqring, tmpring, rstd, self.ps[7], self.psb[7])
                for br in range(3):
                    fw.dma("pool", o3[br].r(slice(None)), srcs[br][0][:, :, gs].rearrange("c p n -> p c n"),
                           reads=[self.b_scr[srcs[br][1]]], writes=o3[br].b())
                for d in range(16):
                    for br in range(3):
                        it += 1
                        w, slot = self.wload(wring, self.I("w_inG")[l, br * 16 + d], 16)
                        ga, gab = self.ps[it % 2], self.psb[it % 2]
                        for k in range(16):
                            self.mm(ga[:, :], w[:, k, :], hT.r((slice(None), k, slice(None))), k == 0, k == 15,
                                    slot.b() + hT.b(k), [gab])
                        w2, slot2 = self.wload(wring, self.I("wbr")[l, br, d], 8)
                        pr, prb = self.ps[2 + it % 2], self.psb[2 + it % 2]
                        for k in range(8):
                            self.mm(pr[:, :], w2[:, k, :], o3[br].r((slice(None), k, slice(None))), k == 0, k == 7,
                                    slot2.b() + o3[br].b(), [prb])
                        sg = sgr.next()
                        self.act(sg[:], ga[:, :], AF.Sigmoid, [gab], sg.b())
                        if br == 0:
                            self.tt(y3[:, d, :].bitcast(F32R), sg[:], pr[:, :], ALU.mult, sg.b() + [prb], xg.b())
                        else:
                            tm = tmr.next()
                            self.tt(tm[:], sg[:], pr[:, :], ALU.mult, sg.b() + [prb], tm.b())
                            out = y3[:, d, :].bitcast(F32R)
                            self.tt(out, y3[:, d, :], tm[:], ALU.add, xg.b() + tm.b(), xg.b())
                for d2 in range(16):
                    it += 1
                    w, slot = self.wload(wring, self.I("wout")[l, d2], 16)
                    bank, bb = self.ps[4 + it % 2], self.psb[4 + it % 2]
                    for k in range(16):
                        self.mm(bank[:, :], w[:, k, :], y3[:, k, :].bitcast(F32R), k == 0, k == 15, slot.b() + xg.b(), [bb])
                    xr = xring.next()
                    fw.dma("sp", xr[:], self.xT[d2, :, gs], reads=[self.b_x[g][d2]], writes=xr.b())
                    self.stt(xr[:], bank[:, :], mp[:, 2, d2:d2 + 1, cd], xr[:], ALU.mult, ALU.add,
                             [bb] + mp.b() + xr.b(), xr.b())
                    fw.dma("sp", self.xT[d2, :, gs], xr[:], reads=xr.b(), writes=[self.b_x[g][d2]])
            fw.emit()

    def phase_ffn(self, l, groups=None, final=False):
        nc, fw = self.nc, self.fw
        moe = (l % 2 == 1)
        nexp = self.moe_experts if moe else 1
        with contextlib.ExitStack() as es:
            xa = Tile(nc, es, "xa", [128, 16 * 512])
            hT = Tile(nc, es, "hT4", [128, 16, 512], split=True)
            aT = [Tile(nc, es, "aT%d" % i, [128, FB, 512], split=True) for i in range(2)]
            wgu = Ring(nc, es, "wgu", [128, 2048], 4)
            wdr = Ring(nc, es, "wdr", [128, FB * 128], 3)
            sglr = Ring(nc, es, "sgl", [128, 512], 2)
            t4r = Ring(nc, es, "t4", [128, 512], 2)
            sqring = Ring(nc, es, "sq4", [128, 512], 2)
            tmpring = Ring(nc, es, "tmp4", [128, 512], 2)
            rstd = Tile(nc, es, "rstd4", [128, 512])
            xring = Ring(nc, es, "xr4", [128, 512], 3)
            self.epsb = Tile(nc, es, "epsb4", [128, 1])
            fw.op("dve", lambda: nc.vector.memset(self.epsb[:], EPS), writes=self.epsb.b())
            if moe:
                rt = Tile(nc, es, "rt", [128, 16, 8])
                fw.dma("pool", rt.r(slice(None)), self.I("router"), writes=rt.b())
                gate = Tile(nc, es, "gate", [128, 4, 8])
                gsm = Ring(nc, es, "gsm", [128, 32], 2)
                gbr = Ring(nc, es, "gb", [128, 128], 2)
                gbcr = Ring(nc, es, "gbc", [128, 512], 2)
            a3 = xa.t[:].rearrange("p (c n) -> p c n", c=16)
            mp = self.modp[l]
            it = 0
            for g in (groups or range(NGRP)):
                cd = 0 if g < 4 else 1
                gs = slice(g * G, (g + 1) * G)
                self.load_x(g, xa)
                self.norm_mod(g, l, 1, xa, hT, sqring, tmpring, rstd, self.ps[7], self.psb[7])
                if moe:
                    for t in range(4):
                        lb, lbb = self.ps[6], self.psb[6]
                        for k in range(16):
                            self.mm(lb[:, t * 8:(t + 1) * 8], hT.r((slice(None), k, slice(t * 128, (t + 1) * 128))),
                                    rt.r((slice(None), k, slice(None))), k == 0, k == 15, hT.b(k) + rt.b(), [lbb])
                        sm = gsm.next()
                        lg = sm[:, 0:8]
                        self.copy("dve", lg, lb[:, t * 8:(t + 1) * 8], [lbb], sm.b())
                        self.rmax(sm[:, 24:25], lg, sm.b(), sm.b())
                        self.ts(sm[:, 8:16], lg, sm[:, 24:25], None, ALU.is_equal, None, sm.b(), sm.b())
                        self.stt(sm[:, 8:16], sm[:, 8:16], -1e30, lg, ALU.mult, ALU.add, sm.b(), sm.b())
                        self.rmax(sm[:, 25:26], sm[:, 8:16], sm.b(), sm.b())
                        self.ts(sm[:, 8:16], lg, sm[:, 25:26], None, ALU.is_ge, None, sm.b(), sm.b())
                        self.ts(sm[:, 26:27], sm[:, 24:25], -1.0, None, ALU.mult, None, sm.b(), sm.b())
                        self.act(sm[:, 16:24], lg, AF.Exp, sm.b(), sm.b(), bias=sm[:, 26:27], scale=1.0)
                        self.tt(sm[:, 16:24], sm[:, 16:24], sm[:, 8:16], ALU.mult, sm.b(), sm.b())
                        self.rsum(sm[:, 27:28], sm[:, 16:24], sm.b(), sm.b())
                        self.recip(sm[:, 27:28], sm[:, 27:28], sm.b(), sm.b())
                        self.ts(gate[:, t, :], sm[:, 16:24], sm[:, 27:28], None, ALU.mult, None, sm.b(), gate.b())
                first = True
                for e in range(nexp):
                    if moe:
                        gbank, gbb = self.ps[6], self.psb[6]
                        for t in range(4):
                            gb = gbr.next()
                            self.copy("dve", gb.r(slice(None)), gate[:, t, e:e + 1].to_broadcast([128, 128]), gate.b(), gb.b())
                            self.mm(gbank[:, t * 128:(t + 1) * 128], gb.r(slice(None)), self.identr.r(slice(None)), True, True,
                                    gb.b() + self.identr.b(), [gbb], sync=True)
                        gbc = gbcr.next()
                        self.copy("act", gbc[:], gbank[:, :], [gbb], gbc.b())
                        wg_d, wu_d, wd_d = self.I("moe_g")[e], self.I("moe_u")[e], self.I("moe_d")[e]
                    else:
                        wg_d, wu_d, wd_d = self.I("ffn_g"), self.I("ffn_u"), self.I("ffn_d")
                    for blk in range(NFF // FB):
                        at = aT[blk % 2]
                        for jj in range(FB):
                            j = blk * FB + jj
                            it += 1
                            wg, sg_ = self.wload(wgu, wg_d[j], 16)
                            gbk, gbkb = self.ps[it % 2], self.psb[it % 2]
                            for k in range(16):
                                self.mm(gbk[:, :], wg[:, k, :], hT.r((slice(None), k, slice(None))), k == 0, k == 15,
                                        sg_.b() + hT.b(k), [gbkb])
                            wu, su_ = self.wload(wgu, wu_d[j], 16)
                            ubk, ubkb = self.ps[2 + it % 2], self.psb[2 + it % 2]
                            for k in range(16):
                                self.mm(ubk[:, :], wu[:, k, :], hT.r((slice(None), k, slice(None))), k == 0, k == 15,
                                        su_.b() + hT.b(k), [ubkb])
                            sgl = sglr.next()
                            self.act(sgl[:], gbk[:, :], AF.Silu, [gbkb], sgl.b())
                            if moe:
                                t4 = t4r.next()
                                self.tt(t4[:], ubk[:, :], gbc[:], ALU.mult, [ubkb] + gbc.b(), t4.b())
                                self.tt(at.r((slice(None), jj, slice(None))), sgl[:], t4[:], ALU.mult, sgl.b() + t4.b(), at.b(jj))
                            else:
                                self.tt(at.r((slice(None), jj, slice(None))), sgl[:], ubk[:, :], ALU.mult, sgl.b() + [ubkb], at.b(jj))
                        for d in range(16):
                            it += 1
                            wd, sd_ = self.wload(wdr, wd_d[blk, d], FB)
                            dbk, dbkb = self.ps[4 + it % 2], self.psb[4 + it % 2]
                            for jj in range(FB):
                                self.mm(dbk[:, :], wd[:, jj, :], at.r((slice(None), jj, slice(None))), jj == 0, jj == FB - 1,
                                        sd_.b() + at.b(jj), [dbkb])
                            if first:
                                self.copy("dve", a3[:, d, :], dbk[:, :], [dbkb], xa.b())
                            else:
                                self.tt(a3[:, d, :], a3[:, d, :], dbk[:, :], ALU.add, xa.b() + [dbkb], xa.b())
                        first = False
                for d2 in range(16):
                    xr = xring.next()
                    fw.dma("sp", xr[:], self.xT[d2, :, gs], reads=[self.b_x[g][d2]], writes=xr.b())
                    self.stt(a3[:, d2, :], a3[:, d2, :], mp[:, 5, d2:d2 + 1, cd], xr[:], ALU.mult, ALU.add,
                             xa.b() + mp.b() + xr.b(), xa.b())
                    if not final:
                        fw.dma("sp", self.xT[d2, :, gs], a3[:, d2, :], reads=xa.b(), writes=[self.b_x[g][d2]])
                if final:
                    self.norm_stats(a3, xa.b(), 16, D, sqring, rstd, self.ps[7], self.psb[7])
                    for d2 in range(16):
                        xr = xring.next()
                        col = R_FG + d2
                        self.stt(xr[:], a3[:, d2, :], self.vT[:, col:col + 1], rstd[:], ALU.mult, ALU.mult,
                                 xa.b() + self.vT.b() + rstd.b(), xr.b())
                        fw.dma("sp", self.o_yT[d2, :, gs], xr[:], reads=xr.b())
            fw.emit()


def _tiles(W):
    K, M = W.shape
    return np.ascontiguousarray(W.reshape(K // 128, 128, M // 128, 128).transpose(2, 1, 0, 3))


def host_consts():
    c = {}
    c["ident"] = np.eye(128, dtype=np.float32)
    p = np.arange(128)
    d = p % 64
    partner = np.where((d % 32) < 16, p + 16, p - 16)
    pm = np.zeros((128, 128), np.float32)
    pm[partner, p] = 1.0
    c["permM"] = pm
    quarter = 16
    inv = (np.float32(10000.0) ** (-np.arange(quarter, dtype=np.float32) / np.float32(quarter))).astype(np.float32)
    n = np.arange(2048)
    rr = (n // 64).astype(np.float32)
    cc = (n % 64).astype(np.float32)
    ang_r = (rr[:, None] * inv[None, :]).astype(np.float32)
    ang_c = (cc[:, None] * inv[None, :]).astype(np.float32)
    C = np.zeros((128, 2048), np.float32)
    S = np.zeros((128, 2048), np.float32)
    for pp in range(128):
        dd = pp % 64
        j = dd % 16
        ang = ang_r[:, j] if dd < 32 else ang_c[:, j]
        C[pp] = np.cos(ang)
        sgn = -1.0 if (dd % 32) < 16 else 1.0
        S[pp] = sgn * np.sin(ang)
    c["ropeC"] = C
    c["ropeS"] = S
    r = np.arange(128)[:, None]
    cidx = np.arange(128)[None, :]
    m = np.zeros((128, 384), np.float32)
    m[:, 0:128] = np.where(cidx >= r, 0.0, -1e30)
    m[:, 256:384] = np.where(cidx <= r, 0.0, -1e30)
    c["maskA"] = m
    for nm, nseq in (("invc_s", 2048), ("invc_p", 256)):
        t = np.arange(nseq)
        tab = np.zeros((4, nseq), np.float32)
        for gi, win in enumerate(POOL_WINDOWS):
            left = win // 2
            right = win - left - 1
            lo = np.maximum(t - left, 0)
            hi = np.minimum(t + right, nseq - 1) + 1
            tab[gi] = (1.0 / (hi - lo).astype(np.float32)).astype(np.float32)
        c[nm] = tab
    return c


def prep_shared(inp):
    sh = dict(host_consts())
    w_in = inp["w_in"]
    colsA = np.concatenate([
        np.arange(0, 1024),
        np.arange(1024, 1088), np.arange(1024, 1088),
        np.arange(1088, 1152), np.arange(1088, 1152),
        np.arange(1152, 1280),
        np.arange(1280, 1792),
        np.arange(1792, 2048),
        np.arange(2048, 2112), np.arange(2048, 2112),
        np.arange(2112, 3136)])
    sh["w_ada"] = np.stack([_tiles(inp["w_ada"][l]) for l in range(DEPTH)])
    sh["w_inA"] = np.stack([_tiles(w_in[l][:, colsA]) for l in range(DEPTH)])
    sh["w_inG"] = np.stack([_tiles(w_in[l][:, 3136:]) for l in range(DEPTH)])
    cq = [np.arange(h * 192, h * 192 + 128) for h in range(8)]
    for j in range(4):
        cq.append(np.concatenate([np.arange((2 * j) * 192 + 128, (2 * j) * 192 + 192),
                                  np.arange((2 * j + 1) * 192 + 128, (2 * j + 1) * 192 + 192)]))
    cq = np.concatenate(cq)
    sh["w_uq"] = np.stack([_tiles(inp["w_uq"][l][:, cq]) for l in range(DEPTH)])
    ckv = np.concatenate([np.arange(h * 256, h * 256 + 128) for h in range(8)] +
                         [np.arange(h * 256 + 128, h * 256 + 256) for h in range(8)])
    sh["w_ukv"] = np.stack([_tiles(inp["w_ukv"][l][:, ckv]) for l in range(DEPTH)])
    sh["poolw"] = np.stack([np.concatenate([_tiles(inp["pool_w"][l][g]) for g in range(4)]) for l in range(DEPTH)])
    sh["wbr"] = np.stack([np.stack([_tiles(inp[k][l]) for k in ("w_branch_a", "w_branch_b", "w_branch_c")])
                          for l in range(DEPTH)])
    sh["wout"] = np.stack([_tiles(inp["w_out"][l]) for l in range(DEPTH)])
    sh["ffn_g"] = _tiles(inp["ffn_w_gate"][0])
    sh["ffn_u"] = _tiles(inp["ffn_w_up"][0])

    def dtiles(wd):
        return np.ascontiguousarray(wd.reshape(4, FB, 128, 16, 128).transpose(0, 3, 2, 1, 4))
    sh["ffn_d"] = dtiles(inp["ffn_w_down"][0])
    sh["router"] = np.ascontiguousarray(inp["router_w"][0].reshape(16, 128, 8).transpose(1, 0, 2))
    sh["moe_g"] = np.stack([_tiles(inp["moe_w_gate"][0][e]) for e in range(NEXP)])
    sh["moe_u"] = np.stack([_tiles(inp["moe_w_up"][0][e]) for e in range(NEXP)])
    sh["moe_d"] = np.stack([dtiles(inp["moe_w_down"][0][e]) for e in range(NEXP)])
    sh["sink"] = np.ascontiguousarray(inp["attn_sink"])
    return sh


def prep_core(inp, i):
    m = {}
    x = np.concatenate([inp["x_sample"][i], inp["x_prompt"][2 * i], inp["x_prompt"][2 * i + 1]], axis=0)
    m["xinT"] = np.ascontiguousarray(x.T.reshape(16, 128, NT))
    v = np.zeros((NROWS, 128), np.float32)
    v[R_C:R_C + 16] = inp["c"][i].reshape(16, 128)
    v[R_CCTX:R_CCTX + 16] = inp["c_ctx"].reshape(16, 128)
    for l in range(DEPTH):
        v[R_LN1(l):R_LN1(l) + 16] = inp["ln1_g"][l].reshape(16, 128)
        v[R_LN2(l):R_LN2(l) + 16] = inp["ln2_g"][l].reshape(16, 128)
        v[R_BADA(l):R_BADA(l) + 96] = inp["b_ada"][l].reshape(96, 128)
        v[R_QG(l):R_QG(l) + 4] = inp["mla_q_norm_g"][l].reshape(4, 128)
        v[R_KVG(l):R_KVG(l) + 2] = inp["mla_kv_norm_g"][l].reshape(2, 128)
        v[R_PSC(l):R_PSC(l) + 8] = inp["pool_scale"][l].reshape(8, 128)
    v[R_FG:R_FG + 16] = inp["final_g"].reshape(16, 128)
    m["vecs"] = v
    ck = inp["cache_attn_k"][i]
    ckT = ck.transpose(0, 2, 3, 1)
    m["ck2T"] = np.ascontiguousarray(np.concatenate([ckT, ckT], axis=2))
    m["cv"] = np.ascontiguousarray(inp["cache_attn_v"][i].reshape(DEPTH, 512, 128))
    m["cckvT"] = np.ascontiguousarray(inp["cache_mla_ckv"][i].transpose(0, 2, 1).reshape(DEPTH, 2, 128, 512))
    krT = inp["cache_mla_krope"][i].transpose(0, 2, 1)
    m["ckr2T"] = np.ascontiguousarray(np.concatenate([krT, krT], axis=1))
    return m


def build_program():
    P = Prog()
    P.alloc_persistent()
    P.phase_prologue()
    for l in range(DEPTH):
        P.phase_proj(l)
        P.phase_attnA(l)
        P.phase_mla(l)
        P.phase_pool(l)
        P.phase_merge(l)
        P.phase_ffn(l, final=(l == DEPTH - 1))
    return P


def kernel(**inputs):
    inp = {k: np.asarray(v, dtype=np.float32) for k, v in inputs.items()}
    P = build_program()
    sh = prep_shared(inp)
    in_maps = []
    for i in range(NCORES):
        m = prep_core(inp, i)
        m.update(sh)
        in_maps.append({k: m[k] for k in P.din})
    res = run_bass_kernel_spmd(P.nc, in_maps, core_ids=list(range(NCORES)))
    y_prompt = np.zeros((16, 256, D), np.float32)
    y_sample = np.zeros((8, 2048, D), np.float32)
    st_k = np.zeros((16, DEPTH, 256, 2, 64), np.float32)
    st_v = np.zeros((16, DEPTH, 256, 2, 64), np.float32)
    st_ckv = np.zeros((16, DEPTH, 256, 256), np.float32)
    st_kr = np.zeros((16, DEPTH, 256, 64), np.float32)
    for i in range(NCORES):
        r = res.results[i]
        y = np.asarray(r["o_yT"]).reshape(D, NT).T
        y_sample[i] = y[0:2048]
        okT = np.asarray(r["o_kT"])
        ov = np.asarray(r["o_v"]).reshape(DEPTH, 512, 128)
        ockv = np.asarray(r["o_ckvT"]).reshape(DEPTH, 256, 512)
        okr = np.asarray(r["o_krT"])
        for j in range(2):
            b = 2 * i + j
            ts = slice(j * 256, (j + 1) * 256)
            y_prompt[b] = y[2048 + j * 256:2048 + (j + 1) * 256]
            st_k[b] = okT[:, :, :, ts].transpose(0, 3, 1, 2)
            st_v[b] = ov[:, ts, :].reshape(DEPTH, 256, 2, 64)
            st_ckv[b] = ockv[:, :, ts].transpose(0, 2, 1)
            st_kr[b] = okr[:, :, ts].transpose(0, 2, 1)
    return (y_prompt, y_sample, st_k, st_v, st_ckv, st_kr)
```

```python
import contextlib
import numpy as np
import concourse.bass as bass
import concourse.mybir as mybir
from concourse.bass_utils import run_bass_kernel_spmd

F32 = mybir.dt.float32
F32R = mybir.dt.float32r
AF = mybir.ActivationFunctionType
ALU = mybir.AluOpType
AX = mybir.AxisListType

NCORES = 8
D = 2048
NT = 2560
G = 512
NGRP = 5
DEPTH = 2
EPS = 1e-6
NFF = 44
FB = 11
NEXP = 8
NS = 17
DBG_GROUPS = None
SEQS = ((0, 2048, True), (2048, 256, False), (2304, 256, False))
POOL_WINDOWS = (2, 4, 8, 16)

R_C, R_CCTX = 0, 16


def R_LN1(l): return 32 + l * 142
def R_LN2(l): return 32 + l * 142 + 16
def R_BADA(l): return 32 + l * 142 + 32
def R_QG(l): return 32 + l * 142 + 128
def R_KVG(l): return 32 + l * 142 + 132
def R_PSC(l): return 32 + l * 142 + 134


R_FG = 32 + 2 * 142
NROWS = 384


class Buf:
    __slots__ = ("w", "r")

    def __init__(self):
        self.w = None
        self.r = {}


class Eng:
    def __init__(self, name, eng, sem):
        self.name = name
        self.eng = eng
        self.sem = sem
        self.cnt = 0
        self.seen = {}
        self.dsems = []
        self.dvals = []
        self.dn = 0
        self.pend = []
        self.prog = []


class FW:
    def __init__(self, nc, es, ndma_sems=8):
        self.nc = nc
        self.engs = {}
        for name, eng in (("pe", nc.tensor), ("act", nc.scalar), ("dve", nc.vector),
                          ("pool", nc.gpsimd), ("sp", nc.sync)):
            sem = es.enter_context(nc.semaphore("s_" + name))
            self.engs[name] = Eng(name, eng, sem)
        for qn in ("sp", "pool"):
            e = self.engs[qn]
            for i in range(ndma_sems):
                e.dsems.append(es.enter_context(nc.semaphore("d_%s%d" % (qn, i))))
                e.dvals.append(0)
        self.ninst = 0

    def _wait(self, e, ticket):
        sem, val, owner = ticket
        if owner is e:
            if e.name == "pe" or val > e.cnt:
                return
        key = id(sem)
        if e.seen.get(key, 0) >= val:
            return
        e.pend.append((sem, val))
        e.seen[key] = val
        self.ninst += 1

    def _deps(self, e, reads, writes):
        for b in reads:
            if b.w is not None:
                self._wait(e, b.w)
        for b in writes:
            if b.w is not None:
                self._wait(e, b.w)
            for t in b.r.values():
                self._wait(e, t)

    def _mark(self, e, ticket, reads, writes, key=None):
        for b in reads:
            b.r[key or e.name] = ticket
        for b in writes:
            b.w = ticket
            b.r = {}

    def op(self, en, fn, reads=(), writes=(), sync=True):
        e = self.engs[en]
        self._deps(e, reads, writes)
        self.ninst += 1
        waits, e.pend = e.pend, []
        if sync:
            e.cnt += 1
            e.prog.append((waits, fn, e.sem, 1))
            t = (e.sem, e.cnt, e)
        else:
            e.prog.append((waits, fn, None, 0))
            t = (e.sem, e.cnt + 1, e)
        self._mark(e, t, reads, writes)

    def dma(self, qn, out, in_, reads=(), writes=(), **kw):
        e = self.engs[qn]
        slot = e.dn % len(e.dsems)
        e.dn += 1
        sem = e.dsems[slot]
        if e.dvals[slot] > 0:
            self._wait(e, (sem, e.dvals[slot], None))
        self._deps(e, reads, writes)
        e.dvals[slot] += 16
        eng = e.eng
        waits, e.pend = e.pend, []
        e.prog.append((waits, (lambda: eng.dma_start(out=out, in_=in_, **kw)), sem, 16))
        self.ninst += 1
        self._mark(e, (sem, e.dvals[slot], None), reads, writes, key=(e.name, slot))

    def dma_custom(self, qn, fn, reads=(), writes=()):
        e = self.engs[qn]
        slot = e.dn % len(e.dsems)
        e.dn += 1
        sem = e.dsems[slot]
        if e.dvals[slot] > 0:
            self._wait(e, (sem, e.dvals[slot], None))
        self._deps(e, reads, writes)
        e.dvals[slot] += 16
        waits, e.pend = e.pend, []
        e.prog.append((waits, fn, sem, 16))
        self.ninst += 1
        self._mark(e, (sem, e.dvals[slot], None), reads, writes, key=(e.name, slot))

    def emit(self):
        sp = self.engs["sp"]
        for qn in ("sp", "pool"):
            q = self.engs[qn]
            for s, v in zip(q.dsems, q.dvals):
                if v > 0:
                    self._wait(sp, (s, v, None))
        for en in ("pe", "act", "dve", "pool"):
            e = self.engs[en]
            if e.cnt > 0:
                self._wait(sp, (e.sem, e.cnt, None))

        def replay(e):
            eng = e.eng
            for waits, fn, sem, inc in e.prog:
                for s, v in waits:
                    eng.wait_ge(s, v)
                ins = fn()
                if sem is not None:
                    ins.then_inc(sem, inc)
            for s, v in e.pend:
                eng.wait_ge(s, v)
            e.prog = []
            e.pend = []

        with self.nc.Block() as block:
            @block.sync
            def _(x):
                replay(self.engs["sp"])

            @block.tensor
            def _(x):
                replay(self.engs["pe"])

            @block.scalar
            def _(x):
                replay(self.engs["act"])

            @block.vector
            def _(x):
                replay(self.engs["dve"])

            @block.gpsimd
            def _(x):
                replay(self.engs["pool"])


class Tile:
    _n = [0]

    def __init__(self, nc, es, name, shape, split=False):
        Tile._n[0] += 1
        self.t = es.enter_context(nc.sbuf_tensor("sb%d_%s" % (Tile._n[0], name), list(shape), F32))
        self.split = split
        if split:
            self.bufs = [Buf() for _ in range(shape[1])]
        else:
            self.bufs = [Buf()]

    def __getitem__(self, idx):
        return self.t[idx]

    def r(self, idx):
        return self.t[idx].bitcast(F32R)

    def b(self, i=None):
        if self.split and i is not None:
            return [self.bufs[i]]
        return list(self.bufs)


class Ring:
    def __init__(self, nc, es, name, shape, n):
        self.tiles = [Tile(nc, es, "%s%d" % (name, i), shape) for i in range(n)]
        self.i = 0

    def next(self):
        t = self.tiles[self.i % len(self.tiles)]
        self.i += 1
        return t


class Prog:
    def __init__(self, stop_after=None, moe_experts=NEXP, scratch_in=(), moe_sparse=True):
        self.stop_after = stop_after
        self.moe_experts = moe_experts
        self.nc = nc = bass.Bass("TRN2", target_bir_lowering=False)
        self.es = es = contextlib.ExitStack()
        self.fw = FW(nc, es)
        self.din = {}
        self.evac_i = 0

        self.ishapes = {}

        def inp(name, shape):
            self.ishapes[name] = list(shape)

        def outp(name, shape):
            return nc.dram_tensor(name, list(shape), F32, kind="ExternalOutput").ap()

        def scr(name, shape):
            return nc.dram_tensor(name, list(shape), F32, kind="Internal").ap()

        inp("xinT", [16, 128, NT])
        inp("vecs", [NROWS, 128])
        inp("sink", [DEPTH, 16])
        inp("ident", [128, 128])
        inp("permM", [128, 128])
        inp("ropeC", [128, 2048])
        inp("ropeS", [128, 2048])
        inp("maskA", [128, 384])
        inp("invc_s", [4, 2048])
        inp("invc_p", [4, 256])
        inp("ck2T", [DEPTH, 2, 128, 512])
        inp("cv", [DEPTH, 512, 128])
        inp("cckvT", [DEPTH, 2, 128, 512])
        inp("ckr2T", [DEPTH, 128, 512])
        inp("w_ada", [DEPTH, 96, 128, 16, 128])
        inp("w_inA", [DEPTH, 26, 128, 16, 128])
        inp("w_inG", [DEPTH, 48, 128, 16, 128])
        inp("w_uq", [DEPTH, 12, 128, 4, 128])
        inp("w_ukv", [DEPTH, 16, 128, 2, 128])
        inp("poolw", [DEPTH, 8, 128, 2, 128])
        inp("wbr", [DEPTH, 3, 16, 128, 8, 128])
        inp("wout", [DEPTH, 16, 128, 16, 128])
        inp("ffn_g", [NFF, 128, 16, 128])
        inp("ffn_u", [NFF, 128, 16, 128])
        inp("ffn_d", [4, 16, 128, FB, 128])
        inp("router", [128, 16, 8])
        inp("moe_g", [NEXP, NFF, 128, 16, 128])
        inp("moe_u", [NEXP, NFF, 128, 16, 128])
        inp("moe_d", [NEXP, 4, 16, 128, FB, 128])
        inp("Umat", [128, 128])
        inp("tokid", [128, NT // 128])
        inp("svals", [128, NS])
        inp("wbase", [128, NFF])
        inp("wdbase", [128, 64])
        inp("Tab0", [NS * 512, 16])
        self.o_yT = outp("o_yT", [16, 128, NT])
        self.o_kT = outp("o_kT", [DEPTH, 2, 64, 512])
        self.o_v = outp("o_v", [DEPTH, 4, 128, 128])
        self.o_ckvT = outp("o_ckvT", [DEPTH, 2, 128, 512])
        self.o_krT = outp("o_krT", [DEPTH, 64, 512])
        def mk(name, shape):
            if name in scratch_in:
                return nc.dram_tensor(name, list(shape), F32, kind="ExternalInput").ap()
            return (outp if stop_after is not None else scr)(name, shape)
        self.xT = mk("s_xT", [16, 128, NT])
        self.qaT = mk("s_qaT", [8, 128, NT])
        self.kaT2 = mk("s_kaT2", [2, 128, NT])
        self.vaTok = mk("s_vaTok", [NT // 128, 128, 128])
        self.qnT = mk("s_qnT", [8, 128, NT])
        self.qrT = mk("s_qrT", [4, 128, NT])
        self.ckvT = mk("s_ckvT", [2, 128, NT])
        self.krT2 = mk("s_krT2", [128, NT])
        self.uT = mk("s_uT", [8, 128, NT])
        self.oaT = mk("s_oaT", [8, 128, NT])
        self.obT = mk("s_obT", [8, 128, NT])
        self.ocT = mk("s_ocT", [8, 128, NT])
        if moe_sparse:
            self.hTok = scr("s_hTok", [NT + 128, D])
            self.Ybuf = scr("s_Y", [2 * NT + 128, D])
            self.Tab = nc.dram_tensor("s_Tab", [NS * 512, 16], mybir.dt.int32, kind="Internal").ap()
        self.b_x = [[Buf() for _ in range(16)] for _ in range(NGRP)]
        self.b_scr = {k: Buf() for k in ("qaT", "kaT2", "vaTok", "qnT", "qrT", "ckvT", "krT2", "uT",
                                         "oaT", "obT", "ocT")}
        self.ps = [es.enter_context(nc.psum_tensor("ps%d" % i, [128, 512], F32)) for i in range(8)]
        self.psb = [Buf() for _ in range(8)]

    def I(self, name, dt=F32):
        if name not in self.din:
            self.din[name] = self.nc.dram_tensor(name, self.ishapes[name], dt, kind="ExternalInput").ap()
        return self.din[name]

    def mm(self, out, lhsT, rhs, start, stop, reads, writes, sync=None):
        nc = self.nc
        if sync is None:
            sync = stop
        self.fw.op("pe", lambda: nc.tensor.matmul(out, lhsT=lhsT, rhs=rhs, start=start, stop=stop),
                   reads=reads, writes=writes, sync=sync)

    def tr(self, out, in_, reads, writes, sync=True):
        nc = self.nc
        ident = self.ident[:]
        self.fw.op("pe", lambda: nc.tensor.transpose(out=out, in_=in_, identity=ident),
                   reads=reads + self.ident.b(), writes=writes, sync=sync)

    def copy(self, en, out, in_, reads, writes):
        nc = self.nc
        if en == "act":
            self.fw.op("act", lambda: nc.scalar.copy(out=out, in_=in_), reads=reads, writes=writes)
        elif en == "dve":
            self.fw.op("dve", lambda: nc.vector.tensor_copy(out=out, in_=in_), reads=reads, writes=writes)
        else:
            self.fw.op("pool", lambda: nc.gpsimd.tensor_copy(out=out, in_=in_), reads=reads, writes=writes)

    def evac_eng(self):
        self.evac_i += 1
        return "act" if self.evac_i % 2 else "dve"

    def act(self, out, in_, func, reads, writes, bias=None, scale=None, accum_out=None):
        nc = self.nc
        kw = {}
        if bias is not None:
            kw["bias"] = bias
        if scale is not None:
            kw["scale"] = scale
        if accum_out is not None:
            kw["accum_out"] = accum_out
        self.fw.op("act", lambda: nc.scalar.activation(out=out, in_=in_, func=func, **kw),
                   reads=reads, writes=writes)

    def tt(self, out, in0, in1, op, reads, writes, en="dve"):
        nc = self.nc
        eng = nc.vector if en == "dve" else nc.gpsimd
        self.fw.op(en, lambda: eng.tensor_tensor(out=out, in0=in0, in1=in1, op=op), reads=reads, writes=writes)

    def ts(self, out, in0, s1, s2, op0, op1, reads, writes):
        nc = self.nc
        if op1 is None:
            self.fw.op("dve", lambda: nc.vector.tensor_scalar(out=out, in0=in0, scalar1=s1, scalar2=None, op0=op0),
                       reads=reads, writes=writes)
        else:
            self.fw.op("dve", lambda: nc.vector.tensor_scalar(out=out, in0=in0, scalar1=s1, scalar2=s2,
                                                              op0=op0, op1=op1), reads=reads, writes=writes)

    def stt(self, out, in0, scalar, in1, op0, op1, reads, writes):
        nc = self.nc
        self.fw.op("dve", lambda: nc.vector.scalar_tensor_tensor(out=out, in0=in0, scalar=scalar, in1=in1,
                                                                 op0=op0, op1=op1), reads=reads, writes=writes)

    def wload(self, ring, dram_tile, nk, ncol=128):
        slot = ring.next()
        dst = slot.t[:, 0:nk * ncol].rearrange("p (k m) -> p k m", k=nk)
        self.fw.dma("pool", dst.bitcast(F32R), dram_tile, writes=slot.b())
        return dst.bitcast(F32R), slot

    def alloc_persistent(self):
        nc, es = self.nc, self.es
        self.ident = Tile(nc, es, "ident", [128, 128])
        self.ones = Tile(nc, es, "ones", [128, 128])
        self.identr = Tile(nc, es, "identr", [128, 128])
        self.vT = Tile(nc, es, "vT", [128, NROWS])
        self.modp = [Tile(nc, es, "modp%d" % l, [128, 6, 16, 2]) for l in range(DEPTH)]
        self.sinkb = Tile(nc, es, "sinkb", [128, DEPTH * 16])

    def phase_prologue(self):
        nc, fw = self.nc, self.fw
        with contextlib.ExitStack() as es:
            vrows = Tile(nc, es, "vrows", [128, 3, 128])
            scT = Tile(nc, es, "scT", [128, 32])
            modT = Tile(nc, es, "modT", [128, 96, 2])
            wring = Ring(nc, es, "wada", [128, 2048], 4)
            fw.dma("sp", self.ident[:], self.I("ident"), writes=self.ident.b())
            fw.dma("sp", vrows[:], self.I("vecs").rearrange("(a p) f -> p a f", p=128), writes=vrows.b())
            fw.dma("sp", self.sinkb[:],
                   self.I("sink").rearrange("l h -> (l h)").partition_broadcast(128), writes=self.sinkb.b())
            ones32 = Tile(nc, es, "ones32", [128, 128])
            fw.op("dve", lambda: nc.vector.memset(ones32[:], 1.0), writes=ones32.b())
            self.copy("act", self.ones.r(slice(None)), ones32[:], ones32.b(), self.ones.b())
            self.copy("act", self.identr.r(slice(None)), self.ident[:], self.ident.b(), self.identr.b())
            for a in range(3):
                self.tr(self.ps[0][:, a * 128:(a + 1) * 128], vrows[:, a, :], vrows.b(), [self.psb[0]])
            self.copy("dve", self.vT[:], self.ps[0][:, 0:384], [self.psb[0]], self.vT.b())
            self.act(scT.r(slice(None)), self.vT[:, 0:32], AF.Silu, self.vT.b(), scT.b())
            sc3 = scT.r(slice(None)).rearrange("p (c k) -> p k c", c=2)
            for l in range(DEPTH):
                for j in range(96):
                    w, slot = self.wload(wring, self.I("w_ada")[l, j], 16)
                    bank = self.ps[1 + (j % 2)]
                    bb = self.psb[1 + (j % 2)]
                    for k in range(16):
                        self.mm(bank[:, 0:2], w[:, k, :], sc3[:, k, :], k == 0, k == 15,
                                slot.b() + scT.b(), [bb])
                    col = R_BADA(l) + j
                    self.act(modT[:, j, :], bank[:, 0:2], AF.Identity, [bb] + self.vT.b(), modT.b(),
                             bias=self.vT[:, col:col + 1])
                mp = self.modp[l]
                for s in range(2):
                    lnr = R_LN1(l) if s == 0 else R_LN2(l)
                    sh, sc, gt = 3 * s, 3 * s + 1, 3 * s + 2
                    for cd in range(2):
                        self.stt(mp[:, 3 * s + 0, :, cd], modT[:, sc * 16:(sc + 1) * 16, cd], 1.0,
                                 self.vT[:, lnr:lnr + 16], ALU.add, ALU.mult,
                                 modT.b() + self.vT.b(), mp.b())
                        self.copy("dve", mp[:, 3 * s + 1, :, cd], modT[:, sh * 16:(sh + 1) * 16, cd], modT.b(), mp.b())
                        self.copy("dve", mp[:, 3 * s + 2, :, cd], modT[:, gt * 16:(gt + 1) * 16, cd], modT.b(), mp.b())
            xt = Ring(nc, es, "xcp", [128, 16 * 512], 2)
            for g in range(NGRP):
                t = xt.next()
                v = t.t[:].rearrange("p (c n) -> p c n", c=16)
                fw.dma("sp", v, self.I("xinT")[:, :, g * G:(g + 1) * G].rearrange("c p n -> p c n"), writes=t.b())
                fw.dma("sp", self.xT[:, :, g * G:(g + 1) * G].rearrange("c p n -> p c n"), v,
                       reads=t.b(), writes=self.b_x[g])
            fw.emit()

    def norm_stats(self, x3, xb, nchunk, nfeat, sqring, rstd, bank, bb):
        nc = self.nc
        for c in range(nchunk):
            sq = sqring.next()
            self.act(sq.r(slice(None)), x3[:, c, :], AF.Square, xb, sq.b())
            self.mm(bank[:, :], self.ones.r(slice(None)), sq.r(slice(None)), c == 0, c == nchunk - 1,
                    self.ones.b() + sq.b(), [bb], sync=True)
        self.act(rstd[:], bank[:, :], AF.Sqrt, [bb] + self.epsb.b(), rstd.b(), bias=self.epsb[:, 0:1], scale=1.0 / nfeat)
        self.fw.op("dve", lambda: nc.vector.reciprocal(out=rstd[:], in_=rstd[:]), reads=rstd.b(), writes=rstd.b())

    def norm_mod(self, g, l, s, xg, hT, sqring, tmpring, rstd, bank, bb):
        cd = 0 if g < 4 else 1
        mp = self.modp[l]
        x3 = xg.t[:].rearrange("p (c n) -> p c n", c=16)
        self.norm_stats(x3, xg.b(), 16, D, sqring, rstd, bank, bb)
        for c in range(16):
            tmp = tmpring.next()
            self.tt(tmp[:], x3[:, c, :], rstd[:], ALU.mult, xg.b() + rstd.b(), tmp.b())
            self.act(hT.r((slice(None), c, slice(None))), tmp[:], AF.Identity, tmp.b() + mp.b(), hT.b(c),
                     bias=mp[:, 3 * s + 1, c:c + 1, cd], scale=mp[:, 3 * s + 0, c:c + 1, cd])

    def load_x(self, g, xg):
        v = xg.t[:].rearrange("p (c n) -> p c n", c=16)
        self.fw.dma("sp", v, self.xT[:, :, g * G:(g + 1) * G].rearrange("c p n -> p c n"),
                    reads=self.b_x[g], writes=xg.b())

    def phase_proj(self, l):
        nc, fw = self.nc, self.fw
        with contextlib.ExitStack() as es:
            xg = Tile(nc, es, "xg", [128, 16 * 512])
            hT = Tile(nc, es, "hT", [128, 16, 512], split=True)
            wring = Ring(nc, es, "w1", [128, 2048], 4)
            wuq = Tile(nc, es, "wuq", [128, 12, 512])
            ropeC = Tile(nc, es, "ropeC", [128, 2048])
            ropeS = Tile(nc, es, "ropeS", [128, 2048])
            permM = Tile(nc, es, "permM", [128, 128])
            cqs = Tile(nc, es, "cqs", [128, 4, 512])
            cqn = Tile(nc, es, "cqn", [128, 4, 512])
            ckvs = Tile(nc, es, "ckvs", [128, 2, 512])
            stg = Ring(nc, es, "stg", [128, 512], 4)
            sqring = Ring(nc, es, "sq", [128, 512], 3)
            tmpring = Ring(nc, es, "tmp", [128, 512], 3)
            xsring = Ring(nc, es, "xs", [128, 512], 2)
            t1ring = Ring(nc, es, "t1", [128, 512], 2)
            rstd = Tile(nc, es, "rstd", [128, 512])
            rstd2 = Tile(nc, es, "rstd2", [128, 512])
            vtok = Ring(nc, es, "vtok", [128, 512], 2)
            self.epsb = Tile(nc, es, "epsb", [128, 1])
            fw.op("dve", lambda: nc.vector.memset(self.epsb[:], EPS), writes=self.epsb.b())
            fw.dma("sp", ropeC[:], self.I("ropeC"), writes=ropeC.b())
            fw.dma("sp", ropeS[:], self.I("ropeS"), writes=ropeS.b())
            fw.dma("pool", permM.r(slice(None)), self.I("permM"), writes=permM.b())
            fw.dma("pool", wuq.t[:].rearrange("p a (k m) -> p a k m", k=4).bitcast(F32R),
                   self.I("w_uq")[l].rearrange("a p k m -> p a k m"), writes=wuq.b())
            wuq4 = wuq.t[:].rearrange("p a (k m) -> p a k m", k=4).bitcast(F32R)
            bi = [0]

            def nextbank():
                bi[0] += 1
                i = bi[0] % 4
                return self.ps[i], self.psb[i]

            def finish_chunk(bank, bb, g, rope, dst, dbuf, P=128, dst2=None):
                st = stg.next()
                if rope and g < 4:
                    xs = xsring.next()
                    self.copy("act", xs.r(slice(None))[0:P], bank[0:P, :], [bb], xs.b())
                    pb, pbb = self.ps[4 + (bi[0] % 2)], self.psb[4 + (bi[0] % 2)]
                    self.mm(pb[0:P, :], permM.r(slice(None))[0:P, 0:P], xs.r(slice(None))[0:P], True, True,
                            permM.b() + xs.b(), [pbb])
                    t1 = t1ring.next()
                    self.tt(t1[0:P], xs[0:P], ropeC[0:P, g * G:(g + 1) * G], ALU.mult, xs.b() + ropeC.b(), t1.b())
                    self.tt(st[0:P], pb[0:P, :], ropeS[0:P, g * G:(g + 1) * G], ALU.mult, [pbb] + ropeS.b(), st.b())
                    self.tt(st[0:P], st[0:P], t1[0:P], ALU.add, st.b() + t1.b(), st.b())
                else:
                    self.copy(self.evac_eng(), st[0:P], bank[0:P, :], [bb], st.b())
                fw.dma("sp", dst, st[0:P], reads=st.b(), writes=[dbuf])
                if dst2 is not None:
                    fw.dma("sp", dst2, st[0:dst2.shape[0]], reads=st.b())
                return st

            for g in (DBG_GROUPS or range(NGRP)):
                gs = slice(g * G, (g + 1) * G)
                self.load_x(g, xg)
                self.norm_mod(g, l, 0, xg, hT, sqring, tmpring, rstd, self.ps[7], self.psb[7])
                for ch in range(26):
                    w, slot = self.wload(wring, self.I("w_inA")[l, ch], 16)
                    bank, bb = nextbank()
                    for k in range(16):
                        self.mm(bank[:, :], w[:, k, :], hT.r((slice(None), k, slice(None))), k == 0, k == 15,
                                slot.b() + hT.b(k), [bb])
                    if ch < 8:
                        finish_chunk(bank, bb, g, True, self.qaT[ch, :, gs], self.b_scr["qaT"])
                    elif ch < 10:
                        d2 = self.o_kT[l, ch - 8, :, :] if g == 4 else None
                        finish_chunk(bank, bb, g, True, self.kaT2[ch - 8, :, gs], self.b_scr["kaT2"], dst2=d2)
                    elif ch == 10:
                        st = stg.next()
                        self.copy(self.evac_eng(), st[:], bank[:, :], [bb], st.b())
                        tb, tbb = self.ps[6], self.psb[6]
                        for t in range(4):
                            self.tr(tb[:, t * 128:(t + 1) * 128], st[:, t * 128:(t + 1) * 128], st.b(), [tbb])
                        vt = vtok.next()
                        self.copy(self.evac_eng(), vt[:], tb[:, :], [tbb], vt.b())
                        fw.dma("sp", self.vaTok[g * 4:(g + 1) * 4].rearrange("t p d -> p t d"),
                               vt.t[:].rearrange("p (t d) -> p t d", t=4), reads=vt.b(), writes=[self.b_scr["vaTok"]])
                        if g == 4:
                            fw.dma("sp", self.o_v[l].rearrange("t p d -> p t d"),
                                   vt.t[:].rearrange("p (t d) -> p t d", t=4), reads=vt.b())
                    elif ch < 15:
                        self.copy(self.evac_eng(), cqs[:, ch - 11, :], bank[:, :], [bb], cqs.b())
                        if ch == 14:
                            self.norm_stats(cqs.t[:], cqs.b(), 4, 512, sqring, rstd2, self.ps[7], self.psb[7])
                            for c in range(4):
                                col = R_QG(l) + c
                                self.stt(cqn.r((slice(None), c, slice(None))), cqs[:, c, :], self.vT[:, col:col + 1],
                                         rstd2[:], ALU.mult, ALU.mult, cqs.b() + rstd2.b() + self.vT.b(), cqn.b())
                            for a in range(12):
                                bank2, bb2 = nextbank()
                                for k in range(4):
                                    self.mm(bank2[:, :], wuq4[:, a, k, :], cqn.r((slice(None), k, slice(None))),
                                            k == 0, k == 3, wuq.b() + cqn.b(), [bb2])
                                if a < 8:
                                    finish_chunk(bank2, bb2, g, False, self.qnT[a, :, gs], self.b_scr["qnT"])
                                else:
                                    finish_chunk(bank2, bb2, g, True, self.qrT[a - 8, :, gs], self.b_scr["qrT"])
                    elif ch < 17:
                        self.copy(self.evac_eng(), ckvs[:, ch - 15, :], bank[:, :], [bb], ckvs.b())
                        if ch == 16:
                            self.norm_stats(ckvs.t[:], ckvs.b(), 2, 256, sqring, rstd2, self.ps[7], self.psb[7])
                            for c in range(2):
                                col = R_KVG(l) + c
                                st = stg.next()
                                self.stt(st[:], ckvs[:, c, :], self.vT[:, col:col + 1], rstd2[:], ALU.mult, ALU.mult,
                                         ckvs.b() + rstd2.b() + self.vT.b(), st.b())
                                fw.dma("sp", self.ckvT[c, :, gs], st[:], reads=st.b(), writes=[self.b_scr["ckvT"]])
                                if g == 4:
                                    fw.dma("sp", self.o_ckvT[l, c], st[:], reads=st.b())
                    elif ch == 17:
                        d2 = self.o_krT[l] if g == 4 else None
                        finish_chunk(bank, bb, g, True, self.krT2[:, gs], self.b_scr["krT2"], dst2=d2)
                    else:
                        finish_chunk(bank, bb, g, False, self.uT[ch - 18, :, gs], self.b_scr["uT"])
            fw.emit()

    def rmax(self, out, in_, reads, writes):
        nc = self.nc
        self.fw.op("dve", lambda: nc.vector.reduce_max(out=out, in_=in_, axis=AX.X), reads=reads, writes=writes)

    def rsum(self, out, in_, reads, writes):
        nc = self.nc
        self.fw.op("dve", lambda: nc.vector.reduce_sum(out=out, in_=in_, axis=AX.X), reads=reads, writes=writes)

    def recip(self, out, in_, reads, writes):
        nc = self.nc
        self.fw.op("dve", lambda: nc.vector.reciprocal(out=out, in_=in_), reads=reads, writes=writes)

    def phase_attnA(self, l, seqs=SEQS, qb_limit=None):
        nc, fw = self.nc, self.fw
        with contextlib.ExitStack() as es:
            kT2 = Tile(nc, es, "kT2", [128, 2, 2560])
            Vt = Tile(nc, es, "Vt", [128, 20, 128])
            qring = Ring(nc, es, "qc", [128, 2048], 2)
            maskA = Tile(nc, es, "maskA", [128, 384])
            Pring = Ring(nc, es, "Pa", [128, 896], 2)
            PTring = Ring(nc, es, "PTa", [128, 896], 2)
            slring = Ring(nc, es, "sl", [128, 384], 2)
            small = Ring(nc, es, "sma", [128, 8], 4)
            rdring = Ring(nc, es, "rda", [128, 2], 2)
            Oqring = Ring(nc, es, "Oqa", [128, 128], 2)
            ostg = Ring(nc, es, "ostga", [128, 512], 2)
            fw.dma("sp", maskA[:], self.I("maskA"), writes=maskA.b())
            for (tok0, n, ctx) in seqs:
                nqb = n // 128
                for g2 in range(2):
                    fw.dma("pool", kT2.r((slice(None), g2, slice(0, n))), self.kaT2[g2, :, tok0:tok0 + n],
                           reads=[self.b_scr["kaT2"]], writes=kT2.b())
                    if ctx:
                        fw.dma("pool", kT2.r((slice(None), g2, slice(n, n + 512))), self.I("ck2T")[l, g2],
                               writes=kT2.b())
                fw.dma("pool", Vt.r((slice(None), slice(0, nqb), slice(None))),
                       self.vaTok[tok0 // 128:tok0 // 128 + nqb].rearrange("t p d -> p t d"),
                       reads=[self.b_scr["vaTok"]], writes=Vt.b())
                if ctx:
                    fw.dma("pool", Vt.r((slice(None), slice(nqb, nqb + 4), slice(None))),
                           self.I("cv")[l].rearrange("(t p) d -> p t d", p=128), writes=Vt.b())
                for c in range(8):
                    g2 = c // 4
                    qc = qring.next()
                    fw.dma("pool", qc.r((slice(None), slice(0, n))), self.qaT[c, :, tok0:tok0 + n],
                           reads=[self.b_scr["qaT"]], writes=qc.b())
                    st = None
                    qbs = list(range(nqb)) if qb_limit is None else list(range(min(nqb, qb_limit)))
                    for qb in qbs:
                        if ctx:
                            kb_lo, kb_hi = max(qb - 1, 0), min(qb + 1, nqb - 1)
                        else:
                            kb_lo, kb_hi = 0, nqb - 1
                        nl = (kb_hi - kb_lo + 1) * 128
                        blocks = list(range(kb_lo, kb_hi + 1)) + ([nqb + i for i in range(4)] if ctx else [])
                        nb = len(blocks)
                        rd = rdring.next()
                        obank, obb = self.ps[6], self.psb[6]
                        for hh in range(2):
                            h = 2 * c + hh
                            pb = hh * 64
                            sbank, sbb = self.ps[hh * 2], self.psb[hh * 2]
                            cbank, cbb = self.ps[hh * 2 + 1], self.psb[hh * 2 + 1]
                            lq = qc.r((slice(pb, pb + 64), slice(qb * 128, (qb + 1) * 128)))
                            self.mm(sbank[:, 0:nl], lq, kT2.r((slice(pb, pb + 64), g2, slice(kb_lo * 128, kb_lo * 128 + nl))),
                                    True, True, qc.b() + kT2.b(), [sbb])
                            if ctx:
                                self.mm(cbank[:, :], lq, kT2.r((slice(pb, pb + 64), g2, slice(n, n + 512))),
                                        True, True, qc.b() + kT2.b(), [cbb])
                            sm = small.next()
                            Pt = Pring.next()
                            if ctx:
                                sl = slring.next()
                                mlo = 128 if qb == 0 else 0
                                self.tt(sl[:, 0:nl], sbank[:, 0:nl], maskA[:, mlo:mlo + nl], ALU.add,
                                        [sbb] + maskA.b(), sl.b())
                                self.rmax(sm[:, 0:1], sl[:, 0:nl], sl.b(), sm.b())
                                self.rmax(sm[:, 1:2], cbank[:, :], [cbb], sm.b())
                                self.tt(sm[:, 2:3], sm[:, 0:1], sm[:, 1:2], ALU.max, sm.b(), sm.b())
                                src_loc, src_b = sl[:, 0:nl], sl.b()
                            else:
                                self.rmax(sm[:, 2:3], sbank[:, 0:nl], [sbb], sm.b())
                                src_loc, src_b = sbank[:, 0:nl], [sbb]
                            scol = l * 16 + h
                            self.ts(sm[:, 3:4], sm[:, 2:3], 0.125, self.sinkb[:, scol:scol + 1], ALU.mult, ALU.max,
                                    sm.b() + self.sinkb.b(), sm.b())
                            self.ts(sm[:, 4:5], sm[:, 3:4], -1.0, None, ALU.mult, None, sm.b(), sm.b())
                            self.act(Pt[:, 0:nl], src_loc, AF.Exp, src_b + sm.b(), Pt.b() + sm.b(),
                                     bias=sm[:, 4:5], scale=0.125, accum_out=sm[:, 5:6])
                            if ctx:
                                self.act(Pt[:, nl:nl + 512], cbank[:, :], AF.Exp, [cbb] + sm.b(), Pt.b() + sm.b(),
                                         bias=sm[:, 4:5], scale=0.125, accum_out=sm[:, 6:7])
                            self.act(sm[:, 7:8], self.sinkb[:, scol:scol + 1], AF.Exp, self.sinkb.b() + sm.b(), sm.b(),
                                     bias=sm[:, 4:5], scale=1.0)
                            self.tt(sm[:, 5:6], sm[:, 5:6], sm[:, 7:8], ALU.add, sm.b(), sm.b())
                            if ctx:
                                self.tt(sm[:, 5:6], sm[:, 5:6], sm[:, 6:7], ALU.add, sm.b(), sm.b())
                            self.recip(rd[:, hh:hh + 1], sm[:, 5:6], sm.b(), rd.b())
                            PT = PTring.next()
                            for i0 in range(0, nb, 4):
                                cnt = min(4, nb - i0)
                                tb, tbb = self.ps[4 + (i0 // 4) % 2], self.psb[4 + (i0 // 4) % 2]
                                for i in range(i0, i0 + cnt):
                                    self.tr(tb[:, (i - i0) * 128:(i - i0 + 1) * 128], Pt[:, i * 128:(i + 1) * 128],
                                            Pt.b(), [tbb])
                                self.copy(self.evac_eng(), PT.r((slice(None), slice(i0 * 128, (i0 + cnt) * 128))),
                                          tb[:, 0:cnt * 128], [tbb], PT.b())
                            for i, blk in enumerate(blocks):
                                self.mm(obank[:, hh * 64:(hh + 1) * 64], PT.r((slice(None), slice(i * 128, (i + 1) * 128))),
                                        Vt.r((slice(None), blk, slice(g2 * 64, (g2 + 1) * 64))), i == 0, i == nb - 1,
                                        PT.b() + Vt.b(), [obb], sync=True)
                        Oq = Oqring.next()
                        self.ts(Oq[:, 0:64], obank[:, 0:64], rd[:, 0:1], None, ALU.mult, None, [obb] + rd.b(), Oq.b())
                        self.ts(Oq[:, 64:128], obank[:, 64:128], rd[:, 1:2], None, ALU.mult, None, [obb] + rd.b(), Oq.b())
                        tb, tbb = self.ps[7], self.psb[7]
                        self.tr(tb[:, 0:128], Oq[:, :], Oq.b(), [tbb])
                        if qb % 4 == 0:
                            st = ostg.next()
                        self.copy(self.evac_eng(), st[:, (qb % 4) * 128:(qb % 4 + 1) * 128], tb[:, 0:128], [tbb], st.b())
                        if qb % 4 == 3 or qb == qbs[-1]:
                            q0 = (qb // 4) * 512
                            wd = (qb % 4 + 1) * 128
                            fw.dma("sp", self.oaT[c, :, tok0 + q0:tok0 + q0 + wd], st[:, 0:wd], reads=st.b(),
                                   writes=[self.b_scr["oaT"]])
            fw.emit()

    def phase_mla(self, l, seqs=SEQS, qb_limit=None, heads=range(8)):
        nc, fw = self.nc, self.fw
        scale = float((128 + 64) ** -0.5)
        with contextlib.ExitStack() as es:
            ckvA = Tile(nc, es, "ckvA", [128, 2, 2560])
            krA = Tile(nc, es, "krA", [128, 2560])
            wukv = Tile(nc, es, "wukv", [128, 16, 256])
            knT = Tile(nc, es, "knT", [128, 2560])
            vh = Tile(nc, es, "vh", [128, 20, 128])
            qnr = Ring(nc, es, "qnm", [128, 2048], 2)
            qrr = Ring(nc, es, "qrm", [128, 2048], 2)
            Pring = Ring(nc, es, "Pm", [128, 2560], 2)
            PTring = Ring(nc, es, "PTm", [128, 2560], 2)
            small = Ring(nc, es, "smm", [128, 16], 4)
            Oqring = Ring(nc, es, "Oqm", [128, 128], 2)
            ostg = Ring(nc, es, "ostgm", [128, 512], 2)
            wukv4 = wukv.t[:].rearrange("p a (k m) -> p a k m", k=2).bitcast(F32R)
            fw.dma("pool", wukv4, self.I("w_ukv")[l].rearrange("a p k m -> p a k m"), writes=wukv.b())
            for (tok0, n, ctx) in seqs:
                nqb = n // 128
                nk = n + (512 if ctx else 0)
                nkb = nk // 128
                kgs = [(s, min(512, nk - s)) for s in range(0, nk, 512)]
                ng = len(kgs)
                for k in range(2):
                    fw.dma("pool", ckvA.r((slice(None), k, slice(0, n))), self.ckvT[k, :, tok0:tok0 + n],
                           reads=[self.b_scr["ckvT"]], writes=ckvA.b())
                    if ctx:
                        fw.dma("pool", ckvA.r((slice(None), k, slice(n, n + 512))), self.I("cckvT")[l, k], writes=ckvA.b())
                fw.dma("pool", krA.r((slice(None), slice(0, n))), self.krT2[:, tok0:tok0 + n],
                       reads=[self.b_scr["krT2"]], writes=krA.b())
                if ctx:
                    fw.dma("pool", krA.r((slice(None), slice(n, n + 512))), self.I("ckr2T")[l], writes=krA.b())
                qr, qr_pair = None, -1
                for h in heads:
                    pb = (h % 2) * 64
                    for gi, (s, w) in enumerate(kgs):
                        bank, bb = self.ps[gi % 4], self.psb[gi % 4]
                        for k in range(2):
                            self.mm(bank[:, 0:w], wukv4[:, h, k, :], ckvA.r((slice(None), k, slice(s, s + w))),
                                    k == 0, k == 1, wukv.b() + ckvA.b(), [bb])
                        self.copy(self.evac_eng(), knT.r((slice(None), slice(s, s + w))), bank[:, 0:w], [bb], knT.b())
                    for kb0 in range(0, nkb, 4):
                        cnt = min(4, nkb - kb0)
                        bank, bb = self.ps[4 + (kb0 // 4) % 2], self.psb[4 + (kb0 // 4) % 2]
                        for kb in range(kb0, kb0 + cnt):
                            for k in range(2):
                                self.mm(bank[:, (kb - kb0) * 128:(kb - kb0 + 1) * 128],
                                        ckvA.r((slice(None), k, slice(kb * 128, (kb + 1) * 128))), wukv4[:, 8 + h, k, :],
                                        k == 0, k == 1, wukv.b() + ckvA.b(), [bb], sync=(k == 1 and kb == kb0 + cnt - 1))
                        self.copy(self.evac_eng(), vh.r((slice(None), slice(kb0, kb0 + cnt), slice(None))),
                                  bank[:, 0:cnt * 128].rearrange("p (a d) -> p a d", a=cnt), [bb], vh.b())
                    qn = qnr.next()
                    fw.dma("pool", qn.r((slice(None), slice(0, n))), self.qnT[h, :, tok0:tok0 + n],
                           reads=[self.b_scr["qnT"]], writes=qn.b())
                    if qr_pair != h // 2:
                        qr_pair = h // 2
                        qr = qrr.next()
                        fw.dma("pool", qr.r((slice(None), slice(0, n))), self.qrT[h // 2, :, tok0:tok0 + n],
                               reads=[self.b_scr["qrT"]], writes=qr.b())
                    st = None
                    qbs = list(range(nqb)) if qb_limit is None else list(range(min(nqb, qb_limit)))
                    for qb in qbs:
                        qsl = slice(qb * 128, (qb + 1) * 128)
                        sm = small.next()
                        Pt = Pring.next()
                        for gi, (s, w) in enumerate(kgs):
                            bank, bb = self.ps[gi], self.psb[gi]
                            self.mm(bank[:, 0:w], qn.r((slice(None), qsl)), knT.r((slice(None), slice(s, s + w))),
                                    True, False, qn.b() + knT.b(), [bb], sync=False)
                            self.mm(bank[:, 0:w], qr.r((slice(pb, pb + 64), qsl)), krA.r((slice(pb, pb + 64), slice(s, s + w))),
                                    False, True, qr.b() + krA.b(), [bb], sync=True)
                            self.rmax(sm[:, gi:gi + 1], bank[:, 0:w], [bb], sm.b())
                        self.rmax(sm[:, 8:9], sm[:, 0:ng], sm.b(), sm.b())
                        self.ts(sm[:, 9:10], sm[:, 8:9], -scale, None, ALU.mult, None, sm.b(), sm.b())
                        for gi, (s, w) in enumerate(kgs):
                            bank, bb = self.ps[gi], self.psb[gi]
                            self.act(Pt[:, s:s + w], bank[:, 0:w], AF.Exp, [bb] + sm.b(), Pt.b() + sm.b(),
                                     bias=sm[:, 9:10], scale=scale, accum_out=sm[:, 10 + gi:11 + gi])
                        self.rsum(sm[:, 15:16], sm[:, 10:10 + ng], sm.b(), sm.b())
                        self.recip(sm[:, 15:16], sm[:, 15:16], sm.b(), sm.b())
                        PT = PTring.next()
                        for kb0 in range(0, nkb, 4):
                            cnt = min(4, nkb - kb0)
                            tb, tbb = self.ps[5 + (kb0 // 4) % 2], self.psb[5 + (kb0 // 4) % 2]
                            for kb in range(kb0, kb0 + cnt):
                                self.tr(tb[:, (kb - kb0) * 128:(kb - kb0 + 1) * 128], Pt[:, kb * 128:(kb + 1) * 128],
                                        Pt.b(), [tbb])
                            self.copy(self.evac_eng(), PT.r((slice(None), slice(kb0 * 128, (kb0 + cnt) * 128))),
                                      tb[:, 0:cnt * 128], [tbb], PT.b())
                        obank, obb = self.ps[7], self.psb[7]
                        for kb in range(nkb):
                            self.mm(obank[:, 0:128], PT.r((slice(None), slice(kb * 128, (kb + 1) * 128))),
                                    vh.r((slice(None), kb, slice(None))), kb == 0, kb == nkb - 1, PT.b() + vh.b(), [obb])
                        Oq = Oqring.next()
                        self.ts(Oq[:, :], obank[:, 0:128], sm[:, 15:16], None, ALU.mult, None, [obb] + sm.b(), Oq.b())
                        tb, tbb = self.ps[5], self.psb[5]
                        self.tr(tb[:, 0:128], Oq[:, :], Oq.b(), [tbb])
                        if qb % 4 == 0:
                            st = ostg.next()
                        self.copy(self.evac_eng(), st[:, (qb % 4) * 128:(qb % 4 + 1) * 128], tb[:, 0:128], [tbb], st.b())
                        if qb % 4 == 3 or qb == qbs[-1]:
                            q0 = (qb // 4) * 512
                            wd = (qb % 4 + 1) * 128
                            fw.dma("sp", self.obT[h, :, tok0 + q0:tok0 + q0 + wd], st[:, 0:wd], reads=st.b(),
                                   writes=[self.b_scr["obT"]])
            fw.emit()

    def phase_pool(self, l, seqs=SEQS):
        nc, fw = self.nc, self.fw
        with contextlib.ExitStack() as es:
            invs = Tile(nc, es, "invs", [128, 4 * 2048])
            invp = Tile(nc, es, "invp", [128, 4 * 256])
            pw = Tile(nc, es, "pw", [128, 8, 256])
            upr = Ring(nc, es, "up", [128, 2064], 2)
            Ar = Ring(nc, es, "Apool", [128, 2064], 3)
            dT = [Tile(nc, es, "dT%d" % i, [128, 2048]) for i in range(2)]
            stg = Ring(nc, es, "pstg", [128, 512], 3)
            fw.dma("sp", invs[:], self.I("invc_s").rearrange("a n -> (a n)").partition_broadcast(128), writes=invs.b())
            fw.dma("sp", invp[:], self.I("invc_p").rearrange("a n -> (a n)").partition_broadcast(128), writes=invp.b())
            pw4 = pw.t[:].rearrange("p a (k m) -> p a k m", k=2).bitcast(F32R)
            fw.dma("pool", pw4, self.I("poolw")[l].rearrange("a p k m -> p a k m"), writes=pw.b())
            bi = 0
            for (tok0, n, ctx) in seqs:
                inv = invs if n == 2048 else invp
                for pg in range(4):
                    win = POOL_WINDOWS[pg]
                    left = win // 2
                    for half in range(2):
                        cc = pg * 2 + half
                        u = upr.next()
                        fw.op("dve", lambda u=u: nc.vector.memset(u[:, 0:8], 0.0), writes=u.b())
                        fw.op("dve", lambda u=u, n=n: nc.vector.memset(u[:, 8 + n:16 + n], 0.0), writes=u.b())
                        fw.dma("sp", u[:, 8:8 + n], self.uT[cc, :, tok0:tok0 + n], reads=[self.b_scr["uT"]], writes=u.b())
                        cur, L, step = u, n + 16, 1
                        while step < win:
                            nxt = Ar.next()
                            self.tt(nxt[:, 0:L - step], cur[:, 0:L - step], cur[:, step:L], ALU.add, cur.b(), nxt.b())
                            cur, L, step = nxt, L - step, step * 2
                        tmp = Ar.next()
                        self.tt(tmp[:, 0:n], cur[:, 8 - left:8 - left + n], inv[:, pg * n:(pg + 1) * n], ALU.mult,
                                cur.b() + inv.b(), tmp.b())
                        self.tt(dT[half].r((slice(None), slice(0, n))), tmp[:, 0:n], u[:, 8:8 + n], ALU.subtract,
                                tmp.b() + u.b(), dT[half].b())
                    for mh in range(2):
                        for tg in range(0, n, 512):
                            w = min(512, n - tg)
                            bi += 1
                            bank, bb = self.ps[bi % 4], self.psb[bi % 4]
                            for k in range(2):
                                self.mm(bank[:, 0:w], pw4[:, pg * 2 + mh, k, :], dT[k].r((slice(None), slice(tg, tg + w))),
                                        k == 0, k == 1, pw.b() + dT[k].b(), [bb])
                            st = stg.next()
                            col = R_PSC(l) + pg * 2 + mh
                            self.act(st[:, 0:w], bank[:, 0:w], AF.Copy, [bb] + self.vT.b(), st.b(),
                                     scale=self.vT[:, col:col + 1])
                            fw.dma("sp", self.ocT[pg * 2 + mh, :, tok0 + tg:tok0 + tg + w], st[:, 0:w], reads=st.b(),
                                   writes=[self.b_scr["ocT"]])
            fw.emit()

    def phase_merge(self, l, groups=None):
        nc, fw = self.nc, self.fw
        with contextlib.ExitStack() as es:
            xg = Tile(nc, es, "xg3", [128, 16 * 512])
            hT = Tile(nc, es, "hT3", [128, 16, 512], split=True)
            o3 = [Tile(nc, es, "o3_%d" % i, [128, 8, 512]) for i in range(3)]
            wring = Ring(nc, es, "w3", [128, 2048], 3)
            sgr = Ring(nc, es, "sg3", [128, 512], 2)
            tmr = Ring(nc, es, "tm3", [128, 512], 2)
            sqring = Ring(nc, es, "sq3", [128, 512], 2)
            tmpring = Ring(nc, es, "tmp3", [128, 512], 2)
            rstd = Tile(nc, es, "rstd3", [128, 512])
            xring = Ring(nc, es, "xr3", [128, 512], 3)
            self.epsb = Tile(nc, es, "epsb3", [128, 1])
            fw.op("dve", lambda: nc.vector.memset(self.epsb[:], EPS), writes=self.epsb.b())
            y3 = xg.t[:].rearrange("p (c n) -> p c n", c=16)
            srcs = [(self.oaT, "oaT"), (self.obT, "obT"), (self.ocT, "ocT")]
            mp = self.modp[l]
            it = 0
            for g in (groups or range(NGRP)):
                cd = 0 if g < 4 else 1
                gs = slice(g * G, (g + 1) * G)
                self.load_x(g, xg)
                self.norm_mod(g, l, 0, xg, hT, sqring, tmpring, rstd, self.ps[7], self.psb[7])
                for br in range(3):
                    fw.dma("pool", o3[br].r(slice(None)), srcs[br][0][:, :, gs].rearrange("c p n -> p c n"),
                           reads=[self.b_scr[srcs[br][1]]], writes=o3[br].b())
                for d in range(16):
                    for br in range(3):
                        it += 1
                        w, slot = self.wload(wring, self.I("w_inG")[l, br * 16 + d], 16)
                        ga, gab = self.ps[it % 2], self.psb[it % 2]
                        for k in range(16):
                            self.mm(ga[:, :], w[:, k, :], hT.r((slice(None), k, slice(None))), k == 0, k == 15,
                                    slot.b() + hT.b(k), [gab])
                        w2, slot2 = self.wload(wring, self.I("wbr")[l, br, d], 8)
                        pr, prb = self.ps[2 + it % 2], self.psb[2 + it % 2]
                        for k in range(8):
                            self.mm(pr[:, :], w2[:, k, :], o3[br].r((slice(None), k, slice(None))), k == 0, k == 7,
                                    slot2.b() + o3[br].b(), [prb])
                        sg = sgr.next()
                        self.act(sg[:], ga[:, :], AF.Sigmoid, [gab], sg.b())
                        if br == 0:
                            self.tt(y3[:, d, :].bitcast(F32R), sg[:], pr[:, :], ALU.mult, sg.b() + [prb], xg.b())
                        else:
                            tm = tmr.next()
                            self.tt(tm[:], sg[:], pr[:, :], ALU.mult, sg.b() + [prb], tm.b())
                            out = y3[:, d, :].bitcast(F32R)
                            self.tt(out, y3[:, d, :], tm[:], ALU.add, xg.b() + tm.b(), xg.b())
                for d2 in range(16):
                    it += 1
                    w, slot = self.wload(wring, self.I("wout")[l, d2], 16)
                    bank, bb = self.ps[4 + it % 2], self.psb[4 + it % 2]
                    for k in range(16):
                        self.mm(bank[:, :], w[:, k, :], y3[:, k, :].bitcast(F32R), k == 0, k == 15, slot.b() + xg.b(), [bb])
                    xr = xring.next()
                    fw.dma("sp", xr[:], self.xT[d2, :, gs], reads=[self.b_x[g][d2]], writes=xr.b())
                    self.stt(xr[:], bank[:, :], mp[:, 2, d2:d2 + 1, cd], xr[:], ALU.mult, ALU.add,
                             [bb] + mp.b() + xr.b(), xr.b())
                    fw.dma("sp", self.xT[d2, :, gs], xr[:], reads=xr.b(), writes=[self.b_x[g][d2]])
            fw.emit()

    def phase_ffn(self, l, groups=None, final=False):
        nc, fw = self.nc, self.fw
        moe = (l % 2 == 1)
        nexp = self.moe_experts if moe else 1
        with contextlib.ExitStack() as es:
            xa = Tile(nc, es, "xa", [128, 16 * 512])
            hT = Tile(nc, es, "hT4", [128, 16, 512], split=True)
            aT = [Tile(nc, es, "aT%d" % i, [128, FB, 512], split=True) for i in range(2)]
            wgu = Ring(nc, es, "wgu", [128, 2048], 4)
            wdr = Ring(nc, es, "wdr", [128, FB * 128], 3)
            sglr = Ring(nc, es, "sgl", [128, 512], 2)
            t4r = Ring(nc, es, "t4", [128, 512], 2)
            sqring = Ring(nc, es, "sq4", [128, 512], 2)
            tmpring = Ring(nc, es, "tmp4", [128, 512], 2)
            rstd = Tile(nc, es, "rstd4", [128, 512])
            xring = Ring(nc, es, "xr4", [128, 512], 3)
            self.epsb = Tile(nc, es, "epsb4", [128, 1])
            fw.op("dve", lambda: nc.vector.memset(self.epsb[:], EPS), writes=self.epsb.b())
            if moe:
                rt = Tile(nc, es, "rt", [128, 16, 8])
                fw.dma("pool", rt.r(slice(None)), self.I("router"), writes=rt.b())
                gate = Tile(nc, es, "gate", [128, 4, 8])
                gsm = Ring(nc, es, "gsm", [128, 32], 2)
                gbr = Ring(nc, es, "gb", [128, 128], 2)
                gbcr = Ring(nc, es, "gbc", [128, 512], 2)
            a3 = xa.t[:].rearrange("p (c n) -> p c n", c=16)
            mp = self.modp[l]
            it = 0
            for g in (groups or range(NGRP)):
                cd = 0 if g < 4 else 1
                gs = slice(g * G, (g + 1) * G)
                self.load_x(g, xa)
                self.norm_mod(g, l, 1, xa, hT, sqring, tmpring, rstd, self.ps[7], self.psb[7])
                if moe:
                    for t in range(4):
                        lb, lbb = self.ps[6], self.psb[6]
                        for k in range(16):
                            self.mm(lb[:, t * 8:(t + 1) * 8], hT.r((slice(None), k, slice(t * 128, (t + 1) * 128))),
                                    rt.r((slice(None), k, slice(None))), k == 0, k == 15, hT.b(k) + rt.b(), [lbb])
                        sm = gsm.next()
                        lg = sm[:, 0:8]
                        self.copy("dve", lg, lb[:, t * 8:(t + 1) * 8], [lbb], sm.b())
                        self.rmax(sm[:, 24:25], lg, sm.b(), sm.b())
                        self.ts(sm[:, 8:16], lg, sm[:, 24:25], None, ALU.is_equal, None, sm.b(), sm.b())
                        self.stt(sm[:, 8:16], sm[:, 8:16], -1e30, lg, ALU.mult, ALU.add, sm.b(), sm.b())
                        self.rmax(sm[:, 25:26], sm[:, 8:16], sm.b(), sm.b())
                        self.ts(sm[:, 8:16], lg, sm[:, 25:26], None, ALU.is_ge, None, sm.b(), sm.b())
                        self.ts(sm[:, 26:27], sm[:, 24:25], -1.0, None, ALU.mult, None, sm.b(), sm.b())
                        self.act(sm[:, 16:24], lg, AF.Exp, sm.b(), sm.b(), bias=sm[:, 26:27], scale=1.0)
                        self.tt(sm[:, 16:24], sm[:, 16:24], sm[:, 8:16], ALU.mult, sm.b(), sm.b())
                        self.rsum(sm[:, 27:28], sm[:, 16:24], sm.b(), sm.b())
                        self.recip(sm[:, 27:28], sm[:, 27:28], sm.b(), sm.b())
                        self.ts(gate[:, t, :], sm[:, 16:24], sm[:, 27:28], None, ALU.mult, None, sm.b(), gate.b())
                first = True
                for e in range(nexp):
                    if moe:
                        gbank, gbb = self.ps[6], self.psb[6]
                        for t in range(4):
                            gb = gbr.next()
                            self.copy("dve", gb.r(slice(None)), gate[:, t, e:e + 1].to_broadcast([128, 128]), gate.b(), gb.b())
                            self.mm(gbank[:, t * 128:(t + 1) * 128], gb.r(slice(None)), self.identr.r(slice(None)), True, True,
                                    gb.b() + self.identr.b(), [gbb], sync=True)
                        gbc = gbcr.next()
                        self.copy("act", gbc[:], gbank[:, :], [gbb], gbc.b())
                        wg_d, wu_d, wd_d = self.I("moe_g")[e], self.I("moe_u")[e], self.I("moe_d")[e]
                    else:
                        wg_d, wu_d, wd_d = self.I("ffn_g"), self.I("ffn_u"), self.I("ffn_d")
                    for blk in range(NFF // FB):
                        at = aT[blk % 2]
                        for jj in range(FB):
                            j = blk * FB + jj
                            it += 1
                            wg, sg_ = self.wload(wgu, wg_d[j], 16)
                            gbk, gbkb = self.ps[it % 2], self.psb[it % 2]
                            for k in range(16):
                                self.mm(gbk[:, :], wg[:, k, :], hT.r((slice(None), k, slice(None))), k == 0, k == 15,
                                        sg_.b() + hT.b(k), [gbkb])
                            wu, su_ = self.wload(wgu, wu_d[j], 16)
                            ubk, ubkb = self.ps[2 + it % 2], self.psb[2 + it % 2]
                            for k in range(16):
                                self.mm(ubk[:, :], wu[:, k, :], hT.r((slice(None), k, slice(None))), k == 0, k == 15,
                                        su_.b() + hT.b(k), [ubkb])
                            sgl = sglr.next()
                            self.act(sgl[:], gbk[:, :], AF.Silu, [gbkb], sgl.b())
                            if moe:
                                t4 = t4r.next()
                                self.tt(t4[:], ubk[:, :], gbc[:], ALU.mult, [ubkb] + gbc.b(), t4.b())
                                self.tt(at.r((slice(None), jj, slice(None))), sgl[:], t4[:], ALU.mult, sgl.b() + t4.b(), at.b(jj))
                            else:
                                self.tt(at.r((slice(None), jj, slice(None))), sgl[:], ubk[:, :], ALU.mult, sgl.b() + [ubkb], at.b(jj))
                        for d in range(16):
                            it += 1
                            wd, sd_ = self.wload(wdr, wd_d[blk, d], FB)
                            dbk, dbkb = self.ps[4 + it % 2], self.psb[4 + it % 2]
                            for jj in range(FB):
                                self.mm(dbk[:, :], wd[:, jj, :], at.r((slice(None), jj, slice(None))), jj == 0, jj == FB - 1,
                                        sd_.b() + at.b(jj), [dbkb])
                            if first:
                                self.copy("dve", a3[:, d, :], dbk[:, :], [dbkb], xa.b())
                            else:
                                self.tt(a3[:, d, :], a3[:, d, :], dbk[:, :], ALU.add, xa.b() + [dbkb], xa.b())
                        first = False
                for d2 in range(16):
                    xr = xring.next()
                    fw.dma("sp", xr[:], self.xT[d2, :, gs], reads=[self.b_x[g][d2]], writes=xr.b())
                    self.stt(a3[:, d2, :], a3[:, d2, :], mp[:, 5, d2:d2 + 1, cd], xr[:], ALU.mult, ALU.add,
                             xa.b() + mp.b() + xr.b(), xa.b())
                    if not final:
                        fw.dma("sp", self.xT[d2, :, gs], a3[:, d2, :], reads=xa.b(), writes=[self.b_x[g][d2]])
                if final:
                    self.norm_stats(a3, xa.b(), 16, D, sqring, rstd, self.ps[7], self.psb[7])
                    for d2 in range(16):
                        xr = xring.next()
                        col = R_FG + d2
                        self.stt(xr[:], a3[:, d2, :], self.vT[:, col:col + 1], rstd[:], ALU.mult, ALU.mult,
                                 xa.b() + self.vT.b() + rstd.b(), xr.b())
                        fw.dma("sp", self.o_yT[d2, :, gs], xr[:], reads=xr.b())
            fw.emit()

    def phase_moe_sparse(self, l, final=False, slots=None, groups=None):
        nc, fw = self.nc, self.fw
        I32 = mybir.dt.int32
        mp = self.modp[l]
        NTI = NT // 128
        hTok, Ybuf, Tab = self.hTok, self.Ybuf, self.Tab
        b_hTok, b_Y, b_Tab = Buf(), Buf(), Buf()
        B3 = [128, NTI, 8]

        def tred(out, in_, op, reads, writes):
            fw.op("dve", lambda: nc.vector.tensor_reduce(out=out, in_=in_, axis=AX.X, op=op), reads=reads, writes=writes)

        with contextlib.ExitStack() as es0:
            esc = Tile(nc, es0, "esc", [128, 2 * NS])
            self.epsb = Tile(nc, es0, "epsb5", [128, 1])
            fw.op("dve", lambda: nc.vector.memset(self.epsb[:], EPS), writes=self.epsb.b())
            with contextlib.ExitStack() as es:
                xa = Tile(nc, es, "xa5", [128, 16 * 512])
                hT = Tile(nc, es, "hT5", [128, 16, 512], split=True)
                sqring = Ring(nc, es, "sq5", [128, 512], 2)
                tmpring = Ring(nc, es, "tmp5", [128, 512], 2)
                rstd = Tile(nc, es, "rstd5", [128, 512])
                hst = Ring(nc, es, "hst5", [128, 2048], 2)
                rt = Tile(nc, es, "rt5", [128, 16, 8])
                Um = Tile(nc, es, "Um", [128, 128])
                Lg = Tile(nc, es, "Lg", B3)
                eq1 = Tile(nc, es, "eq1", B3)
                sel = Tile(nc, es, "sel", B3)
                wk = Tile(nc, es, "wk", B3)
                wk2 = Tile(nc, es, "wk2", B3)
                gt = Tile(nc, es, "gt", B3)
                pos = Tile(nc, es, "pos", B3)
                tot = Tile(nc, es, "tot", B3)
                offs = Tile(nc, es, "offs", B3)
                m1 = Tile(nc, es, "m1", [128, NTI, 1])
                m2 = Tile(nc, es, "m2", [128, NTI, 1])
                sm = Tile(nc, es, "sm5", [128, 96])
                tokid = Tile(nc, es, "tokid", [128, NTI])
                svals = Tile(nc, es, "svals", [128, NS])
                rr = Tile(nc, es, "rr", [128, 2 * NTI])
                zero = Tile(nc, es, "zero5", [128, 2048])
                recs_t = es.enter_context(nc.sbuf_tensor("sb_recs", [128, 2 * NTI, 16], I32))
                ridx_t = es.enter_context(nc.sbuf_tensor("sb_ridx", [128, 2 * NTI], I32))
                b_recs, b_ridx = Buf(), Buf()
                fw.dma("pool", rt.r(slice(None)), self.I("router"), writes=rt.b())
                fw.dma("pool", Um.r(slice(None)), self.I("Umat"), writes=Um.b())
                fw.dma("sp", tokid[:], self.I("tokid"), writes=tokid.b())
                fw.dma("sp", svals[:], self.I("svals"), writes=svals.b())
                fw.dma("sp", Tab[:, :], self.I("Tab0", I32), writes=[b_Tab])
                fw.op("dve", lambda: nc.vector.memset(zero[:], 0.0), writes=zero.b())
                fw.op("dve", lambda: nc.vector.memset(recs_t[:], 0), writes=[b_recs])
                fw.dma("sp", hTok[NT:NT + 128, :], zero[:], reads=zero.b(), writes=[b_hTok])
                for g in range(NGRP):
                    self.load_x(g, xa)
                    self.norm_mod(g, l, 1, xa, hT, sqring, tmpring, rstd, self.ps[7], self.psb[7])
                    for t in range(4):
                        lb, lbb = self.ps[6], self.psb[6]
                        for k in range(16):
                            self.mm(lb[:, t * 8:(t + 1) * 8], hT.r((slice(None), k, slice(t * 128, (t + 1) * 128))),
                                    rt.r((slice(None), k, slice(None))), k == 0, k == 15, hT.b(k) + rt.b(), [lbb])
                        self.copy("dve", Lg[:, g * 4 + t, :], lb[:, t * 8:(t + 1) * 8], [lbb], Lg.b())
                        hs = hst.next()
                        for c4 in range(4):
                            tb, tbb = self.ps[c4], self.psb[c4]
                            for ci in range(4):
                                c = c4 * 4 + ci
                                self.tr(tb[:, ci * 128:(ci + 1) * 128], hT[:, c, t * 128:(t + 1) * 128], hT.b(c), [tbb])
                            self.copy(self.evac_eng(), hs[:, c4 * 512:(c4 + 1) * 512], tb[:, :], [tbb], hs.b())
                        r0 = g * 512 + t * 128
                        fw.dma("sp", hTok[r0:r0 + 128, :], hs[:], reads=hs.b(), writes=[b_hTok])
                tred(m1[:], Lg[:], ALU.max, Lg.b(), m1.b())
                self.tt(eq1[:], Lg[:], m1[:].to_broadcast(B3), ALU.is_equal, Lg.b() + m1.b(), eq1.b())
                self.stt(wk[:], eq1[:], -1e30, Lg[:], ALU.mult, ALU.add, eq1.b() + Lg.b(), wk.b())
                tred(m2[:], wk[:], ALU.max, wk.b(), m2.b())
                self.tt(sel.r(slice(None)), Lg[:], m2[:].to_broadcast(B3), ALU.is_ge, Lg.b() + m2.b(), sel.b())
                self.tt(wk[:], Lg[:], m1[:].to_broadcast(B3), ALU.subtract, Lg.b() + m1.b(), wk.b())
                self.act(wk[:], wk[:], AF.Exp, wk.b(), wk.b())
                self.tt(wk[:], wk[:], sel[:], ALU.mult, wk.b() + sel.b(), wk.b())
                tred(m2[:], wk[:], ALU.add, wk.b(), m2.b())
                self.recip(m2[:], m2[:], m2.b(), m2.b())
                self.tt(gt[:], wk[:], m2[:].to_broadcast(B3), ALU.mult, wk.b() + m2.b(), gt.b())
                pb_, pbb_ = self.ps[0], self.psb[0]
                tb_, tbb_ = self.ps[1], self.psb[1]
                for t in range(NTI):
                    self.mm(pb_[:, t * 8:(t + 1) * 8], Um.r(slice(None)), sel.r((slice(None), t, slice(None))), True, True,
                            Um.b() + sel.b(), [pbb_], sync=(t == NTI - 1))
                for t in range(NTI):
                    self.mm(tb_[:, t * 8:(t + 1) * 8], self.ones.r(slice(None)), sel.r((slice(None), t, slice(None))), True, True,
                            self.ones.b() + sel.b(), [tbb_], sync=(t == NTI - 1))
                self.copy("dve", pos.t[:].rearrange("p a e -> p (a e)"), pb_[:, 0:NTI * 8], [pbb_], pos.b())
                self.copy("act", tot.t[:].rearrange("p a e -> p (a e)"), tb_[:, 0:NTI * 8], [tbb_], tot.b())
                fw.op("dve", lambda: nc.vector.memset(offs[:, 0, :], 0.0), writes=offs.b())
                for t in range(1, NTI):
                    self.tt(offs[:, t, :], offs[:, t - 1, :], tot[:, t - 1, :], ALU.add, offs.b() + tot.b(), offs.b())
                self.tt(pos[:], pos[:], offs[:], ALU.add, pos.b() + offs.b(), pos.b())
                cnt, nsl, tmp8 = sm[:, 0:8], sm[:, 8:16], sm[:, 24:32]
                self.tt(cnt, offs[:, NTI - 1, :], tot[:, NTI - 1, :], ALU.add, offs.b() + tot.b(), sm.b())
                self.ts(nsl, cnt, 0.0, None, ALU.is_gt, None, sm.b(), sm.b())
                for kk in range(1, 5):
                    self.ts(tmp8, cnt, float(512 * kk), None, ALU.is_gt, None, sm.b(), sm.b())
                    self.tt(nsl, nsl, tmp8, ALU.add, sm.b(), sm.b())
                fw.op("dve", lambda: nc.vector.memset(sm[:, 16:17], 0.0), writes=sm.b())
                for e in range(1, NEXP):
                    self.tt(sm[:, 16 + e:17 + e], sm[:, 15 + e:16 + e], sm[:, 7 + e:8 + e], ALU.add, sm.b(), sm.b())
                self.ts(sm[:, 32:40], sm[:, 16:24], 512.0, None, ALU.mult, None, sm.b(), sm.b())
                self.tt(pos[:], pos[:], sm[:, 32:40].rearrange("p (a e) -> p a e", a=1).to_broadcast(B3), ALU.add,
                        pos.b() + sm.b(), pos.b())
                self.tt(wk2[:], sel[:], eq1[:], ALU.subtract, sel.b() + eq1.b(), wk2.b())
                rv = rr.t[:].rearrange("p (k a) -> p k a", k=2)
                recs_f = recs_t[:].bitcast(F32)
                for k_, oh in ((0, eq1), (1, wk2)):
                    ks = slice(k_ * NTI, (k_ + 1) * NTI)
                    self.tt(wk[:], pos[:], oh[:], ALU.mult, pos.b() + oh.b(), wk.b())
                    tred(rv[:, k_, :], wk[:], ALU.add, wk.b(), rr.b())
                    fw.op("dve", lambda ks=ks: nc.vector.tensor_copy(out=recs_t[:, ks, 0], in_=tokid[:]),
                          reads=tokid.b(), writes=[b_recs])
                    fw.op("dve", lambda ks=ks, k_=k_: nc.vector.tensor_scalar(out=recs_t[:, ks, 1], in0=tokid[:],
                                                                             scalar1=float(k_ * NT), scalar2=None, op0=ALU.add),
                          reads=tokid.b(), writes=[b_recs])
                    self.tt(wk[:], gt[:], oh[:], ALU.mult, gt.b() + oh.b(), wk.b())
                    tred(recs_f[:, ks, 2], wk[:], ALU.add, wk.b(), [b_recs])
                fw.op("dve", lambda: nc.vector.tensor_copy(out=ridx_t[:], in_=rr[:]), reads=rr.b(), writes=[b_ridx])
                for i in range(2 * NTI):
                    fw.dma_custom("pool", lambda i=i: nc.gpsimd.indirect_dma_start(
                        out=Tab[:, :], out_offset=bass.IndirectOffsetOnAxis(ap=ridx_t[:, i:i + 1], axis=0),
                        in_=recs_t[:, i, :], in_offset=None), reads=[b_recs, b_ridx], writes=[b_Tab])
                ev = sm[:, 40:40 + NS]
                tmpS = sm[:, 60:60 + NS]
                fw.op("dve", lambda: nc.vector.memset(ev, -1.0), writes=sm.b())
                for e in range(NEXP):
                    self.ts(tmpS, svals[:], sm[:, 16 + e:17 + e], None, ALU.is_ge, None, svals.b() + sm.b(), sm.b())
                    self.tt(ev, ev, tmpS, ALU.add, sm.b(), sm.b())
                self.ts(esc[:, 0:NS], ev, float(NFF * 128), None, ALU.mult, None, sm.b(), esc.b())
                self.ts(esc[:, NS:2 * NS], ev, float(64 * 128), None, ALU.mult, None, sm.b(), esc.b())
                fw.emit()
            with contextlib.ExitStack() as es:
                hTs = Tile(nc, es, "hTs", [128, 16, 512], split=True)
                acc = Tile(nc, es, "acc5", [128, 16 * 512])
                aT = [Tile(nc, es, "aT5_%d" % i, [128, FB, 512], split=True) for i in range(2)]
                wgu = Ring(nc, es, "wgu5", [128, 2048], 4)
                wdr = Ring(nc, es, "wdr5", [128, FB * 128], 3)
                sglr = Ring(nc, es, "sgl5", [128, 512], 2)
                gth = Ring(nc, es, "gth", [128, 2048], 2)
                otl = Ring(nc, es, "otl", [128, 2048], 2)
                wbase = Tile(nc, es, "wbase", [128, NFF])
                wdbase = Tile(nc, es, "wdbase", [128, 64])
                widx_t = [es.enter_context(nc.sbuf_tensor("sb_widx%d" % i, [128, NFF + 64], I32)) for i in range(2)]
                b_widx = [Buf(), Buf()]
                rbs_t = [es.enter_context(nc.sbuf_tensor("sb_rbs%d" % i, [128, 4, 16], I32)) for i in range(2)]
                b_rbs = [Buf(), Buf()]
                fw.dma("sp", wbase[:], self.I("wbase"), writes=wbase.b())
                fw.dma("sp", wdbase[:], self.I("wdbase"), writes=wdbase.b())
                a3 = acc.t[:].rearrange("p (c n) -> p c n", c=16)
                wg_rows = self.I("moe_g").rearrange("e j p k m -> (e j p) (k m)")
                wu_rows = self.I("moe_u").rearrange("e j p k m -> (e j p) (k m)")
                wd_rows = self.I("moe_d").rearrange("e b d p j m -> (e b d p) (j m)")
                it = 0
                for s in (slots if slots is not None else range(NS)):
                    widx, bw = widx_t[s % 2], b_widx[s % 2]
                    rbs, brb = rbs_t[s % 2], b_rbs[s % 2]
                    fw.op("dve", lambda widx=widx, s=s: nc.vector.tensor_scalar(
                        out=widx[:, 0:NFF], in0=wbase[:], scalar1=esc[:, s:s + 1], scalar2=None, op0=ALU.add),
                        reads=wbase.b() + esc.b(), writes=[bw])
                    fw.op("dve", lambda widx=widx, s=s: nc.vector.tensor_scalar(
                        out=widx[:, NFF:NFF + 64], in0=wdbase[:], scalar1=esc[:, NS + s:NS + s + 1], scalar2=None, op0=ALU.add),
                        reads=wdbase.b() + esc.b(), writes=[bw])
                    fw.dma("sp", rbs[:], Tab[s * 512:(s + 1) * 512, :].rearrange("(q p) c -> p q c", p=128),
                           reads=[b_Tab], writes=[brb])
                    for q in range(4):
                        gtile = gth.next()
                        fw.dma_custom("pool", lambda gtile=gtile, rbs=rbs, q=q: nc.gpsimd.indirect_dma_start(
                            out=gtile[:], out_offset=None, in_=hTok[:, :],
                            in_offset=bass.IndirectOffsetOnAxis(ap=rbs[:, q, 0:1], axis=0)),
                            reads=[brb, b_hTok], writes=gtile.b())
                        for c4 in range(4):
                            tb, tbb = self.ps[6 + c4 % 2], self.psb[6 + c4 % 2]
                            for ci in range(4):
                                c = c4 * 4 + ci
                                self.tr(tb[:, ci * 128:(ci + 1) * 128], gtile[:, c * 128:(c + 1) * 128], gtile.b(), [tbb])
                            self.copy(self.evac_eng(), hTs.r((slice(None), slice(c4 * 4, c4 * 4 + 4), slice(q * 128, (q + 1) * 128))),
                                      tb[:, :].rearrange("p (a n) -> p a n", a=4), [tbb],
                                      hTs.b(c4 * 4) + hTs.b(c4 * 4 + 1) + hTs.b(c4 * 4 + 2) + hTs.b(c4 * 4 + 3))

                    def wgather(ring, rows, col, nelem, widx=widx, bw=bw):
                        slot = ring.next()
                        dst = slot.t[:, 0:nelem].bitcast(F32R)
                        fw.dma_custom("pool", lambda: nc.gpsimd.indirect_dma_start(
                            out=dst, out_offset=None, in_=rows,
                            in_offset=bass.IndirectOffsetOnAxis(ap=widx[:, col:col + 1], axis=0)),
                            reads=[bw], writes=slot.b())
                        return slot
                    for blk in range(NFF // FB):
                        at = aT[blk % 2]
                        for jj in range(FB):
                            j = blk * FB + jj
                            it += 1
                            sg_ = wgather(wgu, wg_rows, j, 2048)
                            wg = sg_.t[:].rearrange("p (k m) -> p k m", k=16).bitcast(F32R)
                            gbk, gbkb = self.ps[it % 2], self.psb[it % 2]
                            for k in range(16):
                                self.mm(gbk[:, :], wg[:, k, :], hTs.r((slice(None), k, slice(None))), k == 0, k == 15,
                                        sg_.b() + hTs.b(k), [gbkb])
                            su_ = wgather(wgu, wu_rows, j, 2048)
                            wu = su_.t[:].rearrange("p (k m) -> p k m", k=16).bitcast(F32R)
                            ubk, ubkb = self.ps[2 + it % 2], self.psb[2 + it % 2]
                            for k in range(16):
                                self.mm(ubk[:, :], wu[:, k, :], hTs.r((slice(None), k, slice(None))), k == 0, k == 15,
                                        su_.b() + hTs.b(k), [ubkb])
                            sgl = sglr.next()
                            self.act(sgl[:], gbk[:, :], AF.Silu, [gbkb], sgl.b())
                            self.tt(at.r((slice(None), jj, slice(None))), sgl[:], ubk[:, :], ALU.mult, sgl.b() + [ubkb], at.b(jj))
                        for d in range(16):
                            it += 1
                            sd_ = wgather(wdr, wd_rows, NFF + blk * 16 + d, FB * 128)
                            wd = sd_.t[:].rearrange("p (k m) -> p k m", k=FB).bitcast(F32R)
                            dbk, dbkb = self.ps[4 + it % 2], self.psb[4 + it % 2]
                            for jj in range(FB):
                                self.mm(dbk[:, :], wd[:, jj, :], at.r((slice(None), jj, slice(None))), jj == 0, jj == FB - 1,
                                        sd_.b() + at.b(jj), [dbkb])
                            if blk == 0:
                                self.copy("dve", a3[:, d, :], dbk[:, :], [dbkb], acc.b())
                            else:
                                self.tt(a3[:, d, :], a3[:, d, :], dbk[:, :], ALU.add, acc.b() + [dbkb], acc.b())
                    rbs_f = rbs[:].bitcast(F32)
                    for q in range(4):
                        ot = otl.next()
                        for c4 in range(4):
                            tb, tbb = self.ps[6 + c4 % 2], self.psb[6 + c4 % 2]
                            for ci in range(4):
                                c = c4 * 4 + ci
                                self.tr(tb[:, ci * 128:(ci + 1) * 128], a3[:, c, q * 128:(q + 1) * 128], acc.b(), [tbb])
                            self.ts(ot[:, c4 * 512:(c4 + 1) * 512], tb[:, :], rbs_f[:, q, 2:3], None, ALU.mult, None,
                                    [tbb, brb], ot.b())
                        fw.dma_custom("pool", lambda ot=ot, rbs=rbs, q=q: nc.gpsimd.indirect_dma_start(
                            out=Ybuf[:, :], out_offset=bass.IndirectOffsetOnAxis(ap=rbs[:, q, 1:2], axis=0),
                            in_=ot[:], in_offset=None), reads=ot.b() + [brb], writes=[b_Y])
                fw.emit()
            with contextlib.ExitStack() as es:
                ys = Ring(nc, es, "ys", [128, 2048], 4)
                y2 = Ring(nc, es, "y2", [128, 2048], 2)
                xg = Tile(nc, es, "xg6", [128, 16 * 512])
                sqring = Ring(nc, es, "sq6", [128, 512], 2)
                rstd = Tile(nc, es, "rstd6", [128, 512])
                xring = Ring(nc, es, "xr6", [128, 512], 3)
                x3 = xg.t[:].rearrange("p (c n) -> p c n", c=16)
                for g in (groups or range(NGRP)):
                    cd = 0 if g < 4 else 1
                    gs = slice(g * G, (g + 1) * G)
                    self.load_x(g, xg)
                    yt = []
                    for t in range(4):
                        r0 = g * 512 + t * 128
                        a, b2 = ys.next(), y2.next()
                        fw.dma("sp", a[:], Ybuf[r0:r0 + 128, :], reads=[b_Y], writes=a.b())
                        fw.dma("sp", b2[:], Ybuf[NT + r0:NT + r0 + 128, :], reads=[b_Y], writes=b2.b())
                        self.tt(a[:], a[:], b2[:], ALU.add, a.b() + b2.b(), a.b(), en="pool" if t % 2 else "dve")
                        yt.append(a)
                    for c in range(16):
                        tb, tbb = self.ps[c % 4], self.psb[c % 4]
                        for t in range(4):
                            self.tr(tb[:, t * 128:(t + 1) * 128], yt[t][:, c * 128:(c + 1) * 128], yt[t].b(), [tbb])
                        self.stt(x3[:, c, :], tb[:, :], mp[:, 5, c:c + 1, cd], x3[:, c, :], ALU.mult, ALU.add,
                                 [tbb] + mp.b() + xg.b(), xg.b())
                        if not final:
                            fw.dma("sp", self.xT[c, :, gs], x3[:, c, :], reads=xg.b(), writes=[self.b_x[g][c]])
                    if final:
                        self.norm_stats(x3, xg.b(), 16, D, sqring, rstd, self.ps[7], self.psb[7])
                        for d2 in range(16):
                            xr = xring.next()
                            col = R_FG + d2
                            self.stt(xr[:], x3[:, d2, :], self.vT[:, col:col + 1], rstd[:], ALU.mult, ALU.mult,
                                     xg.b() + self.vT.b() + rstd.b(), xr.b())
                            fw.dma("sp", self.o_yT[d2, :, gs], xr[:], reads=xr.b())
                fw.emit()


def _tiles(W):
    K, M = W.shape
    return np.ascontiguousarray(W.reshape(K // 128, 128, M // 128, 128).transpose(2, 1, 0, 3))


def host_consts():
    c = {}
    c["ident"] = np.eye(128, dtype=np.float32)
    p = np.arange(128)
    d = p % 64
    partner = np.where((d % 32) < 16, p + 16, p - 16)
    pm = np.zeros((128, 128), np.float32)
    pm[partner, p] = 1.0
    c["permM"] = pm
    quarter = 16
    inv = (np.float32(10000.0) ** (-np.arange(quarter, dtype=np.float32) / np.float32(quarter))).astype(np.float32)
    n = np.arange(2048)
    rr = (n // 64).astype(np.float32)
    cc = (n % 64).astype(np.float32)
    ang_r = (rr[:, None] * inv[None, :]).astype(np.float32)
    ang_c = (cc[:, None] * inv[None, :]).astype(np.float32)
    C = np.zeros((128, 2048), np.float32)
    S = np.zeros((128, 2048), np.float32)
    for pp in range(128):
        dd = pp % 64
        j = dd % 16
        ang = ang_r[:, j] if dd < 32 else ang_c[:, j]
        C[pp] = np.cos(ang)
        sgn = -1.0 if (dd % 32) < 16 else 1.0
        S[pp] = sgn * np.sin(ang)
    c["ropeC"] = C
    c["ropeS"] = S
    r = np.arange(128)[:, None]
    cidx = np.arange(128)[None, :]
    m = np.zeros((128, 384), np.float32)
    m[:, 0:128] = np.where(cidx >= r, 0.0, -1e30)
    m[:, 256:384] = np.where(cidx <= r, 0.0, -1e30)
    c["maskA"] = m
    c["Umat"] = np.triu(np.ones((128, 128), np.float32), k=1)
    pp = np.arange(128, dtype=np.float32)[:, None]
    c["tokid"] = (np.arange(NT // 128, dtype=np.float32)[None, :] * 128 + pp).astype(np.float32)
    c["svals"] = np.tile(np.arange(NS, dtype=np.float32)[None, :], (128, 1))
    c["wbase"] = (np.arange(NFF, dtype=np.float32)[None, :] * 128 + pp).astype(np.float32)
    c["wdbase"] = (np.arange(64, dtype=np.float32)[None, :] * 128 + pp).astype(np.float32)
    tab0 = np.zeros((NS * 512, 16), np.int32)
    tab0[:, 0] = NT
    tab0[:, 1] = 2 * NT + (np.arange(NS * 512) % 128)
    c["Tab0"] = tab0
    for nm, nseq in (("invc_s", 2048), ("invc_p", 256)):
        t = np.arange(nseq)
        tab = np.zeros((4, nseq), np.float32)
        for gi, win in enumerate(POOL_WINDOWS):
            left = win // 2
            right = win - left - 1
            lo = np.maximum(t - left, 0)
            hi = np.minimum(t + right, nseq - 1) + 1
            tab[gi] = (1.0 / (hi - lo).astype(np.float32)).astype(np.float32)
        c[nm] = tab
    return c


def prep_shared(inp):
    sh = dict(host_consts())
    w_in = inp["w_in"]
    colsA = np.concatenate([
        np.arange(0, 1024),
        np.arange(1024, 1088), np.arange(1024, 1088),
        np.arange(1088, 1152), np.arange(1088, 1152),
        np.arange(1152, 1280),
        np.arange(1280, 1792),
        np.arange(1792, 2048),
        np.arange(2048, 2112), np.arange(2048, 2112),
        np.arange(2112, 3136)])
    sh["w_ada"] = np.stack([_tiles(inp["w_ada"][l]) for l in range(DEPTH)])
    sh["w_inA"] = np.stack([_tiles(w_in[l][:, colsA]) for l in range(DEPTH)])
    sh["w_inG"] = np.stack([_tiles(w_in[l][:, 3136:]) for l in range(DEPTH)])
    cq = [np.arange(h * 192, h * 192 + 128) for h in range(8)]
    for j in range(4):
        cq.append(np.concatenate([np.arange((2 * j) * 192 + 128, (2 * j) * 192 + 192),
                                  np.arange((2 * j + 1) * 192 + 128, (2 * j + 1) * 192 + 192)]))
    cq = np.concatenate(cq)
    sh["w_uq"] = np.stack([_tiles(inp["w_uq"][l][:, cq]) for l in range(DEPTH)])
    ckv = np.concatenate([np.arange(h * 256, h * 256 + 128) for h in range(8)] +
                         [np.arange(h * 256 + 128, h * 256 + 256) for h in range(8)])
    sh["w_ukv"] = np.stack([_tiles(inp["w_ukv"][l][:, ckv]) for l in range(DEPTH)])
    sh["poolw"] = np.stack([np.concatenate([_tiles(inp["pool_w"][l][g]) for g in range(4)]) for l in range(DEPTH)])
    sh["wbr"] = np.stack([np.stack([_tiles(inp[k][l]) for k in ("w_branch_a", "w_branch_b", "w_branch_c")])
                          for l in range(DEPTH)])
    sh["wout"] = np.stack([_tiles(inp["w_out"][l]) for l in range(DEPTH)])
    sh["ffn_g"] = _tiles(inp["ffn_w_gate"][0])
    sh["ffn_u"] = _tiles(inp["ffn_w_up"][0])

    def dtiles(wd):
        return np.ascontiguousarray(wd.reshape(4, FB, 128, 16, 128).transpose(0, 3, 2, 1, 4))
    sh["ffn_d"] = dtiles(inp["ffn_w_down"][0])
    sh["router"] = np.ascontiguousarray(inp["router_w"][0].reshape(16, 128, 8).transpose(1, 0, 2))
    sh["moe_g"] = np.stack([_tiles(inp["moe_w_gate"][0][e]) for e in range(NEXP)])
    sh["moe_u"] = np.stack([_tiles(inp["moe_w_up"][0][e]) for e in range(NEXP)])
    sh["moe_d"] = np.stack([dtiles(inp["moe_w_down"][0][e]) for e in range(NEXP)])
    sh["sink"] = np.ascontiguousarray(inp["attn_sink"])
    return sh


def prep_core(inp, i):
    m = {}
    x = np.concatenate([inp["x_sample"][i], inp["x_prompt"][2 * i], inp["x_prompt"][2 * i + 1]], axis=0)
    m["xinT"] = np.ascontiguousarray(x.T.reshape(16, 128, NT))
    v = np.zeros((NROWS, 128), np.float32)
    v[R_C:R_C + 16] = inp["c"][i].reshape(16, 128)
    v[R_CCTX:R_CCTX + 16] = inp["c_ctx"].reshape(16, 128)
    for l in range(DEPTH):
        v[R_LN1(l):R_LN1(l) + 16] = inp["ln1_g"][l].reshape(16, 128)
        v[R_LN2(l):R_LN2(l) + 16] = inp["ln2_g"][l].reshape(16, 128)
        v[R_BADA(l):R_BADA(l) + 96] = inp["b_ada"][l].reshape(96, 128)
        v[R_QG(l):R_QG(l) + 4] = inp["mla_q_norm_g"][l].reshape(4, 128)
        v[R_KVG(l):R_KVG(l) + 2] = inp["mla_kv_norm_g"][l].reshape(2, 128)
        v[R_PSC(l):R_PSC(l) + 8] = inp["pool_scale"][l].reshape(8, 128)
    v[R_FG:R_FG + 16] = inp["final_g"].reshape(16, 128)
    m["vecs"] = v
    ck = inp["cache_attn_k"][i]
    ckT = ck.transpose(0, 2, 3, 1)
    m["ck2T"] = np.ascontiguousarray(np.concatenate([ckT, ckT], axis=2))
    m["cv"] = np.ascontiguousarray(inp["cache_attn_v"][i].reshape(DEPTH, 512, 128))
    m["cckvT"] = np.ascontiguousarray(inp["cache_mla_ckv"][i].transpose(0, 2, 1).reshape(DEPTH, 2, 128, 512))
    krT = inp["cache_mla_krope"][i].transpose(0, 2, 1)
    m["ckr2T"] = np.ascontiguousarray(np.concatenate([krT, krT], axis=1))
    return m


def build_program():
    P = Prog()
    P.alloc_persistent()
    P.phase_prologue()
    for l in range(DEPTH):
        P.phase_proj(l)
        P.phase_attnA(l)
        P.phase_mla(l)
        P.phase_pool(l)
        P.phase_merge(l)
        if l % 2 == 1:
            P.phase_moe_sparse(l, final=(l == DEPTH - 1))
        else:
            P.phase_ffn(l, final=(l == DEPTH - 1))
    return P


def kernel(**inputs):
    inp = {k: np.asarray(v, dtype=np.float32) for k, v in inputs.items()}
    P = build_program()
    sh = prep_shared(inp)
    in_maps = []
    for i in range(NCORES):
        m = prep_core(inp, i)
        m.update(sh)
        in_maps.append({k: m[k] for k in P.din})
    res = run_bass_kernel_spmd(P.nc, in_maps, core_ids=list(range(NCORES)))
    y_prompt = np.zeros((16, 256, D), np.float32)
    y_sample = np.zeros((8, 2048, D), np.float32)
    st_k = np.zeros((16, DEPTH, 256, 2, 64), np.float32)
    st_v = np.zeros((16, DEPTH, 256, 2, 64), np.float32)
    st_ckv = np.zeros((16, DEPTH, 256, 256), np.float32)
    st_kr = np.zeros((16, DEPTH, 256, 64), np.float32)
    for i in range(NCORES):
        r = res.results[i]
        y = np.asarray(r["o_yT"]).reshape(D, NT).T
        y_sample[i] = y[0:2048]
        okT = np.asarray(r["o_kT"])
        ov = np.asarray(r["o_v"]).reshape(DEPTH, 512, 128)
        ockv = np.asarray(r["o_ckvT"]).reshape(DEPTH, 256, 512)
        okr = np.asarray(r["o_krT"])
        for j in range(2):
            b = 2 * i + j
            ts = slice(j * 256, (j + 1) * 256)
            y_prompt[b] = y[2048 + j * 256:2048 + (j + 1) * 256]
            st_k[b] = okT[:, :, :, ts].transpose(0, 3, 1, 2)
            st_v[b] = ov[:, ts, :].reshape(DEPTH, 256, 2, 64)
            st_ckv[b] = ockv[:, :, ts].transpose(0, 2, 1)
            st_kr[b] = okr[:, :, ts].transpose(0, 2, 1)
    return (y_prompt, y_sample, st_k, st_v, st_ckv, st_kr)
```

```python
import contextlib
import numpy as np
import concourse.bass as bass
import concourse.mybir as mybir
from concourse.bass_utils import run_bass_kernel_spmd

F32 = mybir.dt.float32
F32R = mybir.dt.float32r
AF = mybir.ActivationFunctionType
ALU = mybir.AluOpType
AX = mybir.AxisListType

NCORES = 8
D = 2048
NT = 2560
G = 512
NGRP = 5
DEPTH = 2
EPS = 1e-6
NFF = 44
FB = 11
NEXP = 8
NS = 17
DBG_GROUPS = None
SEQS = ((0, 2048, True), (2048, 256, False), (2304, 256, False))
POOL_WINDOWS = (2, 4, 8, 16)

R_C, R_CCTX = 0, 16


def R_LN1(l): return 32 + l * 142
def R_LN2(l): return 32 + l * 142 + 16
def R_BADA(l): return 32 + l * 142 + 32
def R_QG(l): return 32 + l * 142 + 128
def R_KVG(l): return 32 + l * 142 + 132
def R_PSC(l): return 32 + l * 142 + 134


R_FG = 32 + 2 * 142
NROWS = 384


class Buf:
    __slots__ = ("w", "r")

    def __init__(self):
        self.w = None
        self.r = {}


class Eng:
    def __init__(self, name, eng, sem):
        self.name = name
        self.eng = eng
        self.sem = sem
        self.cnt = 0
        self.seen = {}
        self.dsems = []
        self.dvals = []
        self.dn = 0
        self.pend = []
        self.prog = []


class FW:
    def __init__(self, nc, es, ndma_sems=8):
        self.nc = nc
        self.engs = {}
        for name, eng in (("pe", nc.tensor), ("act", nc.scalar), ("dve", nc.vector),
                          ("pool", nc.gpsimd), ("sp", nc.sync)):
            sem = es.enter_context(nc.semaphore("s_" + name))
            self.engs[name] = Eng(name, eng, sem)
        for qn in ("sp", "pool"):
            e = self.engs[qn]
            for i in range(ndma_sems):
                e.dsems.append(es.enter_context(nc.semaphore("d_%s%d" % (qn, i))))
                e.dvals.append(0)
        self.ninst = 0

    def _wait(self, e, ticket):
        sem, val, owner = ticket
        if owner is e:
            if e.name == "pe" or val > e.cnt:
                return
        key = id(sem)
        if e.seen.get(key, 0) >= val:
            return
        e.pend.append((sem, val))
        e.seen[key] = val
        self.ninst += 1

    def _deps(self, e, reads, writes):
        for b in reads:
            if b.w is not None:
                self._wait(e, b.w)
        for b in writes:
            if b.w is not None:
                self._wait(e, b.w)
            for t in b.r.values():
                self._wait(e, t)

    def _mark(self, e, ticket, reads, writes, key=None):
        for b in reads:
            b.r[key or e.name] = ticket
        for b in writes:
            b.w = ticket
            b.r = {}

    def op(self, en, fn, reads=(), writes=(), sync=True):
        e = self.engs[en]
        self._deps(e, reads, writes)
        self.ninst += 1
        waits, e.pend = e.pend, []
        if sync:
            e.cnt += 1
            e.prog.append((waits, fn, e.sem, 1))
            t = (e.sem, e.cnt, e)
        else:
            e.prog.append((waits, fn, None, 0))
            t = (e.sem, e.cnt + 1, e)
        self._mark(e, t, reads, writes)

    def dma(self, qn, out, in_, reads=(), writes=(), **kw):
        e = self.engs[qn]
        slot = e.dn % len(e.dsems)
        e.dn += 1
        sem = e.dsems[slot]
        if e.dvals[slot] > 0:
            self._wait(e, (sem, e.dvals[slot], None))
        self._deps(e, reads, writes)
        e.dvals[slot] += 16
        eng = e.eng
        waits, e.pend = e.pend, []
        e.prog.append((waits, (lambda: eng.dma_start(out=out, in_=in_, **kw)), sem, 16))
        self.ninst += 1
        self._mark(e, (sem, e.dvals[slot], None), reads, writes, key=(e.name, slot))

    def dma_custom(self, qn, fn, reads=(), writes=()):
        e = self.engs[qn]
        slot = e.dn % len(e.dsems)
        e.dn += 1
        sem = e.dsems[slot]
        if e.dvals[slot] > 0:
            self._wait(e, (sem, e.dvals[slot], None))
        self._deps(e, reads, writes)
        e.dvals[slot] += 16
        waits, e.pend = e.pend, []
        e.prog.append((waits, fn, sem, 16))
        self.ninst += 1
        self._mark(e, (sem, e.dvals[slot], None), reads, writes, key=(e.name, slot))

    def emit(self):
        sp = self.engs["sp"]
        for qn in ("sp", "pool"):
            q = self.engs[qn]
            for s, v in zip(q.dsems, q.dvals):
                if v > 0:
                    self._wait(sp, (s, v, None))
        for en in ("pe", "act", "dve", "pool"):
            e = self.engs[en]
            if e.cnt > 0:
                self._wait(sp, (e.sem, e.cnt, None))

        def replay(e):
            eng = e.eng
            for waits, fn, sem, inc in e.prog:
                for s, v in waits:
                    eng.wait_ge(s, v)
                ins = fn()
                if sem is not None:
                    ins.then_inc(sem, inc)
            for s, v in e.pend:
                eng.wait_ge(s, v)
            e.prog = []
            e.pend = []

        with self.nc.Block() as block:
            @block.sync
            def _(x):
                replay(self.engs["sp"])

            @block.tensor
            def _(x):
                replay(self.engs["pe"])

            @block.scalar
            def _(x):
                replay(self.engs["act"])

            @block.vector
            def _(x):
                replay(self.engs["dve"])

            @block.gpsimd
            def _(x):
                replay(self.engs["pool"])


class Tile:
    _n = [0]

    def __init__(self, nc, es, name, shape, split=False):
        Tile._n[0] += 1
        self.t = es.enter_context(nc.sbuf_tensor("sb%d_%s" % (Tile._n[0], name), list(shape), F32))
        self.split = split
        if split:
            self.bufs = [Buf() for _ in range(shape[1])]
        else:
            self.bufs = [Buf()]

    def __getitem__(self, idx):
        return self.t[idx]

    def r(self, idx):
        return self.t[idx].bitcast(F32R)

    def b(self, i=None):
        if self.split and i is not None:
            return [self.bufs[i]]
        return list(self.bufs)


class Ring:
    def __init__(self, nc, es, name, shape, n):
        self.tiles = [Tile(nc, es, "%s%d" % (name, i), shape) for i in range(n)]
        self.i = 0

    def next(self):
        t = self.tiles[self.i % len(self.tiles)]
        self.i += 1
        return t


class Prog:
    def __init__(self, stop_after=None, moe_experts=NEXP, scratch_in=(), moe_sparse=True):
        self.stop_after = stop_after
        self.moe_experts = moe_experts
        self.nc = nc = bass.Bass("TRN2", target_bir_lowering=False)
        self.es = es = contextlib.ExitStack()
        self.fw = FW(nc, es)
        self.din = {}
        self.evac_i = 0

        self.ishapes = {}

        def inp(name, shape):
            self.ishapes[name] = list(shape)

        def outp(name, shape):
            return nc.dram_tensor(name, list(shape), F32, kind="ExternalOutput").ap()

        def scr(name, shape):
            return nc.dram_tensor(name, list(shape), F32, kind="Internal").ap()

        inp("xinT", [16, 128, NT])
        inp("vecs", [NROWS, 128])
        inp("sink", [DEPTH, 16])
        inp("ident", [128, 128])
        inp("permM", [128, 128])
        inp("ropeC", [128, 2048])
        inp("ropeS", [128, 2048])
        inp("maskA", [128, 384])
        inp("invc_s", [4, 2048])
        inp("invc_p", [4, 256])
        inp("ck2T", [DEPTH, 2, 128, 512])
        inp("cv", [DEPTH, 512, 128])
        inp("cckvT", [DEPTH, 2, 128, 512])
        inp("ckr2T", [DEPTH, 128, 512])
        inp("w_ada", [DEPTH, 96, 128, 16, 128])
        inp("w_inA", [DEPTH, 26, 128, 16, 128])
        inp("w_inG", [DEPTH, 48, 128, 16, 128])
        inp("w_uq", [DEPTH, 12, 128, 4, 128])
        inp("w_ukv", [DEPTH, 16, 128, 2, 128])
        inp("poolw", [DEPTH, 8, 128, 2, 128])
        inp("wbr", [DEPTH, 3, 16, 128, 8, 128])
        inp("wout", [DEPTH, 16, 128, 16, 128])
        inp("ffn_g", [NFF, 128, 16, 128])
        inp("ffn_u", [NFF, 128, 16, 128])
        inp("ffn_d", [4, 16, 128, FB, 128])
        inp("router", [128, 16, 8])
        inp("moe_g", [NEXP, NFF, 128, 16, 128])
        inp("moe_u", [NEXP, NFF, 128, 16, 128])
        inp("moe_d", [NEXP, 4, 16, 128, FB, 128])
        inp("Umat", [128, 128])
        inp("tokid", [128, NT // 128])
        inp("svals", [128, NS])
        inp("wbase", [128, NFF])
        inp("wdbase", [128, 64])
        inp("Tab0", [NS * 512, 16])
        self.o_yT = outp("o_yT", [16, 128, NT])
        self.o_kT = outp("o_kT", [DEPTH, 2, 64, 512])
        self.o_v = outp("o_v", [DEPTH, 4, 128, 128])
        self.o_ckvT = outp("o_ckvT", [DEPTH, 2, 128, 512])
        self.o_krT = outp("o_krT", [DEPTH, 64, 512])
        def mk(name, shape):
            if name in scratch_in:
                return nc.dram_tensor(name, list(shape), F32, kind="ExternalInput").ap()
            return (outp if stop_after is not None else scr)(name, shape)
        self.xT = mk("s_xT", [16, 128, NT])
        self.qaT = mk("s_qaT", [8, 128, NT])
        self.kaT2 = mk("s_kaT2", [2, 128, NT])
        self.vaTok = mk("s_vaTok", [NT // 128, 128, 128])
        self.qnT = mk("s_qnT", [8, 128, NT])
        self.qrT = mk("s_qrT", [4, 128, NT])
        self.ckvT = mk("s_ckvT", [2, 128, NT])
        self.krT2 = mk("s_krT2", [128, NT])
        self.uT = mk("s_uT", [8, 128, NT])
        self.oaT = mk("s_oaT", [8, 128, NT])
        self.obT = mk("s_obT", [8, 128, NT])
        self.ocT = mk("s_ocT", [8, 128, NT])
        if moe_sparse:
            self.hTok = scr("s_hTok", [NT + 128, D])
            self.Ybuf = scr("s_Y", [2 * NT + 128, D])
            self.Tab = nc.dram_tensor("s_Tab", [NS * 512, 16], mybir.dt.int32, kind="Internal").ap()
        self.b_x = [[Buf() for _ in range(16)] for _ in range(NGRP)]
        self.b_scr = {k: Buf() for k in ("qaT", "kaT2", "vaTok", "qnT", "qrT", "ckvT", "krT2", "uT",
                                         "oaT", "obT", "ocT")}
        self.ps = [es.enter_context(nc.psum_tensor("ps%d" % i, [128, 512], F32)) for i in range(8)]
        self.psb = [Buf() for _ in range(8)]

    def I(self, name, dt=F32):
        if name not in self.din:
            self.din[name] = self.nc.dram_tensor(name, self.ishapes[name], dt, kind="ExternalInput").ap()
        return self.din[name]

    def mm(self, out, lhsT, rhs, start, stop, reads, writes, sync=None):
        nc = self.nc
        if sync is None:
            sync = stop
        self.fw.op("pe", lambda: nc.tensor.matmul(out, lhsT=lhsT, rhs=rhs, start=start, stop=stop),
                   reads=reads, writes=writes, sync=sync)

    def tr(self, out, in_, reads, writes, sync=True):
        nc = self.nc
        ident = self.ident[:]
        self.fw.op("pe", lambda: nc.tensor.transpose(out=out, in_=in_, identity=ident),
                   reads=reads + self.ident.b(), writes=writes, sync=sync)

    def copy(self, en, out, in_, reads, writes):
        nc = self.nc
        if en == "act":
            self.fw.op("act", lambda: nc.scalar.copy(out=out, in_=in_), reads=reads, writes=writes)
        elif en == "dve":
            self.fw.op("dve", lambda: nc.vector.tensor_copy(out=out, in_=in_), reads=reads, writes=writes)
        else:
            self.fw.op("pool", lambda: nc.gpsimd.tensor_copy(out=out, in_=in_), reads=reads, writes=writes)

    def evac_eng(self):
        self.evac_i += 1
        return "act" if self.evac_i % 2 else "dve"

    def act(self, out, in_, func, reads, writes, bias=None, scale=None, accum_out=None):
        nc = self.nc
        kw = {}
        if bias is not None:
            kw["bias"] = bias
        if scale is not None:
            kw["scale"] = scale
        if accum_out is not None:
            kw["accum_out"] = accum_out
        self.fw.op("act", lambda: nc.scalar.activation(out=out, in_=in_, func=func, **kw),
                   reads=reads, writes=writes)

    def tt(self, out, in0, in1, op, reads, writes, en="dve"):
        nc = self.nc
        eng = nc.vector if en == "dve" else nc.gpsimd
        self.fw.op(en, lambda: eng.tensor_tensor(out=out, in0=in0, in1=in1, op=op), reads=reads, writes=writes)

    def ts(self, out, in0, s1, s2, op0, op1, reads, writes):
        nc = self.nc
        if op1 is None:
            self.fw.op("dve", lambda: nc.vector.tensor_scalar(out=out, in0=in0, scalar1=s1, scalar2=None, op0=op0),
                       reads=reads, writes=writes)
        else:
            self.fw.op("dve", lambda: nc.vector.tensor_scalar(out=out, in0=in0, scalar1=s1, scalar2=s2,
                                                              op0=op0, op1=op1), reads=reads, writes=writes)

    def stt(self, out, in0, scalar, in1, op0, op1, reads, writes):
        nc = self.nc
        self.fw.op("dve", lambda: nc.vector.scalar_tensor_tensor(out=out, in0=in0, scalar=scalar, in1=in1,
                                                                 op0=op0, op1=op1), reads=reads, writes=writes)

    def wload(self, ring, dram_tile, nk, ncol=128):
        slot = ring.next()
        dst = slot.t[:, 0:nk * ncol].rearrange("p (k m) -> p k m", k=nk)
        self.fw.dma("pool", dst.bitcast(F32R), dram_tile, writes=slot.b())
        return dst.bitcast(F32R), slot

    def alloc_persistent(self):
        nc, es = self.nc, self.es
        self.ident = Tile(nc, es, "ident", [128, 128])
        self.ones = Tile(nc, es, "ones", [128, 128])
        self.identr = Tile(nc, es, "identr", [128, 128])
        self.vT = Tile(nc, es, "vT", [128, NROWS])
        self.modp = [Tile(nc, es, "modp%d" % l, [128, 6, 16, 2]) for l in range(DEPTH)]
        self.sinkb = Tile(nc, es, "sinkb", [128, DEPTH * 16])

    def phase_prologue(self):
        nc, fw = self.nc, self.fw
        with contextlib.ExitStack() as es:
            vrows = Tile(nc, es, "vrows", [128, 3, 128])
            scT = Tile(nc, es, "scT", [128, 32])
            modT = Tile(nc, es, "modT", [128, 96, 2])
            wring = Ring(nc, es, "wada", [128, 2048], 4)
            fw.dma("sp", self.ident[:], self.I("ident"), writes=self.ident.b())
            fw.dma("sp", vrows[:], self.I("vecs").rearrange("(a p) f -> p a f", p=128), writes=vrows.b())
            fw.dma("sp", self.sinkb[:],
                   self.I("sink").rearrange("l h -> (l h)").partition_broadcast(128), writes=self.sinkb.b())
            ones32 = Tile(nc, es, "ones32", [128, 128])
            fw.op("dve", lambda: nc.vector.memset(ones32[:], 1.0), writes=ones32.b())
            self.copy("act", self.ones.r(slice(None)), ones32[:], ones32.b(), self.ones.b())
            self.copy("act", self.identr.r(slice(None)), self.ident[:], self.ident.b(), self.identr.b())
            for a in range(3):
                self.tr(self.ps[0][:, a * 128:(a + 1) * 128], vrows[:, a, :], vrows.b(), [self.psb[0]])
            self.copy("dve", self.vT[:], self.ps[0][:, 0:384], [self.psb[0]], self.vT.b())
            self.act(scT.r(slice(None)), self.vT[:, 0:32], AF.Silu, self.vT.b(), scT.b())
            sc3 = scT.r(slice(None)).rearrange("p (c k) -> p k c", c=2)
            for l in range(DEPTH):
                for j in range(96):
                    w, slot = self.wload(wring, self.I("w_ada")[l, j], 16)
                    bank = self.ps[1 + (j % 2)]
                    bb = self.psb[1 + (j % 2)]
                    for k in range(16):
                        self.mm(bank[:, 0:2], w[:, k, :], sc3[:, k, :], k == 0, k == 15,
                                slot.b() + scT.b(), [bb])
                    col = R_BADA(l) + j
                    self.act(modT[:, j, :], bank[:, 0:2], AF.Identity, [bb] + self.vT.b(), modT.b(),
                             bias=self.vT[:, col:col + 1])
                mp = self.modp[l]
                for s in range(2):
                    lnr = R_LN1(l) if s == 0 else R_LN2(l)
                    sh, sc, gt = 3 * s, 3 * s + 1, 3 * s + 2
                    for cd in range(2):
                        self.stt(mp[:, 3 * s + 0, :, cd], modT[:, sc * 16:(sc + 1) * 16, cd], 1.0,
                                 self.vT[:, lnr:lnr + 16], ALU.add, ALU.mult,
                                 modT.b() + self.vT.b(), mp.b())
                        self.copy("dve", mp[:, 3 * s + 1, :, cd], modT[:, sh * 16:(sh + 1) * 16, cd], modT.b(), mp.b())
                        self.copy("dve", mp[:, 3 * s + 2, :, cd], modT[:, gt * 16:(gt + 1) * 16, cd], modT.b(), mp.b())
            xt = Ring(nc, es, "xcp", [128, 16 * 512], 2)
            for g in range(NGRP):
                t = xt.next()
                v = t.t[:].rearrange("p (c n) -> p c n", c=16)
                fw.dma("sp", v, self.I("xinT")[:, :, g * G:(g + 1) * G].rearrange("c p n -> p c n"), writes=t.b())
                fw.dma("sp", self.xT[:, :, g * G:(g + 1) * G].rearrange("c p n -> p c n"), v,
                       reads=t.b(), writes=self.b_x[g])
            fw.emit()

    def norm_stats(self, x3, xb, nchunk, nfeat, sqring, rstd, bank, bb):
        nc = self.nc
        for c in range(nchunk):
            sq = sqring.next()
            self.act(sq.r(slice(None)), x3[:, c, :], AF.Square, xb, sq.b())
            self.mm(bank[:, :], self.ones.r(slice(None)), sq.r(slice(None)), c == 0, c == nchunk - 1,
                    self.ones.b() + sq.b(), [bb], sync=True)
        self.act(rstd[:], bank[:, :], AF.Sqrt, [bb] + self.epsb.b(), rstd.b(), bias=self.epsb[:, 0:1], scale=1.0 / nfeat)
        self.fw.op("dve", lambda: nc.vector.reciprocal(out=rstd[:], in_=rstd[:]), reads=rstd.b(), writes=rstd.b())

    def norm_mod(self, g, l, s, xg, hT, sqring, tmpring, rstd, bank, bb):
        cd = 0 if g < 4 else 1
        mp = self.modp[l]
        x3 = xg.t[:].rearrange("p (c n) -> p c n", c=16)
        self.norm_stats(x3, xg.b(), 16, D, sqring, rstd, bank, bb)
        for c in range(16):
            tmp = tmpring.next()
            self.tt(tmp[:], x3[:, c, :], rstd[:], ALU.mult, xg.b() + rstd.b(), tmp.b())
            self.act(hT.r((slice(None), c, slice(None))), tmp[:], AF.Identity, tmp.b() + mp.b(), hT.b(c),
                     bias=mp[:, 3 * s + 1, c:c + 1, cd], scale=mp[:, 3 * s + 0, c:c + 1, cd])

    def load_x(self, g, xg):
        v = xg.t[:].rearrange("p (c n) -> p c n", c=16)
        self.fw.dma("sp", v, self.xT[:, :, g * G:(g + 1) * G].rearrange("c p n -> p c n"),
                    reads=self.b_x[g], writes=xg.b())

    def phase_proj(self, l):
        nc, fw = self.nc, self.fw
        with contextlib.ExitStack() as es:
            xg = Tile(nc, es, "xg", [128, 16 * 512])
            hT = Tile(nc, es, "hT", [128, 16, 512], split=True)
            wring = Ring(nc, es, "w1", [128, 2048], 4)
            wuq = Tile(nc, es, "wuq", [128, 12, 512])
            ropeC = Tile(nc, es, "ropeC", [128, 2048])
            ropeS = Tile(nc, es, "ropeS", [128, 2048])
            permM = Tile(nc, es, "permM", [128, 128])
            cqs = Tile(nc, es, "cqs", [128, 4, 512])
            cqn = Tile(nc, es, "cqn", [128, 4, 512])
            ckvs = Tile(nc, es, "ckvs", [128, 2, 512])
            stg = Ring(nc, es, "stg", [128, 512], 4)
            sqring = Ring(nc, es, "sq", [128, 512], 3)
            tmpring = Ring(nc, es, "tmp", [128, 512], 3)
            xsring = Ring(nc, es, "xs", [128, 512], 2)
            t1ring = Ring(nc, es, "t1", [128, 512], 2)
            rstd = Tile(nc, es, "rstd", [128, 512])
            rstd2 = Tile(nc, es, "rstd2", [128, 512])
            vtok = Ring(nc, es, "vtok", [128, 512], 2)
            self.epsb = Tile(nc, es, "epsb", [128, 1])
            fw.op("dve", lambda: nc.vector.memset(self.epsb[:], EPS), writes=self.epsb.b())
            fw.dma("sp", ropeC[:], self.I("ropeC"), writes=ropeC.b())
            fw.dma("sp", ropeS[:], self.I("ropeS"), writes=ropeS.b())
            fw.dma("pool", permM.r(slice(None)), self.I("permM"), writes=permM.b())
            fw.dma("pool", wuq.t[:].rearrange("p a (k m) -> p a k m", k=4).bitcast(F32R),
                   self.I("w_uq")[l].rearrange("a p k m -> p a k m"), writes=wuq.b())
            wuq4 = wuq.t[:].rearrange("p a (k m) -> p a k m", k=4).bitcast(F32R)
            bi = [0]

            def nextbank():
                bi[0] += 1
                i = bi[0] % 4
                return self.ps[i], self.psb[i]

            def finish_chunk(bank, bb, g, rope, dst, dbuf, P=128, dst2=None):
                st = stg.next()
                if rope and g < 4:
                    xs = xsring.next()
                    self.copy("act", xs.r(slice(None))[0:P], bank[0:P, :], [bb], xs.b())
                    pb, pbb = self.ps[4 + (bi[0] % 2)], self.psb[4 + (bi[0] % 2)]
                    self.mm(pb[0:P, :], permM.r(slice(None))[0:P, 0:P], xs.r(slice(None))[0:P], True, True,
                            permM.b() + xs.b(), [pbb])
                    t1 = t1ring.next()
                    self.tt(t1[0:P], xs[0:P], ropeC[0:P, g * G:(g + 1) * G], ALU.mult, xs.b() + ropeC.b(), t1.b())
                    self.tt(st[0:P], pb[0:P, :], ropeS[0:P, g * G:(g + 1) * G], ALU.mult, [pbb] + ropeS.b(), st.b())
                    self.tt(st[0:P], st[0:P], t1[0:P], ALU.add, st.b() + t1.b(), st.b())
                else:
                    self.copy(self.evac_eng(), st[0:P], bank[0:P, :], [bb], st.b())
                fw.dma("sp", dst, st[0:P], reads=st.b())
                if dst2 is not None:
                    fw.dma("sp", dst2, st[0:dst2.shape[0]], reads=st.b())
                return st

            for g in (DBG_GROUPS or range(NGRP)):
                gs = slice(g * G, (g + 1) * G)
                self.load_x(g, xg)
                self.norm_mod(g, l, 0, xg, hT, sqring, tmpring, rstd, self.ps[7], self.psb[7])
                for ch in range(26):
                    w, slot = self.wload(wring, self.I("w_inA")[l, ch], 16)
                    bank, bb = nextbank()
                    for k in range(16):
                        self.mm(bank[:, :], w[:, k, :], hT.r((slice(None), k, slice(None))), k == 0, k == 15,
                                slot.b() + hT.b(k), [bb])
                    if ch < 8:
                        finish_chunk(bank, bb, g, True, self.qaT[ch, :, gs], self.b_scr["qaT"])
                    elif ch < 10:
                        d2 = self.o_kT[l, ch - 8, :, :] if g == 4 else None
                        finish_chunk(bank, bb, g, True, self.kaT2[ch - 8, :, gs], self.b_scr["kaT2"], dst2=d2)
                    elif ch == 10:
                        st = stg.next()
                        self.copy(self.evac_eng(), st[:], bank[:, :], [bb], st.b())
                        tb, tbb = self.ps[6], self.psb[6]
                        for t in range(4):
                            self.tr(tb[:, t * 128:(t + 1) * 128], st[:, t * 128:(t + 1) * 128], st.b(), [tbb])
                        vt = vtok.next()
                        self.copy(self.evac_eng(), vt[:], tb[:, :], [tbb], vt.b())
                        fw.dma("sp", self.vaTok[g * 4:(g + 1) * 4].rearrange("t p d -> p t d"),
                               vt.t[:].rearrange("p (t d) -> p t d", t=4), reads=vt.b())
                        if g == 4:
                            fw.dma("sp", self.o_v[l].rearrange("t p d -> p t d"),
                                   vt.t[:].rearrange("p (t d) -> p t d", t=4), reads=vt.b())
                    elif ch < 15:
                        self.copy(self.evac_eng(), cqs[:, ch - 11, :], bank[:, :], [bb], cqs.b())
                        if ch == 14:
                            self.norm_stats(cqs.t[:], cqs.b(), 4, 512, sqring, rstd2, self.ps[7], self.psb[7])
                            for c in range(4):
                                col = R_QG(l) + c
                                self.stt(cqn.r((slice(None), c, slice(None))), cqs[:, c, :], self.vT[:, col:col + 1],
                                         rstd2[:], ALU.mult, ALU.mult, cqs.b() + rstd2.b() + self.vT.b(), cqn.b())
                            for a in range(12):
                                bank2, bb2 = nextbank()
                                for k in range(4):
                                    self.mm(bank2[:, :], wuq4[:, a, k, :], cqn.r((slice(None), k, slice(None))),
                                            k == 0, k == 3, wuq.b() + cqn.b(), [bb2])
                                if a < 8:
                                    finish_chunk(bank2, bb2, g, False, self.qnT[a, :, gs], self.b_scr["qnT"])
                                else:
                                    finish_chunk(bank2, bb2, g, True, self.qrT[a - 8, :, gs], self.b_scr["qrT"])
                    elif ch < 17:
                        self.copy(self.evac_eng(), ckvs[:, ch - 15, :], bank[:, :], [bb], ckvs.b())
                        if ch == 16:
                            self.norm_stats(ckvs.t[:], ckvs.b(), 2, 256, sqring, rstd2, self.ps[7], self.psb[7])
                            for c in range(2):
                                col = R_KVG(l) + c
                                st = stg.next()
                                self.stt(st[:], ckvs[:, c, :], self.vT[:, col:col + 1], rstd2[:], ALU.mult, ALU.mult,
                                         ckvs.b() + rstd2.b() + self.vT.b(), st.b())
                                fw.dma("sp", self.ckvT[c, :, gs], st[:], reads=st.b())
                                if g == 4:
                                    fw.dma("sp", self.o_ckvT[l, c], st[:], reads=st.b())
                    elif ch == 17:
                        d2 = self.o_krT[l] if g == 4 else None
                        finish_chunk(bank, bb, g, True, self.krT2[:, gs], self.b_scr["krT2"], dst2=d2)
                    else:
                        finish_chunk(bank, bb, g, False, self.uT[ch - 18, :, gs], self.b_scr["uT"])
            fw.emit()

    def rmax(self, out, in_, reads, writes):
        nc = self.nc
        self.fw.op("dve", lambda: nc.vector.reduce_max(out=out, in_=in_, axis=AX.X), reads=reads, writes=writes)

    def rsum(self, out, in_, reads, writes):
        nc = self.nc
        self.fw.op("dve", lambda: nc.vector.reduce_sum(out=out, in_=in_, axis=AX.X), reads=reads, writes=writes)

    def recip(self, out, in_, reads, writes):
        nc = self.nc
        self.fw.op("dve", lambda: nc.vector.reciprocal(out=out, in_=in_), reads=reads, writes=writes)

    def phase_attnA(self, l, seqs=SEQS, qb_limit=None):
        nc, fw = self.nc, self.fw
        with contextlib.ExitStack() as es:
            kT2 = Tile(nc, es, "kT2", [128, 2, 2560])
            Vt = Tile(nc, es, "Vt", [128, 20, 128])
            qring = Ring(nc, es, "qc", [128, 2048], 2)
            maskA = Tile(nc, es, "maskA", [128, 384])
            Pring = Ring(nc, es, "Pa", [128, 896], 2)
            PTring = Ring(nc, es, "PTa", [128, 896], 2)
            slring = Ring(nc, es, "sl", [128, 384], 2)
            small = Ring(nc, es, "sma", [128, 8], 4)
            rdring = Ring(nc, es, "rda", [128, 2], 2)
            Oqring = Ring(nc, es, "Oqa", [128, 128], 2)
            ostg = Ring(nc, es, "ostga", [128, 512], 2)
            fw.dma("sp", maskA[:], self.I("maskA"), writes=maskA.b())
            for (tok0, n, ctx) in seqs:
                nqb = n // 128
                for g2 in range(2):
                    fw.dma("pool", kT2.r((slice(None), g2, slice(0, n))), self.kaT2[g2, :, tok0:tok0 + n],
                           writes=kT2.b())
                    if ctx:
                        fw.dma("pool", kT2.r((slice(None), g2, slice(n, n + 512))), self.I("ck2T")[l, g2],
                               writes=kT2.b())
                fw.dma("pool", Vt.r((slice(None), slice(0, nqb), slice(None))),
                       self.vaTok[tok0 // 128:tok0 // 128 + nqb].rearrange("t p d -> p t d"),
                       writes=Vt.b())
                if ctx:
                    fw.dma("pool", Vt.r((slice(None), slice(nqb, nqb + 4), slice(None))),
                           self.I("cv")[l].rearrange("(t p) d -> p t d", p=128), writes=Vt.b())
                for c in range(8):
                    g2 = c // 4
                    qc = qring.next()
                    fw.dma("pool", qc.r((slice(None), slice(0, n))), self.qaT[c, :, tok0:tok0 + n],
                           writes=qc.b())
                    st = None
                    qbs = list(range(nqb)) if qb_limit is None else list(range(min(nqb, qb_limit)))
                    for qb in qbs:
                        if ctx:
                            kb_lo, kb_hi = max(qb - 1, 0), min(qb + 1, nqb - 1)
                        else:
                            kb_lo, kb_hi = 0, nqb - 1
                        nl = (kb_hi - kb_lo + 1) * 128
                        blocks = list(range(kb_lo, kb_hi + 1)) + ([nqb + i for i in range(4)] if ctx else [])
                        nb = len(blocks)
                        rd = rdring.next()
                        obank, obb = self.ps[6], self.psb[6]
                        for hh in range(2):
                            h = 2 * c + hh
                            pb = hh * 64
                            sbank, sbb = self.ps[hh * 2], self.psb[hh * 2]
                            cbank, cbb = self.ps[hh * 2 + 1], self.psb[hh * 2 + 1]
                            lq = qc.r((slice(pb, pb + 64), slice(qb * 128, (qb + 1) * 128)))
                            self.mm(sbank[:, 0:nl], lq, kT2.r((slice(pb, pb + 64), g2, slice(kb_lo * 128, kb_lo * 128 + nl))),
                                    True, True, qc.b() + kT2.b(), [sbb])
                            if ctx:
                                self.mm(cbank[:, :], lq, kT2.r((slice(pb, pb + 64), g2, slice(n, n + 512))),
                                        True, True, qc.b() + kT2.b(), [cbb])
                            sm = small.next()
                            Pt = Pring.next()
                            if ctx:
                                sl = slring.next()
                                mlo = 128 if qb == 0 else 0
                                self.tt(sl[:, 0:nl], sbank[:, 0:nl], maskA[:, mlo:mlo + nl], ALU.add,
                                        [sbb] + maskA.b(), sl.b())
                                self.rmax(sm[:, 0:1], sl[:, 0:nl], sl.b(), sm.b())
                                self.rmax(sm[:, 1:2], cbank[:, :], [cbb], sm.b())
                                self.tt(sm[:, 2:3], sm[:, 0:1], sm[:, 1:2], ALU.max, sm.b(), sm.b())
                                src_loc, src_b = sl[:, 0:nl], sl.b()
                            else:
                                self.rmax(sm[:, 2:3], sbank[:, 0:nl], [sbb], sm.b())
                                src_loc, src_b = sbank[:, 0:nl], [sbb]
                            scol = l * 16 + h
                            self.ts(sm[:, 3:4], sm[:, 2:3], 0.125, self.sinkb[:, scol:scol + 1], ALU.mult, ALU.max,
                                    sm.b() + self.sinkb.b(), sm.b())
                            self.ts(sm[:, 4:5], sm[:, 3:4], -1.0, None, ALU.mult, None, sm.b(), sm.b())
                            self.act(Pt[:, 0:nl], src_loc, AF.Exp, src_b + sm.b(), Pt.b() + sm.b(),
                                     bias=sm[:, 4:5], scale=0.125, accum_out=sm[:, 5:6])
                            if ctx:
                                self.act(Pt[:, nl:nl + 512], cbank[:, :], AF.Exp, [cbb] + sm.b(), Pt.b() + sm.b(),
                                         bias=sm[:, 4:5], scale=0.125, accum_out=sm[:, 6:7])
                            self.act(sm[:, 7:8], self.sinkb[:, scol:scol + 1], AF.Exp, self.sinkb.b() + sm.b(), sm.b(),
                                     bias=sm[:, 4:5], scale=1.0)
                            self.tt(sm[:, 5:6], sm[:, 5:6], sm[:, 7:8], ALU.add, sm.b(), sm.b())
                            if ctx:
                                self.tt(sm[:, 5:6], sm[:, 5:6], sm[:, 6:7], ALU.add, sm.b(), sm.b())
                            self.recip(rd[:, hh:hh + 1], sm[:, 5:6], sm.b(), rd.b())
                            PT = PTring.next()
                            for i0 in range(0, nb, 4):
                                cnt = min(4, nb - i0)
                                tb, tbb = self.ps[4 + (i0 // 4) % 2], self.psb[4 + (i0 // 4) % 2]
                                for i in range(i0, i0 + cnt):
                                    self.tr(tb[:, (i - i0) * 128:(i - i0 + 1) * 128], Pt[:, i * 128:(i + 1) * 128],
                                            Pt.b(), [tbb])
                                self.copy(self.evac_eng(), PT.r((slice(None), slice(i0 * 128, (i0 + cnt) * 128))),
                                          tb[:, 0:cnt * 128], [tbb], PT.b())
                            for i, blk in enumerate(blocks):
                                self.mm(obank[:, hh * 64:(hh + 1) * 64], PT.r((slice(None), slice(i * 128, (i + 1) * 128))),
                                        Vt.r((slice(None), blk, slice(g2 * 64, (g2 + 1) * 64))), i == 0, i == nb - 1,
                                        PT.b() + Vt.b(), [obb], sync=True)
                        Oq = Oqring.next()
                        self.ts(Oq[:, 0:64], obank[:, 0:64], rd[:, 0:1], None, ALU.mult, None, [obb] + rd.b(), Oq.b())
                        self.ts(Oq[:, 64:128], obank[:, 64:128], rd[:, 1:2], None, ALU.mult, None, [obb] + rd.b(), Oq.b())
                        tb, tbb = self.ps[7], self.psb[7]
                        self.tr(tb[:, 0:128], Oq[:, :], Oq.b(), [tbb])
                        if qb % 4 == 0:
                            st = ostg.next()
                        self.copy(self.evac_eng(), st[:, (qb % 4) * 128:(qb % 4 + 1) * 128], tb[:, 0:128], [tbb], st.b())
                        if qb % 4 == 3 or qb == qbs[-1]:
                            q0 = (qb // 4) * 512
                            wd = (qb % 4 + 1) * 128
                            fw.dma("sp", self.oaT[c, :, tok0 + q0:tok0 + q0 + wd], st[:, 0:wd], reads=st.b())
            fw.emit()

    def phase_mla(self, l, seqs=SEQS, qb_limit=None, heads=range(8)):
        nc, fw = self.nc, self.fw
        scale = float((128 + 64) ** -0.5)
        with contextlib.ExitStack() as es:
            ckvA = Tile(nc, es, "ckvA", [128, 2, 2560])
            krA = Tile(nc, es, "krA", [128, 2560])
            wukv = Tile(nc, es, "wukv", [128, 16, 256])
            knT = Tile(nc, es, "knT", [128, 2560])
            vh = Tile(nc, es, "vh", [128, 20, 128])
            qnr = Ring(nc, es, "qnm", [128, 2048], 2)
            qrr = Ring(nc, es, "qrm", [128, 2048], 2)
            Pring = Ring(nc, es, "Pm", [128, 2560], 2)
            PTring = Ring(nc, es, "PTm", [128, 2560], 2)
            small = Ring(nc, es, "smm", [128, 16], 4)
            Oqring = Ring(nc, es, "Oqm", [128, 128], 2)
            ostg = Ring(nc, es, "ostgm", [128, 512], 2)
            wukv4 = wukv.t[:].rearrange("p a (k m) -> p a k m", k=2).bitcast(F32R)
            fw.dma("pool", wukv4, self.I("w_ukv")[l].rearrange("a p k m -> p a k m"), writes=wukv.b())
            for (tok0, n, ctx) in seqs:
                nqb = n // 128
                nk = n + (512 if ctx else 0)
                nkb = nk // 128
                kgs = [(s, min(512, nk - s)) for s in range(0, nk, 512)]
                ng = len(kgs)
                for k in range(2):
                    fw.dma("pool", ckvA.r((slice(None), k, slice(0, n))), self.ckvT[k, :, tok0:tok0 + n],
                           writes=ckvA.b())
                    if ctx:
                        fw.dma("pool", ckvA.r((slice(None), k, slice(n, n + 512))), self.I("cckvT")[l, k], writes=ckvA.b())
                fw.dma("pool", krA.r((slice(None), slice(0, n))), self.krT2[:, tok0:tok0 + n],
                       writes=krA.b())
                if ctx:
                    fw.dma("pool", krA.r((slice(None), slice(n, n + 512))), self.I("ckr2T")[l], writes=krA.b())
                qr, qr_pair = None, -1
                for h in heads:
                    pb = (h % 2) * 64
                    for gi, (s, w) in enumerate(kgs):
                        bank, bb = self.ps[gi % 4], self.psb[gi % 4]
                        for k in range(2):
                            self.mm(bank[:, 0:w], wukv4[:, h, k, :], ckvA.r((slice(None), k, slice(s, s + w))),
                                    k == 0, k == 1, wukv.b() + ckvA.b(), [bb])
                        self.copy(self.evac_eng(), knT.r((slice(None), slice(s, s + w))), bank[:, 0:w], [bb], knT.b())
                    for kb0 in range(0, nkb, 4):
                        cnt = min(4, nkb - kb0)
                        bank, bb = self.ps[4 + (kb0 // 4) % 2], self.psb[4 + (kb0 // 4) % 2]
                        for kb in range(kb0, kb0 + cnt):
                            for k in range(2):
                                self.mm(bank[:, (kb - kb0) * 128:(kb - kb0 + 1) * 128],
                                        ckvA.r((slice(None), k, slice(kb * 128, (kb + 1) * 128))), wukv4[:, 8 + h, k, :],
                                        k == 0, k == 1, wukv.b() + ckvA.b(), [bb], sync=(k == 1 and kb == kb0 + cnt - 1))
                        self.copy(self.evac_eng(), vh.r((slice(None), slice(kb0, kb0 + cnt), slice(None))),
                                  bank[:, 0:cnt * 128].rearrange("p (a d) -> p a d", a=cnt), [bb], vh.b())
                    qn = qnr.next()
                    fw.dma("pool", qn.r((slice(None), slice(0, n))), self.qnT[h, :, tok0:tok0 + n],
                           writes=qn.b())
                    if qr_pair != h // 2:
                        qr_pair = h // 2
                        qr = qrr.next()
                        fw.dma("pool", qr.r((slice(None), slice(0, n))), self.qrT[h // 2, :, tok0:tok0 + n],
                               writes=qr.b())
                    st = None
                    qbs = list(range(nqb)) if qb_limit is None else list(range(min(nqb, qb_limit)))
                    for qb in qbs:
                        qsl = slice(qb * 128, (qb + 1) * 128)
                        sm = small.next()
                        Pt = Pring.next()
                        for gi, (s, w) in enumerate(kgs):
                            bank, bb = self.ps[gi], self.psb[gi]
                            self.mm(bank[:, 0:w], qn.r((slice(None), qsl)), knT.r((slice(None), slice(s, s + w))),
                                    True, False, qn.b() + knT.b(), [bb], sync=False)
                            self.mm(bank[:, 0:w], qr.r((slice(pb, pb + 64), qsl)), krA.r((slice(pb, pb + 64), slice(s, s + w))),
                                    False, True, qr.b() + krA.b(), [bb], sync=True)
                            self.rmax(sm[:, gi:gi + 1], bank[:, 0:w], [bb], sm.b())
                        self.rmax(sm[:, 8:9], sm[:, 0:ng], sm.b(), sm.b())
                        self.ts(sm[:, 9:10], sm[:, 8:9], -scale, None, ALU.mult, None, sm.b(), sm.b())
                        for gi, (s, w) in enumerate(kgs):
                            bank, bb = self.ps[gi], self.psb[gi]
                            self.act(Pt[:, s:s + w], bank[:, 0:w], AF.Exp, [bb] + sm.b(), Pt.b() + sm.b(),
                                     bias=sm[:, 9:10], scale=scale, accum_out=sm[:, 10 + gi:11 + gi])
                        self.rsum(sm[:, 15:16], sm[:, 10:10 + ng], sm.b(), sm.b())
                        self.recip(sm[:, 15:16], sm[:, 15:16], sm.b(), sm.b())
                        PT = PTring.next()
                        for kb0 in range(0, nkb, 4):
                            cnt = min(4, nkb - kb0)
                            tb, tbb = self.ps[5 + (kb0 // 4) % 2], self.psb[5 + (kb0 // 4) % 2]
                            for kb in range(kb0, kb0 + cnt):
                                self.tr(tb[:, (kb - kb0) * 128:(kb - kb0 + 1) * 128], Pt[:, kb * 128:(kb + 1) * 128],
                                        Pt.b(), [tbb])
                            self.copy(self.evac_eng(), PT.r((slice(None), slice(kb0 * 128, (kb0 + cnt) * 128))),
                                      tb[:, 0:cnt * 128], [tbb], PT.b())
                        obank, obb = self.ps[7], self.psb[7]
                        for kb in range(nkb):
                            self.mm(obank[:, 0:128], PT.r((slice(None), slice(kb * 128, (kb + 1) * 128))),
                                    vh.r((slice(None), kb, slice(None))), kb == 0, kb == nkb - 1, PT.b() + vh.b(), [obb])
                        Oq = Oqring.next()
                        self.ts(Oq[:, :], obank[:, 0:128], sm[:, 15:16], None, ALU.mult, None, [obb] + sm.b(), Oq.b())
                        tb, tbb = self.ps[5], self.psb[5]
                        self.tr(tb[:, 0:128], Oq[:, :], Oq.b(), [tbb])
                        if qb % 4 == 0:
                            st = ostg.next()
                        self.copy(self.evac_eng(), st[:, (qb % 4) * 128:(qb % 4 + 1) * 128], tb[:, 0:128], [tbb], st.b())
                        if qb % 4 == 3 or qb == qbs[-1]:
                            q0 = (qb // 4) * 512
                            wd = (qb % 4 + 1) * 128
                            fw.dma("sp", self.obT[h, :, tok0 + q0:tok0 + q0 + wd], st[:, 0:wd], reads=st.b())
            fw.emit()

    def phase_pool(self, l, seqs=SEQS):
        nc, fw = self.nc, self.fw
        with contextlib.ExitStack() as es:
            invs = Tile(nc, es, "invs", [128, 4 * 2048])
            invp = Tile(nc, es, "invp", [128, 4 * 256])
            pw = Tile(nc, es, "pw", [128, 8, 256])
            upr = Ring(nc, es, "up", [128, 2064], 2)
            Ar = Ring(nc, es, "Apool", [128, 2064], 3)
            dT = [Tile(nc, es, "dT%d" % i, [128, 2048]) for i in range(2)]
            stg = Ring(nc, es, "pstg", [128, 512], 3)
            fw.dma("sp", invs[:], self.I("invc_s").rearrange("a n -> (a n)").partition_broadcast(128), writes=invs.b())
            fw.dma("sp", invp[:], self.I("invc_p").rearrange("a n -> (a n)").partition_broadcast(128), writes=invp.b())
            pw4 = pw.t[:].rearrange("p a (k m) -> p a k m", k=2).bitcast(F32R)
            fw.dma("pool", pw4, self.I("poolw")[l].rearrange("a p k m -> p a k m"), writes=pw.b())
            bi = 0
            for (tok0, n, ctx) in seqs:
                inv = invs if n == 2048 else invp
                for pg in range(4):
                    win = POOL_WINDOWS[pg]
                    left = win // 2
                    for half in range(2):
                        cc = pg * 2 + half
                        u = upr.next()
                        fw.op("dve", lambda u=u: nc.vector.memset(u[:, 0:8], 0.0), writes=u.b())
                        fw.op("dve", lambda u=u, n=n: nc.vector.memset(u[:, 8 + n:16 + n], 0.0), writes=u.b())
                        fw.dma("sp", u[:, 8:8 + n], self.uT[cc, :, tok0:tok0 + n], writes=u.b())
                        cur, L, step = u, n + 16, 1
                        while step < win:
                            nxt = Ar.next()
                            self.tt(nxt[:, 0:L - step], cur[:, 0:L - step], cur[:, step:L], ALU.add, cur.b(), nxt.b())
                            cur, L, step = nxt, L - step, step * 2
                        tmp = Ar.next()
                        self.tt(tmp[:, 0:n], cur[:, 8 - left:8 - left + n], inv[:, pg * n:(pg + 1) * n], ALU.mult,
                                cur.b() + inv.b(), tmp.b())
                        self.tt(dT[half].r((slice(None), slice(0, n))), tmp[:, 0:n], u[:, 8:8 + n], ALU.subtract,
                                tmp.b() + u.b(), dT[half].b())
                    for mh in range(2):
                        for tg in range(0, n, 512):
                            w = min(512, n - tg)
                            bi += 1
                            bank, bb = self.ps[bi % 4], self.psb[bi % 4]
                            for k in range(2):
                                self.mm(bank[:, 0:w], pw4[:, pg * 2 + mh, k, :], dT[k].r((slice(None), slice(tg, tg + w))),
                                        k == 0, k == 1, pw.b() + dT[k].b(), [bb])
                            st = stg.next()
                            col = R_PSC(l) + pg * 2 + mh
                            self.act(st[:, 0:w], bank[:, 0:w], AF.Copy, [bb] + self.vT.b(), st.b(),
                                     scale=self.vT[:, col:col + 1])
                            fw.dma("sp", self.ocT[pg * 2 + mh, :, tok0 + tg:tok0 + tg + w], st[:, 0:w], reads=st.b())
            fw.emit()

    def phase_merge(self, l, groups=None):
        nc, fw = self.nc, self.fw
        with contextlib.ExitStack() as es:
            xg = Tile(nc, es, "xg3", [128, 16 * 512])
            hT = Tile(nc, es, "hT3", [128, 16, 512], split=True)
            o3 = [Tile(nc, es, "o3_%d" % i, [128, 8, 512]) for i in range(3)]
            wring = Ring(nc, es, "w3", [128, 2048], 6)
            sgr = Ring(nc, es, "sg3", [128, 512], 2)
            tmr = Ring(nc, es, "tm3", [128, 512], 2)
            sqring = Ring(nc, es, "sq3", [128, 512], 2)
            tmpring = Ring(nc, es, "tmp3", [128, 512], 2)
            rstd = Tile(nc, es, "rstd3", [128, 512])
            xring = Ring(nc, es, "xr3", [128, 512], 3)
            self.epsb = Tile(nc, es, "epsb3", [128, 1])
            fw.op("dve", lambda: nc.vector.memset(self.epsb[:], EPS), writes=self.epsb.b())
            y3 = xg.t[:].rearrange("p (c n) -> p c n", c=16)
            srcs = [(self.oaT, "oaT"), (self.obT, "obT"), (self.ocT, "ocT")]
            mp = self.modp[l]
            it = 0
            for g in (groups or range(NGRP)):
                cd = 0 if g < 4 else 1
                gs = slice(g * G, (g + 1) * G)
                self.load_x(g, xg)
                self.norm_mod(g, l, 0, xg, hT, sqring, tmpring, rstd, self.ps[7], self.psb[7])
                for br in range(3):
                    fw.dma("pool", o3[br].r(slice(None)), srcs[br][0][:, :, gs].rearrange("c p n -> p c n"),
                           writes=o3[br].b())
                for d in range(16):
                    for br in range(3):
                        it += 1
                        w, slot = self.wload(wring, self.I("w_inG")[l, br * 16 + d], 16)
                        ga, gab = self.ps[it % 2], self.psb[it % 2]
                        for k in range(16):
                            self.mm(ga[:, :], w[:, k, :], hT.r((slice(None), k, slice(None))), k == 0, k == 15,
                                    slot.b() + hT.b(k), [gab])
                        w2, slot2 = self.wload(wring, self.I("wbr")[l, br, d], 8)
                        pr, prb = self.ps[2 + it % 2], self.psb[2 + it % 2]
                        for k in range(8):
                            self.mm(pr[:, :], w2[:, k, :], o3[br].r((slice(None), k, slice(None))), k == 0, k == 7,
                                    slot2.b() + o3[br].b(), [prb])
                        sg = sgr.next()
                        self.act(sg[:], ga[:, :], AF.Sigmoid, [gab], sg.b())
                        if br == 0:
                            self.tt(y3[:, d, :].bitcast(F32R), sg[:], pr[:, :], ALU.mult, sg.b() + [prb], xg.b())
                        else:
                            tm = tmr.next()
                            self.tt(tm[:], sg[:], pr[:, :], ALU.mult, sg.b() + [prb], tm.b())
                            out = y3[:, d, :].bitcast(F32R)
                            self.tt(out, y3[:, d, :], tm[:], ALU.add, xg.b() + tm.b(), xg.b())
                for d2 in range(16):
                    it += 1
                    w, slot = self.wload(wring, self.I("wout")[l, d2], 16)
                    bank, bb = self.ps[4 + it % 2], self.psb[4 + it % 2]
                    for k in range(16):
                        self.mm(bank[:, :], w[:, k, :], y3[:, k, :].bitcast(F32R), k == 0, k == 15, slot.b() + xg.b(), [bb])
                    xr = xring.next()
                    fw.dma("sp", xr[:], self.xT[d2, :, gs], reads=[self.b_x[g][d2]], writes=xr.b())
                    self.stt(xr[:], bank[:, :], mp[:, 2, d2:d2 + 1, cd], xr[:], ALU.mult, ALU.add,
                             [bb] + mp.b() + xr.b(), xr.b())
                    fw.dma("sp", self.xT[d2, :, gs], xr[:], reads=xr.b(), writes=[self.b_x[g][d2]])
            fw.emit()

    def phase_ffn(self, l, groups=None, final=False):
        nc, fw = self.nc, self.fw
        moe = (l % 2 == 1)
        nexp = self.moe_experts if moe else 1
        with contextlib.ExitStack() as es:
            xa = Tile(nc, es, "xa", [128, 16 * 512])
            hT = Tile(nc, es, "hT4", [128, 16, 512], split=True)
            aT = [Tile(nc, es, "aT%d" % i, [128, FB, 512], split=True) for i in range(2)]
            wgu = Ring(nc, es, "wgu", [128, 2048], 6)
            wdr = Ring(nc, es, "wdr", [128, FB * 128], 3)
            sglr = Ring(nc, es, "sgl", [128, 512], 2)
            t4r = Ring(nc, es, "t4", [128, 512], 2)
            sqring = Ring(nc, es, "sq4", [128, 512], 2)
            tmpring = Ring(nc, es, "tmp4", [128, 512], 2)
            rstd = Tile(nc, es, "rstd4", [128, 512])
            xring = Ring(nc, es, "xr4", [128, 512], 3)
            self.epsb = Tile(nc, es, "epsb4", [128, 1])
            fw.op("dve", lambda: nc.vector.memset(self.epsb[:], EPS), writes=self.epsb.b())
            if moe:
                rt = Tile(nc, es, "rt", [128, 16, 8])
                fw.dma("pool", rt.r(slice(None)), self.I("router"), writes=rt.b())
                gate = Tile(nc, es, "gate", [128, 4, 8])
                gsm = Ring(nc, es, "gsm", [128, 32], 2)
                gbr = Ring(nc, es, "gb", [128, 128], 2)
                gbcr = Ring(nc, es, "gbc", [128, 512], 2)
            a3 = xa.t[:].rearrange("p (c n) -> p c n", c=16)
            mp = self.modp[l]
            it = 0
            for g in (groups or range(NGRP)):
                cd = 0 if g < 4 else 1
                gs = slice(g * G, (g + 1) * G)
                self.load_x(g, xa)
                self.norm_mod(g, l, 1, xa, hT, sqring, tmpring, rstd, self.ps[7], self.psb[7])
                if moe:
                    for t in range(4):
                        lb, lbb = self.ps[6], self.psb[6]
                        for k in range(16):
                            self.mm(lb[:, t * 8:(t + 1) * 8], hT.r((slice(None), k, slice(t * 128, (t + 1) * 128))),
                                    rt.r((slice(None), k, slice(None))), k == 0, k == 15, hT.b(k) + rt.b(), [lbb])
                        sm = gsm.next()
                        lg = sm[:, 0:8]
                        self.copy("dve", lg, lb[:, t * 8:(t + 1) * 8], [lbb], sm.b())
                        self.rmax(sm[:, 24:25], lg, sm.b(), sm.b())
                        self.ts(sm[:, 8:16], lg, sm[:, 24:25], None, ALU.is_equal, None, sm.b(), sm.b())
                        self.stt(sm[:, 8:16], sm[:, 8:16], -1e30, lg, ALU.mult, ALU.add, sm.b(), sm.b())
                        self.rmax(sm[:, 25:26], sm[:, 8:16], sm.b(), sm.b())
                        self.ts(sm[:, 8:16], lg, sm[:, 25:26], None, ALU.is_ge, None, sm.b(), sm.b())
                        self.ts(sm[:, 26:27], sm[:, 24:25], -1.0, None, ALU.mult, None, sm.b(), sm.b())
                        self.act(sm[:, 16:24], lg, AF.Exp, sm.b(), sm.b(), bias=sm[:, 26:27], scale=1.0)
                        self.tt(sm[:, 16:24], sm[:, 16:24], sm[:, 8:16], ALU.mult, sm.b(), sm.b())
                        self.rsum(sm[:, 27:28], sm[:, 16:24], sm.b(), sm.b())
                        self.recip(sm[:, 27:28], sm[:, 27:28], sm.b(), sm.b())
                        self.ts(gate[:, t, :], sm[:, 16:24], sm[:, 27:28], None, ALU.mult, None, sm.b(), gate.b())
                first = True
                for e in range(nexp):
                    if moe:
                        gbank, gbb = self.ps[6], self.psb[6]
                        for t in range(4):
                            gb = gbr.next()
                            self.copy("dve", gb.r(slice(None)), gate[:, t, e:e + 1].to_broadcast([128, 128]), gate.b(), gb.b())
                            self.mm(gbank[:, t * 128:(t + 1) * 128], gb.r(slice(None)), self.identr.r(slice(None)), True, True,
                                    gb.b() + self.identr.b(), [gbb], sync=True)
                        gbc = gbcr.next()
                        self.copy("act", gbc[:], gbank[:, :], [gbb], gbc.b())
                        wg_d, wu_d, wd_d = self.I("moe_g")[e], self.I("moe_u")[e], self.I("moe_d")[e]
                    else:
                        wg_d, wu_d, wd_d = self.I("ffn_g"), self.I("ffn_u"), self.I("ffn_d")
                    for blk in range(NFF // FB):
                        at = aT[blk % 2]
                        for jj in range(FB):
                            j = blk * FB + jj
                            it += 1
                            wg, sg_ = self.wload(wgu, wg_d[j], 16)
                            gbk, gbkb = self.ps[it % 2], self.psb[it % 2]
                            for k in range(16):
                                self.mm(gbk[:, :], wg[:, k, :], hT.r((slice(None), k, slice(None))), k == 0, k == 15,
                                        sg_.b() + hT.b(k), [gbkb])
                            wu, su_ = self.wload(wgu, wu_d[j], 16)
                            ubk, ubkb = self.ps[2 + it % 2], self.psb[2 + it % 2]
                            for k in range(16):
                                self.mm(ubk[:, :], wu[:, k, :], hT.r((slice(None), k, slice(None))), k == 0, k == 15,
                                        su_.b() + hT.b(k), [ubkb])
                            sgl = sglr.next()
                            self.act(sgl[:], gbk[:, :], AF.Silu, [gbkb], sgl.b())
                            if moe:
                                t4 = t4r.next()
                                self.tt(t4[:], ubk[:, :], gbc[:], ALU.mult, [ubkb] + gbc.b(), t4.b())
                                self.tt(at.r((slice(None), jj, slice(None))), sgl[:], t4[:], ALU.mult, sgl.b() + t4.b(), at.b(jj))
                            else:
                                self.tt(at.r((slice(None), jj, slice(None))), sgl[:], ubk[:, :], ALU.mult, sgl.b() + [ubkb], at.b(jj))
                        for d in range(16):
                            it += 1
                            wd, sd_ = self.wload(wdr, wd_d[blk, d], FB)
                            dbk, dbkb = self.ps[4 + it % 2], self.psb[4 + it % 2]
                            for jj in range(FB):
                                self.mm(dbk[:, :], wd[:, jj, :], at.r((slice(None), jj, slice(None))), jj == 0, jj == FB - 1,
                                        sd_.b() + at.b(jj), [dbkb])
                            if first:
                                self.copy("dve", a3[:, d, :], dbk[:, :], [dbkb], xa.b())
                            else:
                                self.tt(a3[:, d, :], a3[:, d, :], dbk[:, :], ALU.add, xa.b() + [dbkb], xa.b())
                        first = False
                for d2 in range(16):
                    xr = xring.next()
                    fw.dma("sp", xr[:], self.xT[d2, :, gs], reads=[self.b_x[g][d2]], writes=xr.b())
                    self.stt(a3[:, d2, :], a3[:, d2, :], mp[:, 5, d2:d2 + 1, cd], xr[:], ALU.mult, ALU.add,
                             xa.b() + mp.b() + xr.b(), xa.b())
                    if not final:
                        fw.dma("sp", self.xT[d2, :, gs], a3[:, d2, :], reads=xa.b(), writes=[self.b_x[g][d2]])
                if final:
                    self.norm_stats(a3, xa.b(), 16, D, sqring, rstd, self.ps[7], self.psb[7])
                    for d2 in range(16):
                        xr = xring.next()
                        col = R_FG + d2
                        self.stt(xr[:], a3[:, d2, :], self.vT[:, col:col + 1], rstd[:], ALU.mult, ALU.mult,
                                 xa.b() + self.vT.b() + rstd.b(), xr.b())
                        fw.dma("sp", self.o_yT[d2, :, gs], xr[:], reads=xr.b())
            fw.emit()

    def phase_moe_sparse(self, l, final=False, slots=None, groups=None):
        nc, fw = self.nc, self.fw
        I32 = mybir.dt.int32
        mp = self.modp[l]
        NTI = NT // 128
        hTok, Ybuf, Tab = self.hTok, self.Ybuf, self.Tab
        b_hTok, b_Y, b_Tab = Buf(), Buf(), Buf()
        B3 = [128, NTI, 8]

        def tred(out, in_, op, reads, writes):
            fw.op("dve", lambda: nc.vector.tensor_reduce(out=out, in_=in_, axis=AX.X, op=op), reads=reads, writes=writes)

        with contextlib.ExitStack() as es0:
            esc = Tile(nc, es0, "esc", [128, 2 * NS])
            self.epsb = Tile(nc, es0, "epsb5", [128, 1])
            fw.op("dve", lambda: nc.vector.memset(self.epsb[:], EPS), writes=self.epsb.b())
            with contextlib.ExitStack() as es:
                xa = Tile(nc, es, "xa5", [128, 16 * 512])
                hT = Tile(nc, es, "hT5", [128, 16, 512], split=True)
                sqring = Ring(nc, es, "sq5", [128, 512], 2)
                tmpring = Ring(nc, es, "tmp5", [128, 512], 2)
                rstd = Tile(nc, es, "rstd5", [128, 512])
                hst = Ring(nc, es, "hst5", [128, 2048], 2)
                rt = Tile(nc, es, "rt5", [128, 16, 8])
                Um = Tile(nc, es, "Um", [128, 128])
                Lg = Tile(nc, es, "Lg", B3)
                eq1 = Tile(nc, es, "eq1", B3)
                sel = Tile(nc, es, "sel", B3)
                wk = Tile(nc, es, "wk", B3)
                wk2 = Tile(nc, es, "wk2", B3)
                gt = Tile(nc, es, "gt", B3)
                pos = Tile(nc, es, "pos", B3)
                tot = Tile(nc, es, "tot", B3)
                offs = Tile(nc, es, "offs", B3)
                m1 = Tile(nc, es, "m1", [128, NTI, 1])
                m2 = Tile(nc, es, "m2", [128, NTI, 1])
                sm = Tile(nc, es, "sm5", [128, 96])
                tokid = Tile(nc, es, "tokid", [128, NTI])
                svals = Tile(nc, es, "svals", [128, NS])
                rr = Tile(nc, es, "rr", [128, 2 * NTI])
                zero = Tile(nc, es, "zero5", [128, 2048])
                recs_t = es.enter_context(nc.sbuf_tensor("sb_recs", [128, 2 * NTI, 16], I32))
                ridx_t = es.enter_context(nc.sbuf_tensor("sb_ridx", [128, 2 * NTI], I32))
                b_recs, b_ridx = Buf(), Buf()
                fw.dma("pool", rt.r(slice(None)), self.I("router"), writes=rt.b())
                fw.dma("pool", Um.r(slice(None)), self.I("Umat"), writes=Um.b())
                fw.dma("sp", tokid[:], self.I("tokid"), writes=tokid.b())
                fw.dma("sp", svals[:], self.I("svals"), writes=svals.b())
                fw.dma("sp", Tab[:, :], self.I("Tab0", I32), writes=[b_Tab])
                fw.op("dve", lambda: nc.vector.memset(zero[:], 0.0), writes=zero.b())
                fw.op("dve", lambda: nc.vector.memset(recs_t[:], 0), writes=[b_recs])
                fw.dma("sp", hTok[NT:NT + 128, :], zero[:], reads=zero.b(), writes=[b_hTok])
                for g in range(NGRP):
                    self.load_x(g, xa)
                    self.norm_mod(g, l, 1, xa, hT, sqring, tmpring, rstd, self.ps[7], self.psb[7])
                    for t in range(4):
                        lb, lbb = self.ps[6], self.psb[6]
                        for k in range(16):
                            self.mm(lb[:, t * 8:(t + 1) * 8], hT.r((slice(None), k, slice(t * 128, (t + 1) * 128))),
                                    rt.r((slice(None), k, slice(None))), k == 0, k == 15, hT.b(k) + rt.b(), [lbb])
                        self.copy("dve", Lg[:, g * 4 + t, :], lb[:, t * 8:(t + 1) * 8], [lbb], Lg.b())
                        hs = hst.next()
                        for c4 in range(4):
                            tb, tbb = self.ps[c4], self.psb[c4]
                            for ci in range(4):
                                c = c4 * 4 + ci
                                self.tr(tb[:, ci * 128:(ci + 1) * 128], hT[:, c, t * 128:(t + 1) * 128], hT.b(c), [tbb])
                            self.copy(self.evac_eng(), hs[:, c4 * 512:(c4 + 1) * 512], tb[:, :], [tbb], hs.b())
                        r0 = g * 512 + t * 128
                        fw.dma("sp", hTok[r0:r0 + 128, :], hs[:], reads=hs.b(), writes=[b_hTok])
                tred(m1[:], Lg[:], ALU.max, Lg.b(), m1.b())
                self.tt(eq1[:], Lg[:], m1[:].to_broadcast(B3), ALU.is_equal, Lg.b() + m1.b(), eq1.b())
                self.stt(wk[:], eq1[:], -1e30, Lg[:], ALU.mult, ALU.add, eq1.b() + Lg.b(), wk.b())
                tred(m2[:], wk[:], ALU.max, wk.b(), m2.b())
                self.tt(sel.r(slice(None)), Lg[:], m2[:].to_broadcast(B3), ALU.is_ge, Lg.b() + m2.b(), sel.b())
                self.tt(wk[:], Lg[:], m1[:].to_broadcast(B3), ALU.subtract, Lg.b() + m1.b(), wk.b())
                self.act(wk[:], wk[:], AF.Exp, wk.b(), wk.b())
                self.tt(wk[:], wk[:], sel[:], ALU.mult, wk.b() + sel.b(), wk.b())
                tred(m2[:], wk[:], ALU.add, wk.b(), m2.b())
                self.recip(m2[:], m2[:], m2.b(), m2.b())
                self.tt(gt[:], wk[:], m2[:].to_broadcast(B3), ALU.mult, wk.b() + m2.b(), gt.b())
                pb_, pbb_ = self.ps[0], self.psb[0]
                tb_, tbb_ = self.ps[1], self.psb[1]
                for t in range(NTI):
                    self.mm(pb_[:, t * 8:(t + 1) * 8], Um.r(slice(None)), sel.r((slice(None), t, slice(None))), True, True,
                            Um.b() + sel.b(), [pbb_], sync=(t == NTI - 1))
                for t in range(NTI):
                    self.mm(tb_[:, t * 8:(t + 1) * 8], self.ones.r(slice(None)), sel.r((slice(None), t, slice(None))), True, True,
                            self.ones.b() + sel.b(), [tbb_], sync=(t == NTI - 1))
                self.copy("dve", pos.t[:].rearrange("p a e -> p (a e)"), pb_[:, 0:NTI * 8], [pbb_], pos.b())
                self.copy("act", tot.t[:].rearrange("p a e -> p (a e)"), tb_[:, 0:NTI * 8], [tbb_], tot.b())
                fw.op("dve", lambda: nc.vector.memset(offs[:, 0, :], 0.0), writes=offs.b())
                for t in range(1, NTI):
                    self.tt(offs[:, t, :], offs[:, t - 1, :], tot[:, t - 1, :], ALU.add, offs.b() + tot.b(), offs.b())
                self.tt(pos[:], pos[:], offs[:], ALU.add, pos.b() + offs.b(), pos.b())
                cnt, nsl, tmp8 = sm[:, 0:8], sm[:, 8:16], sm[:, 24:32]
                self.tt(cnt, offs[:, NTI - 1, :], tot[:, NTI - 1, :], ALU.add, offs.b() + tot.b(), sm.b())
                self.ts(nsl, cnt, 0.0, None, ALU.is_gt, None, sm.b(), sm.b())
                for kk in range(1, 5):
                    self.ts(tmp8, cnt, float(512 * kk), None, ALU.is_gt, None, sm.b(), sm.b())
                    self.tt(nsl, nsl, tmp8, ALU.add, sm.b(), sm.b())
                fw.op("dve", lambda: nc.vector.memset(sm[:, 16:17], 0.0), writes=sm.b())
                for e in range(1, NEXP):
                    self.tt(sm[:, 16 + e:17 + e], sm[:, 15 + e:16 + e], sm[:, 7 + e:8 + e], ALU.add, sm.b(), sm.b())
                self.ts(sm[:, 32:40], sm[:, 16:24], 512.0, None, ALU.mult, None, sm.b(), sm.b())
                self.tt(pos[:], pos[:], sm[:, 32:40].rearrange("p (a e) -> p a e", a=1).to_broadcast(B3), ALU.add,
                        pos.b() + sm.b(), pos.b())
                self.tt(wk2[:], sel[:], eq1[:], ALU.subtract, sel.b() + eq1.b(), wk2.b())
                rv = rr.t[:].rearrange("p (k a) -> p k a", k=2)
                recs_f = recs_t[:].bitcast(F32)
                for k_, oh in ((0, eq1), (1, wk2)):
                    ks = slice(k_ * NTI, (k_ + 1) * NTI)
                    self.tt(wk[:], pos[:], oh[:], ALU.mult, pos.b() + oh.b(), wk.b())
                    tred(rv[:, k_, :], wk[:], ALU.add, wk.b(), rr.b())
                    fw.op("dve", lambda ks=ks: nc.vector.tensor_copy(out=recs_t[:, ks, 0], in_=tokid[:]),
                          reads=tokid.b(), writes=[b_recs])
                    fw.op("dve", lambda ks=ks, k_=k_: nc.vector.tensor_scalar(out=recs_t[:, ks, 1], in0=tokid[:],
                                                                             scalar1=float(k_ * NT), scalar2=None, op0=ALU.add),
                          reads=tokid.b(), writes=[b_recs])
                    self.tt(wk[:], gt[:], oh[:], ALU.mult, gt.b() + oh.b(), wk.b())
                    tred(recs_f[:, ks, 2], wk[:], ALU.add, wk.b(), [b_recs])
                fw.op("dve", lambda: nc.vector.tensor_copy(out=ridx_t[:], in_=rr[:]), reads=rr.b(), writes=[b_ridx])
                for i in range(2 * NTI):
                    fw.dma_custom("pool", lambda i=i: nc.gpsimd.indirect_dma_start(
                        out=Tab[:, :], out_offset=bass.IndirectOffsetOnAxis(ap=ridx_t[:, i:i + 1], axis=0),
                        in_=recs_t[:, i, :], in_offset=None), reads=[b_recs, b_ridx], writes=[b_Tab])
                ev = sm[:, 40:40 + NS]
                tmpS = sm[:, 60:60 + NS]
                fw.op("dve", lambda: nc.vector.memset(ev, -1.0), writes=sm.b())
                for e in range(NEXP):
                    self.ts(tmpS, svals[:], sm[:, 16 + e:17 + e], None, ALU.is_ge, None, svals.b() + sm.b(), sm.b())
                    self.tt(ev, ev, tmpS, ALU.add, sm.b(), sm.b())
                self.ts(esc[:, 0:NS], ev, float(NFF * 128), None, ALU.mult, None, sm.b(), esc.b())
                self.ts(esc[:, NS:2 * NS], ev, float(64 * 128), None, ALU.mult, None, sm.b(), esc.b())
                fw.emit()
            with contextlib.ExitStack() as es:
                hTs = Tile(nc, es, "hTs", [128, 16, 512], split=True)
                acc = Tile(nc, es, "acc5", [128, 16 * 512])
                aT = [Tile(nc, es, "aT5_%d" % i, [128, FB, 512], split=True) for i in range(2)]
                wgu = Ring(nc, es, "wgu5", [128, 2048], 4)
                wdr = Ring(nc, es, "wdr5", [128, FB * 128], 3)
                sglr = Ring(nc, es, "sgl5", [128, 512], 2)
                gth = Ring(nc, es, "gth", [128, 2048], 2)
                otl = Ring(nc, es, "otl", [128, 2048], 2)
                wbase = Tile(nc, es, "wbase", [128, NFF])
                wdbase = Tile(nc, es, "wdbase", [128, 64])
                widx_t = [es.enter_context(nc.sbuf_tensor("sb_widx%d" % i, [128, NFF + 64], I32)) for i in range(2)]
                b_widx = [Buf(), Buf()]
                rbs_t = [es.enter_context(nc.sbuf_tensor("sb_rbs%d" % i, [128, 4, 16], I32)) for i in range(2)]
                b_rbs = [Buf(), Buf()]
                fw.dma("sp", wbase[:], self.I("wbase"), writes=wbase.b())
                fw.dma("sp", wdbase[:], self.I("wdbase"), writes=wdbase.b())
                a3 = acc.t[:].rearrange("p (c n) -> p c n", c=16)
                wg_rows = self.I("moe_g").rearrange("e j p k m -> (e j p) (k m)")
                wu_rows = self.I("moe_u").rearrange("e j p k m -> (e j p) (k m)")
                wd_rows = self.I("moe_d").rearrange("e b d p j m -> (e b d p) (j m)")
                it = 0
                for s in (slots if slots is not None else range(NS)):
                    widx, bw = widx_t[s % 2], b_widx[s % 2]
                    rbs, brb = rbs_t[s % 2], b_rbs[s % 2]
                    fw.op("dve", lambda widx=widx, s=s: nc.vector.tensor_scalar(
                        out=widx[:, 0:NFF], in0=wbase[:], scalar1=esc[:, s:s + 1], scalar2=None, op0=ALU.add),
                        reads=wbase.b() + esc.b(), writes=[bw])
                    fw.op("dve", lambda widx=widx, s=s: nc.vector.tensor_scalar(
                        out=widx[:, NFF:NFF + 64], in0=wdbase[:], scalar1=esc[:, NS + s:NS + s + 1], scalar2=None, op0=ALU.add),
                        reads=wdbase.b() + esc.b(), writes=[bw])
                    fw.dma("sp", rbs[:], Tab[s * 512:(s + 1) * 512, :].rearrange("(q p) c -> p q c", p=128),
                           reads=[b_Tab], writes=[brb])
                    for q in range(4):
                        gtile = gth.next()
                        fw.dma_custom("pool", lambda gtile=gtile, rbs=rbs, q=q: nc.gpsimd.indirect_dma_start(
                            out=gtile[:], out_offset=None, in_=hTok[:, :],
                            in_offset=bass.IndirectOffsetOnAxis(ap=rbs[:, q, 0:1], axis=0)),
                            reads=[brb, b_hTok], writes=gtile.b())
                        for c4 in range(4):
                            tb, tbb = self.ps[6 + c4 % 2], self.psb[6 + c4 % 2]
                            for ci in range(4):
                                c = c4 * 4 + ci
                                self.tr(tb[:, ci * 128:(ci + 1) * 128], gtile[:, c * 128:(c + 1) * 128], gtile.b(), [tbb])
                            self.copy(self.evac_eng(), hTs.r((slice(None), slice(c4 * 4, c4 * 4 + 4), slice(q * 128, (q + 1) * 128))),
                                      tb[:, :].rearrange("p (a n) -> p a n", a=4), [tbb],
                                      hTs.b(c4 * 4) + hTs.b(c4 * 4 + 1) + hTs.b(c4 * 4 + 2) + hTs.b(c4 * 4 + 3))

                    def wgather(ring, rows, col, nelem, widx=widx, bw=bw):
                        slot = ring.next()
                        dst = slot.t[:, 0:nelem].bitcast(F32R)
                        fw.dma_custom("pool", lambda: nc.gpsimd.indirect_dma_start(
                            out=dst, out_offset=None, in_=rows,
                            in_offset=bass.IndirectOffsetOnAxis(ap=widx[:, col:col + 1], axis=0)),
                            reads=[bw], writes=slot.b())
                        return slot
                    for blk in range(NFF // FB):
                        at = aT[blk % 2]
                        for jj in range(FB):
                            j = blk * FB + jj
                            it += 1
                            sg_ = wgather(wgu, wg_rows, j, 2048)
                            wg = sg_.t[:].rearrange("p (k m) -> p k m", k=16).bitcast(F32R)
                            gbk, gbkb = self.ps[it % 2], self.psb[it % 2]
                            for k in range(16):
                                self.mm(gbk[:, :], wg[:, k, :], hTs.r((slice(None), k, slice(None))), k == 0, k == 15,
                                        sg_.b() + hTs.b(k), [gbkb])
                            su_ = wgather(wgu, wu_rows, j, 2048)
                            wu = su_.t[:].rearrange("p (k m) -> p k m", k=16).bitcast(F32R)
                            ubk, ubkb = self.ps[2 + it % 2], self.psb[2 + it % 2]
                            for k in range(16):
                                self.mm(ubk[:, :], wu[:, k, :], hTs.r((slice(None), k, slice(None))), k == 0, k == 15,
                                        su_.b() + hTs.b(k), [ubkb])
                            sgl = sglr.next()
                            self.act(sgl[:], gbk[:, :], AF.Silu, [gbkb], sgl.b())
                            self.tt(at.r((slice(None), jj, slice(None))), sgl[:], ubk[:, :], ALU.mult, sgl.b() + [ubkb], at.b(jj))
                        for d in range(16):
                            it += 1
                            sd_ = wgather(wdr, wd_rows, NFF + blk * 16 + d, FB * 128)
                            wd = sd_.t[:].rearrange("p (k m) -> p k m", k=FB).bitcast(F32R)
                            dbk, dbkb = self.ps[4 + it % 2], self.psb[4 + it % 2]
                            for jj in range(FB):
                                self.mm(dbk[:, :], wd[:, jj, :], at.r((slice(None), jj, slice(None))), jj == 0, jj == FB - 1,
                                        sd_.b() + at.b(jj), [dbkb])
                            if blk == 0:
                                self.copy("dve", a3[:, d, :], dbk[:, :], [dbkb], acc.b())
                            else:
                                self.tt(a3[:, d, :], a3[:, d, :], dbk[:, :], ALU.add, acc.b() + [dbkb], acc.b())
                    rbs_f = rbs[:].bitcast(F32)
                    for q in range(4):
                        ot = otl.next()
                        for c4 in range(4):
                            tb, tbb = self.ps[6 + c4 % 2], self.psb[6 + c4 % 2]
                            for ci in range(4):
                                c = c4 * 4 + ci
                                self.tr(tb[:, ci * 128:(ci + 1) * 128], a3[:, c, q * 128:(q + 1) * 128], acc.b(), [tbb])
                            self.ts(ot[:, c4 * 512:(c4 + 1) * 512], tb[:, :], rbs_f[:, q, 2:3], None, ALU.mult, None,
                                    [tbb, brb], ot.b())
                        fw.dma_custom("pool", lambda ot=ot, rbs=rbs, q=q: nc.gpsimd.indirect_dma_start(
                            out=Ybuf[:, :], out_offset=bass.IndirectOffsetOnAxis(ap=rbs[:, q, 1:2], axis=0),
                            in_=ot[:], in_offset=None), reads=ot.b() + [brb], writes=[b_Y])
                fw.emit()
            with contextlib.ExitStack() as es:
                ys = Ring(nc, es, "ys", [128, 2048], 4)
                y2 = Ring(nc, es, "y2", [128, 2048], 2)
                xg = Tile(nc, es, "xg6", [128, 16 * 512])
                sqring = Ring(nc, es, "sq6", [128, 512], 2)
                rstd = Tile(nc, es, "rstd6", [128, 512])
                xring = Ring(nc, es, "xr6", [128, 512], 3)
                x3 = xg.t[:].rearrange("p (c n) -> p c n", c=16)
                for g in (groups or range(NGRP)):
                    cd = 0 if g < 4 else 1
                    gs = slice(g * G, (g + 1) * G)
                    self.load_x(g, xg)
                    yt = []
                    for t in range(4):
                        r0 = g * 512 + t * 128
                        a, b2 = ys.next(), y2.next()
                        fw.dma("sp", a[:], Ybuf[r0:r0 + 128, :], reads=[b_Y], writes=a.b())
                        fw.dma("sp", b2[:], Ybuf[NT + r0:NT + r0 + 128, :], reads=[b_Y], writes=b2.b())
                        self.tt(a[:], a[:], b2[:], ALU.add, a.b() + b2.b(), a.b(), en="pool" if t % 2 else "dve")
                        yt.append(a)
                    for c in range(16):
                        tb, tbb = self.ps[c % 4], self.psb[c % 4]
                        for t in range(4):
                            self.tr(tb[:, t * 128:(t + 1) * 128], yt[t][:, c * 128:(c + 1) * 128], yt[t].b(), [tbb])
                        self.stt(x3[:, c, :], tb[:, :], mp[:, 5, c:c + 1, cd], x3[:, c, :], ALU.mult, ALU.add,
                                 [tbb] + mp.b() + xg.b(), xg.b())
                        if not final:
                            fw.dma("sp", self.xT[c, :, gs], x3[:, c, :], reads=xg.b(), writes=[self.b_x[g][c]])
                    if final:
                        self.norm_stats(x3, xg.b(), 16, D, sqring, rstd, self.ps[7], self.psb[7])
                        for d2 in range(16):
                            xr = xring.next()
                            col = R_FG + d2
                            self.stt(xr[:], x3[:, d2, :], self.vT[:, col:col + 1], rstd[:], ALU.mult, ALU.mult,
                                     xg.b() + self.vT.b() + rstd.b(), xr.b())
                            fw.dma("sp", self.o_yT[d2, :, gs], xr[:], reads=xr.b())
                fw.emit()


def _tiles(W):
    K, M = W.shape
    return np.ascontiguousarray(W.reshape(K // 128, 128, M // 128, 128).transpose(2, 1, 0, 3))


def host_consts():
    c = {}
    c["ident"] = np.eye(128, dtype=np.float32)
    p = np.arange(128)
    d = p % 64
    partner = np.where((d % 32) < 16, p + 16, p - 16)
    pm = np.zeros((128, 128), np.float32)
    pm[partner, p] = 1.0
    c["permM"] = pm
    quarter = 16
    inv = (np.float32(10000.0) ** (-np.arange(quarter, dtype=np.float32) / np.float32(quarter))).astype(np.float32)
    n = np.arange(2048)
    rr = (n // 64).astype(np.float32)
    cc = (n % 64).astype(np.float32)
    ang_r = (rr[:, None] * inv[None, :]).astype(np.float32)
    ang_c = (cc[:, None] * inv[None, :]).astype(np.float32)
    C = np.zeros((128, 2048), np.float32)
    S = np.zeros((128, 2048), np.float32)
    for pp in range(128):
        dd = pp % 64
        j = dd % 16
        ang = ang_r[:, j] if dd < 32 else ang_c[:, j]
        C[pp] = np.cos(ang)
        sgn = -1.0 if (dd % 32) < 16 else 1.0
        S[pp] = sgn * np.sin(ang)
    c["ropeC"] = C
    c["ropeS"] = S
    r = np.arange(128)[:, None]
    cidx = np.arange(128)[None, :]
    m = np.zeros((128, 384), np.float32)
    m[:, 0:128] = np.where(cidx >= r, 0.0, -1e30)
    m[:, 256:384] = np.where(cidx <= r, 0.0, -1e30)
    c["maskA"] = m
    c["Umat"] = np.triu(np.ones((128, 128), np.float32), k=1)
    pp = np.arange(128, dtype=np.float32)[:, None]
    c["tokid"] = (np.arange(NT // 128, dtype=np.float32)[None, :] * 128 + pp).astype(np.float32)
    c["svals"] = np.tile(np.arange(NS, dtype=np.float32)[None, :], (128, 1))
    c["wbase"] = (np.arange(NFF, dtype=np.float32)[None, :] * 128 + pp).astype(np.float32)
    c["wdbase"] = (np.arange(64, dtype=np.float32)[None, :] * 128 + pp).astype(np.float32)
    tab0 = np.zeros((NS * 512, 16), np.int32)
    tab0[:, 0] = NT
    tab0[:, 1] = 2 * NT + (np.arange(NS * 512) % 128)
    c["Tab0"] = tab0
    for nm, nseq in (("invc_s", 2048), ("invc_p", 256)):
        t = np.arange(nseq)
        tab = np.zeros((4, nseq), np.float32)
        for gi, win in enumerate(POOL_WINDOWS):
            left = win // 2
            right = win - left - 1
            lo = np.maximum(t - left, 0)
            hi = np.minimum(t + right, nseq - 1) + 1
            tab[gi] = (1.0 / (hi - lo).astype(np.float32)).astype(np.float32)
        c[nm] = tab
    return c


def prep_shared(inp):
    sh = dict(host_consts())
    w_in = inp["w_in"]
    colsA = np.concatenate([
        np.arange(0, 1024),
        np.arange(1024, 1088), np.arange(1024, 1088),
        np.arange(1088, 1152), np.arange(1088, 1152),
        np.arange(1152, 1280),
        np.arange(1280, 1792),
        np.arange(1792, 2048),
        np.arange(2048, 2112), np.arange(2048, 2112),
        np.arange(2112, 3136)])
    sh["w_ada"] = np.stack([_tiles(inp["w_ada"][l]) for l in range(DEPTH)])
    sh["w_inA"] = np.stack([_tiles(w_in[l][:, colsA]) for l in range(DEPTH)])
    sh["w_inG"] = np.stack([_tiles(w_in[l][:, 3136:]) for l in range(DEPTH)])
    cq = [np.arange(h * 192, h * 192 + 128) for h in range(8)]
    for j in range(4):
        cq.append(np.concatenate([np.arange((2 * j) * 192 + 128, (2 * j) * 192 + 192),
                                  np.arange((2 * j + 1) * 192 + 128, (2 * j + 1) * 192 + 192)]))
    cq = np.concatenate(cq)
    sh["w_uq"] = np.stack([_tiles(inp["w_uq"][l][:, cq]) for l in range(DEPTH)])
    ckv = np.concatenate([np.arange(h * 256, h * 256 + 128) for h in range(8)] +
                         [np.arange(h * 256 + 128, h * 256 + 256) for h in range(8)])
    sh["w_ukv"] = np.stack([_tiles(inp["w_ukv"][l][:, ckv]) for l in range(DEPTH)])
    sh["poolw"] = np.stack([np.concatenate([_tiles(inp["pool_w"][l][g]) for g in range(4)]) for l in range(DEPTH)])
    sh["wbr"] = np.stack([np.stack([_tiles(inp[k][l]) for k in ("w_branch_a", "w_branch_b", "w_branch_c")])
                          for l in range(DEPTH)])
    sh["wout"] = np.stack([_tiles(inp["w_out"][l]) for l in range(DEPTH)])
    sh["ffn_g"] = _tiles(inp["ffn_w_gate"][0])
    sh["ffn_u"] = _tiles(inp["ffn_w_up"][0])

    def dtiles(wd):
        return np.ascontiguousarray(wd.reshape(4, FB, 128, 16, 128).transpose(0, 3, 2, 1, 4))
    sh["ffn_d"] = dtiles(inp["ffn_w_down"][0])
    sh["router"] = np.ascontiguousarray(inp["router_w"][0].reshape(16, 128, 8).transpose(1, 0, 2))
    sh["moe_g"] = np.stack([_tiles(inp["moe_w_gate"][0][e]) for e in range(NEXP)])
    sh["moe_u"] = np.stack([_tiles(inp["moe_w_up"][0][e]) for e in range(NEXP)])
    sh["moe_d"] = np.stack([dtiles(inp["moe_w_down"][0][e]) for e in range(NEXP)])
    sh["sink"] = np.ascontiguousarray(inp["attn_sink"])
    return sh


def prep_core(inp, i):
    m = {}
    x = np.concatenate([inp["x_sample"][i], inp["x_prompt"][2 * i], inp["x_prompt"][2 * i + 1]], axis=0)
    m["xinT"] = np.ascontiguousarray(x.T.reshape(16, 128, NT))
    v = np.zeros((NROWS, 128), np.float32)
    v[R_C:R_C + 16] = inp["c"][i].reshape(16, 128)
    v[R_CCTX:R_CCTX + 16] = inp["c_ctx"].reshape(16, 128)
    for l in range(DEPTH):
        v[R_LN1(l):R_LN1(l) + 16] = inp["ln1_g"][l].reshape(16, 128)
        v[R_LN2(l):R_LN2(l) + 16] = inp["ln2_g"][l].reshape(16, 128)
        v[R_BADA(l):R_BADA(l) + 96] = inp["b_ada"][l].reshape(96, 128)
        v[R_QG(l):R_QG(l) + 4] = inp["mla_q_norm_g"][l].reshape(4, 128)
        v[R_KVG(l):R_KVG(l) + 2] = inp["mla_kv_norm_g"][l].reshape(2, 128)
        v[R_PSC(l):R_PSC(l) + 8] = inp["pool_scale"][l].reshape(8, 128)
    v[R_FG:R_FG + 16] = inp["final_g"].reshape(16, 128)
    m["vecs"] = v
    ck = inp["cache_attn_k"][i]
    ckT = ck.transpose(0, 2, 3, 1)
    m["ck2T"] = np.ascontiguousarray(np.concatenate([ckT, ckT], axis=2))
    m["cv"] = np.ascontiguousarray(inp["cache_attn_v"][i].reshape(DEPTH, 512, 128))
    m["cckvT"] = np.ascontiguousarray(inp["cache_mla_ckv"][i].transpose(0, 2, 1).reshape(DEPTH, 2, 128, 512))
    krT = inp["cache_mla_krope"][i].transpose(0, 2, 1)
    m["ckr2T"] = np.ascontiguousarray(np.concatenate([krT, krT], axis=1))
    return m


def build_program():
    P = Prog()
    P.alloc_persistent()
    P.phase_prologue()
    for l in range(DEPTH):
        P.phase_proj(l)
        P.phase_attnA(l)
        P.phase_mla(l)
        P.phase_pool(l)
        P.phase_merge(l)
        if l % 2 == 1:
            P.phase_moe_sparse(l, final=(l == DEPTH - 1))
        else:
            P.phase_ffn(l, final=(l == DEPTH - 1))
    return P


def kernel(**inputs):
    inp = {k: np.asarray(v, dtype=np.float32) for k, v in inputs.items()}
    P = build_program()
    sh = prep_shared(inp)
    in_maps = []
    for i in range(NCORES):
        m = prep_core(inp, i)
        m.update(sh)
        in_maps.append({k: m[k] for k in P.din})
    res = run_bass_kernel_spmd(P.nc, in_maps, core_ids=list(range(NCORES)))
    y_prompt = np.zeros((16, 256, D), np.float32)
    y_sample = np.zeros((8, 2048, D), np.float32)
    st_k = np.zeros((16, DEPTH, 256, 2, 64), np.float32)
    st_v = np.zeros((16, DEPTH, 256, 2, 64), np.float32)
    st_ckv = np.zeros((16, DEPTH, 256, 256), np.float32)
    st_kr = np.zeros((16, DEPTH, 256, 64), np.float32)
    for i in range(NCORES):
        r = res.results[i]
        y = np.asarray(r["o_yT"]).reshape(D, NT).T
        y_sample[i] = y[0:2048]
        okT = np.asarray(r["o_kT"])
        ov = np.asarray(r["o_v"]).reshape(DEPTH, 512, 128)
        ockv = np.asarray(r["o_ckvT"]).reshape(DEPTH, 256, 512)
        okr = np.asarray(r["o_krT"])
        for j in range(2):
            b = 2 * i + j
            ts = slice(j * 256, (j + 1) * 256)
            y_prompt[b] = y[2048 + j * 256:2048 + (j + 1) * 256]
            st_k[b] = okT[:, :, :, ts].transpose(0, 3, 1, 2)
            st_v[b] = ov[:, ts, :].reshape(DEPTH, 256, 2, 64)
            st_ckv[b] = ockv[:, :, ts].transpose(0, 2, 1)
            st_kr[b] = okr[:, :, ts].transpose(0, 2, 1)
    return (y_prompt, y_sample, st_k, st_v, st_ckv, st_kr)
```

```python
import contextlib
import numpy as np
import concourse.bass as bass
import concourse.mybir as mybir
from concourse.bass_utils import run_bass_kernel_spmd

F32 = mybir.dt.float32
F32R = mybir.dt.float32r
BF16 = mybir.dt.bfloat16
AF = mybir.ActivationFunctionType
ALU = mybir.AluOpType
AX = mybir.AxisListType

NCORES = 8
D = 2048
NT = 2560
G = 512
NGRP = 5
DEPTH = 2
EPS = 1e-6
NFF = 44
FB = 11
NEXP = 8
NS = 17
DBG_GROUPS = None
SEQS = ((0, 2048, True), (2048, 256, False), (2304, 256, False))
POOL_WINDOWS = (2, 4, 8, 16)

R_C, R_CCTX = 0, 16


def R_LN1(l): return 32 + l * 142
def R_LN2(l): return 32 + l * 142 + 16
def R_BADA(l): return 32 + l * 142 + 32
def R_QG(l): return 32 + l * 142 + 128
def R_KVG(l): return 32 + l * 142 + 132
def R_PSC(l): return 32 + l * 142 + 134


R_FG = 32 + 2 * 142
NROWS = 384


class Buf:
    __slots__ = ("w", "r")

    def __init__(self):
        self.w = None
        self.r = {}


class Eng:
    def __init__(self, name, eng, sem):
        self.name = name
        self.eng = eng
        self.sem = sem
        self.cnt = 0
        self.seen = {}
        self.dsems = []
        self.dvals = []
        self.dn = 0
        self.pend = []
        self.prog = []


class FW:
    def __init__(self, nc, es, ndma_sems=8):
        self.nc = nc
        self.engs = {}
        for name, eng in (("pe", nc.tensor), ("act", nc.scalar), ("dve", nc.vector),
                          ("pool", nc.gpsimd), ("sp", nc.sync)):
            sem = es.enter_context(nc.semaphore("s_" + name))
            self.engs[name] = Eng(name, eng, sem)
        for qn in ("sp", "pool"):
            e = self.engs[qn]
            for i in range(ndma_sems):
                e.dsems.append(es.enter_context(nc.semaphore("d_%s%d" % (qn, i))))
                e.dvals.append(0)
        self.ninst = 0

    def _wait(self, e, ticket):
        sem, val, owner = ticket
        if owner is e:
            if e.name == "pe" or val > e.cnt:
                return
        key = id(sem)
        if e.seen.get(key, 0) >= val:
            return
        e.pend.append((sem, val))
        e.seen[key] = val
        self.ninst += 1

    def _deps(self, e, reads, writes):
        for b in reads:
            if b.w is not None:
                self._wait(e, b.w)
        for b in writes:
            if b.w is not None:
                self._wait(e, b.w)
            for t in b.r.values():
                self._wait(e, t)

    def _mark(self, e, ticket, reads, writes, key=None):
        for b in reads:
            b.r[key or e.name] = ticket
        for b in writes:
            b.w = ticket
            b.r = {}

    def op(self, en, fn, reads=(), writes=(), sync=True):
        e = self.engs[en]
        self._deps(e, reads, writes)
        self.ninst += 1
        waits, e.pend = e.pend, []
        if sync:
            e.cnt += 1
            e.prog.append((waits, fn, e.sem, 1))
            t = (e.sem, e.cnt, e)
        else:
            e.prog.append((waits, fn, None, 0))
            t = (e.sem, e.cnt + 1, e)
        self._mark(e, t, reads, writes)

    def dma(self, qn, out, in_, reads=(), writes=(), **kw):
        e = self.engs[qn]
        slot = e.dn % len(e.dsems)
        e.dn += 1
        sem = e.dsems[slot]
        if e.dvals[slot] > 0:
            self._wait(e, (sem, e.dvals[slot], None))
        self._deps(e, reads, writes)
        e.dvals[slot] += 16
        eng = e.eng
        waits, e.pend = e.pend, []
        e.prog.append((waits, (lambda: eng.dma_start(out=out, in_=in_, **kw)), sem, 16))
        self.ninst += 1
        self._mark(e, (sem, e.dvals[slot], None), reads, writes, key=(e.name, slot))

    def dma_custom(self, qn, fn, reads=(), writes=()):
        e = self.engs[qn]
        slot = e.dn % len(e.dsems)
        e.dn += 1
        sem = e.dsems[slot]
        if e.dvals[slot] > 0:
            self._wait(e, (sem, e.dvals[slot], None))
        self._deps(e, reads, writes)
        e.dvals[slot] += 16
        waits, e.pend = e.pend, []
        e.prog.append((waits, fn, sem, 16))
        self.ninst += 1
        self._mark(e, (sem, e.dvals[slot], None), reads, writes, key=(e.name, slot))

    def emit(self):
        sp = self.engs["sp"]
        for qn in ("sp", "pool"):
            q = self.engs[qn]
            for s, v in zip(q.dsems, q.dvals):
                if v > 0:
                    self._wait(sp, (s, v, None))
        for en in ("pe", "act", "dve", "pool"):
            e = self.engs[en]
            if e.cnt > 0:
                self._wait(sp, (e.sem, e.cnt, None))

        def replay(e):
            eng = e.eng
            for waits, fn, sem, inc in e.prog:
                for s, v in waits:
                    eng.wait_ge(s, v)
                ins = fn()
                if sem is not None:
                    ins.then_inc(sem, inc)
            for s, v in e.pend:
                eng.wait_ge(s, v)
            e.prog = []
            e.pend = []

        with self.nc.Block() as block:
            @block.sync
            def _(x):
                replay(self.engs["sp"])

            @block.tensor
            def _(x):
                replay(self.engs["pe"])

            @block.scalar
            def _(x):
                replay(self.engs["act"])

            @block.vector
            def _(x):
                replay(self.engs["dve"])

            @block.gpsimd
            def _(x):
                replay(self.engs["pool"])


class Tile:
    _n = [0]

    def __init__(self, nc, es, name, shape, split=False, dt=None):
        Tile._n[0] += 1
        self.t = es.enter_context(nc.sbuf_tensor("sb%d_%s" % (Tile._n[0], name), list(shape), dt or F32))
        self.split = split
        if split:
            self.bufs = [Buf() for _ in range(shape[1])]
        else:
            self.bufs = [Buf()]

    def __getitem__(self, idx):
        return self.t[idx]

    def r(self, idx):
        return self.t[idx].bitcast(F32R)

    def b(self, i=None):
        if self.split and i is not None:
            return [self.bufs[i]]
        return list(self.bufs)


class Ring:
    def __init__(self, nc, es, name, shape, n, dt=None):
        self.tiles = [Tile(nc, es, "%s%d" % (name, i), shape, dt=dt) for i in range(n)]
        self.i = 0

    def next(self):
        t = self.tiles[self.i % len(self.tiles)]
        self.i += 1
        return t


class Prog:
    def __init__(self, stop_after=None, moe_experts=NEXP, scratch_in=(), moe_sparse=True):
        self.stop_after = stop_after
        self.moe_experts = moe_experts
        self.nc = nc = bass.Bass("TRN2", target_bir_lowering=False)
        self.es = es = contextlib.ExitStack()
        self.fw = FW(nc, es)
        self.din = {}
        self.evac_i = 0

        self.ishapes = {}

        def inp(name, shape):
            self.ishapes[name] = list(shape)

        def outp(name, shape):
            return nc.dram_tensor(name, list(shape), F32, kind="ExternalOutput").ap()

        def scr(name, shape):
            return nc.dram_tensor(name, list(shape), F32, kind="Internal").ap()

        inp("xinT", [16, 128, NT])
        inp("vecs", [NROWS, 128])
        inp("sink", [DEPTH, 16])
        inp("ident", [128, 128])
        inp("permM", [128, 128])
        inp("ropeC", [128, 2048])
        inp("ropeS", [128, 2048])
        inp("maskA", [128, 384])
        inp("invc_s", [4, 2048])
        inp("invc_p", [4, 256])
        inp("ck2T", [DEPTH, 2, 128, 512])
        inp("cv", [DEPTH, 512, 128])
        inp("cckvT", [DEPTH, 2, 128, 512])
        inp("ckr2T", [DEPTH, 128, 512])
        inp("w_ada", [DEPTH, 96, 128, 16, 128])
        inp("w_inA", [DEPTH, 26, 128, 16, 128])
        inp("w_inG", [DEPTH, 48, 128, 16, 128])
        inp("w_uq", [DEPTH, 12, 128, 4, 128])
        inp("w_ukv", [DEPTH, 16, 128, 2, 128])
        inp("poolw", [DEPTH, 8, 128, 2, 128])
        inp("wbr", [DEPTH, 3, 16, 128, 8, 128])
        inp("wout", [DEPTH, 16, 128, 16, 128])
        inp("ffn_g", [NFF, 128, 16, 128])
        inp("ffn_u", [NFF, 128, 16, 128])
        inp("ffn_d", [4, 16, 128, FB, 128])
        inp("router", [128, 16, 8])
        inp("moe_g", [NEXP, NFF, 128, 16, 128])
        inp("moe_u", [NEXP, NFF, 128, 16, 128])
        inp("moe_d", [NEXP, 4, 16, 128, FB, 128])
        inp("Umat", [128, 128])
        inp("tokid", [128, NT // 128])
        inp("svals", [128, NS])
        inp("wbase", [128, NFF])
        inp("wdbase", [128, 64])
        inp("Tab0", [NS * 512, 16])
        self.o_yT = outp("o_yT", [16, 128, NT])
        self.o_kT = outp("o_kT", [DEPTH, 2, 64, 512])
        self.o_v = outp("o_v", [DEPTH, 4, 128, 128])
        self.o_ckvT = outp("o_ckvT", [DEPTH, 2, 128, 512])
        self.o_krT = outp("o_krT", [DEPTH, 64, 512])
        def mk(name, shape):
            if name in scratch_in:
                return nc.dram_tensor(name, list(shape), F32, kind="ExternalInput").ap()
            return (outp if stop_after is not None else scr)(name, shape)
        self.xT = mk("s_xT", [16, 128, NT])
        self.qaT = mk("s_qaT", [8, 128, NT])
        self.kaT2 = mk("s_kaT2", [2, 128, NT])
        self.vaTok = mk("s_vaTok", [NT // 128, 128, 128])
        self.qnT = mk("s_qnT", [8, 128, NT])
        self.qrT = mk("s_qrT", [4, 128, NT])
        self.ckvT = mk("s_ckvT", [2, 128, NT])
        self.krT2 = mk("s_krT2", [128, NT])
        self.uT = mk("s_uT", [8, 128, NT])
        self.oaT = mk("s_oaT", [8, 128, NT])
        self.obT = mk("s_obT", [8, 128, NT])
        self.ocT = mk("s_ocT", [8, 128, NT])
        if moe_sparse:
            self.hTok = scr("s_hTok", [NT + 128, D])
            self.Ybuf = scr("s_Y", [2 * NT + 128, D])
            self.Tab = nc.dram_tensor("s_Tab", [NS * 512, 16], mybir.dt.int32, kind="Internal").ap()
        self.b_x = [[Buf() for _ in range(16)] for _ in range(NGRP)]
        self.b_scr = {k: Buf() for k in ("qaT", "kaT2", "vaTok", "qnT", "qrT", "ckvT", "krT2", "uT",
                                         "oaT", "obT", "ocT")}
        self.ps = [es.enter_context(nc.psum_tensor("ps%d" % i, [128, 512], F32)) for i in range(8)]
        self.psb = [Buf() for _ in range(8)]

    def I(self, name, dt=F32):
        if name not in self.din:
            self.din[name] = self.nc.dram_tensor(name, self.ishapes[name], dt, kind="ExternalInput").ap()
        return self.din[name]

    def mm(self, out, lhsT, rhs, start, stop, reads, writes, sync=None):
        nc = self.nc
        if sync is None:
            sync = stop
        self.fw.op("pe", lambda: nc.tensor.matmul(out, lhsT=lhsT, rhs=rhs, start=start, stop=stop),
                   reads=reads, writes=writes, sync=sync)

    def tr(self, out, in_, reads, writes, sync=True):
        nc = self.nc
        ident = self.ident[:]
        self.fw.op("pe", lambda: nc.tensor.transpose(out=out, in_=in_, identity=ident),
                   reads=reads + self.ident.b(), writes=writes, sync=sync)

    def copy(self, en, out, in_, reads, writes):
        nc = self.nc
        if en == "act":
            self.fw.op("act", lambda: nc.scalar.copy(out=out, in_=in_), reads=reads, writes=writes)
        elif en == "dve":
            self.fw.op("dve", lambda: nc.vector.tensor_copy(out=out, in_=in_), reads=reads, writes=writes)
        else:
            self.fw.op("pool", lambda: nc.gpsimd.tensor_copy(out=out, in_=in_), reads=reads, writes=writes)

    def evac_eng(self):
        self.evac_i += 1
        return "act" if self.evac_i % 2 else "dve"

    def act(self, out, in_, func, reads, writes, bias=None, scale=None, accum_out=None):
        nc = self.nc
        kw = {}
        if bias is not None:
            kw["bias"] = bias
        if scale is not None:
            kw["scale"] = scale
        if accum_out is not None:
            kw["accum_out"] = accum_out
        self.fw.op("act", lambda: nc.scalar.activation(out=out, in_=in_, func=func, **kw),
                   reads=reads, writes=writes)

    def tt(self, out, in0, in1, op, reads, writes, en="dve"):
        nc = self.nc
        eng = nc.vector if en == "dve" else nc.gpsimd
        self.fw.op(en, lambda: eng.tensor_tensor(out=out, in0=in0, in1=in1, op=op), reads=reads, writes=writes)

    def ts(self, out, in0, s1, s2, op0, op1, reads, writes):
        nc = self.nc
        if op1 is None:
            self.fw.op("dve", lambda: nc.vector.tensor_scalar(out=out, in0=in0, scalar1=s1, scalar2=None, op0=op0),
                       reads=reads, writes=writes)
        else:
            self.fw.op("dve", lambda: nc.vector.tensor_scalar(out=out, in0=in0, scalar1=s1, scalar2=s2,
                                                              op0=op0, op1=op1), reads=reads, writes=writes)

    def stt(self, out, in0, scalar, in1, op0, op1, reads, writes):
        nc = self.nc
        self.fw.op("dve", lambda: nc.vector.scalar_tensor_tensor(out=out, in0=in0, scalar=scalar, in1=in1,
                                                                 op0=op0, op1=op1), reads=reads, writes=writes)

    def wload(self, ring, dram_tile, nk, ncol=128):
        slot = ring.next()
        dst = slot.t[:, 0:nk * ncol].rearrange("p (k m) -> p k m", k=nk)
        self.fw.dma("pool", dst.bitcast(F32R), dram_tile, writes=slot.b())
        return dst.bitcast(F32R), slot

    def alloc_persistent(self):
        nc, es = self.nc, self.es
        self.ident = Tile(nc, es, "ident", [128, 128])
        self.ones = Tile(nc, es, "ones", [128, 128])
        self.identr = Tile(nc, es, "identr", [128, 128])
        self.vT = Tile(nc, es, "vT", [128, NROWS])
        self.modp = [Tile(nc, es, "modp%d" % l, [128, 6, 16, 2]) for l in range(DEPTH)]
        self.sinkb = Tile(nc, es, "sinkb", [128, DEPTH * 16])

    def phase_prologue(self):
        nc, fw = self.nc, self.fw
        with contextlib.ExitStack() as es:
            vrows = Tile(nc, es, "vrows", [128, 3, 128])
            scT = Tile(nc, es, "scT", [128, 32])
            modT = Tile(nc, es, "modT", [128, 96, 2])
            wring = Ring(nc, es, "wada", [128, 2048], 4)
            fw.dma("sp", self.ident[:], self.I("ident"), writes=self.ident.b())
            fw.dma("sp", vrows[:], self.I("vecs").rearrange("(a p) f -> p a f", p=128), writes=vrows.b())
            fw.dma("sp", self.sinkb[:],
                   self.I("sink").rearrange("l h -> (l h)").partition_broadcast(128), writes=self.sinkb.b())
            ones32 = Tile(nc, es, "ones32", [128, 128])
            fw.op("dve", lambda: nc.vector.memset(ones32[:], 1.0), writes=ones32.b())
            self.copy("act", self.ones.r(slice(None)), ones32[:], ones32.b(), self.ones.b())
            self.copy("act", self.identr.r(slice(None)), self.ident[:], self.ident.b(), self.identr.b())
            for a in range(3):
                self.tr(self.ps[0][:, a * 128:(a + 1) * 128], vrows[:, a, :], vrows.b(), [self.psb[0]])
            self.copy("dve", self.vT[:], self.ps[0][:, 0:384], [self.psb[0]], self.vT.b())
            self.act(scT.r(slice(None)), self.vT[:, 0:32], AF.Silu, self.vT.b(), scT.b())
            sc3 = scT.r(slice(None)).rearrange("p (c k) -> p k c", c=2)
            for l in range(DEPTH):
                for j in range(96):
                    w, slot = self.wload(wring, self.I("w_ada")[l, j], 16)
                    bank = self.ps[1 + (j % 2)]
                    bb = self.psb[1 + (j % 2)]
                    for k in range(16):
                        self.mm(bank[:, 0:2], w[:, k, :], sc3[:, k, :], k == 0, k == 15,
                                slot.b() + scT.b(), [bb])
                    col = R_BADA(l) + j
                    self.act(modT[:, j, :], bank[:, 0:2], AF.Identity, [bb] + self.vT.b(), modT.b(),
                             bias=self.vT[:, col:col + 1])
                mp = self.modp[l]
                for s in range(2):
                    lnr = R_LN1(l) if s == 0 else R_LN2(l)
                    sh, sc, gt = 3 * s, 3 * s + 1, 3 * s + 2
                    for cd in range(2):
                        self.stt(mp[:, 3 * s + 0, :, cd], modT[:, sc * 16:(sc + 1) * 16, cd], 1.0,
                                 self.vT[:, lnr:lnr + 16], ALU.add, ALU.mult,
                                 modT.b() + self.vT.b(), mp.b())
                        self.copy("dve", mp[:, 3 * s + 1, :, cd], modT[:, sh * 16:(sh + 1) * 16, cd], modT.b(), mp.b())
                        self.copy("dve", mp[:, 3 * s + 2, :, cd], modT[:, gt * 16:(gt + 1) * 16, cd], modT.b(), mp.b())
            xt = Ring(nc, es, "xcp", [128, 16 * 512], 2)
            for g in range(NGRP):
                t = xt.next()
                v = t.t[:].rearrange("p (c n) -> p c n", c=16)
                fw.dma("sp", v, self.I("xinT")[:, :, g * G:(g + 1) * G].rearrange("c p n -> p c n"), writes=t.b())
                fw.dma("sp", self.xT[:, :, g * G:(g + 1) * G].rearrange("c p n -> p c n"), v,
                       reads=t.b(), writes=self.b_x[g])
            fw.emit()

    def norm_stats(self, x3, xb, nchunk, nfeat, sqring, rstd, bank, bb):
        nc = self.nc
        for c in range(nchunk):
            sq = sqring.next()
            self.act(sq.r(slice(None)), x3[:, c, :], AF.Square, xb, sq.b())
            self.mm(bank[:, :], self.ones.r(slice(None)), sq.r(slice(None)), c == 0, c == nchunk - 1,
                    self.ones.b() + sq.b(), [bb], sync=True)
        self.act(rstd[:], bank[:, :], AF.Sqrt, [bb] + self.epsb.b(), rstd.b(), bias=self.epsb[:, 0:1], scale=1.0 / nfeat)
        self.fw.op("dve", lambda: nc.vector.reciprocal(out=rstd[:], in_=rstd[:]), reads=rstd.b(), writes=rstd.b())

    def norm_mod(self, g, l, s, xg, hT, sqring, tmpring, rstd, bank, bb):
        cd = 0 if g < 4 else 1
        mp = self.modp[l]
        x3 = xg.t[:].rearrange("p (c n) -> p c n", c=16)
        self.norm_stats(x3, xg.b(), 16, D, sqring, rstd, bank, bb)
        for c in range(16):
            tmp = tmpring.next()
            self.tt(tmp[:], x3[:, c, :], rstd[:], ALU.mult, xg.b() + rstd.b(), tmp.b())
            self.act(hT.r((slice(None), c, slice(None))), tmp[:], AF.Identity, tmp.b() + mp.b(), hT.b(c),
                     bias=mp[:, 3 * s + 1, c:c + 1, cd], scale=mp[:, 3 * s + 0, c:c + 1, cd])

    def load_x(self, g, xg):
        v = xg.t[:].rearrange("p (c n) -> p c n", c=16)
        self.fw.dma("sp", v, self.xT[:, :, g * G:(g + 1) * G].rearrange("c p n -> p c n"),
                    reads=self.b_x[g], writes=xg.b())

    def phase_proj(self, l):
        nc, fw = self.nc, self.fw
        with contextlib.ExitStack() as es:
            xg = Tile(nc, es, "xg", [128, 16 * 512])
            hT = Tile(nc, es, "hT", [128, 16, 512], split=True)
            wring = Ring(nc, es, "w1", [128, 2048], 4)
            wuq = Tile(nc, es, "wuq", [128, 12, 512])
            ropeC = Tile(nc, es, "ropeC", [128, 2048])
            ropeS = Tile(nc, es, "ropeS", [128, 2048])
            permM = Tile(nc, es, "permM", [128, 128])
            cqs = Tile(nc, es, "cqs", [128, 4, 512])
            cqn = Tile(nc, es, "cqn", [128, 4, 512])
            ckvs = Tile(nc, es, "ckvs", [128, 2, 512])
            stg = Ring(nc, es, "stg", [128, 512], 4)
            sqring = Ring(nc, es, "sq", [128, 512], 3)
            tmpring = Ring(nc, es, "tmp", [128, 512], 3)
            xsring = Ring(nc, es, "xs", [128, 512], 2)
            t1ring = Ring(nc, es, "t1", [128, 512], 2)
            rstd = Tile(nc, es, "rstd", [128, 512])
            rstd2 = Tile(nc, es, "rstd2", [128, 512])
            vtok = Ring(nc, es, "vtok", [128, 512], 2)
            self.epsb = Tile(nc, es, "epsb", [128, 1])
            fw.op("dve", lambda: nc.vector.memset(self.epsb[:], EPS), writes=self.epsb.b())
            fw.dma("sp", ropeC[:], self.I("ropeC"), writes=ropeC.b())
            fw.dma("sp", ropeS[:], self.I("ropeS"), writes=ropeS.b())
            fw.dma("pool", permM.r(slice(None)), self.I("permM"), writes=permM.b())
            fw.dma("pool", wuq.t[:].rearrange("p a (k m) -> p a k m", k=4).bitcast(F32R),
                   self.I("w_uq")[l].rearrange("a p k m -> p a k m"), writes=wuq.b())
            wuq4 = wuq.t[:].rearrange("p a (k m) -> p a k m", k=4).bitcast(F32R)
            bi = [0]

            def nextbank():
                bi[0] += 1
                i = bi[0] % 4
                return self.ps[i], self.psb[i]

            def finish_chunk(bank, bb, g, rope, dst, dbuf, P=128, dst2=None):
                st = stg.next()
                if rope and g < 4:
                    xs = xsring.next()
                    self.copy("act", xs.r(slice(None))[0:P], bank[0:P, :], [bb], xs.b())
                    pb, pbb = self.ps[4 + (bi[0] % 2)], self.psb[4 + (bi[0] % 2)]
                    self.mm(pb[0:P, :], permM.r(slice(None))[0:P, 0:P], xs.r(slice(None))[0:P], True, True,
                            permM.b() + xs.b(), [pbb])
                    t1 = t1ring.next()
                    self.tt(t1[0:P], xs[0:P], ropeC[0:P, g * G:(g + 1) * G], ALU.mult, xs.b() + ropeC.b(), t1.b())
                    self.tt(st[0:P], pb[0:P, :], ropeS[0:P, g * G:(g + 1) * G], ALU.mult, [pbb] + ropeS.b(), st.b())
                    self.tt(st[0:P], st[0:P], t1[0:P], ALU.add, st.b() + t1.b(), st.b())
                else:
                    self.copy(self.evac_eng(), st[0:P], bank[0:P, :], [bb], st.b())
                fw.dma("sp", dst, st[0:P], reads=st.b())
                if dst2 is not None:
                    fw.dma("sp", dst2, st[0:dst2.shape[0]], reads=st.b())
                return st

            for g in (DBG_GROUPS or range(NGRP)):
                gs = slice(g * G, (g + 1) * G)
                self.load_x(g, xg)
                self.norm_mod(g, l, 0, xg, hT, sqring, tmpring, rstd, self.ps[7], self.psb[7])
                for ch in range(26):
                    w, slot = self.wload(wring, self.I("w_inA")[l, ch], 16)
                    bank, bb = nextbank()
                    for k in range(16):
                        self.mm(bank[:, :], w[:, k, :], hT.r((slice(None), k, slice(None))), k == 0, k == 15,
                                slot.b() + hT.b(k), [bb])
                    if ch < 8:
                        finish_chunk(bank, bb, g, True, self.qaT[ch, :, gs], self.b_scr["qaT"])
                    elif ch < 10:
                        d2 = self.o_kT[l, ch - 8, :, :] if g == 4 else None
                        finish_chunk(bank, bb, g, True, self.kaT2[ch - 8, :, gs], self.b_scr["kaT2"], dst2=d2)
                    elif ch == 10:
                        st = stg.next()
                        self.copy(self.evac_eng(), st[:], bank[:, :], [bb], st.b())
                        tb, tbb = self.ps[6], self.psb[6]
                        for t in range(4):
                            self.tr(tb[:, t * 128:(t + 1) * 128], st[:, t * 128:(t + 1) * 128], st.b(), [tbb])
                        vt = vtok.next()
                        self.copy(self.evac_eng(), vt[:], tb[:, :], [tbb], vt.b())
                        fw.dma("sp", self.vaTok[g * 4:(g + 1) * 4].rearrange("t p d -> p t d"),
                               vt.t[:].rearrange("p (t d) -> p t d", t=4), reads=vt.b())
                        if g == 4:
                            fw.dma("sp", self.o_v[l].rearrange("t p d -> p t d"),
                                   vt.t[:].rearrange("p (t d) -> p t d", t=4), reads=vt.b())
                    elif ch < 15:
                        self.copy(self.evac_eng(), cqs[:, ch - 11, :], bank[:, :], [bb], cqs.b())
                        if ch == 14:
                            self.norm_stats(cqs.t[:], cqs.b(), 4, 512, sqring, rstd2, self.ps[7], self.psb[7])
                            for c in range(4):
                                col = R_QG(l) + c
                                self.stt(cqn.r((slice(None), c, slice(None))), cqs[:, c, :], self.vT[:, col:col + 1],
                                         rstd2[:], ALU.mult, ALU.mult, cqs.b() + rstd2.b() + self.vT.b(), cqn.b())
                            for a in range(12):
                                bank2, bb2 = nextbank()
                                for k in range(4):
                                    self.mm(bank2[:, :], wuq4[:, a, k, :], cqn.r((slice(None), k, slice(None))),
                                            k == 0, k == 3, wuq.b() + cqn.b(), [bb2])
                                if a < 8:
                                    finish_chunk(bank2, bb2, g, False, self.qnT[a, :, gs], self.b_scr["qnT"])
                                else:
                                    finish_chunk(bank2, bb2, g, True, self.qrT[a - 8, :, gs], self.b_scr["qrT"])
                    elif ch < 17:
                        self.copy(self.evac_eng(), ckvs[:, ch - 15, :], bank[:, :], [bb], ckvs.b())
                        if ch == 16:
                            self.norm_stats(ckvs.t[:], ckvs.b(), 2, 256, sqring, rstd2, self.ps[7], self.psb[7])
                            for c in range(2):
                                col = R_KVG(l) + c
                                st = stg.next()
                                self.stt(st[:], ckvs[:, c, :], self.vT[:, col:col + 1], rstd2[:], ALU.mult, ALU.mult,
                                         ckvs.b() + rstd2.b() + self.vT.b(), st.b())
                                fw.dma("sp", self.ckvT[c, :, gs], st[:], reads=st.b())
                                if g == 4:
                                    fw.dma("sp", self.o_ckvT[l, c], st[:], reads=st.b())
                    elif ch == 17:
                        d2 = self.o_krT[l] if g == 4 else None
                        finish_chunk(bank, bb, g, True, self.krT2[:, gs], self.b_scr["krT2"], dst2=d2)
                    else:
                        finish_chunk(bank, bb, g, False, self.uT[ch - 18, :, gs], self.b_scr["uT"])
            fw.emit()

    def rmax(self, out, in_, reads, writes):
        nc = self.nc
        self.fw.op("dve", lambda: nc.vector.reduce_max(out=out, in_=in_, axis=AX.X), reads=reads, writes=writes)

    def rsum(self, out, in_, reads, writes):
        nc = self.nc
        self.fw.op("dve", lambda: nc.vector.reduce_sum(out=out, in_=in_, axis=AX.X), reads=reads, writes=writes)

    def recip(self, out, in_, reads, writes):
        nc = self.nc
        self.fw.op("dve", lambda: nc.vector.reciprocal(out=out, in_=in_), reads=reads, writes=writes)

    def phase_attnA(self, l, seqs=SEQS, qb_limit=None):
        nc, fw = self.nc, self.fw
        with contextlib.ExitStack() as es:
            kT2 = Tile(nc, es, "kT2", [128, 2, 2560])
            Vt = Tile(nc, es, "Vt", [128, 20, 128])
            qring = Ring(nc, es, "qc", [128, 2048], 2)
            maskA = Tile(nc, es, "maskA", [128, 384])
            Pring = Ring(nc, es, "Pa", [128, 896], 5)
            PTring = Ring(nc, es, "PTa", [128, 896], 2)
            slring = Ring(nc, es, "sl", [128, 384], 2)
            small = Ring(nc, es, "sma", [128, 8], 6)
            rdring = Ring(nc, es, "rda", [128, 2], 3)
            Oqring = Ring(nc, es, "Oqa", [128, 128], 2)
            ostg = Ring(nc, es, "ostga", [128, 512], 2)
            fw.dma("sp", maskA[:], self.I("maskA"), writes=maskA.b())
            for (tok0, n, ctx) in seqs:
                nqb = n // 128
                for g2 in range(2):
                    fw.dma("pool", kT2.r((slice(None), g2, slice(0, n))), self.kaT2[g2, :, tok0:tok0 + n],
                           writes=kT2.b())
                    if ctx:
                        fw.dma("pool", kT2.r((slice(None), g2, slice(n, n + 512))), self.I("ck2T")[l, g2],
                               writes=kT2.b())
                fw.dma("pool", Vt.r((slice(None), slice(0, nqb), slice(None))),
                       self.vaTok[tok0 // 128:tok0 // 128 + nqb].rearrange("t p d -> p t d"),
                       writes=Vt.b())
                if ctx:
                    fw.dma("pool", Vt.r((slice(None), slice(nqb, nqb + 4), slice(None))),
                           self.I("cv")[l].rearrange("(t p) d -> p t d", p=128), writes=Vt.b())
                for c in range(8):
                    g2 = c // 4
                    qc = qring.next()
                    fw.dma("pool", qc.r((slice(None), slice(0, n))), self.qaT[c, :, tok0:tok0 + n],
                           writes=qc.b())
                    qbs = list(range(nqb)) if qb_limit is None else list(range(min(nqb, qb_limit)))
                    stbox = [None]

                    def stageA(qb, c=c, g2=g2, qc=qc):
                        if ctx:
                            kb_lo, kb_hi = max(qb - 1, 0), min(qb + 1, nqb - 1)
                        else:
                            kb_lo, kb_hi = 0, nqb - 1
                        nl = (kb_hi - kb_lo + 1) * 128
                        blocks = list(range(kb_lo, kb_hi + 1)) + ([nqb + i for i in range(4)] if ctx else [])
                        rd = rdring.next()
                        pts = []
                        for hh in range(2):
                            h = 2 * c + hh
                            pb = hh * 64
                            sbank, sbb = self.ps[hh * 2], self.psb[hh * 2]
                            cbank, cbb = self.ps[hh * 2 + 1], self.psb[hh * 2 + 1]
                            lq = qc.r((slice(pb, pb + 64), slice(qb * 128, (qb + 1) * 128)))
                            self.mm(sbank[:, 0:nl], lq, kT2.r((slice(pb, pb + 64), g2, slice(kb_lo * 128, kb_lo * 128 + nl))),
                                    True, True, qc.b() + kT2.b(), [sbb])
                            if ctx:
                                self.mm(cbank[:, :], lq, kT2.r((slice(pb, pb + 64), g2, slice(n, n + 512))),
                                        True, True, qc.b() + kT2.b(), [cbb])
                            sm = small.next()
                            Pt = Pring.next()
                            if ctx:
                                mlo = 128 if qb == 0 else 0
                                self.tt(Pt[:, 0:nl], sbank[:, 0:nl], maskA[:, mlo:mlo + nl], ALU.add,
                                        [sbb] + maskA.b(), Pt.b())
                                self.rmax(sm[:, 0:1], Pt[:, 0:nl], Pt.b(), sm.b())
                                fw.op("dve", lambda cbank=cbank, Pt=Pt, sm=sm, nl=nl: nc.vector.tensor_scalar(
                                    out=Pt[:, nl:nl + 512], in0=cbank[:, :], scalar1=1.0, scalar2=None, op0=ALU.mult,
                                    op1=ALU.max, accum_out=sm[:, 1:2]), reads=[cbb], writes=Pt.b() + sm.b())
                                self.tt(sm[:, 2:3], sm[:, 0:1], sm[:, 1:2], ALU.max, sm.b(), sm.b())
                            else:
                                fw.op("dve", lambda sbank=sbank, Pt=Pt, sm=sm, nl=nl: nc.vector.tensor_scalar(
                                    out=Pt[:, 0:nl], in0=sbank[:, 0:nl], scalar1=1.0, scalar2=None, op0=ALU.mult,
                                    op1=ALU.max, accum_out=sm[:, 2:3]), reads=[sbb], writes=Pt.b() + sm.b())
                            scol = l * 16 + h
                            self.ts(sm[:, 3:4], sm[:, 2:3], 0.125, self.sinkb[:, scol:scol + 1], ALU.mult, ALU.max,
                                    sm.b() + self.sinkb.b(), sm.b())
                            self.ts(sm[:, 4:5], sm[:, 3:4], -1.0, None, ALU.mult, None, sm.b(), sm.b())
                            self.act(Pt[:, 0:nl], Pt[:, 0:nl], AF.Exp, Pt.b() + sm.b(), Pt.b() + sm.b(),
                                     bias=sm[:, 4:5], scale=0.125, accum_out=sm[:, 5:6])
                            if ctx:
                                self.act(Pt[:, nl:nl + 512], Pt[:, nl:nl + 512], AF.Exp, Pt.b() + sm.b(), Pt.b() + sm.b(),
                                         bias=sm[:, 4:5], scale=0.125, accum_out=sm[:, 6:7])
                            self.act(sm[:, 7:8], self.sinkb[:, scol:scol + 1], AF.Exp, self.sinkb.b() + sm.b(), sm.b(),
                                     bias=sm[:, 4:5], scale=1.0)
                            self.tt(sm[:, 5:6], sm[:, 5:6], sm[:, 7:8], ALU.add, sm.b(), sm.b())
                            if ctx:
                                self.tt(sm[:, 5:6], sm[:, 5:6], sm[:, 6:7], ALU.add, sm.b(), sm.b())
                            self.recip(rd[:, hh:hh + 1], sm[:, 5:6], sm.b(), rd.b())
                            pts.append(Pt)
                        return blocks, rd, pts

                    def stageB(qb, blocks, rd, pts, c=c, g2=g2):
                        nb = len(blocks)
                        obank, obb = self.ps[6], self.psb[6]
                        for hh in range(2):
                            Pt = pts[hh]
                            PT = PTring.next()
                            for i0 in range(0, nb, 4):
                                cnt = min(4, nb - i0)
                                tb, tbb = self.ps[4 + (i0 // 4) % 2], self.psb[4 + (i0 // 4) % 2]
                                for i in range(i0, i0 + cnt):
                                    self.tr(tb[:, (i - i0) * 128:(i - i0 + 1) * 128], Pt[:, i * 128:(i + 1) * 128],
                                            Pt.b(), [tbb])
                                self.copy(self.evac_eng(), PT.r((slice(None), slice(i0 * 128, (i0 + cnt) * 128))),
                                          tb[:, 0:cnt * 128], [tbb], PT.b())
                            for i, blk in enumerate(blocks):
                                self.mm(obank[:, hh * 64:(hh + 1) * 64], PT.r((slice(None), slice(i * 128, (i + 1) * 128))),
                                        Vt.r((slice(None), blk, slice(g2 * 64, (g2 + 1) * 64))), i == 0, i == nb - 1,
                                        PT.b() + Vt.b(), [obb], sync=True)
                        Oq = Oqring.next()
                        self.ts(Oq[:, 0:64], obank[:, 0:64], rd[:, 0:1], None, ALU.mult, None, [obb] + rd.b(), Oq.b())
                        self.ts(Oq[:, 64:128], obank[:, 64:128], rd[:, 1:2], None, ALU.mult, None, [obb] + rd.b(), Oq.b())
                        tb, tbb = self.ps[7], self.psb[7]
                        self.tr(tb[:, 0:128], Oq[:, :], Oq.b(), [tbb])
                        if qb % 4 == 0:
                            stbox[0] = ostg.next()
                        st = stbox[0]
                        self.copy(self.evac_eng(), st[:, (qb % 4) * 128:(qb % 4 + 1) * 128], tb[:, 0:128], [tbb], st.b())
                        if qb % 4 == 3 or qb == qbs[-1]:
                            q0 = (qb // 4) * 512
                            wd = (qb % 4 + 1) * 128
                            fw.dma("sp", self.oaT[c, :, tok0 + q0:tok0 + q0 + wd], st[:, 0:wd], reads=st.b())

                    nxt = stageA(qbs[0])
                    for ii, qb in enumerate(qbs):
                        cur = nxt
                        if ii + 1 < len(qbs):
                            nxt = stageA(qbs[ii + 1])
                        stageB(qb, *cur)
            fw.emit()

    def phase_mla(self, l, seqs=SEQS, qb_limit=None, heads=range(8)):
        nc, fw = self.nc, self.fw
        scale = float((128 + 64) ** -0.5)
        with contextlib.ExitStack() as es:
            ckvA = Tile(nc, es, "ckvA", [128, 2, 2560])
            krA = Tile(nc, es, "krA", [128, 2560])
            wukv = Tile(nc, es, "wukv", [128, 16, 256])
            knT = Tile(nc, es, "knT", [128, 2560])
            vh = Tile(nc, es, "vh", [128, 20, 128], dt=BF16)
            identb = Tile(nc, es, "identb", [128, 128], dt=BF16)
            Pbring = Ring(nc, es, "Pbm", [128, 2560], 3, dt=BF16)
            qnr = Ring(nc, es, "qnm", [128, 2048], 2)
            qrr = Ring(nc, es, "qrm", [128, 2048], 2)
            Pring = Ring(nc, es, "Pm", [128, 2560], 3)
            PTring = Ring(nc, es, "PTm", [128, 2560], 2, dt=BF16)
            small = Ring(nc, es, "smm", [128, 16], 4)
            Oqring = Ring(nc, es, "Oqm", [128, 128], 2)
            ostg = Ring(nc, es, "ostgm", [128, 512], 2)
            self.copy("dve", identb[:], self.ident[:], self.ident.b(), identb.b())
            wukv4 = wukv.t[:].rearrange("p a (k m) -> p a k m", k=2).bitcast(F32R)
            fw.dma("pool", wukv4, self.I("w_ukv")[l].rearrange("a p k m -> p a k m"), writes=wukv.b())
            for (tok0, n, ctx) in seqs:
                nqb = n // 128
                nk = n + (512 if ctx else 0)
                nkb = nk // 128
                kgs = [(s, min(512, nk - s)) for s in range(0, nk, 512)]
                ng = len(kgs)
                for k in range(2):
                    fw.dma("pool", ckvA.r((slice(None), k, slice(0, n))), self.ckvT[k, :, tok0:tok0 + n],
                           writes=ckvA.b())
                    if ctx:
                        fw.dma("pool", ckvA.r((slice(None), k, slice(n, n + 512))), self.I("cckvT")[l, k], writes=ckvA.b())
                fw.dma("pool", krA.r((slice(None), slice(0, n))), self.krT2[:, tok0:tok0 + n],
                       writes=krA.b())
                if ctx:
                    fw.dma("pool", krA.r((slice(None), slice(n, n + 512))), self.I("ckr2T")[l], writes=krA.b())
                qr, qr_pair = None, -1
                for h in heads:
                    pb = (h % 2) * 64
                    for gi, (s, w) in enumerate(kgs):
                        bank, bb = self.ps[gi % 4], self.psb[gi % 4]
                        for k in range(2):
                            self.mm(bank[:, 0:w], wukv4[:, h, k, :], ckvA.r((slice(None), k, slice(s, s + w))),
                                    k == 0, k == 1, wukv.b() + ckvA.b(), [bb])
                        self.copy(self.evac_eng(), knT.r((slice(None), slice(s, s + w))), bank[:, 0:w], [bb], knT.b())
                    for kb0 in range(0, nkb, 4):
                        cnt = min(4, nkb - kb0)
                        bank, bb = self.ps[4 + (kb0 // 4) % 2], self.psb[4 + (kb0 // 4) % 2]
                        for kb in range(kb0, kb0 + cnt):
                            for k in range(2):
                                self.mm(bank[:, (kb - kb0) * 128:(kb - kb0 + 1) * 128],
                                        ckvA.r((slice(None), k, slice(kb * 128, (kb + 1) * 128))), wukv4[:, 8 + h, k, :],
                                        k == 0, k == 1, wukv.b() + ckvA.b(), [bb], sync=(k == 1 and kb == kb0 + cnt - 1))
                        self.copy(self.evac_eng(), vh[:, kb0:kb0 + cnt, :],
                                  bank[:, 0:cnt * 128].rearrange("p (a d) -> p a d", a=cnt), [bb], vh.b())
                    qn = qnr.next()
                    fw.dma("pool", qn.r((slice(None), slice(0, n))), self.qnT[h, :, tok0:tok0 + n],
                           writes=qn.b())
                    if qr_pair != h // 2:
                        qr_pair = h // 2
                        qr = qrr.next()
                        fw.dma("pool", qr.r((slice(None), slice(0, n))), self.qrT[h // 2, :, tok0:tok0 + n],
                               writes=qr.b())
                    qbs = list(range(nqb)) if qb_limit is None else list(range(min(nqb, qb_limit)))
                    stbox = [None]

                    def stageA(qb, qn=qn, qr=qr, pb=pb):
                        qsl = slice(qb * 128, (qb + 1) * 128)
                        sm = small.next()
                        Pt = Pring.next()
                        for gi, (s, w) in enumerate(kgs):
                            bank, bb = self.ps[gi], self.psb[gi]
                            self.mm(bank[:, 0:w], qn.r((slice(None), qsl)), knT.r((slice(None), slice(s, s + w))),
                                    True, False, qn.b() + knT.b(), [bb], sync=False)
                            self.mm(bank[:, 0:w], qr.r((slice(pb, pb + 64), qsl)), krA.r((slice(pb, pb + 64), slice(s, s + w))),
                                    False, True, qr.b() + krA.b(), [bb], sync=True)
                            fw.op("dve", lambda bank=bank, w=w, s=s, gi=gi, Pt=Pt, sm=sm: nc.vector.tensor_scalar(
                                out=Pt[:, s:s + w], in0=bank[:, 0:w], scalar1=1.0, scalar2=None, op0=ALU.mult, op1=ALU.max,
                                accum_out=sm[:, gi:gi + 1]), reads=[bb], writes=Pt.b() + sm.b())
                        self.rmax(sm[:, 8:9], sm[:, 0:ng], sm.b(), sm.b())
                        self.ts(sm[:, 9:10], sm[:, 8:9], -scale, None, ALU.mult, None, sm.b(), sm.b())
                        Pb = Pbring.next()
                        for gi, (s, w) in enumerate(kgs):
                            self.act(Pb[:, s:s + w], Pt[:, s:s + w], AF.Exp, Pt.b() + sm.b(), Pb.b() + sm.b(),
                                     bias=sm[:, 9:10], scale=scale, accum_out=sm[:, 10 + gi:11 + gi])
                        self.rsum(sm[:, 15:16], sm[:, 10:10 + ng], sm.b(), sm.b())
                        self.recip(sm[:, 15:16], sm[:, 15:16], sm.b(), sm.b())
                        return sm, Pb

                    def stageB(qb, sm, Pt, h=h):
                        PT = PTring.next()
                        for kb0 in range(0, nkb, 4):
                            cnt = min(4, nkb - kb0)
                            tbb = self.psb[5 + (kb0 // 4) % 2]
                            tb = self.ps[5 + (kb0 // 4) % 2][:, :].bitcast(BF16)
                            for kb in range(kb0, kb0 + cnt):
                                fw.op("pe", lambda tb=tb, kb=kb, kb0=kb0, Pt=Pt: nc.tensor.transpose(
                                    out=tb[:, (kb - kb0) * 128:(kb - kb0 + 1) * 128], in_=Pt[:, kb * 128:(kb + 1) * 128],
                                    identity=identb[:]), reads=Pt.b() + identb.b(), writes=[tbb])
                            self.copy(self.evac_eng(), PT[:, kb0 * 128:(kb0 + cnt) * 128],
                                      tb[:, 0:cnt * 128], [tbb], PT.b())
                        obank, obb = self.ps[7], self.psb[7]
                        for kb in range(nkb):
                            self.mm(obank[:, 0:128], PT[:, kb * 128:(kb + 1) * 128],
                                    vh[:, kb, :], kb == 0, kb == nkb - 1, PT.b() + vh.b(), [obb])
                        Oq = Oqring.next()
                        self.ts(Oq[:, :], obank[:, 0:128], sm[:, 15:16], None, ALU.mult, None, [obb] + sm.b(), Oq.b())
                        tb, tbb = self.ps[5], self.psb[5]
                        self.tr(tb[:, 0:128], Oq[:, :], Oq.b(), [tbb])
                        if qb % 4 == 0:
                            stbox[0] = ostg.next()
                        st = stbox[0]
                        self.copy(self.evac_eng(), st[:, (qb % 4) * 128:(qb % 4 + 1) * 128], tb[:, 0:128], [tbb], st.b())
                        if qb % 4 == 3 or qb == qbs[-1]:
                            q0 = (qb // 4) * 512
                            wd = (qb % 4 + 1) * 128
                            fw.dma("sp", self.obT[h, :, tok0 + q0:tok0 + q0 + wd], st[:, 0:wd], reads=st.b())

                    nxt = stageA(qbs[0])
                    for ii, qb in enumerate(qbs):
                        cur = nxt
                        if ii + 1 < len(qbs):
                            nxt = stageA(qbs[ii + 1])
                        stageB(qb, *cur)
            fw.emit()

    def phase_pool(self, l, seqs=SEQS):
        nc, fw = self.nc, self.fw
        with contextlib.ExitStack() as es:
            invs = Tile(nc, es, "invs", [128, 4 * 2048])
            invp = Tile(nc, es, "invp", [128, 4 * 256])
            pw = Tile(nc, es, "pw", [128, 8, 256])
            upr = Ring(nc, es, "up", [128, 2064], 2)
            Ar = Ring(nc, es, "Apool", [128, 2064], 3)
            dT = [Tile(nc, es, "dT%d" % i, [128, 2048]) for i in range(2)]
            stg = Ring(nc, es, "pstg", [128, 512], 3)
            fw.dma("sp", invs[:], self.I("invc_s").rearrange("a n -> (a n)").partition_broadcast(128), writes=invs.b())
            fw.dma("sp", invp[:], self.I("invc_p").rearrange("a n -> (a n)").partition_broadcast(128), writes=invp.b())
            pw4 = pw.t[:].rearrange("p a (k m) -> p a k m", k=2).bitcast(F32R)
            fw.dma("pool", pw4, self.I("poolw")[l].rearrange("a p k m -> p a k m"), writes=pw.b())
            bi = 0
            for (tok0, n, ctx) in seqs:
                inv = invs if n == 2048 else invp
                for pg in range(4):
                    win = POOL_WINDOWS[pg]
                    left = win // 2
                    for half in range(2):
                        cc = pg * 2 + half
                        u = upr.next()
                        fw.op("dve", lambda u=u: nc.vector.memset(u[:, 0:8], 0.0), writes=u.b())
                        fw.op("dve", lambda u=u, n=n: nc.vector.memset(u[:, 8 + n:16 + n], 0.0), writes=u.b())
                        fw.dma("sp", u[:, 8:8 + n], self.uT[cc, :, tok0:tok0 + n], writes=u.b())
                        cur, L, step = u, n + 16, 1
                        while step < win:
                            nxt = Ar.next()
                            self.tt(nxt[:, 0:L - step], cur[:, 0:L - step], cur[:, step:L], ALU.add, cur.b(), nxt.b())
                            cur, L, step = nxt, L - step, step * 2
                        tmp = Ar.next()
                        self.tt(tmp[:, 0:n], cur[:, 8 - left:8 - left + n], inv[:, pg * n:(pg + 1) * n], ALU.mult,
                                cur.b() + inv.b(), tmp.b())
                        self.tt(dT[half].r((slice(None), slice(0, n))), tmp[:, 0:n], u[:, 8:8 + n], ALU.subtract,
                                tmp.b() + u.b(), dT[half].b())
                    for mh in range(2):
                        for tg in range(0, n, 512):
                            w = min(512, n - tg)
                            bi += 1
                            bank, bb = self.ps[bi % 4], self.psb[bi % 4]
                            for k in range(2):
                                self.mm(bank[:, 0:w], pw4[:, pg * 2 + mh, k, :], dT[k].r((slice(None), slice(tg, tg + w))),
                                        k == 0, k == 1, pw.b() + dT[k].b(), [bb])
                            st = stg.next()
                            col = R_PSC(l) + pg * 2 + mh
                            self.act(st[:, 0:w], bank[:, 0:w], AF.Copy, [bb] + self.vT.b(), st.b(),
                                     scale=self.vT[:, col:col + 1])
                            fw.dma("sp", self.ocT[pg * 2 + mh, :, tok0 + tg:tok0 + tg + w], st[:, 0:w], reads=st.b())
            fw.emit()

    def phase_merge(self, l, groups=None):
        nc, fw = self.nc, self.fw
        with contextlib.ExitStack() as es:
            xg = Tile(nc, es, "xg3", [128, 16 * 512])
            hT = Tile(nc, es, "hT3", [128, 16, 512], split=True)
            o3 = [Tile(nc, es, "o3_%d" % i, [128, 8, 512]) for i in range(3)]
            wring = Ring(nc, es, "w3", [128, 2048], 6)
            sgr = Ring(nc, es, "sg3", [128, 512], 2)
            tmr = Ring(nc, es, "tm3", [128, 512], 2)
            sqring = Ring(nc, es, "sq3", [128, 512], 2)
            tmpring = Ring(nc, es, "tmp3", [128, 512], 2)
            rstd = Tile(nc, es, "rstd3", [128, 512])
            xring = Ring(nc, es, "xr3", [128, 512], 3)
            self.epsb = Tile(nc, es, "epsb3", [128, 1])
            fw.op("dve", lambda: nc.vector.memset(self.epsb[:], EPS), writes=self.epsb.b())
            y3 = xg.t[:].rearrange("p (c n) -> p c n", c=16)
            srcs = [(self.oaT, "oaT"), (self.obT, "obT"), (self.ocT, "ocT")]
            mp = self.modp[l]
            it = 0
            for g in (groups or range(NGRP)):
                cd = 0 if g < 4 else 1
                gs = slice(g * G, (g + 1) * G)
                self.load_x(g, xg)
                self.norm_mod(g, l, 0, xg, hT, sqring, tmpring, rstd, self.ps[7], self.psb[7])
                for br in range(3):
                    fw.dma("pool", o3[br].r(slice(None)), srcs[br][0][:, :, gs].rearrange("c p n -> p c n"),
                           writes=o3[br].b())
                for d in range(16):
                    for br in range(3):
                        it += 1
                        w, slot = self.wload(wring, self.I("w_inG")[l, br * 16 + d], 16)
                        ga, gab = self.ps[it % 2], self.psb[it % 2]
                        for k in range(16):
                            self.mm(ga[:, :], w[:, k, :], hT.r((slice(None), k, slice(None))), k == 0, k == 15,
                                    slot.b() + hT.b(k), [gab])
                        w2, slot2 = self.wload(wring, self.I("wbr")[l, br, d], 8)
                        pr, prb = self.ps[2 + it % 2], self.psb[2 + it % 2]
                        for k in range(8):
                            self.mm(pr[:, :], w2[:, k, :], o3[br].r((slice(None), k, slice(None))), k == 0, k == 7,
                                    slot2.b() + o3[br].b(), [prb])
                        sg = sgr.next()
                        self.act(sg[:], ga[:, :], AF.Sigmoid, [gab], sg.b())
                        if br == 0:
                            self.tt(y3[:, d, :].bitcast(F32R), sg[:], pr[:, :], ALU.mult, sg.b() + [prb], xg.b())
                        else:
                            tm = tmr.next()
                            self.tt(tm[:], sg[:], pr[:, :], ALU.mult, sg.b() + [prb], tm.b())
                            out = y3[:, d, :].bitcast(F32R)
                            self.tt(out, y3[:, d, :], tm[:], ALU.add, xg.b() + tm.b(), xg.b())
                for d2 in range(16):
                    it += 1
                    w, slot = self.wload(wring, self.I("wout")[l, d2], 16)
                    bank, bb = self.ps[4 + it % 2], self.psb[4 + it % 2]
                    for k in range(16):
                        self.mm(bank[:, :], w[:, k, :], y3[:, k, :].bitcast(F32R), k == 0, k == 15, slot.b() + xg.b(), [bb])
                    xr = xring.next()
                    fw.dma("sp", xr[:], self.xT[d2, :, gs], reads=[self.b_x[g][d2]], writes=xr.b())
                    self.stt(xr[:], bank[:, :], mp[:, 2, d2:d2 + 1, cd], xr[:], ALU.mult, ALU.add,
                             [bb] + mp.b() + xr.b(), xr.b())
                    fw.dma("sp", self.xT[d2, :, gs], xr[:], reads=xr.b(), writes=[self.b_x[g][d2]])
            fw.emit()

    def phase_ffn(self, l, groups=None, final=False):
        nc, fw = self.nc, self.fw
        moe = (l % 2 == 1)
        nexp = self.moe_experts if moe else 1
        with contextlib.ExitStack() as es:
            xa = Tile(nc, es, "xa", [128, 16 * 512])
            hT = Tile(nc, es, "hT4", [128, 16, 512], split=True)
            aT = [Tile(nc, es, "aT%d" % i, [128, FB, 512], split=True) for i in range(2)]
            wgu = Ring(nc, es, "wgu", [128, 2048], 6)
            wdr = Ring(nc, es, "wdr", [128, FB * 128], 3)
            sglr = Ring(nc, es, "sgl", [128, 512], 2)
            t4r = Ring(nc, es, "t4", [128, 512], 2)
            sqring = Ring(nc, es, "sq4", [128, 512], 2)
            tmpring = Ring(nc, es, "tmp4", [128, 512], 2)
            rstd = Tile(nc, es, "rstd4", [128, 512])
            xring = Ring(nc, es, "xr4", [128, 512], 3)
            self.epsb = Tile(nc, es, "epsb4", [128, 1])
            fw.op("dve", lambda: nc.vector.memset(self.epsb[:], EPS), writes=self.epsb.b())
            if moe:
                rt = Tile(nc, es, "rt", [128, 16, 8])
                fw.dma("pool", rt.r(slice(None)), self.I("router"), writes=rt.b())
                gate = Tile(nc, es, "gate", [128, 4, 8])
                gsm = Ring(nc, es, "gsm", [128, 32], 2)
                gbr = Ring(nc, es, "gb", [128, 128], 2)
                gbcr = Ring(nc, es, "gbc", [128, 512], 2)
            a3 = xa.t[:].rearrange("p (c n) -> p c n", c=16)
            mp = self.modp[l]
            it = 0
            for g in (groups or range(NGRP)):
                cd = 0 if g < 4 else 1
                gs = slice(g * G, (g + 1) * G)
                self.load_x(g, xa)
                self.norm_mod(g, l, 1, xa, hT, sqring, tmpring, rstd, self.ps[7], self.psb[7])
                if moe:
                    for t in range(4):
                        lb, lbb = self.ps[6], self.psb[6]
                        for k in range(16):
                            self.mm(lb[:, t * 8:(t + 1) * 8], hT.r((slice(None), k, slice(t * 128, (t + 1) * 128))),
                                    rt.r((slice(None), k, slice(None))), k == 0, k == 15, hT.b(k) + rt.b(), [lbb])
                        sm = gsm.next()
                        lg = sm[:, 0:8]
                        self.copy("dve", lg, lb[:, t * 8:(t + 1) * 8], [lbb], sm.b())
                        self.rmax(sm[:, 24:25], lg, sm.b(), sm.b())
                        self.ts(sm[:, 8:16], lg, sm[:, 24:25], None, ALU.is_equal, None, sm.b(), sm.b())
                        self.stt(sm[:, 8:16], sm[:, 8:16], -1e30, lg, ALU.mult, ALU.add, sm.b(), sm.b())
                        self.rmax(sm[:, 25:26], sm[:, 8:16], sm.b(), sm.b())
                        self.ts(sm[:, 8:16], lg, sm[:, 25:26], None, ALU.is_ge, None, sm.b(), sm.b())
                        self.ts(sm[:, 26:27], sm[:, 24:25], -1.0, None, ALU.mult, None, sm.b(), sm.b())
                        self.act(sm[:, 16:24], lg, AF.Exp, sm.b(), sm.b(), bias=sm[:, 26:27], scale=1.0)
                        self.tt(sm[:, 16:24], sm[:, 16:24], sm[:, 8:16], ALU.mult, sm.b(), sm.b())
                        self.rsum(sm[:, 27:28], sm[:, 16:24], sm.b(), sm.b())
                        self.recip(sm[:, 27:28], sm[:, 27:28], sm.b(), sm.b())
                        self.ts(gate[:, t, :], sm[:, 16:24], sm[:, 27:28], None, ALU.mult, None, sm.b(), gate.b())
                first = True
                for e in range(nexp):
                    if moe:
                        gbank, gbb = self.ps[6], self.psb[6]
                        for t in range(4):
                            gb = gbr.next()
                            self.copy("dve", gb.r(slice(None)), gate[:, t, e:e + 1].to_broadcast([128, 128]), gate.b(), gb.b())
                            self.mm(gbank[:, t * 128:(t + 1) * 128], gb.r(slice(None)), self.identr.r(slice(None)), True, True,
                                    gb.b() + self.identr.b(), [gbb], sync=True)
                        gbc = gbcr.next()
                        self.copy("act", gbc[:], gbank[:, :], [gbb], gbc.b())
                        wg_d, wu_d, wd_d = self.I("moe_g")[e], self.I("moe_u")[e], self.I("moe_d")[e]
                    else:
                        wg_d, wu_d, wd_d = self.I("ffn_g"), self.I("ffn_u"), self.I("ffn_d")
                    for blk in range(NFF // FB):
                        at = aT[blk % 2]
                        for jj in range(FB):
                            j = blk * FB + jj
                            it += 1
                            wg, sg_ = self.wload(wgu, wg_d[j], 16)
                            gbk, gbkb = self.ps[it % 2], self.psb[it % 2]
                            for k in range(16):
                                self.mm(gbk[:, :], wg[:, k, :], hT.r((slice(None), k, slice(None))), k == 0, k == 15,
                                        sg_.b() + hT.b(k), [gbkb])
                            wu, su_ = self.wload(wgu, wu_d[j], 16)
                            ubk, ubkb = self.ps[2 + it % 2], self.psb[2 + it % 2]
                            for k in range(16):
                                self.mm(ubk[:, :], wu[:, k, :], hT.r((slice(None), k, slice(None))), k == 0, k == 15,
                                        su_.b() + hT.b(k), [ubkb])
                            sgl = sglr.next()
                            self.act(sgl[:], gbk[:, :], AF.Silu, [gbkb], sgl.b())
                            if moe:
                                t4 = t4r.next()
                                self.tt(t4[:], ubk[:, :], gbc[:], ALU.mult, [ubkb] + gbc.b(), t4.b())
                                self.tt(at.r((slice(None), jj, slice(None))), sgl[:], t4[:], ALU.mult, sgl.b() + t4.b(), at.b(jj))
                            else:
                                self.tt(at.r((slice(None), jj, slice(None))), sgl[:], ubk[:, :], ALU.mult, sgl.b() + [ubkb], at.b(jj))
                        for d in range(16):
                            it += 1
                            wd, sd_ = self.wload(wdr, wd_d[blk, d], FB)
                            dbk, dbkb = self.ps[4 + it % 2], self.psb[4 + it % 2]
                            for jj in range(FB):
                                self.mm(dbk[:, :], wd[:, jj, :], at.r((slice(None), jj, slice(None))), jj == 0, jj == FB - 1,
                                        sd_.b() + at.b(jj), [dbkb])
                            if first:
                                self.copy("dve", a3[:, d, :], dbk[:, :], [dbkb], xa.b())
                            else:
                                self.tt(a3[:, d, :], a3[:, d, :], dbk[:, :], ALU.add, xa.b() + [dbkb], xa.b())
                        first = False
                for d2 in range(16):
                    xr = xring.next()
                    fw.dma("sp", xr[:], self.xT[d2, :, gs], reads=[self.b_x[g][d2]], writes=xr.b())
                    self.stt(a3[:, d2, :], a3[:, d2, :], mp[:, 5, d2:d2 + 1, cd], xr[:], ALU.mult, ALU.add,
                             xa.b() + mp.b() + xr.b(), xa.b())
                    if not final:
                        fw.dma("sp", self.xT[d2, :, gs], a3[:, d2, :], reads=xa.b(), writes=[self.b_x[g][d2]])
                if final:
                    self.norm_stats(a3, xa.b(), 16, D, sqring, rstd, self.ps[7], self.psb[7])
                    for d2 in range(16):
                        xr = xring.next()
                        col = R_FG + d2
                        self.stt(xr[:], a3[:, d2, :], self.vT[:, col:col + 1], rstd[:], ALU.mult, ALU.mult,
                                 xa.b() + self.vT.b() + rstd.b(), xr.b())
                        fw.dma("sp", self.o_yT[d2, :, gs], xr[:], reads=xr.b())
            fw.emit()

    def phase_moe_sparse(self, l, final=False, slots=None, groups=None):
        nc, fw = self.nc, self.fw
        I32 = mybir.dt.int32
        mp = self.modp[l]
        NTI = NT // 128
        hTok, Ybuf, Tab = self.hTok, self.Ybuf, self.Tab
        b_hTok, b_Y, b_Tab = Buf(), Buf(), Buf()
        B3 = [128, NTI, 8]

        def tred(out, in_, op, reads, writes):
            fw.op("dve", lambda: nc.vector.tensor_reduce(out=out, in_=in_, axis=AX.X, op=op), reads=reads, writes=writes)

        with contextlib.ExitStack() as es0:
            esc = Tile(nc, es0, "esc", [128, 2 * NS])
            self.epsb = Tile(nc, es0, "epsb5", [128, 1])
            fw.op("dve", lambda: nc.vector.memset(self.epsb[:], EPS), writes=self.epsb.b())
            with contextlib.ExitStack() as es:
                xa = Tile(nc, es, "xa5", [128, 16 * 512])
                hT = Tile(nc, es, "hT5", [128, 16, 512], split=True)
                sqring = Ring(nc, es, "sq5", [128, 512], 2)
                tmpring = Ring(nc, es, "tmp5", [128, 512], 2)
                rstd = Tile(nc, es, "rstd5", [128, 512])
                hst = Ring(nc, es, "hst5", [128, 2048], 2)
                rt = Tile(nc, es, "rt5", [128, 16, 8])
                Um = Tile(nc, es, "Um", [128, 128])
                Lg = Tile(nc, es, "Lg", B3)
                eq1 = Tile(nc, es, "eq1", B3)
                sel = Tile(nc, es, "sel", B3)
                wk = Tile(nc, es, "wk", B3)
                wk2 = Tile(nc, es, "wk2", B3)
                gt = Tile(nc, es, "gt", B3)
                pos = Tile(nc, es, "pos", B3)
                tot = Tile(nc, es, "tot", B3)
                offs = Tile(nc, es, "offs", B3)
                m1 = Tile(nc, es, "m1", [128, NTI, 1])
                m2 = Tile(nc, es, "m2", [128, NTI, 1])
                sm = Tile(nc, es, "sm5", [128, 96])
                tokid = Tile(nc, es, "tokid", [128, NTI])
                svals = Tile(nc, es, "svals", [128, NS])
                rr = Tile(nc, es, "rr", [128, 2 * NTI])
                zero = Tile(nc, es, "zero5", [128, 2048])
                recs_t = es.enter_context(nc.sbuf_tensor("sb_recs", [128, 2 * NTI, 16], I32))
                ridx_t = es.enter_context(nc.sbuf_tensor("sb_ridx", [128, 2 * NTI], I32))
                b_recs, b_ridx = Buf(), Buf()
                fw.dma("pool", rt.r(slice(None)), self.I("router"), writes=rt.b())
                fw.dma("pool", Um.r(slice(None)), self.I("Umat"), writes=Um.b())
                fw.dma("sp", tokid[:], self.I("tokid"), writes=tokid.b())
                fw.dma("sp", svals[:], self.I("svals"), writes=svals.b())
                fw.dma("sp", Tab[:, :], self.I("Tab0", I32), writes=[b_Tab])
                fw.op("dve", lambda: nc.vector.memset(zero[:], 0.0), writes=zero.b())
                fw.op("dve", lambda: nc.vector.memset(recs_t[:], 0), writes=[b_recs])
                fw.dma("sp", hTok[NT:NT + 128, :], zero[:], reads=zero.b(), writes=[b_hTok])
                for g in range(NGRP):
                    self.load_x(g, xa)
                    self.norm_mod(g, l, 1, xa, hT, sqring, tmpring, rstd, self.ps[7], self.psb[7])
                    for t in range(4):
                        lb, lbb = self.ps[6], self.psb[6]
                        for k in range(16):
                            self.mm(lb[:, t * 8:(t + 1) * 8], hT.r((slice(None), k, slice(t * 128, (t + 1) * 128))),
                                    rt.r((slice(None), k, slice(None))), k == 0, k == 15, hT.b(k) + rt.b(), [lbb])
                        self.copy("dve", Lg[:, g * 4 + t, :], lb[:, t * 8:(t + 1) * 8], [lbb], Lg.b())
                        hs = hst.next()
                        for c4 in range(4):
                            tb, tbb = self.ps[c4], self.psb[c4]
                            for ci in range(4):
                                c = c4 * 4 + ci
                                self.tr(tb[:, ci * 128:(ci + 1) * 128], hT[:, c, t * 128:(t + 1) * 128], hT.b(c), [tbb])
                            self.copy(self.evac_eng(), hs[:, c4 * 512:(c4 + 1) * 512], tb[:, :], [tbb], hs.b())
                        r0 = g * 512 + t * 128
                        fw.dma("sp", hTok[r0:r0 + 128, :], hs[:], reads=hs.b(), writes=[b_hTok])
                tred(m1[:], Lg[:], ALU.max, Lg.b(), m1.b())
                self.tt(eq1[:], Lg[:], m1[:].to_broadcast(B3), ALU.is_equal, Lg.b() + m1.b(), eq1.b())
                self.stt(wk[:], eq1[:], -1e30, Lg[:], ALU.mult, ALU.add, eq1.b() + Lg.b(), wk.b())
                tred(m2[:], wk[:], ALU.max, wk.b(), m2.b())
                self.tt(sel.r(slice(None)), Lg[:], m2[:].to_broadcast(B3), ALU.is_ge, Lg.b() + m2.b(), sel.b())
                self.tt(wk[:], Lg[:], m1[:].to_broadcast(B3), ALU.subtract, Lg.b() + m1.b(), wk.b())
                self.act(wk[:], wk[:], AF.Exp, wk.b(), wk.b())
                self.tt(wk[:], wk[:], sel[:], ALU.mult, wk.b() + sel.b(), wk.b())
                tred(m2[:], wk[:], ALU.add, wk.b(), m2.b())
                self.recip(m2[:], m2[:], m2.b(), m2.b())
                self.tt(gt[:], wk[:], m2[:].to_broadcast(B3), ALU.mult, wk.b() + m2.b(), gt.b())
                pb_, pbb_ = self.ps[0], self.psb[0]
                tb_, tbb_ = self.ps[1], self.psb[1]
                for t in range(NTI):
                    self.mm(pb_[:, t * 8:(t + 1) * 8], Um.r(slice(None)), sel.r((slice(None), t, slice(None))), True, True,
                            Um.b() + sel.b(), [pbb_], sync=(t == NTI - 1))
                for t in range(NTI):
                    self.mm(tb_[:, t * 8:(t + 1) * 8], self.ones.r(slice(None)), sel.r((slice(None), t, slice(None))), True, True,
                            self.ones.b() + sel.b(), [tbb_], sync=(t == NTI - 1))
                self.copy("dve", pos.t[:].rearrange("p a e -> p (a e)"), pb_[:, 0:NTI * 8], [pbb_], pos.b())
                self.copy("act", tot.t[:].rearrange("p a e -> p (a e)"), tb_[:, 0:NTI * 8], [tbb_], tot.b())
                fw.op("dve", lambda: nc.vector.memset(offs[:, 0, :], 0.0), writes=offs.b())
                for t in range(1, NTI):
                    self.tt(offs[:, t, :], offs[:, t - 1, :], tot[:, t - 1, :], ALU.add, offs.b() + tot.b(), offs.b())
                self.tt(pos[:], pos[:], offs[:], ALU.add, pos.b() + offs.b(), pos.b())
                cnt, nsl, tmp8 = sm[:, 0:8], sm[:, 8:16], sm[:, 24:32]
                self.tt(cnt, offs[:, NTI - 1, :], tot[:, NTI - 1, :], ALU.add, offs.b() + tot.b(), sm.b())
                self.ts(nsl, cnt, 0.0, None, ALU.is_gt, None, sm.b(), sm.b())
                for kk in range(1, 5):
                    self.ts(tmp8, cnt, float(512 * kk), None, ALU.is_gt, None, sm.b(), sm.b())
                    self.tt(nsl, nsl, tmp8, ALU.add, sm.b(), sm.b())
                fw.op("dve", lambda: nc.vector.memset(sm[:, 16:17], 0.0), writes=sm.b())
                for e in range(1, NEXP):
                    self.tt(sm[:, 16 + e:17 + e], sm[:, 15 + e:16 + e], sm[:, 7 + e:8 + e], ALU.add, sm.b(), sm.b())
                self.ts(sm[:, 32:40], sm[:, 16:24], 512.0, None, ALU.mult, None, sm.b(), sm.b())
                self.tt(pos[:], pos[:], sm[:, 32:40].rearrange("p (a e) -> p a e", a=1).to_broadcast(B3), ALU.add,
                        pos.b() + sm.b(), pos.b())
                self.tt(wk2[:], sel[:], eq1[:], ALU.subtract, sel.b() + eq1.b(), wk2.b())
                rv = rr.t[:].rearrange("p (k a) -> p k a", k=2)
                recs_f = recs_t[:].bitcast(F32)
                for k_, oh in ((0, eq1), (1, wk2)):
                    ks = slice(k_ * NTI, (k_ + 1) * NTI)
                    self.tt(wk[:], pos[:], oh[:], ALU.mult, pos.b() + oh.b(), wk.b())
                    tred(rv[:, k_, :], wk[:], ALU.add, wk.b(), rr.b())
                    fw.op("dve", lambda ks=ks: nc.vector.tensor_copy(out=recs_t[:, ks, 0], in_=tokid[:]),
                          reads=tokid.b(), writes=[b_recs])
                    fw.op("dve", lambda ks=ks, k_=k_: nc.vector.tensor_scalar(out=recs_t[:, ks, 1], in0=tokid[:],
                                                                             scalar1=float(k_ * NT), scalar2=None, op0=ALU.add),
                          reads=tokid.b(), writes=[b_recs])
                    self.tt(wk[:], gt[:], oh[:], ALU.mult, gt.b() + oh.b(), wk.b())
                    tred(recs_f[:, ks, 2], wk[:], ALU.add, wk.b(), [b_recs])
                fw.op("dve", lambda: nc.vector.tensor_copy(out=ridx_t[:], in_=rr[:]), reads=rr.b(), writes=[b_ridx])
                for i in range(2 * NTI):
                    fw.dma_custom("pool", lambda i=i: nc.gpsimd.indirect_dma_start(
                        out=Tab[:, :], out_offset=bass.IndirectOffsetOnAxis(ap=ridx_t[:, i:i + 1], axis=0),
                        in_=recs_t[:, i, :], in_offset=None), reads=[b_recs, b_ridx], writes=[b_Tab])
                ev = sm[:, 40:40 + NS]
                tmpS = sm[:, 60:60 + NS]
                fw.op("dve", lambda: nc.vector.memset(ev, -1.0), writes=sm.b())
                for e in range(NEXP):
                    self.ts(tmpS, svals[:], sm[:, 16 + e:17 + e], None, ALU.is_ge, None, svals.b() + sm.b(), sm.b())
                    self.tt(ev, ev, tmpS, ALU.add, sm.b(), sm.b())
                self.ts(esc[:, 0:NS], ev, float(NFF * 128), None, ALU.mult, None, sm.b(), esc.b())
                self.ts(esc[:, NS:2 * NS], ev, float(64 * 128), None, ALU.mult, None, sm.b(), esc.b())
                fw.emit()
            with contextlib.ExitStack() as es:
                hTs = Tile(nc, es, "hTs", [128, 16, 512], split=True)
                acc = Tile(nc, es, "acc5", [128, 16 * 512])
                aT = [Tile(nc, es, "aT5_%d" % i, [128, FB, 512], split=True) for i in range(2)]
                wgu = Ring(nc, es, "wgu5", [128, 2048], 4)
                wdr = Ring(nc, es, "wdr5", [128, FB * 128], 3)
                sglr = Ring(nc, es, "sgl5", [128, 512], 2)
                gth = Ring(nc, es, "gth", [128, 2048], 2)
                otl = Ring(nc, es, "otl", [128, 2048], 2)
                wbase = Tile(nc, es, "wbase", [128, NFF])
                wdbase = Tile(nc, es, "wdbase", [128, 64])
                widx_t = [es.enter_context(nc.sbuf_tensor("sb_widx%d" % i, [128, NFF + 64], I32)) for i in range(2)]
                b_widx = [Buf(), Buf()]
                rbs_t = [es.enter_context(nc.sbuf_tensor("sb_rbs%d" % i, [128, 4, 16], I32)) for i in range(2)]
                b_rbs = [Buf(), Buf()]
                fw.dma("sp", wbase[:], self.I("wbase"), writes=wbase.b())
                fw.dma("sp", wdbase[:], self.I("wdbase"), writes=wdbase.b())
                a3 = acc.t[:].rearrange("p (c n) -> p c n", c=16)
                wg_rows = self.I("moe_g").rearrange("e j p k m -> (e j p) (k m)")
                wu_rows = self.I("moe_u").rearrange("e j p k m -> (e j p) (k m)")
                wd_rows = self.I("moe_d").rearrange("e b d p j m -> (e b d p) (j m)")
                it = 0
                for s in (slots if slots is not None else range(NS)):
                    widx, bw = widx_t[s % 2], b_widx[s % 2]
                    rbs, brb = rbs_t[s % 2], b_rbs[s % 2]
                    fw.op("dve", lambda widx=widx, s=s: nc.vector.tensor_scalar(
                        out=widx[:, 0:NFF], in0=wbase[:], scalar1=esc[:, s:s + 1], scalar2=None, op0=ALU.add),
                        reads=wbase.b() + esc.b(), writes=[bw])
                    fw.op("dve", lambda widx=widx, s=s: nc.vector.tensor_scalar(
                        out=widx[:, NFF:NFF + 64], in0=wdbase[:], scalar1=esc[:, NS + s:NS + s + 1], scalar2=None, op0=ALU.add),
                        reads=wdbase.b() + esc.b(), writes=[bw])
                    fw.dma("sp", rbs[:], Tab[s * 512:(s + 1) * 512, :].rearrange("(q p) c -> p q c", p=128),
                           reads=[b_Tab], writes=[brb])
                    for q in range(4):
                        gtile = gth.next()
                        fw.dma_custom("pool", lambda gtile=gtile, rbs=rbs, q=q: nc.gpsimd.indirect_dma_start(
                            out=gtile[:], out_offset=None, in_=hTok[:, :],
                            in_offset=bass.IndirectOffsetOnAxis(ap=rbs[:, q, 0:1], axis=0)),
                            reads=[brb, b_hTok], writes=gtile.b())
                        for c4 in range(4):
                            tb, tbb = self.ps[6 + c4 % 2], self.psb[6 + c4 % 2]
                            for ci in range(4):
                                c = c4 * 4 + ci
                                self.tr(tb[:, ci * 128:(ci + 1) * 128], gtile[:, c * 128:(c + 1) * 128], gtile.b(), [tbb])
                            self.copy(self.evac_eng(), hTs.r((slice(None), slice(c4 * 4, c4 * 4 + 4), slice(q * 128, (q + 1) * 128))),
                                      tb[:, :].rearrange("p (a n) -> p a n", a=4), [tbb],
                                      hTs.b(c4 * 4) + hTs.b(c4 * 4 + 1) + hTs.b(c4 * 4 + 2) + hTs.b(c4 * 4 + 3))

                    def wgather(ring, rows, col, nelem, widx=widx, bw=bw):
                        slot = ring.next()
                        dst = slot.t[:, 0:nelem].bitcast(F32R)
                        fw.dma_custom("pool", lambda: nc.gpsimd.indirect_dma_start(
                            out=dst, out_offset=None, in_=rows,
                            in_offset=bass.IndirectOffsetOnAxis(ap=widx[:, col:col + 1], axis=0)),
                            reads=[bw], writes=slot.b())
                        return slot
                    for blk in range(NFF // FB):
                        at = aT[blk % 2]
                        for jj in range(FB):
                            j = blk * FB + jj
                            it += 1
                            sg_ = wgather(wgu, wg_rows, j, 2048)
                            wg = sg_.t[:].rearrange("p (k m) -> p k m", k=16).bitcast(F32R)
                            gbk, gbkb = self.ps[it % 2], self.psb[it % 2]
                            for k in range(16):
                                self.mm(gbk[:, :], wg[:, k, :], hTs.r((slice(None), k, slice(None))), k == 0, k == 15,
                                        sg_.b() + hTs.b(k), [gbkb])
                            su_ = wgather(wgu, wu_rows, j, 2048)
                            wu = su_.t[:].rearrange("p (k m) -> p k m", k=16).bitcast(F32R)
                            ubk, ubkb = self.ps[2 + it % 2], self.psb[2 + it % 2]
                            for k in range(16):
                                self.mm(ubk[:, :], wu[:, k, :], hTs.r((slice(None), k, slice(None))), k == 0, k == 15,
                                        su_.b() + hTs.b(k), [ubkb])
                            sgl = sglr.next()
                            self.act(sgl[:], gbk[:, :], AF.Silu, [gbkb], sgl.b())
                            self.tt(at.r((slice(None), jj, slice(None))), sgl[:], ubk[:, :], ALU.mult, sgl.b() + [ubkb], at.b(jj))
                        for d in range(16):
                            it += 1
                            sd_ = wgather(wdr, wd_rows, NFF + blk * 16 + d, FB * 128)
                            wd = sd_.t[:].rearrange("p (k m) -> p k m", k=FB).bitcast(F32R)
                            dbk, dbkb = self.ps[4 + it % 2], self.psb[4 + it % 2]
                            for jj in range(FB):
                                self.mm(dbk[:, :], wd[:, jj, :], at.r((slice(None), jj, slice(None))), jj == 0, jj == FB - 1,
                                        sd_.b() + at.b(jj), [dbkb])
                            if blk == 0:
                                self.copy("dve", a3[:, d, :], dbk[:, :], [dbkb], acc.b())
                            else:
                                self.tt(a3[:, d, :], a3[:, d, :], dbk[:, :], ALU.add, acc.b() + [dbkb], acc.b())
                    rbs_f = rbs[:].bitcast(F32)
                    for q in range(4):
                        ot = otl.next()
                        for c4 in range(4):
                            tb, tbb = self.ps[6 + c4 % 2], self.psb[6 + c4 % 2]
                            for ci in range(4):
                                c = c4 * 4 + ci
                                self.tr(tb[:, ci * 128:(ci + 1) * 128], a3[:, c, q * 128:(q + 1) * 128], acc.b(), [tbb])
                            self.ts(ot[:, c4 * 512:(c4 + 1) * 512], tb[:, :], rbs_f[:, q, 2:3], None, ALU.mult, None,
                                    [tbb, brb], ot.b())
                        fw.dma_custom("pool", lambda ot=ot, rbs=rbs, q=q: nc.gpsimd.indirect_dma_start(
                            out=Ybuf[:, :], out_offset=bass.IndirectOffsetOnAxis(ap=rbs[:, q, 1:2], axis=0),
                            in_=ot[:], in_offset=None), reads=ot.b() + [brb], writes=[b_Y])
                fw.emit()
            with contextlib.ExitStack() as es:
                ys = Ring(nc, es, "ys", [128, 2048], 4)
                y2 = Ring(nc, es, "y2", [128, 2048], 2)
                xg = Tile(nc, es, "xg6", [128, 16 * 512])
                sqring = Ring(nc, es, "sq6", [128, 512], 2)
                rstd = Tile(nc, es, "rstd6", [128, 512])
                xring = Ring(nc, es, "xr6", [128, 512], 3)
                x3 = xg.t[:].rearrange("p (c n) -> p c n", c=16)
                for g in (groups or range(NGRP)):
                    cd = 0 if g < 4 else 1
                    gs = slice(g * G, (g + 1) * G)
                    self.load_x(g, xg)
                    yt = []
                    for t in range(4):
                        r0 = g * 512 + t * 128
                        a, b2 = ys.next(), y2.next()
                        fw.dma("sp", a[:], Ybuf[r0:r0 + 128, :], reads=[b_Y], writes=a.b())
                        fw.dma("sp", b2[:], Ybuf[NT + r0:NT + r0 + 128, :], reads=[b_Y], writes=b2.b())
                        self.tt(a[:], a[:], b2[:], ALU.add, a.b() + b2.b(), a.b(), en="pool" if t % 2 else "dve")
                        yt.append(a)
                    for c in range(16):
                        tb, tbb = self.ps[c % 4], self.psb[c % 4]
                        for t in range(4):
                            self.tr(tb[:, t * 128:(t + 1) * 128], yt[t][:, c * 128:(c + 1) * 128], yt[t].b(), [tbb])
                        self.stt(x3[:, c, :], tb[:, :], mp[:, 5, c:c + 1, cd], x3[:, c, :], ALU.mult, ALU.add,
                                 [tbb] + mp.b() + xg.b(), xg.b())
                        if not final:
                            fw.dma("sp", self.xT[c, :, gs], x3[:, c, :], reads=xg.b(), writes=[self.b_x[g][c]])
                    if final:
                        self.norm_stats(x3, xg.b(), 16, D, sqring, rstd, self.ps[7], self.psb[7])
                        for d2 in range(16):
                            xr = xring.next()
                            col = R_FG + d2
                            self.stt(xr[:], x3[:, d2, :], self.vT[:, col:col + 1], rstd[:], ALU.mult, ALU.mult,
                                     xg.b() + self.vT.b() + rstd.b(), xr.b())
                            fw.dma("sp", self.o_yT[d2, :, gs], xr[:], reads=xr.b())
                fw.emit()


def _tiles(W):
    K, M = W.shape
    return np.ascontiguousarray(W.reshape(K // 128, 128, M // 128, 128).transpose(2, 1, 0, 3))


def host_consts():
    c = {}
    c["ident"] = np.eye(128, dtype=np.float32)
    p = np.arange(128)
    d = p % 64
    partner = np.where((d % 32) < 16, p + 16, p - 16)
    pm = np.zeros((128, 128), np.float32)
    pm[partner, p] = 1.0
    c["permM"] = pm
    quarter = 16
    inv = (np.float32(10000.0) ** (-np.arange(quarter, dtype=np.float32) / np.float32(quarter))).astype(np.float32)
    n = np.arange(2048)
    rr = (n // 64).astype(np.float32)
    cc = (n % 64).astype(np.float32)
    ang_r = (rr[:, None] * inv[None, :]).astype(np.float32)
    ang_c = (cc[:, None] * inv[None, :]).astype(np.float32)
    C = np.zeros((128, 2048), np.float32)
    S = np.zeros((128, 2048), np.float32)
    for pp in range(128):
        dd = pp % 64
        j = dd % 16
        ang = ang_r[:, j] if dd < 32 else ang_c[:, j]
        C[pp] = np.cos(ang)
        sgn = -1.0 if (dd % 32) < 16 else 1.0
        S[pp] = sgn * np.sin(ang)
    c["ropeC"] = C
    c["ropeS"] = S
    r = np.arange(128)[:, None]
    cidx = np.arange(128)[None, :]
    m = np.zeros((128, 384), np.float32)
    m[:, 0:128] = np.where(cidx >= r, 0.0, -1e30)
    m[:, 256:384] = np.where(cidx <= r, 0.0, -1e30)
    c["maskA"] = m
    c["Umat"] = np.triu(np.ones((128, 128), np.float32), k=1)
    pp = np.arange(128, dtype=np.float32)[:, None]
    c["tokid"] = (np.arange(NT // 128, dtype=np.float32)[None, :] * 128 + pp).astype(np.float32)
    c["svals"] = np.tile(np.arange(NS, dtype=np.float32)[None, :], (128, 1))
    c["wbase"] = (np.arange(NFF, dtype=np.float32)[None, :] * 128 + pp).astype(np.float32)
    c["wdbase"] = (np.arange(64, dtype=np.float32)[None, :] * 128 + pp).astype(np.float32)
    tab0 = np.zeros((NS * 512, 16), np.int32)
    tab0[:, 0] = NT
    tab0[:, 1] = 2 * NT + (np.arange(NS * 512) % 128)
    c["Tab0"] = tab0
    for nm, nseq in (("invc_s", 2048), ("invc_p", 256)):
        t = np.arange(nseq)
        tab = np.zeros((4, nseq), np.float32)
        for gi, win in enumerate(POOL_WINDOWS):
            left = win // 2
            right = win - left - 1
            lo = np.maximum(t - left, 0)
            hi = np.minimum(t + right, nseq - 1) + 1
            tab[gi] = (1.0 / (hi - lo).astype(np.float32)).astype(np.float32)
        c[nm] = tab
    return c


def prep_shared(inp):
    sh = dict(host_consts())
    w_in = inp["w_in"]
    colsA = np.concatenate([
        np.arange(0, 1024),
        np.arange(1024, 1088), np.arange(1024, 1088),
        np.arange(1088, 1152), np.arange(1088, 1152),
        np.arange(1152, 1280),
        np.arange(1280, 1792),
        np.arange(1792, 2048),
        np.arange(2048, 2112), np.arange(2048, 2112),
        np.arange(2112, 3136)])
    sh["w_ada"] = np.stack([_tiles(inp["w_ada"][l]) for l in range(DEPTH)])
    sh["w_inA"] = np.stack([_tiles(w_in[l][:, colsA]) for l in range(DEPTH)])
    sh["w_inG"] = np.stack([_tiles(w_in[l][:, 3136:]) for l in range(DEPTH)])
    cq = [np.arange(h * 192, h * 192 + 128) for h in range(8)]
    for j in range(4):
        cq.append(np.concatenate([np.arange((2 * j) * 192 + 128, (2 * j) * 192 + 192),
                                  np.arange((2 * j + 1) * 192 + 128, (2 * j + 1) * 192 + 192)]))
    cq = np.concatenate(cq)
    sh["w_uq"] = np.stack([_tiles(inp["w_uq"][l][:, cq]) for l in range(DEPTH)])
    ckv = np.concatenate([np.arange(h * 256, h * 256 + 128) for h in range(8)] +
                         [np.arange(h * 256 + 128, h * 256 + 256) for h in range(8)])
    sh["w_ukv"] = np.stack([_tiles(inp["w_ukv"][l][:, ckv]) for l in range(DEPTH)])
    sh["poolw"] = np.stack([np.concatenate([_tiles(inp["pool_w"][l][g]) for g in range(4)]) for l in range(DEPTH)])
    sh["wbr"] = np.stack([np.stack([_tiles(inp[k][l]) for k in ("w_branch_a", "w_branch_b", "w_branch_c")])
                          for l in range(DEPTH)])
    sh["wout"] = np.stack([_tiles(inp["w_out"][l]) for l in range(DEPTH)])
    sh["ffn_g"] = _tiles(inp["ffn_w_gate"][0])
    sh["ffn_u"] = _tiles(inp["ffn_w_up"][0])

    def dtiles(wd):
        return np.ascontiguousarray(wd.reshape(4, FB, 128, 16, 128).transpose(0, 3, 2, 1, 4))
    sh["ffn_d"] = dtiles(inp["ffn_w_down"][0])
    sh["router"] = np.ascontiguousarray(inp["router_w"][0].reshape(16, 128, 8).transpose(1, 0, 2))
    sh["moe_g"] = np.stack([_tiles(inp["moe_w_gate"][0][e]) for e in range(NEXP)])
    sh["moe_u"] = np.stack([_tiles(inp["moe_w_up"][0][e]) for e in range(NEXP)])
    sh["moe_d"] = np.stack([dtiles(inp["moe_w_down"][0][e]) for e in range(NEXP)])
    sh["sink"] = np.ascontiguousarray(inp["attn_sink"])
    return sh


def prep_core(inp, i):
    m = {}
    x = np.concatenate([inp["x_sample"][i], inp["x_prompt"][2 * i], inp["x_prompt"][2 * i + 1]], axis=0)
    m["xinT"] = np.ascontiguousarray(x.T.reshape(16, 128, NT))
    v = np.zeros((NROWS, 128), np.float32)
    v[R_C:R_C + 16] = inp["c"][i].reshape(16, 128)
    v[R_CCTX:R_CCTX + 16] = inp["c_ctx"].reshape(16, 128)
    for l in range(DEPTH):
        v[R_LN1(l):R_LN1(l) + 16] = inp["ln1_g"][l].reshape(16, 128)
        v[R_LN2(l):R_LN2(l) + 16] = inp["ln2_g"][l].reshape(16, 128)
        v[R_BADA(l):R_BADA(l) + 96] = inp["b_ada"][l].reshape(96, 128)
        v[R_QG(l):R_QG(l) + 4] = inp["mla_q_norm_g"][l].reshape(4, 128)
        v[R_KVG(l):R_KVG(l) + 2] = inp["mla_kv_norm_g"][l].reshape(2, 128)
        v[R_PSC(l):R_PSC(l) + 8] = inp["pool_scale"][l].reshape(8, 128)
    v[R_FG:R_FG + 16] = inp["final_g"].reshape(16, 128)
    m["vecs"] = v
    ck = inp["cache_attn_k"][i]
    ckT = ck.transpose(0, 2, 3, 1)
    m["ck2T"] = np.ascontiguousarray(np.concatenate([ckT, ckT], axis=2))
    m["cv"] = np.ascontiguousarray(inp["cache_attn_v"][i].reshape(DEPTH, 512, 128))
    m["cckvT"] = np.ascontiguousarray(inp["cache_mla_ckv"][i].transpose(0, 2, 1).reshape(DEPTH, 2, 128, 512))
    krT = inp["cache_mla_krope"][i].transpose(0, 2, 1)
    m["ckr2T"] = np.ascontiguousarray(np.concatenate([krT, krT], axis=1))
    return m


def build_program():
    P = Prog()
    P.alloc_persistent()
    P.phase_prologue()
    for l in range(DEPTH):
        P.phase_proj(l)
        P.phase_attnA(l)
        P.phase_mla(l)
        P.phase_pool(l)
        P.phase_merge(l)
        if l % 2 == 1:
            P.phase_moe_sparse(l, final=(l == DEPTH - 1))
        else:
            P.phase_ffn(l, final=(l == DEPTH - 1))
    return P


def kernel(**inputs):
    inp = {k: np.asarray(v, dtype=np.float32) for k, v in inputs.items()}
    P = build_program()
    sh = prep_shared(inp)
    in_maps = []
    for i in range(NCORES):
        m = prep_core(inp, i)
        m.update(sh)
        in_maps.append({k: m[k] for k in P.din})
    res = run_bass_kernel_spmd(P.nc, in_maps, core_ids=list(range(NCORES)))
    y_prompt = np.zeros((16, 256, D), np.float32)
    y_sample = np.zeros((8, 2048, D), np.float32)
    st_k = np.zeros((16, DEPTH, 256, 2, 64), np.float32)
    st_v = np.zeros((16, DEPTH, 256, 2, 64), np.float32)
    st_ckv = np.zeros((16, DEPTH, 256, 256), np.float32)
    st_kr = np.zeros((16, DEPTH, 256, 64), np.float32)
    for i in range(NCORES):
        r = res.results[i]
        y = np.asarray(r["o_yT"]).reshape(D, NT).T
        y_sample[i] = y[0:2048]
        okT = np.asarray(r["o_kT"])
        ov = np.asarray(r["o_v"]).reshape(DEPTH, 512, 128)
        ockv = np.asarray(r["o_ckvT"]).reshape(DEPTH, 256, 512)
        okr = np.asarray(r["o_krT"])
        for j in range(2):
            b = 2 * i + j
            ts = slice(j * 256, (j + 1) * 256)
            y_prompt[b] = y[2048 + j * 256:2048 + (j + 1) * 256]
            st_k[b] = okT[:, :, :, ts].transpose(0, 3, 1, 2)
            st_v[b] = ov[:, ts, :].reshape(DEPTH, 256, 2, 64)
            st_ckv[b] = ockv[:, :, ts].transpose(0, 2, 1)
            st_kr[b] = okr[:, :, ts].transpose(0, 2, 1)
    return (y_prompt, y_sample, st_k, st_v, st_ckv, st_kr)
```

```python
import contextlib
import numpy as np
import concourse.bass as bass
import concourse.mybir as mybir
from concourse.bass_utils import run_bass_kernel_spmd

F32 = mybir.dt.float32
F32R = mybir.dt.float32r
BF16 = mybir.dt.bfloat16
AF = mybir.ActivationFunctionType
ALU = mybir.AluOpType
AX = mybir.AxisListType

NCORES = 8
D = 2048
NT = 2560
G = 512
NGRP = 5
DEPTH = 2
EPS = 1e-6
NFF = 44
FB = 11
NEXP = 8
NS = 17
DBG_GROUPS = None
SEQS = ((0, 2048, True), (2048, 256, False), (2304, 256, False))
POOL_WINDOWS = (2, 4, 8, 16)

R_C, R_CCTX = 0, 16


def R_LN1(l): return 32 + l * 142
def R_LN2(l): return 32 + l * 142 + 16
def R_BADA(l): return 32 + l * 142 + 32
def R_QG(l): return 32 + l * 142 + 128
def R_KVG(l): return 32 + l * 142 + 132
def R_PSC(l): return 32 + l * 142 + 134


R_FG = 32 + 2 * 142
NROWS = 384


class Buf:
    __slots__ = ("w", "r")

    def __init__(self):
        self.w = None
        self.r = {}


class Eng:
    def __init__(self, name, eng, sem):
        self.name = name
        self.eng = eng
        self.sem = sem
        self.cnt = 0
        self.seen = {}
        self.dsems = []
        self.dvals = []
        self.dn = 0
        self.pend = []
        self.prog = []


class FW:
    def __init__(self, nc, es, ndma_sems=8):
        self.nc = nc
        self.engs = {}
        for name, eng in (("pe", nc.tensor), ("act", nc.scalar), ("dve", nc.vector),
                          ("pool", nc.gpsimd), ("sp", nc.sync)):
            sem = es.enter_context(nc.semaphore("s_" + name))
            self.engs[name] = Eng(name, eng, sem)
        for qn in ("sp", "pool"):
            e = self.engs[qn]
            for i in range(ndma_sems):
                e.dsems.append(es.enter_context(nc.semaphore("d_%s%d" % (qn, i))))
                e.dvals.append(0)
        self.ninst = 0

    def _wait(self, e, ticket):
        sem, val, owner = ticket
        if owner is e:
            if e.name == "pe" or val > e.cnt:
                return
        key = id(sem)
        if e.seen.get(key, 0) >= val:
            return
        e.pend.append((sem, val))
        e.seen[key] = val
        self.ninst += 1

    def _deps(self, e, reads, writes):
        for b in reads:
            if b.w is not None:
                self._wait(e, b.w)
        for b in writes:
            if b.w is not None:
                self._wait(e, b.w)
            for t in b.r.values():
                self._wait(e, t)

    def _mark(self, e, ticket, reads, writes, key=None):
        for b in reads:
            b.r[key or e.name] = ticket
        for b in writes:
            b.w = ticket
            b.r = {}

    def op(self, en, fn, reads=(), writes=(), sync=True):
        e = self.engs[en]
        self._deps(e, reads, writes)
        self.ninst += 1
        waits, e.pend = e.pend, []
        if sync:
            e.cnt += 1
            e.prog.append((waits, fn, e.sem, 1))
            t = (e.sem, e.cnt, e)
        else:
            e.prog.append((waits, fn, None, 0))
            t = (e.sem, e.cnt + 1, e)
        self._mark(e, t, reads, writes)

    def dma(self, qn, out, in_, reads=(), writes=(), **kw):
        e = self.engs[qn]
        slot = e.dn % len(e.dsems)
        e.dn += 1
        sem = e.dsems[slot]
        if e.dvals[slot] > 0:
            self._wait(e, (sem, e.dvals[slot], None))
        self._deps(e, reads, writes)
        e.dvals[slot] += 16
        eng = e.eng
        waits, e.pend = e.pend, []
        e.prog.append((waits, (lambda: eng.dma_start(out=out, in_=in_, **kw)), sem, 16))
        self.ninst += 1
        self._mark(e, (sem, e.dvals[slot], None), reads, writes, key=(e.name, slot))

    def dma_custom(self, qn, fn, reads=(), writes=()):
        e = self.engs[qn]
        slot = e.dn % len(e.dsems)
        e.dn += 1
        sem = e.dsems[slot]
        if e.dvals[slot] > 0:
            self._wait(e, (sem, e.dvals[slot], None))
        self._deps(e, reads, writes)
        e.dvals[slot] += 16
        waits, e.pend = e.pend, []
        e.prog.append((waits, fn, sem, 16))
        self.ninst += 1
        self._mark(e, (sem, e.dvals[slot], None), reads, writes, key=(e.name, slot))

    def emit(self):
        sp = self.engs["sp"]
        for qn in ("sp", "pool"):
            q = self.engs[qn]
            for s, v in zip(q.dsems, q.dvals):
                if v > 0:
                    self._wait(sp, (s, v, None))
        for en in ("pe", "act", "dve", "pool"):
            e = self.engs[en]
            if e.cnt > 0:
                self._wait(sp, (e.sem, e.cnt, None))

        def replay(e):
            eng = e.eng
            for waits, fn, sem, inc in e.prog:
                for s, v in waits:
                    eng.wait_ge(s, v)
                ins = fn()
                if sem is not None:
                    ins.then_inc(sem, inc)
            for s, v in e.pend:
                eng.wait_ge(s, v)
            e.prog = []
            e.pend = []

        with self.nc.Block() as block:
            @block.sync
            def _(x):
                replay(self.engs["sp"])

            @block.tensor
            def _(x):
                replay(self.engs["pe"])

            @block.scalar
            def _(x):
                replay(self.engs["act"])

            @block.vector
            def _(x):
                replay(self.engs["dve"])

            @block.gpsimd
            def _(x):
                replay(self.engs["pool"])


class Tile:
    _n = [0]

    def __init__(self, nc, es, name, shape, split=False, dt=None):
        Tile._n[0] += 1
        self.t = es.enter_context(nc.sbuf_tensor("sb%d_%s" % (Tile._n[0], name), list(shape), dt or F32))
        self.split = split
        if split:
            self.bufs = [Buf() for _ in range(shape[1])]
        else:
            self.bufs = [Buf()]

    def __getitem__(self, idx):
        return self.t[idx]

    def r(self, idx):
        return self.t[idx].bitcast(F32R)

    def b(self, i=None):
        if self.split and i is not None:
            return [self.bufs[i]]
        return list(self.bufs)


class Ring:
    def __init__(self, nc, es, name, shape, n, dt=None):
        self.tiles = [Tile(nc, es, "%s%d" % (name, i), shape, dt=dt) for i in range(n)]
        self.i = 0

    def next(self):
        t = self.tiles[self.i % len(self.tiles)]
        self.i += 1
        return t


class Prog:
    def __init__(self, stop_after=None, moe_experts=NEXP, scratch_in=(), moe_sparse=True):
        self.stop_after = stop_after
        self.moe_experts = moe_experts
        self.nc = nc = bass.Bass("TRN2", target_bir_lowering=False)
        self.es = es = contextlib.ExitStack()
        self.fw = FW(nc, es)
        self.din = {}
        self.evac_i = 0

        self.ishapes = {}

        def inp(name, shape):
            self.ishapes[name] = list(shape)

        def outp(name, shape):
            return nc.dram_tensor(name, list(shape), F32, kind="ExternalOutput").ap()

        def scr(name, shape):
            return nc.dram_tensor(name, list(shape), F32, kind="Internal").ap()

        inp("xinT", [16, 128, NT])
        inp("vecs", [NROWS, 128])
        inp("sink", [DEPTH, 16])
        inp("ident", [128, 128])
        inp("permM", [128, 128])
        inp("ropeC", [128, 2048])
        inp("ropeS", [128, 2048])
        inp("maskA", [128, 384])
        inp("invc_s", [4, 2048])
        inp("invc_p", [4, 256])
        inp("ck2T", [DEPTH, 2, 128, 512])
        inp("cv", [DEPTH, 512, 128])
        inp("cckvT", [DEPTH, 2, 128, 512])
        inp("ckr2T", [DEPTH, 128, 512])
        inp("w_ada", [DEPTH, 96, 128, 16, 128])
        inp("w_inA", [DEPTH, 26, 128, 16, 128])
        inp("w_inG", [DEPTH, 48, 128, 16, 128])
        inp("w_uq", [DEPTH, 12, 128, 4, 128])
        inp("w_ukv", [DEPTH, 16, 128, 2, 128])
        inp("poolw", [DEPTH, 8, 128, 2, 128])
        inp("wbr", [DEPTH, 3, 16, 128, 8, 128])
        inp("wout", [DEPTH, 16, 128, 16, 128])
        inp("ffn_g", [NFF, 128, 16, 128])
        inp("ffn_u", [NFF, 128, 16, 128])
        inp("ffn_d", [4, 16, 128, FB, 128])
        inp("router", [128, 16, 8])
        inp("moe_g", [NEXP, NFF, 128, 16, 128])
        inp("moe_u", [NEXP, NFF, 128, 16, 128])
        inp("moe_d", [NEXP, 4, 16, 128, FB, 128])
        inp("Umat", [128, 128])
        inp("tokid", [128, NT // 128])
        inp("svals", [128, NS])
        inp("wbase", [128, NFF])
        inp("wdbase", [128, 64])
        inp("Tab0", [NS * 512, 16])
        self.o_yT = outp("o_yT", [16, 128, NT])
        self.o_kT = outp("o_kT", [DEPTH, 2, 64, 512])
        self.o_v = outp("o_v", [DEPTH, 4, 128, 128])
        self.o_ckvT = outp("o_ckvT", [DEPTH, 2, 128, 512])
        self.o_krT = outp("o_krT", [DEPTH, 64, 512])
        def mk(name, shape):
            if name in scratch_in:
                return nc.dram_tensor(name, list(shape), F32, kind="ExternalInput").ap()
            return (outp if stop_after is not None else scr)(name, shape)
        self.xT = mk("s_xT", [16, 128, NT])
        self.qaT = mk("s_qaT", [8, 128, NT])
        self.kaT2 = mk("s_kaT2", [2, 128, NT])
        self.vaTok = mk("s_vaTok", [NT // 128, 128, 128])
        self.qnT = mk("s_qnT", [8, 128, NT])
        self.qrT = mk("s_qrT", [4, 128, NT])
        self.ckvT = mk("s_ckvT", [2, 128, NT])
        self.krT2 = mk("s_krT2", [128, NT])
        self.uT = mk("s_uT", [8, 128, NT])
        self.oaT = mk("s_oaT", [8, 128, NT])
        self.obT = mk("s_obT", [8, 128, NT])
        self.ocT = mk("s_ocT", [8, 128, NT])
        if moe_sparse:
            self.hTok = scr("s_hTok", [NT + 128, D])
            self.Ybuf = scr("s_Y", [2 * NT + 128, D])
            self.Tab = nc.dram_tensor("s_Tab", [NS * 512, 16], mybir.dt.int32, kind="Internal").ap()
        self.b_x = [[Buf() for _ in range(16)] for _ in range(NGRP)]
        self.b_scr = {k: Buf() for k in ("qaT", "kaT2", "vaTok", "qnT", "qrT", "ckvT", "krT2", "uT",
                                         "oaT", "obT", "ocT")}
        self.ps = [es.enter_context(nc.psum_tensor("ps%d" % i, [128, 512], F32)) for i in range(8)]
        self.psb = [Buf() for _ in range(8)]

    def I(self, name, dt=F32):
        if name not in self.din:
            self.din[name] = self.nc.dram_tensor(name, self.ishapes[name], dt, kind="ExternalInput").ap()
        return self.din[name]

    def mm(self, out, lhsT, rhs, start, stop, reads, writes, sync=None):
        nc = self.nc
        if sync is None:
            sync = stop
        self.fw.op("pe", lambda: nc.tensor.matmul(out, lhsT=lhsT, rhs=rhs, start=start, stop=stop),
                   reads=reads, writes=writes, sync=sync)

    def tr(self, out, in_, reads, writes, sync=True):
        nc = self.nc
        ident = self.ident[:]
        self.fw.op("pe", lambda: nc.tensor.transpose(out=out, in_=in_, identity=ident),
                   reads=reads + self.ident.b(), writes=writes, sync=sync)

    def copy(self, en, out, in_, reads, writes):
        nc = self.nc
        if en == "act":
            self.fw.op("act", lambda: nc.scalar.copy(out=out, in_=in_), reads=reads, writes=writes)
        elif en == "dve":
            self.fw.op("dve", lambda: nc.vector.tensor_copy(out=out, in_=in_), reads=reads, writes=writes)
        else:
            self.fw.op("pool", lambda: nc.gpsimd.tensor_copy(out=out, in_=in_), reads=reads, writes=writes)

    def evac_eng(self):
        self.evac_i += 1
        return "act" if self.evac_i % 2 else "dve"

    def act(self, out, in_, func, reads, writes, bias=None, scale=None, accum_out=None):
        nc = self.nc
        kw = {}
        if bias is not None:
            kw["bias"] = bias
        if scale is not None:
            kw["scale"] = scale
        if accum_out is not None:
            kw["accum_out"] = accum_out
        self.fw.op("act", lambda: nc.scalar.activation(out=out, in_=in_, func=func, **kw),
                   reads=reads, writes=writes)

    def tt(self, out, in0, in1, op, reads, writes, en="dve"):
        nc = self.nc
        eng = nc.vector if en == "dve" else nc.gpsimd
        self.fw.op(en, lambda: eng.tensor_tensor(out=out, in0=in0, in1=in1, op=op), reads=reads, writes=writes)

    def ts(self, out, in0, s1, s2, op0, op1, reads, writes):
        nc = self.nc
        if op1 is None:
            self.fw.op("dve", lambda: nc.vector.tensor_scalar(out=out, in0=in0, scalar1=s1, scalar2=None, op0=op0),
                       reads=reads, writes=writes)
        else:
            self.fw.op("dve", lambda: nc.vector.tensor_scalar(out=out, in0=in0, scalar1=s1, scalar2=s2,
                                                              op0=op0, op1=op1), reads=reads, writes=writes)

    def stt(self, out, in0, scalar, in1, op0, op1, reads, writes):
        nc = self.nc
        self.fw.op("dve", lambda: nc.vector.scalar_tensor_tensor(out=out, in0=in0, scalar=scalar, in1=in1,
                                                                 op0=op0, op1=op1), reads=reads, writes=writes)

    def wload(self, ring, dram_tile, nk, ncol=128):
        slot = ring.next()
        dst = slot.t[:, 0:nk * ncol].rearrange("p (k m) -> p k m", k=nk)
        self.fw.dma("pool", dst.bitcast(F32R), dram_tile, writes=slot.b())
        return dst.bitcast(F32R), slot

    def alloc_persistent(self):
        nc, es = self.nc, self.es
        self.ident = Tile(nc, es, "ident", [128, 128])
        self.ones = Tile(nc, es, "ones", [128, 128])
        self.identr = Tile(nc, es, "identr", [128, 128])
        self.vT = Tile(nc, es, "vT", [128, NROWS])
        self.modp = [Tile(nc, es, "modp%d" % l, [128, 6, 16, 2]) for l in range(DEPTH)]
        self.sinkb = Tile(nc, es, "sinkb", [128, DEPTH * 16])

    def phase_prologue(self):
        nc, fw = self.nc, self.fw
        with contextlib.ExitStack() as es:
            vrows = Tile(nc, es, "vrows", [128, 3, 128])
            scT = Tile(nc, es, "scT", [128, 32])
            modT = Tile(nc, es, "modT", [128, 96, 2])
            wring = Ring(nc, es, "wada", [128, 2048], 4)
            fw.dma("sp", self.ident[:], self.I("ident"), writes=self.ident.b())
            fw.dma("sp", vrows[:], self.I("vecs").rearrange("(a p) f -> p a f", p=128), writes=vrows.b())
            fw.dma("sp", self.sinkb[:],
                   self.I("sink").rearrange("l h -> (l h)").partition_broadcast(128), writes=self.sinkb.b())
            ones32 = Tile(nc, es, "ones32", [128, 128])
            fw.op("dve", lambda: nc.vector.memset(ones32[:], 1.0), writes=ones32.b())
            self.copy("act", self.ones.r(slice(None)), ones32[:], ones32.b(), self.ones.b())
            self.copy("act", self.identr.r(slice(None)), self.ident[:], self.ident.b(), self.identr.b())
            for a in range(3):
                self.tr(self.ps[0][:, a * 128:(a + 1) * 128], vrows[:, a, :], vrows.b(), [self.psb[0]])
            self.copy("dve", self.vT[:], self.ps[0][:, 0:384], [self.psb[0]], self.vT.b())
            self.act(scT.r(slice(None)), self.vT[:, 0:32], AF.Silu, self.vT.b(), scT.b())
            sc3 = scT.r(slice(None)).rearrange("p (c k) -> p k c", c=2)
            for l in range(DEPTH):
                for j in range(96):
                    w, slot = self.wload(wring, self.I("w_ada")[l, j], 16)
                    bank = self.ps[1 + (j % 2)]
                    bb = self.psb[1 + (j % 2)]
                    for k in range(16):
                        self.mm(bank[:, 0:2], w[:, k, :], sc3[:, k, :], k == 0, k == 15,
                                slot.b() + scT.b(), [bb])
                    col = R_BADA(l) + j
                    self.act(modT[:, j, :], bank[:, 0:2], AF.Identity, [bb] + self.vT.b(), modT.b(),
                             bias=self.vT[:, col:col + 1])
                mp = self.modp[l]
                for s in range(2):
                    lnr = R_LN1(l) if s == 0 else R_LN2(l)
                    sh, sc, gt = 3 * s, 3 * s + 1, 3 * s + 2
                    for cd in range(2):
                        self.stt(mp[:, 3 * s + 0, :, cd], modT[:, sc * 16:(sc + 1) * 16, cd], 1.0,
                                 self.vT[:, lnr:lnr + 16], ALU.add, ALU.mult,
                                 modT.b() + self.vT.b(), mp.b())
                        self.copy("dve", mp[:, 3 * s + 1, :, cd], modT[:, sh * 16:(sh + 1) * 16, cd], modT.b(), mp.b())
                        self.copy("dve", mp[:, 3 * s + 2, :, cd], modT[:, gt * 16:(gt + 1) * 16, cd], modT.b(), mp.b())
            xt = Ring(nc, es, "xcp", [128, 16 * 512], 2)
            for g in range(NGRP):
                t = xt.next()
                v = t.t[:].rearrange("p (c n) -> p c n", c=16)
                fw.dma("sp", v, self.I("xinT")[:, :, g * G:(g + 1) * G].rearrange("c p n -> p c n"), writes=t.b())
                fw.dma("sp", self.xT[:, :, g * G:(g + 1) * G].rearrange("c p n -> p c n"), v,
                       reads=t.b(), writes=self.b_x[g])
            fw.emit()

    def norm_stats(self, x3, xb, nchunk, nfeat, sqring, rstd, bank, bb):
        nc = self.nc
        for c in range(nchunk):
            sq = sqring.next()
            self.act(sq.r(slice(None)), x3[:, c, :], AF.Square, xb, sq.b())
            self.mm(bank[:, :], self.ones.r(slice(None)), sq.r(slice(None)), c == 0, c == nchunk - 1,
                    self.ones.b() + sq.b(), [bb], sync=True)
        self.act(rstd[:], bank[:, :], AF.Sqrt, [bb] + self.epsb.b(), rstd.b(), bias=self.epsb[:, 0:1], scale=1.0 / nfeat)
        self.fw.op("dve", lambda: nc.vector.reciprocal(out=rstd[:], in_=rstd[:]), reads=rstd.b(), writes=rstd.b())

    def norm_mod(self, g, l, s, xg, hT, sqring, tmpring, rstd, bank, bb):
        cd = 0 if g < 4 else 1
        mp = self.modp[l]
        x3 = xg.t[:].rearrange("p (c n) -> p c n", c=16)
        self.norm_stats(x3, xg.b(), 16, D, sqring, rstd, bank, bb)
        for c in range(16):
            tmp = tmpring.next()
            self.tt(tmp[:], x3[:, c, :], rstd[:], ALU.mult, xg.b() + rstd.b(), tmp.b())
            self.act(hT.r((slice(None), c, slice(None))), tmp[:], AF.Identity, tmp.b() + mp.b(), hT.b(c),
                     bias=mp[:, 3 * s + 1, c:c + 1, cd], scale=mp[:, 3 * s + 0, c:c + 1, cd])

    def load_x(self, g, xg):
        v = xg.t[:].rearrange("p (c n) -> p c n", c=16)
        self.fw.dma("sp", v, self.xT[:, :, g * G:(g + 1) * G].rearrange("c p n -> p c n"),
                    reads=self.b_x[g], writes=xg.b())

    def phase_proj(self, l):
        nc, fw = self.nc, self.fw
        with contextlib.ExitStack() as es:
            xg = Tile(nc, es, "xg", [128, 16 * 512])
            hT = Tile(nc, es, "hT", [128, 16, 512], split=True)
            wring = Ring(nc, es, "w1", [128, 2048], 4)
            wuq = Tile(nc, es, "wuq", [128, 12, 512])
            ropeC = Tile(nc, es, "ropeC", [128, 2048])
            ropeS = Tile(nc, es, "ropeS", [128, 2048])
            permM = Tile(nc, es, "permM", [128, 128])
            cqs = Tile(nc, es, "cqs", [128, 4, 512])
            cqn = Tile(nc, es, "cqn", [128, 4, 512])
            ckvs = Tile(nc, es, "ckvs", [128, 2, 512])
            stg = Ring(nc, es, "stg", [128, 512], 4)
            sqring = Ring(nc, es, "sq", [128, 512], 3)
            tmpring = Ring(nc, es, "tmp", [128, 512], 3)
            xsring = Ring(nc, es, "xs", [128, 512], 2)
            t1ring = Ring(nc, es, "t1", [128, 512], 2)
            rstd = Tile(nc, es, "rstd", [128, 512])
            rstd2 = Tile(nc, es, "rstd2", [128, 512])
            vtok = Ring(nc, es, "vtok", [128, 512], 2)
            self.epsb = Tile(nc, es, "epsb", [128, 1])
            fw.op("dve", lambda: nc.vector.memset(self.epsb[:], EPS), writes=self.epsb.b())
            fw.dma("sp", ropeC[:], self.I("ropeC"), writes=ropeC.b())
            fw.dma("sp", ropeS[:], self.I("ropeS"), writes=ropeS.b())
            fw.dma("pool", permM.r(slice(None)), self.I("permM"), writes=permM.b())
            fw.dma("pool", wuq.t[:].rearrange("p a (k m) -> p a k m", k=4).bitcast(F32R),
                   self.I("w_uq")[l].rearrange("a p k m -> p a k m"), writes=wuq.b())
            wuq4 = wuq.t[:].rearrange("p a (k m) -> p a k m", k=4).bitcast(F32R)
            bi = [0]

            def nextbank():
                bi[0] += 1
                i = bi[0] % 4
                return self.ps[i], self.psb[i]

            def finish_chunk(bank, bb, g, rope, dst, dbuf, P=128, dst2=None):
                st = stg.next()
                if rope and g < 4:
                    xs = xsring.next()
                    self.copy("act", xs.r(slice(None))[0:P], bank[0:P, :], [bb], xs.b())
                    pb, pbb = self.ps[4 + (bi[0] % 2)], self.psb[4 + (bi[0] % 2)]
                    self.mm(pb[0:P, :], permM.r(slice(None))[0:P, 0:P], xs.r(slice(None))[0:P], True, True,
                            permM.b() + xs.b(), [pbb])
                    t1 = t1ring.next()
                    self.tt(t1[0:P], xs[0:P], ropeC[0:P, g * G:(g + 1) * G], ALU.mult, xs.b() + ropeC.b(), t1.b())
                    self.tt(st[0:P], pb[0:P, :], ropeS[0:P, g * G:(g + 1) * G], ALU.mult, [pbb] + ropeS.b(), st.b())
                    self.tt(st[0:P], st[0:P], t1[0:P], ALU.add, st.b() + t1.b(), st.b())
                else:
                    self.copy(self.evac_eng(), st[0:P], bank[0:P, :], [bb], st.b())
                fw.dma("sp", dst, st[0:P], reads=st.b())
                if dst2 is not None:
                    fw.dma("sp", dst2, st[0:dst2.shape[0]], reads=st.b())
                return st

            for g in (DBG_GROUPS or range(NGRP)):
                gs = slice(g * G, (g + 1) * G)
                self.load_x(g, xg)
                self.norm_mod(g, l, 0, xg, hT, sqring, tmpring, rstd, self.ps[7], self.psb[7])
                for ch in range(26):
                    w, slot = self.wload(wring, self.I("w_inA")[l, ch], 16)
                    bank, bb = nextbank()
                    for k in range(16):
                        self.mm(bank[:, :], w[:, k, :], hT.r((slice(None), k, slice(None))), k == 0, k == 15,
                                slot.b() + hT.b(k), [bb])
                    if ch < 8:
                        finish_chunk(bank, bb, g, True, self.qaT[ch, :, gs], self.b_scr["qaT"])
                    elif ch < 10:
                        d2 = self.o_kT[l, ch - 8, :, :] if g == 4 else None
                        finish_chunk(bank, bb, g, True, self.kaT2[ch - 8, :, gs], self.b_scr["kaT2"], dst2=d2)
                    elif ch == 10:
                        st = stg.next()
                        self.copy(self.evac_eng(), st[:], bank[:, :], [bb], st.b())
                        tb, tbb = self.ps[6], self.psb[6]
                        for t in range(4):
                            self.tr(tb[:, t * 128:(t + 1) * 128], st[:, t * 128:(t + 1) * 128], st.b(), [tbb])
                        vt = vtok.next()
                        self.copy(self.evac_eng(), vt[:], tb[:, :], [tbb], vt.b())
                        fw.dma("sp", self.vaTok[g * 4:(g + 1) * 4].rearrange("t p d -> p t d"),
                               vt.t[:].rearrange("p (t d) -> p t d", t=4), reads=vt.b())
                        if g == 4:
                            fw.dma("sp", self.o_v[l].rearrange("t p d -> p t d"),
                                   vt.t[:].rearrange("p (t d) -> p t d", t=4), reads=vt.b())
                    elif ch < 15:
                        self.copy(self.evac_eng(), cqs[:, ch - 11, :], bank[:, :], [bb], cqs.b())
                        if ch == 14:
                            self.norm_stats(cqs.t[:], cqs.b(), 4, 512, sqring, rstd2, self.ps[7], self.psb[7])
                            for c in range(4):
                                col = R_QG(l) + c
                                self.stt(cqn.r((slice(None), c, slice(None))), cqs[:, c, :], self.vT[:, col:col + 1],
                                         rstd2[:], ALU.mult, ALU.mult, cqs.b() + rstd2.b() + self.vT.b(), cqn.b())
                            for a in range(12):
                                bank2, bb2 = nextbank()
                                for k in range(4):
                                    self.mm(bank2[:, :], wuq4[:, a, k, :], cqn.r((slice(None), k, slice(None))),
                                            k == 0, k == 3, wuq.b() + cqn.b(), [bb2])
                                if a < 8:
                                    finish_chunk(bank2, bb2, g, False, self.qnT[a, :, gs], self.b_scr["qnT"])
                                else:
                                    finish_chunk(bank2, bb2, g, True, self.qrT[a - 8, :, gs], self.b_scr["qrT"])
                    elif ch < 17:
                        self.copy(self.evac_eng(), ckvs[:, ch - 15, :], bank[:, :], [bb], ckvs.b())
                        if ch == 16:
                            self.norm_stats(ckvs.t[:], ckvs.b(), 2, 256, sqring, rstd2, self.ps[7], self.psb[7])
                            for c in range(2):
                                col = R_KVG(l) + c
                                st = stg.next()
                                self.stt(st[:], ckvs[:, c, :], self.vT[:, col:col + 1], rstd2[:], ALU.mult, ALU.mult,
                                         ckvs.b() + rstd2.b() + self.vT.b(), st.b())
                                fw.dma("sp", self.ckvT[c, :, gs], st[:], reads=st.b())
                                if g == 4:
                                    fw.dma("sp", self.o_ckvT[l, c], st[:], reads=st.b())
                    elif ch == 17:
                        d2 = self.o_krT[l] if g == 4 else None
                        finish_chunk(bank, bb, g, True, self.krT2[:, gs], self.b_scr["krT2"], dst2=d2)
                    else:
                        finish_chunk(bank, bb, g, False, self.uT[ch - 18, :, gs], self.b_scr["uT"])
            fw.emit()

    def rmax(self, out, in_, reads, writes):
        nc = self.nc
        self.fw.op("dve", lambda: nc.vector.reduce_max(out=out, in_=in_, axis=AX.X), reads=reads, writes=writes)

    def rsum(self, out, in_, reads, writes):
        nc = self.nc
        self.fw.op("dve", lambda: nc.vector.reduce_sum(out=out, in_=in_, axis=AX.X), reads=reads, writes=writes)

    def recip(self, out, in_, reads, writes):
        nc = self.nc
        self.fw.op("dve", lambda: nc.vector.reciprocal(out=out, in_=in_), reads=reads, writes=writes)

    def phase_attnA(self, l, seqs=SEQS, qb_limit=None):
        nc, fw = self.nc, self.fw
        with contextlib.ExitStack() as es:
            kT2 = Tile(nc, es, "kT2", [128, 2, 2560])
            Vt = Tile(nc, es, "Vt", [128, 20, 128], dt=BF16)
            identbA = Tile(nc, es, "identbA", [128, 128], dt=BF16)
            PbringA = Ring(nc, es, "PbA", [128, 896], 5, dt=BF16)
            qring = Ring(nc, es, "qc", [128, 2048], 2)
            maskA = Tile(nc, es, "maskA", [128, 384])
            Pring = Ring(nc, es, "Pa", [128, 896], 5)
            PTring = Ring(nc, es, "PTa", [128, 896], 3, dt=BF16)
            slring = Ring(nc, es, "sl", [128, 384], 2)
            small = Ring(nc, es, "sma", [128, 16], 4)
            rdring = Ring(nc, es, "rda", [128, 2], 3)
            Oqring = Ring(nc, es, "Oqa", [128, 128], 2)
            ostg = Ring(nc, es, "ostga", [128, 512], 2)
            fw.dma("sp", maskA[:], self.I("maskA"), writes=maskA.b())
            self.copy("dve", identbA[:], self.ident[:], self.ident.b(), identbA.b())
            for (tok0, n, ctx) in seqs:
                nqb = n // 128
                for g2 in range(2):
                    fw.dma("pool", kT2.r((slice(None), g2, slice(0, n))), self.kaT2[g2, :, tok0:tok0 + n],
                           writes=kT2.b())
                    if ctx:
                        fw.dma("pool", kT2.r((slice(None), g2, slice(n, n + 512))), self.I("ck2T")[l, g2],
                               writes=kT2.b())
                fw.dma("pool", Vt[:, 0:nqb, :],
                       self.vaTok[tok0 // 128:tok0 // 128 + nqb].rearrange("t p d -> p t d"),
                       writes=Vt.b())
                if ctx:
                    fw.dma("pool", Vt[:, nqb:nqb + 4, :],
                           self.I("cv")[l].rearrange("(t p) d -> p t d", p=128), writes=Vt.b())
                for c in range(8):
                    g2 = c // 4
                    qc = qring.next()
                    fw.dma("pool", qc.r((slice(None), slice(0, n))), self.qaT[c, :, tok0:tok0 + n],
                           writes=qc.b())
                    qbs = list(range(nqb)) if qb_limit is None else list(range(min(nqb, qb_limit)))
                    stbox = [None]

                    def stageA(qb, c=c, g2=g2, qc=qc):
                        if ctx:
                            kb_lo, kb_hi = max(qb - 1, 0), min(qb + 1, nqb - 1)
                        else:
                            kb_lo, kb_hi = 0, nqb - 1
                        nl = (kb_hi - kb_lo + 1) * 128
                        blocks = list(range(kb_lo, kb_hi + 1)) + ([nqb + i for i in range(4)] if ctx else [])
                        rd = rdring.next()
                        sm = small.next()
                        pts = []
                        for hh in range(2):
                            pb = hh * 64
                            sbank, sbb = self.ps[hh * 2], self.psb[hh * 2]
                            cbank, cbb = self.ps[hh * 2 + 1], self.psb[hh * 2 + 1]
                            lq = qc.r((slice(pb, pb + 64), slice(qb * 128, (qb + 1) * 128)))
                            self.mm(sbank[:, 0:nl], lq, kT2.r((slice(pb, pb + 64), g2, slice(kb_lo * 128, kb_lo * 128 + nl))),
                                    True, True, qc.b() + kT2.b(), [sbb])
                            if ctx:
                                self.mm(cbank[:, :], lq, kT2.r((slice(pb, pb + 64), g2, slice(n, n + 512))),
                                        True, True, qc.b() + kT2.b(), [cbb])
                            Pt = Pring.next()
                            if ctx:
                                mlo = 128 if qb == 0 else 0
                                self.tt(Pt[:, 0:nl], sbank[:, 0:nl], maskA[:, mlo:mlo + nl], ALU.add,
                                        [sbb] + maskA.b(), Pt.b())
                                self.rmax(sm[:, hh:hh + 1], Pt[:, 0:nl], Pt.b(), sm.b())
                                fw.op("dve", lambda cbank=cbank, Pt=Pt, sm=sm, nl=nl, hh=hh: nc.vector.tensor_scalar(
                                    out=Pt[:, nl:nl + 512], in0=cbank[:, :], scalar1=1.0, scalar2=None, op0=ALU.mult,
                                    op1=ALU.max, accum_out=sm[:, 2 + hh:3 + hh]), reads=[cbb], writes=Pt.b() + sm.b())
                            else:
                                fw.op("dve", lambda sbank=sbank, Pt=Pt, sm=sm, nl=nl, hh=hh: nc.vector.tensor_scalar(
                                    out=Pt[:, 0:nl], in0=sbank[:, 0:nl], scalar1=1.0, scalar2=None, op0=ALU.mult,
                                    op1=ALU.max, accum_out=sm[:, 4 + hh:5 + hh]), reads=[sbb], writes=Pt.b() + sm.b())
                            pts.append(Pt)
                        scol = l * 16 + 2 * c
                        sk2 = self.sinkb[:, scol:scol + 2]
                        if ctx:
                            self.tt(sm[:, 4:6], sm[:, 0:2], sm[:, 2:4], ALU.max, sm.b(), sm.b())
                        self.stt(sm[:, 6:8], sm[:, 4:6], 0.125, sk2, ALU.mult, ALU.max, sm.b() + self.sinkb.b(), sm.b())
                        self.ts(sm[:, 8:10], sm[:, 6:8], -1.0, None, ALU.mult, None, sm.b(), sm.b())
                        pbs = []
                        for hh in range(2):
                            Pt = pts[hh]
                            Pb = PbringA.next()
                            self.act(Pb[:, 0:nl], Pt[:, 0:nl], AF.Exp, Pt.b() + sm.b(), Pb.b() + sm.b(),
                                     bias=sm[:, 8 + hh:9 + hh], scale=0.125, accum_out=sm[:, 10 + hh:11 + hh])
                            if ctx:
                                self.act(Pb[:, nl:nl + 512], Pt[:, nl:nl + 512], AF.Exp, Pt.b() + sm.b(), Pb.b() + sm.b(),
                                         bias=sm[:, 8 + hh:9 + hh], scale=0.125, accum_out=sm[:, 12 + hh:13 + hh])
                            pbs.append(Pb)
                        self.tt(sm[:, 14:16], sk2, sm[:, 8:10], ALU.add, self.sinkb.b() + sm.b(), sm.b())
                        self.act(sm[:, 14:16], sm[:, 14:16], AF.Exp, sm.b(), sm.b())
                        self.tt(sm[:, 10:12], sm[:, 10:12], sm[:, 14:16], ALU.add, sm.b(), sm.b())
                        if ctx:
                            self.tt(sm[:, 10:12], sm[:, 10:12], sm[:, 12:14], ALU.add, sm.b(), sm.b())
                        self.recip(rd[:, 0:2], sm[:, 10:12], sm.b(), rd.b())
                        return blocks, rd, pbs

                    def stageB(qb, blocks, rd, pts, c=c, g2=g2):
                        nb = len(blocks)
                        obank, obb = self.ps[6], self.psb[6]
                        for hh in range(2):
                            Pt = pts[hh]
                            PT = PTring.next()
                            for i0 in range(0, nb, 4):
                                cnt = min(4, nb - i0)
                                tbb = self.psb[4 + (i0 // 4) % 2]
                                tb = self.ps[4 + (i0 // 4) % 2][:, :].bitcast(BF16)
                                for i in range(i0, i0 + cnt):
                                    fw.op("pe", lambda tb=tb, i=i, i0=i0, Pt=Pt: nc.tensor.transpose(
                                        out=tb[:, (i - i0) * 128:(i - i0 + 1) * 128], in_=Pt[:, i * 128:(i + 1) * 128],
                                        identity=identbA[:]), reads=Pt.b() + identbA.b(), writes=[tbb])
                                self.copy(self.evac_eng(), PT[:, i0 * 128:(i0 + cnt) * 128],
                                          tb[:, 0:cnt * 128], [tbb], PT.b())
                            for i, blk in enumerate(blocks):
                                self.mm(obank[:, hh * 64:(hh + 1) * 64], PT[:, i * 128:(i + 1) * 128],
                                        Vt[:, blk, g2 * 64:(g2 + 1) * 64], i == 0, i == nb - 1,
                                        PT.b() + Vt.b(), [obb], sync=True)
                        Oq = Oqring.next()
                        self.ts(Oq[:, 0:64], obank[:, 0:64], rd[:, 0:1], None, ALU.mult, None, [obb] + rd.b(), Oq.b())
                        self.ts(Oq[:, 64:128], obank[:, 64:128], rd[:, 1:2], None, ALU.mult, None, [obb] + rd.b(), Oq.b())
                        tb, tbb = self.ps[7], self.psb[7]
                        self.tr(tb[:, 0:128], Oq[:, :], Oq.b(), [tbb])
                        if qb % 4 == 0:
                            stbox[0] = ostg.next()
                        st = stbox[0]
                        self.copy(self.evac_eng(), st[:, (qb % 4) * 128:(qb % 4 + 1) * 128], tb[:, 0:128], [tbb], st.b())
                        if qb % 4 == 3 or qb == qbs[-1]:
                            q0 = (qb // 4) * 512
                            wd = (qb % 4 + 1) * 128
                            fw.dma("sp", self.oaT[c, :, tok0 + q0:tok0 + q0 + wd], st[:, 0:wd], reads=st.b())

                    nxt = stageA(qbs[0])
                    for ii, qb in enumerate(qbs):
                        cur = nxt
                        if ii + 1 < len(qbs):
                            nxt = stageA(qbs[ii + 1])
                        stageB(qb, *cur)
            fw.emit()

    def phase_mla(self, l, seqs=SEQS, qb_limit=None, heads=range(8)):
        nc, fw = self.nc, self.fw
        scale = float((128 + 64) ** -0.5)
        with contextlib.ExitStack() as es:
            ckvA = Tile(nc, es, "ckvA", [128, 2, 2560])
            krA = Tile(nc, es, "krA", [128, 2560])
            wukv = Tile(nc, es, "wukv", [128, 16, 256])
            knT = Tile(nc, es, "knT", [128, 2560])
            vh = Tile(nc, es, "vh", [128, 20, 128], dt=BF16)
            identb = Tile(nc, es, "identb", [128, 128], dt=BF16)
            Pbring = Ring(nc, es, "Pbm", [128, 2560], 3, dt=BF16)
            qnr = Ring(nc, es, "qnm", [128, 2048], 2)
            qrr = Ring(nc, es, "qrm", [128, 2048], 2)
            Pring = Ring(nc, es, "Pm", [128, 2560], 3)
            PTring = Ring(nc, es, "PTm", [128, 2560], 2, dt=BF16)
            small = Ring(nc, es, "smm", [128, 16], 4)
            Oqring = Ring(nc, es, "Oqm", [128, 128], 2)
            ostg = Ring(nc, es, "ostgm", [128, 512], 2)
            self.copy("dve", identb[:], self.ident[:], self.ident.b(), identb.b())
            wukv4 = wukv.t[:].rearrange("p a (k m) -> p a k m", k=2).bitcast(F32R)
            fw.dma("pool", wukv4, self.I("w_ukv")[l].rearrange("a p k m -> p a k m"), writes=wukv.b())
            for (tok0, n, ctx) in seqs:
                nqb = n // 128
                nk = n + (512 if ctx else 0)
                nkb = nk // 128
                kgs = [(s, min(512, nk - s)) for s in range(0, nk, 512)]
                ng = len(kgs)
                for k in range(2):
                    fw.dma("pool", ckvA.r((slice(None), k, slice(0, n))), self.ckvT[k, :, tok0:tok0 + n],
                           writes=ckvA.b())
                    if ctx:
                        fw.dma("pool", ckvA.r((slice(None), k, slice(n, n + 512))), self.I("cckvT")[l, k], writes=ckvA.b())
                fw.dma("pool", krA.r((slice(None), slice(0, n))), self.krT2[:, tok0:tok0 + n],
                       writes=krA.b())
                if ctx:
                    fw.dma("pool", krA.r((slice(None), slice(n, n + 512))), self.I("ckr2T")[l], writes=krA.b())
                qr, qr_pair = None, -1
                for h in heads:
                    pb = (h % 2) * 64
                    for gi, (s, w) in enumerate(kgs):
                        bank, bb = self.ps[gi % 4], self.psb[gi % 4]
                        for k in range(2):
                            self.mm(bank[:, 0:w], wukv4[:, h, k, :], ckvA.r((slice(None), k, slice(s, s + w))),
                                    k == 0, k == 1, wukv.b() + ckvA.b(), [bb])
                        self.copy(self.evac_eng(), knT.r((slice(None), slice(s, s + w))), bank[:, 0:w], [bb], knT.b())
                    for kb0 in range(0, nkb, 4):
                        cnt = min(4, nkb - kb0)
                        bank, bb = self.ps[4 + (kb0 // 4) % 2], self.psb[4 + (kb0 // 4) % 2]
                        for kb in range(kb0, kb0 + cnt):
                            for k in range(2):
                                self.mm(bank[:, (kb - kb0) * 128:(kb - kb0 + 1) * 128],
                                        ckvA.r((slice(None), k, slice(kb * 128, (kb + 1) * 128))), wukv4[:, 8 + h, k, :],
                                        k == 0, k == 1, wukv.b() + ckvA.b(), [bb], sync=(k == 1 and kb == kb0 + cnt - 1))
                        self.copy(self.evac_eng(), vh[:, kb0:kb0 + cnt, :],
                                  bank[:, 0:cnt * 128].rearrange("p (a d) -> p a d", a=cnt), [bb], vh.b())
                    qn = qnr.next()
                    fw.dma("pool", qn.r((slice(None), slice(0, n))), self.qnT[h, :, tok0:tok0 + n],
                           writes=qn.b())
                    if qr_pair != h // 2:
                        qr_pair = h // 2
                        qr = qrr.next()
                        fw.dma("pool", qr.r((slice(None), slice(0, n))), self.qrT[h // 2, :, tok0:tok0 + n],
                               writes=qr.b())
                    qbs = list(range(nqb)) if qb_limit is None else list(range(min(nqb, qb_limit)))
                    stbox = [None]

                    def stageA(qb, qn=qn, qr=qr, pb=pb):
                        qsl = slice(qb * 128, (qb + 1) * 128)
                        sm = small.next()
                        Pt = Pring.next()
                        for gi, (s, w) in enumerate(kgs):
                            bank, bb = self.ps[gi], self.psb[gi]
                            self.mm(bank[:, 0:w], qn.r((slice(None), qsl)), knT.r((slice(None), slice(s, s + w))),
                                    True, False, qn.b() + knT.b(), [bb], sync=False)
                            self.mm(bank[:, 0:w], qr.r((slice(pb, pb + 64), qsl)), krA.r((slice(pb, pb + 64), slice(s, s + w))),
                                    False, True, qr.b() + krA.b(), [bb], sync=True)
                            fw.op("dve", lambda bank=bank, w=w, s=s, gi=gi, Pt=Pt, sm=sm: nc.vector.tensor_scalar(
                                out=Pt[:, s:s + w], in0=bank[:, 0:w], scalar1=1.0, scalar2=None, op0=ALU.mult, op1=ALU.max,
                                accum_out=sm[:, gi:gi + 1]), reads=[bb], writes=Pt.b() + sm.b())
                        self.rmax(sm[:, 8:9], sm[:, 0:ng], sm.b(), sm.b())
                        self.ts(sm[:, 9:10], sm[:, 8:9], -scale, None, ALU.mult, None, sm.b(), sm.b())
                        Pb = Pbring.next()
                        for gi, (s, w) in enumerate(kgs):
                            self.act(Pb[:, s:s + w], Pt[:, s:s + w], AF.Exp, Pt.b() + sm.b(), Pb.b() + sm.b(),
                                     bias=sm[:, 9:10], scale=scale, accum_out=sm[:, 10 + gi:11 + gi])
                        self.rsum(sm[:, 15:16], sm[:, 10:10 + ng], sm.b(), sm.b())
                        self.recip(sm[:, 15:16], sm[:, 15:16], sm.b(), sm.b())
                        return sm, Pb

                    def stageB(qb, sm, Pt, h=h):
                        PT = PTring.next()
                        for kb0 in range(0, nkb, 4):
                            cnt = min(4, nkb - kb0)
                            tbb = self.psb[5 + (kb0 // 4) % 2]
                            tb = self.ps[5 + (kb0 // 4) % 2][:, :].bitcast(BF16)
                            for kb in range(kb0, kb0 + cnt):
                                fw.op("pe", lambda tb=tb, kb=kb, kb0=kb0, Pt=Pt: nc.tensor.transpose(
                                    out=tb[:, (kb - kb0) * 128:(kb - kb0 + 1) * 128], in_=Pt[:, kb * 128:(kb + 1) * 128],
                                    identity=identb[:]), reads=Pt.b() + identb.b(), writes=[tbb])
                            self.copy(self.evac_eng(), PT[:, kb0 * 128:(kb0 + cnt) * 128],
                                      tb[:, 0:cnt * 128], [tbb], PT.b())
                        obank, obb = self.ps[7], self.psb[7]
                        for kb in range(nkb):
                            self.mm(obank[:, 0:128], PT[:, kb * 128:(kb + 1) * 128],
                                    vh[:, kb, :], kb == 0, kb == nkb - 1, PT.b() + vh.b(), [obb])
                        Oq = Oqring.next()
                        self.ts(Oq[:, :], obank[:, 0:128], sm[:, 15:16], None, ALU.mult, None, [obb] + sm.b(), Oq.b())
                        tb, tbb = self.ps[5], self.psb[5]
                        self.tr(tb[:, 0:128], Oq[:, :], Oq.b(), [tbb])
                        if qb % 4 == 0:
                            stbox[0] = ostg.next()
                        st = stbox[0]
                        self.copy(self.evac_eng(), st[:, (qb % 4) * 128:(qb % 4 + 1) * 128], tb[:, 0:128], [tbb], st.b())
                        if qb % 4 == 3 or qb == qbs[-1]:
                            q0 = (qb // 4) * 512
                            wd = (qb % 4 + 1) * 128
                            fw.dma("sp", self.obT[h, :, tok0 + q0:tok0 + q0 + wd], st[:, 0:wd], reads=st.b())

                    nxt = stageA(qbs[0])
                    for ii, qb in enumerate(qbs):
                        cur = nxt
                        if ii + 1 < len(qbs):
                            nxt = stageA(qbs[ii + 1])
                        stageB(qb, *cur)
            fw.emit()

    def phase_pool(self, l, seqs=SEQS):
        nc, fw = self.nc, self.fw
        with contextlib.ExitStack() as es:
            invs = Tile(nc, es, "invs", [128, 4 * 2048])
            invp = Tile(nc, es, "invp", [128, 4 * 256])
            pw = Tile(nc, es, "pw", [128, 8, 256])
            upr = Ring(nc, es, "up", [128, 2064], 2)
            Ar = Ring(nc, es, "Apool", [128, 2064], 3)
            dT = [Tile(nc, es, "dT%d" % i, [128, 2048]) for i in range(2)]
            stg = Ring(nc, es, "pstg", [128, 512], 3)
            fw.dma("sp", invs[:], self.I("invc_s").rearrange("a n -> (a n)").partition_broadcast(128), writes=invs.b())
            fw.dma("sp", invp[:], self.I("invc_p").rearrange("a n -> (a n)").partition_broadcast(128), writes=invp.b())
            pw4 = pw.t[:].rearrange("p a (k m) -> p a k m", k=2).bitcast(F32R)
            fw.dma("pool", pw4, self.I("poolw")[l].rearrange("a p k m -> p a k m"), writes=pw.b())
            bi = 0
            for (tok0, n, ctx) in seqs:
                inv = invs if n == 2048 else invp
                for pg in range(4):
                    win = POOL_WINDOWS[pg]
                    left = win // 2
                    for half in range(2):
                        cc = pg * 2 + half
                        u = upr.next()
                        fw.op("dve", lambda u=u: nc.vector.memset(u[:, 0:8], 0.0), writes=u.b())
                        fw.op("dve", lambda u=u, n=n: nc.vector.memset(u[:, 8 + n:16 + n], 0.0), writes=u.b())
                        fw.dma("sp", u[:, 8:8 + n], self.uT[cc, :, tok0:tok0 + n], writes=u.b())
                        cur, L, step = u, n + 16, 1
                        while step < win:
                            nxt = Ar.next()
                            self.tt(nxt[:, 0:L - step], cur[:, 0:L - step], cur[:, step:L], ALU.add, cur.b(), nxt.b())
                            cur, L, step = nxt, L - step, step * 2
                        tmp = Ar.next()
                        self.tt(tmp[:, 0:n], cur[:, 8 - left:8 - left + n], inv[:, pg * n:(pg + 1) * n], ALU.mult,
                                cur.b() + inv.b(), tmp.b())
                        self.tt(dT[half].r((slice(None), slice(0, n))), tmp[:, 0:n], u[:, 8:8 + n], ALU.subtract,
                                tmp.b() + u.b(), dT[half].b())
                    for mh in range(2):
                        for tg in range(0, n, 512):
                            w = min(512, n - tg)
                            bi += 1
                            bank, bb = self.ps[bi % 4], self.psb[bi % 4]
                            for k in range(2):
                                self.mm(bank[:, 0:w], pw4[:, pg * 2 + mh, k, :], dT[k].r((slice(None), slice(tg, tg + w))),
                                        k == 0, k == 1, pw.b() + dT[k].b(), [bb])
                            st = stg.next()
                            col = R_PSC(l) + pg * 2 + mh
                            self.act(st[:, 0:w], bank[:, 0:w], AF.Copy, [bb] + self.vT.b(), st.b(),
                                     scale=self.vT[:, col:col + 1])
                            fw.dma("sp", self.ocT[pg * 2 + mh, :, tok0 + tg:tok0 + tg + w], st[:, 0:w], reads=st.b())
            fw.emit()

    def phase_merge(self, l, groups=None):
        nc, fw = self.nc, self.fw
        with contextlib.ExitStack() as es:
            xg = Tile(nc, es, "xg3", [128, 16 * 512])
            hT = Tile(nc, es, "hT3", [128, 16, 512], split=True)
            o3 = [Tile(nc, es, "o3_%d" % i, [128, 8, 512]) for i in range(3)]
            wring = Ring(nc, es, "w3", [128, 2048], 6)
            sgr = Ring(nc, es, "sg3", [128, 512], 2)
            tmr = Ring(nc, es, "tm3", [128, 512], 2)
            sqring = Ring(nc, es, "sq3", [128, 512], 2)
            tmpring = Ring(nc, es, "tmp3", [128, 512], 2)
            rstd = Tile(nc, es, "rstd3", [128, 512])
            xring = Ring(nc, es, "xr3", [128, 512], 3)
            self.epsb = Tile(nc, es, "epsb3", [128, 1])
            fw.op("dve", lambda: nc.vector.memset(self.epsb[:], EPS), writes=self.epsb.b())
            y3 = xg.t[:].rearrange("p (c n) -> p c n", c=16)
            srcs = [(self.oaT, "oaT"), (self.obT, "obT"), (self.ocT, "ocT")]
            mp = self.modp[l]
            it = 0
            for g in (groups or range(NGRP)):
                cd = 0 if g < 4 else 1
                gs = slice(g * G, (g + 1) * G)
                self.load_x(g, xg)
                self.norm_mod(g, l, 0, xg, hT, sqring, tmpring, rstd, self.ps[7], self.psb[7])
                for br in range(3):
                    fw.dma("pool", o3[br].r(slice(None)), srcs[br][0][:, :, gs].rearrange("c p n -> p c n"),
                           writes=o3[br].b())
                for d in range(16):
                    for br in range(3):
                        it += 1
                        w, slot = self.wload(wring, self.I("w_inG")[l, br * 16 + d], 16)
                        ga, gab = self.ps[it % 2], self.psb[it % 2]
                        for k in range(16):
                            self.mm(ga[:, :], w[:, k, :], hT.r((slice(None), k, slice(None))), k == 0, k == 15,
                                    slot.b() + hT.b(k), [gab])
                        w2, slot2 = self.wload(wring, self.I("wbr")[l, br, d], 8)
                        pr, prb = self.ps[2 + it % 2], self.psb[2 + it % 2]
                        for k in range(8):
                            self.mm(pr[:, :], w2[:, k, :], o3[br].r((slice(None), k, slice(None))), k == 0, k == 7,
                                    slot2.b() + o3[br].b(), [prb])
                        sg = sgr.next()
                        self.act(sg[:], ga[:, :], AF.Sigmoid, [gab], sg.b())
                        if br == 0:
                            self.tt(y3[:, d, :].bitcast(F32R), sg[:], pr[:, :], ALU.mult, sg.b() + [prb], xg.b())
                        else:
                            tm = tmr.next()
                            self.tt(tm[:], sg[:], pr[:, :], ALU.mult, sg.b() + [prb], tm.b())
                            out = y3[:, d, :].bitcast(F32R)
                            self.tt(out, y3[:, d, :], tm[:], ALU.add, xg.b() + tm.b(), xg.b())
                for d2 in range(16):
                    it += 1
                    w, slot = self.wload(wring, self.I("wout")[l, d2], 16)
                    bank, bb = self.ps[4 + it % 2], self.psb[4 + it % 2]
                    for k in range(16):
                        self.mm(bank[:, :], w[:, k, :], y3[:, k, :].bitcast(F32R), k == 0, k == 15, slot.b() + xg.b(), [bb])
                    xr = xring.next()
                    fw.dma("sp", xr[:], self.xT[d2, :, gs], reads=[self.b_x[g][d2]], writes=xr.b())
                    self.stt(xr[:], bank[:, :], mp[:, 2, d2:d2 + 1, cd], xr[:], ALU.mult, ALU.add,
                             [bb] + mp.b() + xr.b(), xr.b())
                    fw.dma("sp", self.xT[d2, :, gs], xr[:], reads=xr.b(), writes=[self.b_x[g][d2]])
            fw.emit()

    def phase_ffn(self, l, groups=None, final=False):
        nc, fw = self.nc, self.fw
        moe = (l % 2 == 1)
        nexp = self.moe_experts if moe else 1
        with contextlib.ExitStack() as es:
            xa = Tile(nc, es, "xa", [128, 16 * 512])
            hT = Tile(nc, es, "hT4", [128, 16, 512], split=True)
            aT = [Tile(nc, es, "aT%d" % i, [128, FB, 512], split=True) for i in range(2)]
            wgu = Ring(nc, es, "wgu", [128, 2048], 6)
            wdr = Ring(nc, es, "wdr", [128, FB * 128], 3)
            sglr = Ring(nc, es, "sgl", [128, 512], 2)
            t4r = Ring(nc, es, "t4", [128, 512], 2)
            sqring = Ring(nc, es, "sq4", [128, 512], 2)
            tmpring = Ring(nc, es, "tmp4", [128, 512], 2)
            rstd = Tile(nc, es, "rstd4", [128, 512])
            xring = Ring(nc, es, "xr4", [128, 512], 3)
            self.epsb = Tile(nc, es, "epsb4", [128, 1])
            fw.op("dve", lambda: nc.vector.memset(self.epsb[:], EPS), writes=self.epsb.b())
            if moe:
                rt = Tile(nc, es, "rt", [128, 16, 8])
                fw.dma("pool", rt.r(slice(None)), self.I("router"), writes=rt.b())
                gate = Tile(nc, es, "gate", [128, 4, 8])
                gsm = Ring(nc, es, "gsm", [128, 32], 2)
                gbr = Ring(nc, es, "gb", [128, 128], 2)
                gbcr = Ring(nc, es, "gbc", [128, 512], 2)
            a3 = xa.t[:].rearrange("p (c n) -> p c n", c=16)
            mp = self.modp[l]
            it = 0
            for g in (groups or range(NGRP)):
                cd = 0 if g < 4 else 1
                gs = slice(g * G, (g + 1) * G)
                self.load_x(g, xa)
                self.norm_mod(g, l, 1, xa, hT, sqring, tmpring, rstd, self.ps[7], self.psb[7])
                if moe:
                    for t in range(4):
                        lb, lbb = self.ps[6], self.psb[6]
                        for k in range(16):
                            self.mm(lb[:, t * 8:(t + 1) * 8], hT.r((slice(None), k, slice(t * 128, (t + 1) * 128))),
                                    rt.r((slice(None), k, slice(None))), k == 0, k == 15, hT.b(k) + rt.b(), [lbb])
                        sm = gsm.next()
                        lg = sm[:, 0:8]
                        self.copy("dve", lg, lb[:, t * 8:(t + 1) * 8], [lbb], sm.b())
                        self.rmax(sm[:, 24:25], lg, sm.b(), sm.b())
                        self.ts(sm[:, 8:16], lg, sm[:, 24:25], None, ALU.is_equal, None, sm.b(), sm.b())
                        self.stt(sm[:, 8:16], sm[:, 8:16], -1e30, lg, ALU.mult, ALU.add, sm.b(), sm.b())
                        self.rmax(sm[:, 25:26], sm[:, 8:16], sm.b(), sm.b())
                        self.ts(sm[:, 8:16], lg, sm[:, 25:26], None, ALU.is_ge, None, sm.b(), sm.b())
                        self.ts(sm[:, 26:27], sm[:, 24:25], -1.0, None, ALU.mult, None, sm.b(), sm.b())
                        self.act(sm[:, 16:24], lg, AF.Exp, sm.b(), sm.b(), bias=sm[:, 26:27], scale=1.0)
                        self.tt(sm[:, 16:24], sm[:, 16:24], sm[:, 8:16], ALU.mult, sm.b(), sm.b())
                        self.rsum(sm[:, 27:28], sm[:, 16:24], sm.b(), sm.b())
                        self.recip(sm[:, 27:28], sm[:, 27:28], sm.b(), sm.b())
                        self.ts(gate[:, t, :], sm[:, 16:24], sm[:, 27:28], None, ALU.mult, None, sm.b(), gate.b())
                first = True
                for e in range(nexp):
                    if moe:
                        gbank, gbb = self.ps[6], self.psb[6]
                        for t in range(4):
                            gb = gbr.next()
                            self.copy("dve", gb.r(slice(None)), gate[:, t, e:e + 1].to_broadcast([128, 128]), gate.b(), gb.b())
                            self.mm(gbank[:, t * 128:(t + 1) * 128], gb.r(slice(None)), self.identr.r(slice(None)), True, True,
                                    gb.b() + self.identr.b(), [gbb], sync=True)
                        gbc = gbcr.next()
                        self.copy("act", gbc[:], gbank[:, :], [gbb], gbc.b())
                        wg_d, wu_d, wd_d = self.I("moe_g")[e], self.I("moe_u")[e], self.I("moe_d")[e]
                    else:
                        wg_d, wu_d, wd_d = self.I("ffn_g"), self.I("ffn_u"), self.I("ffn_d")
                    for blk in range(NFF // FB):
                        at = aT[blk % 2]
                        for jj in range(FB):
                            j = blk * FB + jj
                            it += 1
                            wg, sg_ = self.wload(wgu, wg_d[j], 16)
                            gbk, gbkb = self.ps[it % 2], self.psb[it % 2]
                            for k in range(16):
                                self.mm(gbk[:, :], wg[:, k, :], hT.r((slice(None), k, slice(None))), k == 0, k == 15,
                                        sg_.b() + hT.b(k), [gbkb])
                            wu, su_ = self.wload(wgu, wu_d[j], 16)
                            ubk, ubkb = self.ps[2 + it % 2], self.psb[2 + it % 2]
                            for k in range(16):
                                self.mm(ubk[:, :], wu[:, k, :], hT.r((slice(None), k, slice(None))), k == 0, k == 15,
                                        su_.b() + hT.b(k), [ubkb])
                            sgl = sglr.next()
                            self.act(sgl[:], gbk[:, :], AF.Silu, [gbkb], sgl.b())
                            if moe:
                                t4 = t4r.next()
                                self.tt(t4[:], ubk[:, :], gbc[:], ALU.mult, [ubkb] + gbc.b(), t4.b())
                                self.tt(at.r((slice(None), jj, slice(None))), sgl[:], t4[:], ALU.mult, sgl.b() + t4.b(), at.b(jj))
                            else:
                                self.tt(at.r((slice(None), jj, slice(None))), sgl[:], ubk[:, :], ALU.mult, sgl.b() + [ubkb], at.b(jj))
                        for d in range(16):
                            it += 1
                            wd, sd_ = self.wload(wdr, wd_d[blk, d], FB)
                            dbk, dbkb = self.ps[4 + it % 2], self.psb[4 + it % 2]
                            for jj in range(FB):
                                self.mm(dbk[:, :], wd[:, jj, :], at.r((slice(None), jj, slice(None))), jj == 0, jj == FB - 1,
                                        sd_.b() + at.b(jj), [dbkb])
                            if first:
                                self.copy("dve", a3[:, d, :], dbk[:, :], [dbkb], xa.b())
                            else:
                                self.tt(a3[:, d, :], a3[:, d, :], dbk[:, :], ALU.add, xa.b() + [dbkb], xa.b())
                        first = False
                for d2 in range(16):
                    xr = xring.next()
                    fw.dma("sp", xr[:], self.xT[d2, :, gs], reads=[self.b_x[g][d2]], writes=xr.b())
                    self.stt(a3[:, d2, :], a3[:, d2, :], mp[:, 5, d2:d2 + 1, cd], xr[:], ALU.mult, ALU.add,
                             xa.b() + mp.b() + xr.b(), xa.b())
                    if not final:
                        fw.dma("sp", self.xT[d2, :, gs], a3[:, d2, :], reads=xa.b(), writes=[self.b_x[g][d2]])
                if final:
                    self.norm_stats(a3, xa.b(), 16, D, sqring, rstd, self.ps[7], self.psb[7])
                    for d2 in range(16):
                        xr = xring.next()
                        col = R_FG + d2
                        self.stt(xr[:], a3[:, d2, :], self.vT[:, col:col + 1], rstd[:], ALU.mult, ALU.mult,
                                 xa.b() + self.vT.b() + rstd.b(), xr.b())
                        fw.dma("sp", self.o_yT[d2, :, gs], xr[:], reads=xr.b())
            fw.emit()

    def phase_moe_sparse(self, l, final=False, slots=None, groups=None):
        nc, fw = self.nc, self.fw
        I32 = mybir.dt.int32
        mp = self.modp[l]
        NTI = NT // 128
        hTok, Ybuf, Tab = self.hTok, self.Ybuf, self.Tab
        b_hTok, b_Y, b_Tab = Buf(), Buf(), Buf()
        B3 = [128, NTI, 8]

        def tred(out, in_, op, reads, writes):
            fw.op("dve", lambda: nc.vector.tensor_reduce(out=out, in_=in_, axis=AX.X, op=op), reads=reads, writes=writes)

        with contextlib.ExitStack() as es0:
            esc = Tile(nc, es0, "esc", [128, 2 * NS])
            self.epsb = Tile(nc, es0, "epsb5", [128, 1])
            fw.op("dve", lambda: nc.vector.memset(self.epsb[:], EPS), writes=self.epsb.b())
            with contextlib.ExitStack() as es:
                xa = Tile(nc, es, "xa5", [128, 16 * 512])
                hT = Tile(nc, es, "hT5", [128, 16, 512], split=True)
                sqring = Ring(nc, es, "sq5", [128, 512], 2)
                tmpring = Ring(nc, es, "tmp5", [128, 512], 2)
                rstd = Tile(nc, es, "rstd5", [128, 512])
                hst = Ring(nc, es, "hst5", [128, 2048], 2)
                rt = Tile(nc, es, "rt5", [128, 16, 8])
                Um = Tile(nc, es, "Um", [128, 128])
                Lg = Tile(nc, es, "Lg", B3)
                eq1 = Tile(nc, es, "eq1", B3)
                sel = Tile(nc, es, "sel", B3)
                wk = Tile(nc, es, "wk", B3)
                wk2 = Tile(nc, es, "wk2", B3)
                gt = Tile(nc, es, "gt", B3)
                pos = Tile(nc, es, "pos", B3)
                tot = Tile(nc, es, "tot", B3)
                offs = Tile(nc, es, "offs", B3)
                m1 = Tile(nc, es, "m1", [128, NTI, 1])
                m2 = Tile(nc, es, "m2", [128, NTI, 1])
                sm = Tile(nc, es, "sm5", [128, 96])
                tokid = Tile(nc, es, "tokid", [128, NTI])
                svals = Tile(nc, es, "svals", [128, NS])
                rr = Tile(nc, es, "rr", [128, 2 * NTI])
                zero = Tile(nc, es, "zero5", [128, 2048])
                recs_t = es.enter_context(nc.sbuf_tensor("sb_recs", [128, 2 * NTI, 16], I32))
                ridx_t = es.enter_context(nc.sbuf_tensor("sb_ridx", [128, 2 * NTI], I32))
                b_recs, b_ridx = Buf(), Buf()
                fw.dma("pool", rt.r(slice(None)), self.I("router"), writes=rt.b())
                fw.dma("pool", Um.r(slice(None)), self.I("Umat"), writes=Um.b())
                fw.dma("sp", tokid[:], self.I("tokid"), writes=tokid.b())
                fw.dma("sp", svals[:], self.I("svals"), writes=svals.b())
                fw.dma("sp", Tab[:, :], self.I("Tab0", I32), writes=[b_Tab])
                fw.op("dve", lambda: nc.vector.memset(zero[:], 0.0), writes=zero.b())
                fw.op("dve", lambda: nc.vector.memset(recs_t[:], 0), writes=[b_recs])
                fw.dma("sp", hTok[NT:NT + 128, :], zero[:], reads=zero.b(), writes=[b_hTok])
                for g in range(NGRP):
                    self.load_x(g, xa)
                    self.norm_mod(g, l, 1, xa, hT, sqring, tmpring, rstd, self.ps[7], self.psb[7])
                    for t in range(4):
                        lb, lbb = self.ps[6], self.psb[6]
                        for k in range(16):
                            self.mm(lb[:, t * 8:(t + 1) * 8], hT.r((slice(None), k, slice(t * 128, (t + 1) * 128))),
                                    rt.r((slice(None), k, slice(None))), k == 0, k == 15, hT.b(k) + rt.b(), [lbb])
                        self.copy("dve", Lg[:, g * 4 + t, :], lb[:, t * 8:(t + 1) * 8], [lbb], Lg.b())
                        hs = hst.next()
                        for c4 in range(4):
                            tb, tbb = self.ps[c4], self.psb[c4]
                            for ci in range(4):
                                c = c4 * 4 + ci
                                self.tr(tb[:, ci * 128:(ci + 1) * 128], hT[:, c, t * 128:(t + 1) * 128], hT.b(c), [tbb])
                            self.copy(self.evac_eng(), hs[:, c4 * 512:(c4 + 1) * 512], tb[:, :], [tbb], hs.b())
                        r0 = g * 512 + t * 128
                        fw.dma("sp", hTok[r0:r0 + 128, :], hs[:], reads=hs.b(), writes=[b_hTok])
                tred(m1[:], Lg[:], ALU.max, Lg.b(), m1.b())
                self.tt(eq1[:], Lg[:], m1[:].to_broadcast(B3), ALU.is_equal, Lg.b() + m1.b(), eq1.b())
                self.stt(wk[:], eq1[:], -1e30, Lg[:], ALU.mult, ALU.add, eq1.b() + Lg.b(), wk.b())
                tred(m2[:], wk[:], ALU.max, wk.b(), m2.b())
                self.tt(sel.r(slice(None)), Lg[:], m2[:].to_broadcast(B3), ALU.is_ge, Lg.b() + m2.b(), sel.b())
                self.tt(wk[:], Lg[:], m1[:].to_broadcast(B3), ALU.subtract, Lg.b() + m1.b(), wk.b())
                self.act(wk[:], wk[:], AF.Exp, wk.b(), wk.b())
                self.tt(wk[:], wk[:], sel[:], ALU.mult, wk.b() + sel.b(), wk.b())
                tred(m2[:], wk[:], ALU.add, wk.b(), m2.b())
                self.recip(m2[:], m2[:], m2.b(), m2.b())
                self.tt(gt[:], wk[:], m2[:].to_broadcast(B3), ALU.mult, wk.b() + m2.b(), gt.b())
                pb_, pbb_ = self.ps[0], self.psb[0]
                tb_, tbb_ = self.ps[1], self.psb[1]
                for t in range(NTI):
                    self.mm(pb_[:, t * 8:(t + 1) * 8], Um.r(slice(None)), sel.r((slice(None), t, slice(None))), True, True,
                            Um.b() + sel.b(), [pbb_], sync=(t == NTI - 1))
                for t in range(NTI):
                    self.mm(tb_[:, t * 8:(t + 1) * 8], self.ones.r(slice(None)), sel.r((slice(None), t, slice(None))), True, True,
                            self.ones.b() + sel.b(), [tbb_], sync=(t == NTI - 1))
                self.copy("dve", pos.t[:].rearrange("p a e -> p (a e)"), pb_[:, 0:NTI * 8], [pbb_], pos.b())
                self.copy("act", tot.t[:].rearrange("p a e -> p (a e)"), tb_[:, 0:NTI * 8], [tbb_], tot.b())
                fw.op("dve", lambda: nc.vector.memset(offs[:, 0, :], 0.0), writes=offs.b())
                for t in range(1, NTI):
                    self.tt(offs[:, t, :], offs[:, t - 1, :], tot[:, t - 1, :], ALU.add, offs.b() + tot.b(), offs.b())
                self.tt(pos[:], pos[:], offs[:], ALU.add, pos.b() + offs.b(), pos.b())
                cnt, nsl, tmp8 = sm[:, 0:8], sm[:, 8:16], sm[:, 24:32]
                self.tt(cnt, offs[:, NTI - 1, :], tot[:, NTI - 1, :], ALU.add, offs.b() + tot.b(), sm.b())
                self.ts(nsl, cnt, 0.0, None, ALU.is_gt, None, sm.b(), sm.b())
                for kk in range(1, 5):
                    self.ts(tmp8, cnt, float(512 * kk), None, ALU.is_gt, None, sm.b(), sm.b())
                    self.tt(nsl, nsl, tmp8, ALU.add, sm.b(), sm.b())
                fw.op("dve", lambda: nc.vector.memset(sm[:, 16:17], 0.0), writes=sm.b())
                for e in range(1, NEXP):
                    self.tt(sm[:, 16 + e:17 + e], sm[:, 15 + e:16 + e], sm[:, 7 + e:8 + e], ALU.add, sm.b(), sm.b())
                self.ts(sm[:, 32:40], sm[:, 16:24], 512.0, None, ALU.mult, None, sm.b(), sm.b())
                self.tt(pos[:], pos[:], sm[:, 32:40].rearrange("p (a e) -> p a e", a=1).to_broadcast(B3), ALU.add,
                        pos.b() + sm.b(), pos.b())
                self.tt(wk2[:], sel[:], eq1[:], ALU.subtract, sel.b() + eq1.b(), wk2.b())
                rv = rr.t[:].rearrange("p (k a) -> p k a", k=2)
                recs_f = recs_t[:].bitcast(F32)
                for k_, oh in ((0, eq1), (1, wk2)):
                    ks = slice(k_ * NTI, (k_ + 1) * NTI)
                    self.tt(wk[:], pos[:], oh[:], ALU.mult, pos.b() + oh.b(), wk.b())
                    tred(rv[:, k_, :], wk[:], ALU.add, wk.b(), rr.b())
                    fw.op("dve", lambda ks=ks: nc.vector.tensor_copy(out=recs_t[:, ks, 0], in_=tokid[:]),
                          reads=tokid.b(), writes=[b_recs])
                    fw.op("dve", lambda ks=ks, k_=k_: nc.vector.tensor_scalar(out=recs_t[:, ks, 1], in0=tokid[:],
                                                                             scalar1=float(k_ * NT), scalar2=None, op0=ALU.add),
                          reads=tokid.b(), writes=[b_recs])
                    self.tt(wk[:], gt[:], oh[:], ALU.mult, gt.b() + oh.b(), wk.b())
                    tred(recs_f[:, ks, 2], wk[:], ALU.add, wk.b(), [b_recs])
                fw.op("dve", lambda: nc.vector.tensor_copy(out=ridx_t[:], in_=rr[:]), reads=rr.b(), writes=[b_ridx])
                for i in range(2 * NTI):
                    fw.dma_custom("pool", lambda i=i: nc.gpsimd.indirect_dma_start(
                        out=Tab[:, :], out_offset=bass.IndirectOffsetOnAxis(ap=ridx_t[:, i:i + 1], axis=0),
                        in_=recs_t[:, i, :], in_offset=None), reads=[b_recs, b_ridx], writes=[b_Tab])
                ev = sm[:, 40:40 + NS]
                tmpS = sm[:, 60:60 + NS]
                fw.op("dve", lambda: nc.vector.memset(ev, -1.0), writes=sm.b())
                for e in range(NEXP):
                    self.ts(tmpS, svals[:], sm[:, 16 + e:17 + e], None, ALU.is_ge, None, svals.b() + sm.b(), sm.b())
                    self.tt(ev, ev, tmpS, ALU.add, sm.b(), sm.b())
                self.ts(esc[:, 0:NS], ev, float(NFF * 128), None, ALU.mult, None, sm.b(), esc.b())
                self.ts(esc[:, NS:2 * NS], ev, float(64 * 128), None, ALU.mult, None, sm.b(), esc.b())
                fw.emit()
            with contextlib.ExitStack() as es:
                hTs = Tile(nc, es, "hTs", [128, 16, 512], split=True)
                acc = Tile(nc, es, "acc5", [128, 16 * 512])
                aT = [Tile(nc, es, "aT5_%d" % i, [128, FB, 512], split=True) for i in range(2)]
                wgu = Ring(nc, es, "wgu5", [128, 2048], 4)
                wdr = Ring(nc, es, "wdr5", [128, FB * 128], 3)
                sglr = Ring(nc, es, "sgl5", [128, 512], 2)
                gth = Ring(nc, es, "gth", [128, 2048], 2)
                otl = Ring(nc, es, "otl", [128, 2048], 2)
                wbase = Tile(nc, es, "wbase", [128, NFF])
                wdbase = Tile(nc, es, "wdbase", [128, 64])
                widx_t = [es.enter_context(nc.sbuf_tensor("sb_widx%d" % i, [128, NFF + 64], I32)) for i in range(2)]
                b_widx = [Buf(), Buf()]
                rbs_t = [es.enter_context(nc.sbuf_tensor("sb_rbs%d" % i, [128, 4, 16], I32)) for i in range(2)]
                b_rbs = [Buf(), Buf()]
                fw.dma("sp", wbase[:], self.I("wbase"), writes=wbase.b())
                fw.dma("sp", wdbase[:], self.I("wdbase"), writes=wdbase.b())
                a3 = acc.t[:].rearrange("p (c n) -> p c n", c=16)
                wg_rows = self.I("moe_g").rearrange("e j p k m -> (e j p) (k m)")
                wu_rows = self.I("moe_u").rearrange("e j p k m -> (e j p) (k m)")
                wd_rows = self.I("moe_d").rearrange("e b d p j m -> (e b d p) (j m)")
                it = 0
                for s in (slots if slots is not None else range(NS)):
                    widx, bw = widx_t[s % 2], b_widx[s % 2]
                    rbs, brb = rbs_t[s % 2], b_rbs[s % 2]
                    fw.op("dve", lambda widx=widx, s=s: nc.vector.tensor_scalar(
                        out=widx[:, 0:NFF], in0=wbase[:], scalar1=esc[:, s:s + 1], scalar2=None, op0=ALU.add),
                        reads=wbase.b() + esc.b(), writes=[bw])
                    fw.op("dve", lambda widx=widx, s=s: nc.vector.tensor_scalar(
                        out=widx[:, NFF:NFF + 64], in0=wdbase[:], scalar1=esc[:, NS + s:NS + s + 1], scalar2=None, op0=ALU.add),
                        reads=wdbase.b() + esc.b(), writes=[bw])
                    fw.dma("sp", rbs[:], Tab[s * 512:(s + 1) * 512, :].rearrange("(q p) c -> p q c", p=128),
                           reads=[b_Tab], writes=[brb])
                    for q in range(4):
                        gtile = gth.next()
                        fw.dma_custom("pool", lambda gtile=gtile, rbs=rbs, q=q: nc.gpsimd.indirect_dma_start(
                            out=gtile[:], out_offset=None, in_=hTok[:, :],
                            in_offset=bass.IndirectOffsetOnAxis(ap=rbs[:, q, 0:1], axis=0)),
                            reads=[brb, b_hTok], writes=gtile.b())
                        for c4 in range(4):
                            tb, tbb = self.ps[6 + c4 % 2], self.psb[6 + c4 % 2]
                            for ci in range(4):
                                c = c4 * 4 + ci
                                self.tr(tb[:, ci * 128:(ci + 1) * 128], gtile[:, c * 128:(c + 1) * 128], gtile.b(), [tbb])
                            self.copy(self.evac_eng(), hTs.r((slice(None), slice(c4 * 4, c4 * 4 + 4), slice(q * 128, (q + 1) * 128))),
                                      tb[:, :].rearrange("p (a n) -> p a n", a=4), [tbb],
                                      hTs.b(c4 * 4) + hTs.b(c4 * 4 + 1) + hTs.b(c4 * 4 + 2) + hTs.b(c4 * 4 + 3))

                    def wgather(ring, rows, col, nelem, widx=widx, bw=bw):
                        slot = ring.next()
                        dst = slot.t[:, 0:nelem].bitcast(F32R)
                        fw.dma_custom("pool", lambda: nc.gpsimd.indirect_dma_start(
                            out=dst, out_offset=None, in_=rows,
                            in_offset=bass.IndirectOffsetOnAxis(ap=widx[:, col:col + 1], axis=0)),
                            reads=[bw], writes=slot.b())
                        return slot
                    for blk in range(NFF // FB):
                        at = aT[blk % 2]
                        for jj in range(FB):
                            j = blk * FB + jj
                            it += 1
                            sg_ = wgather(wgu, wg_rows, j, 2048)
                            wg = sg_.t[:].rearrange("p (k m) -> p k m", k=16).bitcast(F32R)
                            gbk, gbkb = self.ps[it % 2], self.psb[it % 2]
                            for k in range(16):
                                self.mm(gbk[:, :], wg[:, k, :], hTs.r((slice(None), k, slice(None))), k == 0, k == 15,
                                        sg_.b() + hTs.b(k), [gbkb])
                            su_ = wgather(wgu, wu_rows, j, 2048)
                            wu = su_.t[:].rearrange("p (k m) -> p k m", k=16).bitcast(F32R)
                            ubk, ubkb = self.ps[2 + it % 2], self.psb[2 + it % 2]
                            for k in range(16):
                                self.mm(ubk[:, :], wu[:, k, :], hTs.r((slice(None), k, slice(None))), k == 0, k == 15,
                                        su_.b() + hTs.b(k), [ubkb])
                            sgl = sglr.next()
                            self.act(sgl[:], gbk[:, :], AF.Silu, [gbkb], sgl.b())
                            self.tt(at.r((slice(None), jj, slice(None))), sgl[:], ubk[:, :], ALU.mult, sgl.b() + [ubkb], at.b(jj))
                        for d in range(16):
                            it += 1
                            sd_ = wgather(wdr, wd_rows, NFF + blk * 16 + d, FB * 128)
                            wd = sd_.t[:].rearrange("p (k m) -> p k m", k=FB).bitcast(F32R)
                            dbk, dbkb = self.ps[4 + it % 2], self.psb[4 + it % 2]
                            for jj in range(FB):
                                self.mm(dbk[:, :], wd[:, jj, :], at.r((slice(None), jj, slice(None))), jj == 0, jj == FB - 1,
                                        sd_.b() + at.b(jj), [dbkb])
                            if blk == 0:
                                self.copy("dve", a3[:, d, :], dbk[:, :], [dbkb], acc.b())
                            else:
                                self.tt(a3[:, d, :], a3[:, d, :], dbk[:, :], ALU.add, acc.b() + [dbkb], acc.b())
                    rbs_f = rbs[:].bitcast(F32)
                    for q in range(4):
                        ot = otl.next()
                        for c4 in range(4):
                            tb, tbb = self.ps[6 + c4 % 2], self.psb[6 + c4 % 2]
                            for ci in range(4):
                                c = c4 * 4 + ci
                                self.tr(tb[:, ci * 128:(ci + 1) * 128], a3[:, c, q * 128:(q + 1) * 128], acc.b(), [tbb])
                            self.ts(ot[:, c4 * 512:(c4 + 1) * 512], tb[:, :], rbs_f[:, q, 2:3], None, ALU.mult, None,
                                    [tbb, brb], ot.b())
                        fw.dma_custom("pool", lambda ot=ot, rbs=rbs, q=q: nc.gpsimd.indirect_dma_start(
                            out=Ybuf[:, :], out_offset=bass.IndirectOffsetOnAxis(ap=rbs[:, q, 1:2], axis=0),
                            in_=ot[:], in_offset=None), reads=ot.b() + [brb], writes=[b_Y])
                fw.emit()
            with contextlib.ExitStack() as es:
                ys = Ring(nc, es, "ys", [128, 2048], 4)
                y2 = Ring(nc, es, "y2", [128, 2048], 2)
                xg = Tile(nc, es, "xg6", [128, 16 * 512])
                sqring = Ring(nc, es, "sq6", [128, 512], 2)
                rstd = Tile(nc, es, "rstd6", [128, 512])
                xring = Ring(nc, es, "xr6", [128, 512], 3)
                x3 = xg.t[:].rearrange("p (c n) -> p c n", c=16)
                for g in (groups or range(NGRP)):
                    cd = 0 if g < 4 else 1
                    gs = slice(g * G, (g + 1) * G)
                    self.load_x(g, xg)
                    yt = []
                    for t in range(4):
                        r0 = g * 512 + t * 128
                        a, b2 = ys.next(), y2.next()
                        fw.dma("sp", a[:], Ybuf[r0:r0 + 128, :], reads=[b_Y], writes=a.b())
                        fw.dma("sp", b2[:], Ybuf[NT + r0:NT + r0 + 128, :], reads=[b_Y], writes=b2.b())
                        self.tt(a[:], a[:], b2[:], ALU.add, a.b() + b2.b(), a.b(), en="pool" if t % 2 else "dve")
                        yt.append(a)
                    for c in range(16):
                        tb, tbb = self.ps[c % 4], self.psb[c % 4]
                        for t in range(4):
                            self.tr(tb[:, t * 128:(t + 1) * 128], yt[t][:, c * 128:(c + 1) * 128], yt[t].b(), [tbb])
                        self.stt(x3[:, c, :], tb[:, :], mp[:, 5, c:c + 1, cd], x3[:, c, :], ALU.mult, ALU.add,
                                 [tbb] + mp.b() + xg.b(), xg.b())
                        if not final:
                            fw.dma("sp", self.xT[c, :, gs], x3[:, c, :], reads=xg.b(), writes=[self.b_x[g][c]])
                    if final:
                        self.norm_stats(x3, xg.b(), 16, D, sqring, rstd, self.ps[7], self.psb[7])
                        for d2 in range(16):
                            xr = xring.next()
                            col = R_FG + d2
                            self.stt(xr[:], x3[:, d2, :], self.vT[:, col:col + 1], rstd[:], ALU.mult, ALU.mult,
                                     xg.b() + self.vT.b() + rstd.b(), xr.b())
                            fw.dma("sp", self.o_yT[d2, :, gs], xr[:], reads=xr.b())
                fw.emit()


def _tiles(W):
    K, M = W.shape
    return np.ascontiguousarray(W.reshape(K // 128, 128, M // 128, 128).transpose(2, 1, 0, 3))


def host_consts():
    c = {}
    c["ident"] = np.eye(128, dtype=np.float32)
    p = np.arange(128)
    d = p % 64
    partner = np.where((d % 32) < 16, p + 16, p - 16)
    pm = np.zeros((128, 128), np.float32)
    pm[partner, p] = 1.0
    c["permM"] = pm
    quarter = 16
    inv = (np.float32(10000.0) ** (-np.arange(quarter, dtype=np.float32) / np.float32(quarter))).astype(np.float32)
    n = np.arange(2048)
    rr = (n // 64).astype(np.float32)
    cc = (n % 64).astype(np.float32)
    ang_r = (rr[:, None] * inv[None, :]).astype(np.float32)
    ang_c = (cc[:, None] * inv[None, :]).astype(np.float32)
    C = np.zeros((128, 2048), np.float32)
    S = np.zeros((128, 2048), np.float32)
    for pp in range(128):
        dd = pp % 64
        j = dd % 16
        ang = ang_r[:, j] if dd < 32 else ang_c[:, j]
        C[pp] = np.cos(ang)
        sgn = -1.0 if (dd % 32) < 16 else 1.0
        S[pp] = sgn * np.sin(ang)
    c["ropeC"] = C
    c["ropeS"] = S
    r = np.arange(128)[:, None]
    cidx = np.arange(128)[None, :]
    m = np.zeros((128, 384), np.float32)
    m[:, 0:128] = np.where(cidx >= r, 0.0, -1e30)
    m[:, 256:384] = np.where(cidx <= r, 0.0, -1e30)
    c["maskA"] = m
    c["Umat"] = np.triu(np.ones((128, 128), np.float32), k=1)
    pp = np.arange(128, dtype=np.float32)[:, None]
    c["tokid"] = (np.arange(NT // 128, dtype=np.float32)[None, :] * 128 + pp).astype(np.float32)
    c["svals"] = np.tile(np.arange(NS, dtype=np.float32)[None, :], (128, 1))
    c["wbase"] = (np.arange(NFF, dtype=np.float32)[None, :] * 128 + pp).astype(np.float32)
    c["wdbase"] = (np.arange(64, dtype=np.float32)[None, :] * 128 + pp).astype(np.float32)
    tab0 = np.zeros((NS * 512, 16), np.int32)
    tab0[:, 0] = NT
    tab0[:, 1] = 2 * NT + (np.arange(NS * 512) % 128)
    c["Tab0"] = tab0
    for nm, nseq in (("invc_s", 2048), ("invc_p", 256)):
        t = np.arange(nseq)
        tab = np.zeros((4, nseq), np.float32)
        for gi, win in enumerate(POOL_WINDOWS):
            left = win // 2
            right = win - left - 1
            lo = np.maximum(t - left, 0)
            hi = np.minimum(t + right, nseq - 1) + 1
            tab[gi] = (1.0 / (hi - lo).astype(np.float32)).astype(np.float32)
        c[nm] = tab
    return c


def prep_shared(inp):
    sh = dict(host_consts())
    w_in = inp["w_in"]
    colsA = np.concatenate([
        np.arange(0, 1024),
        np.arange(1024, 1088), np.arange(1024, 1088),
        np.arange(1088, 1152), np.arange(1088, 1152),
        np.arange(1152, 1280),
        np.arange(1280, 1792),
        np.arange(1792, 2048),
        np.arange(2048, 2112), np.arange(2048, 2112),
        np.arange(2112, 3136)])
    sh["w_ada"] = np.stack([_tiles(inp["w_ada"][l]) for l in range(DEPTH)])
    sh["w_inA"] = np.stack([_tiles(w_in[l][:, colsA]) for l in range(DEPTH)])
    sh["w_inG"] = np.stack([_tiles(w_in[l][:, 3136:]) for l in range(DEPTH)])
    cq = [np.arange(h * 192, h * 192 + 128) for h in range(8)]
    for j in range(4):
        cq.append(np.concatenate([np.arange((2 * j) * 192 + 128, (2 * j) * 192 + 192),
                                  np.arange((2 * j + 1) * 192 + 128, (2 * j + 1) * 192 + 192)]))
    cq = np.concatenate(cq)
    sh["w_uq"] = np.stack([_tiles(inp["w_uq"][l][:, cq]) for l in range(DEPTH)])
    ckv = np.concatenate([np.arange(h * 256, h * 256 + 128) for h in range(8)] +
                         [np.arange(h * 256 + 128, h * 256 + 256) for h in range(8)])
    sh["w_ukv"] = np.stack([_tiles(inp["w_ukv"][l][:, ckv]) for l in range(DEPTH)])
    sh["poolw"] = np.stack([np.concatenate([_tiles(inp["pool_w"][l][g]) for g in range(4)]) for l in range(DEPTH)])
    sh["wbr"] = np.stack([np.stack([_tiles(inp[k][l]) for k in ("w_branch_a", "w_branch_b", "w_branch_c")])
                          for l in range(DEPTH)])
    sh["wout"] = np.stack([_tiles(inp["w_out"][l]) for l in range(DEPTH)])
    sh["ffn_g"] = _tiles(inp["ffn_w_gate"][0])
    sh["ffn_u"] = _tiles(inp["ffn_w_up"][0])

    def dtiles(wd):
        return np.ascontiguousarray(wd.reshape(4, FB, 128, 16, 128).transpose(0, 3, 2, 1, 4))
    sh["ffn_d"] = dtiles(inp["ffn_w_down"][0])
    sh["router"] = np.ascontiguousarray(inp["router_w"][0].reshape(16, 128, 8).transpose(1, 0, 2))
    sh["moe_g"] = np.stack([_tiles(inp["moe_w_gate"][0][e]) for e in range(NEXP)])
    sh["moe_u"] = np.stack([_tiles(inp["moe_w_up"][0][e]) for e in range(NEXP)])
    sh["moe_d"] = np.stack([dtiles(inp["moe_w_down"][0][e]) for e in range(NEXP)])
    sh["sink"] = np.ascontiguousarray(inp["attn_sink"])
    return sh


def prep_core(inp, i):
    m = {}
    x = np.concatenate([inp["x_sample"][i], inp["x_prompt"][2 * i], inp["x_prompt"][2 * i + 1]], axis=0)
    m["xinT"] = np.ascontiguousarray(x.T.reshape(16, 128, NT))
    v = np.zeros((NROWS, 128), np.float32)
    v[R_C:R_C + 16] = inp["c"][i].reshape(16, 128)
    v[R_CCTX:R_CCTX + 16] = inp["c_ctx"].reshape(16, 128)
    for l in range(DEPTH):
        v[R_LN1(l):R_LN1(l) + 16] = inp["ln1_g"][l].reshape(16, 128)
        v[R_LN2(l):R_LN2(l) + 16] = inp["ln2_g"][l].reshape(16, 128)
        v[R_BADA(l):R_BADA(l) + 96] = inp["b_ada"][l].reshape(96, 128)
        v[R_QG(l):R_QG(l) + 4] = inp["mla_q_norm_g"][l].reshape(4, 128)
        v[R_KVG(l):R_KVG(l) + 2] = inp["mla_kv_norm_g"][l].reshape(2, 128)
        v[R_PSC(l):R_PSC(l) + 8] = inp["pool_scale"][l].reshape(8, 128)
    v[R_FG:R_FG + 16] = inp["final_g"].reshape(16, 128)
    m["vecs"] = v
    ck = inp["cache_attn_k"][i]
    ckT = ck.transpose(0, 2, 3, 1)
    m["ck2T"] = np.ascontiguousarray(np.concatenate([ckT, ckT], axis=2))
    m["cv"] = np.ascontiguousarray(inp["cache_attn_v"][i].reshape(DEPTH, 512, 128))
    m["cckvT"] = np.ascontiguousarray(inp["cache_mla_ckv"][i].transpose(0, 2, 1).reshape(DEPTH, 2, 128, 512))
    krT = inp["cache_mla_krope"][i].transpose(0, 2, 1)
    m["ckr2T"] = np.ascontiguousarray(np.concatenate([krT, krT], axis=1))
    return m


def build_program():
    P = Prog()
    P.alloc_persistent()
    P.phase_prologue()
    for l in range(DEPTH):
        P.phase_proj(l)
        P.phase_attnA(l)
        P.phase_mla(l)
        P.phase_pool(l)
        P.phase_merge(l)
        if l % 2 == 1:
            P.phase_moe_sparse(l, final=(l == DEPTH - 1))
        else:
            P.phase_ffn(l, final=(l == DEPTH - 1))
    return P


def kernel(**inputs):
    inp = {k: np.asarray(v, dtype=np.float32) for k, v in inputs.items()}
    P = build_program()
    sh = prep_shared(inp)
    in_maps = []
    for i in range(NCORES):
        m = prep_core(inp, i)
        m.update(sh)
        in_maps.append({k: m[k] for k in P.din})
    res = run_bass_kernel_spmd(P.nc, in_maps, core_ids=list(range(NCORES)))
    y_prompt = np.zeros((16, 256, D), np.float32)
    y_sample = np.zeros((8, 2048, D), np.float32)
    st_k = np.zeros((16, DEPTH, 256, 2, 64), np.float32)
    st_v = np.zeros((16, DEPTH, 256, 2, 64), np.float32)
    st_ckv = np.zeros((16, DEPTH, 256, 256), np.float32)
    st_kr = np.zeros((16, DEPTH, 256, 64), np.float32)
    for i in range(NCORES):
        r = res.results[i]
        y = np.asarray(r["o_yT"]).reshape(D, NT).T
        y_sample[i] = y[0:2048]
        okT = np.asarray(r["o_kT"])
        ov = np.asarray(r["o_v"]).reshape(DEPTH, 512, 128)
        ockv = np.asarray(r["o_ckvT"]).reshape(DEPTH, 256, 512)
        okr = np.asarray(r["o_krT"])
        for j in range(2):
            b = 2 * i + j
            ts = slice(j * 256, (j + 1) * 256)
            y_prompt[b] = y[2048 + j * 256:2048 + (j + 1) * 256]
            st_k[b] = okT[:, :, :, ts].transpose(0, 3, 1, 2)
            st_v[b] = ov[:, ts, :].reshape(DEPTH, 256, 2, 64)
            st_ckv[b] = ockv[:, :, ts].transpose(0, 2, 1)
            st_kr[b] = okr[:, :, ts].transpose(0, 2, 1)
    return (y_prompt, y_sample, st_k, st_v, st_ckv, st_kr)
```

```python
import contextlib
import numpy as np
import concourse.bass as bass
import concourse.mybir as mybir
from concourse.bass_utils import run_bass_kernel_spmd

F32 = mybir.dt.float32
F32R = mybir.dt.float32r
BF16 = mybir.dt.bfloat16
AF = mybir.ActivationFunctionType
ALU = mybir.AluOpType
AX = mybir.AxisListType

NCORES = 8
D = 2048
NT = 2560
G = 512
NGRP = 5
DEPTH = 2
EPS = 1e-6
NFF = 44
FB = 11
NEXP = 8
NS = 17
DBG_GROUPS = None
SEQS = ((0, 2048, True), (2048, 256, False), (2304, 256, False))
POOL_WINDOWS = (2, 4, 8, 16)

R_C, R_CCTX = 0, 16


def R_LN1(l): return 32 + l * 142
def R_LN2(l): return 32 + l * 142 + 16
def R_BADA(l): return 32 + l * 142 + 32
def R_QG(l): return 32 + l * 142 + 128
def R_KVG(l): return 32 + l * 142 + 132
def R_PSC(l): return 32 + l * 142 + 134


R_FG = 32 + 2 * 142
NROWS = 384


class Buf:
    __slots__ = ("w", "r")

    def __init__(self):
        self.w = None
        self.r = {}


class Eng:
    def __init__(self, name, eng, sem):
        self.name = name
        self.eng = eng
        self.sem = sem
        self.cnt = 0
        self.seen = {}
        self.dsems = []
        self.dvals = []
        self.dn = 0
        self.pend = []
        self.prog = []


class FW:
    def __init__(self, nc, es, ndma_sems=8):
        self.nc = nc
        self.engs = {}
        for name, eng in (("pe", nc.tensor), ("act", nc.scalar), ("dve", nc.vector),
                          ("pool", nc.gpsimd), ("sp", nc.sync)):
            sem = es.enter_context(nc.semaphore("s_" + name))
            self.engs[name] = Eng(name, eng, sem)
        for qn in ("sp", "pool"):
            e = self.engs[qn]
            for i in range(ndma_sems):
                e.dsems.append(es.enter_context(nc.semaphore("d_%s%d" % (qn, i))))
                e.dvals.append(0)
        self.ninst = 0

    def _wait(self, e, ticket):
        sem, val, owner = ticket
        if owner is e:
            if e.name == "pe" or val > e.cnt:
                return
        key = id(sem)
        if e.seen.get(key, 0) >= val:
            return
        e.pend.append((sem, val))
        e.seen[key] = val
        self.ninst += 1

    def _deps(self, e, reads, writes):
        for b in reads:
            if b.w is not None:
                self._wait(e, b.w)
        for b in writes:
            if b.w is not None:
                self._wait(e, b.w)
            for t in b.r.values():
                self._wait(e, t)

    def _mark(self, e, ticket, reads, writes, key=None):
        for b in reads:
            b.r[key or e.name] = ticket
        for b in writes:
            b.w = ticket
            b.r = {}

    def op(self, en, fn, reads=(), writes=(), sync=True):
        e = self.engs[en]
        self._deps(e, reads, writes)
        self.ninst += 1
        waits, e.pend = e.pend, []
        if sync:
            e.cnt += 1
            e.prog.append((waits, fn, e.sem, 1))
            t = (e.sem, e.cnt, e)
        else:
            e.prog.append((waits, fn, None, 0))
            t = (e.sem, e.cnt + 1, e)
        self._mark(e, t, reads, writes)

    def dma(self, qn, out, in_, reads=(), writes=(), **kw):
        e = self.engs[qn]
        slot = e.dn % len(e.dsems)
        e.dn += 1
        sem = e.dsems[slot]
        if e.dvals[slot] > 0:
            self._wait(e, (sem, e.dvals[slot], None))
        self._deps(e, reads, writes)
        e.dvals[slot] += 16
        eng = e.eng
        waits, e.pend = e.pend, []
        e.prog.append((waits, (lambda: eng.dma_start(out=out, in_=in_, **kw)), sem, 16))
        self.ninst += 1
        self._mark(e, (sem, e.dvals[slot], None), reads, writes, key=(e.name, slot))

    def dma_custom(self, qn, fn, reads=(), writes=()):
        e = self.engs[qn]
        slot = e.dn % len(e.dsems)
        e.dn += 1
        sem = e.dsems[slot]
        if e.dvals[slot] > 0:
            self._wait(e, (sem, e.dvals[slot], None))
        self._deps(e, reads, writes)
        e.dvals[slot] += 16
        waits, e.pend = e.pend, []
        e.prog.append((waits, fn, sem, 16))
        self.ninst += 1
        self._mark(e, (sem, e.dvals[slot], None), reads, writes, key=(e.name, slot))

    def emit(self):
        sp = self.engs["sp"]
        for qn in ("sp", "pool"):
            q = self.engs[qn]
            for s, v in zip(q.dsems, q.dvals):
                if v > 0:
                    self._wait(sp, (s, v, None))
        for en in ("pe", "act", "dve", "pool"):
            e = self.engs[en]
            if e.cnt > 0:
                self._wait(sp, (e.sem, e.cnt, None))

        def replay(e):
            eng = e.eng
            for waits, fn, sem, inc in e.prog:
                for s, v in waits:
                    eng.wait_ge(s, v)
                ins = fn()
                if sem is not None:
                    ins.then_inc(sem, inc)
            for s, v in e.pend:
                eng.wait_ge(s, v)
            e.prog = []
            e.pend = []

        with self.nc.Block() as block:
            @block.sync
            def _(x):
                replay(self.engs["sp"])

            @block.tensor
            def _(x):
                replay(self.engs["pe"])

            @block.scalar
            def _(x):
                replay(self.engs["act"])

            @block.vector
            def _(x):
                replay(self.engs["dve"])

            @block.gpsimd
            def _(x):
                replay(self.engs["pool"])


class Tile:
    _n = [0]

    def __init__(self, nc, es, name, shape, split=False, dt=None):
        Tile._n[0] += 1
        self.t = es.enter_context(nc.sbuf_tensor("sb%d_%s" % (Tile._n[0], name), list(shape), dt or F32))
        self.split = split
        if split:
            self.bufs = [Buf() for _ in range(shape[1])]
        else:
            self.bufs = [Buf()]

    def __getitem__(self, idx):
        return self.t[idx]

    def r(self, idx):
        return self.t[idx].bitcast(F32R)

    def b(self, i=None):
        if self.split and i is not None:
            return [self.bufs[i]]
        return list(self.bufs)


class Ring:
    def __init__(self, nc, es, name, shape, n, dt=None):
        self.tiles = [Tile(nc, es, "%s%d" % (name, i), shape, dt=dt) for i in range(n)]
        self.i = 0

    def next(self):
        t = self.tiles[self.i % len(self.tiles)]
        self.i += 1
        return t


class Prog:
    def __init__(self, stop_after=None, moe_experts=NEXP, scratch_in=(), moe_sparse=True):
        self.stop_after = stop_after
        self.moe_experts = moe_experts
        self.nc = nc = bass.Bass("TRN2", target_bir_lowering=False)
        self.es = es = contextlib.ExitStack()
        self.fw = FW(nc, es)
        self.din = {}
        self.evac_i = 0

        self.ishapes = {}

        def inp(name, shape):
            self.ishapes[name] = list(shape)

        def outp(name, shape):
            return nc.dram_tensor(name, list(shape), F32, kind="ExternalOutput").ap()

        def scr(name, shape):
            return nc.dram_tensor(name, list(shape), F32, kind="Internal").ap()

        inp("xinT", [16, 128, NT])
        inp("vecs", [NROWS, 128])
        inp("sink", [DEPTH, 16])
        inp("ident", [128, 128])
        inp("permM", [128, 128])
        inp("ropeC", [128, 2048])
        inp("ropeS", [128, 2048])
        inp("maskA", [128, 384])
        inp("invc_s", [4, 2048])
        inp("invc_p", [4, 256])
        inp("ck2T", [DEPTH, 2, 128, 512])
        inp("cv", [DEPTH, 512, 128])
        inp("cckvT", [DEPTH, 2, 128, 512])
        inp("ckr2T", [DEPTH, 128, 512])
        inp("w_ada", [DEPTH, 96, 128, 16, 128])
        inp("w_inA", [DEPTH, 26, 128, 16, 128])
        inp("w_inG", [DEPTH, 48, 128, 16, 128])
        inp("w_uq", [DEPTH, 12, 128, 4, 128])
        inp("w_ukv", [DEPTH, 16, 128, 2, 128])
        inp("poolw", [DEPTH, 8, 128, 2, 128])
        inp("wbr", [DEPTH, 3, 16, 128, 8, 128])
        inp("wout", [DEPTH, 16, 128, 16, 128])
        inp("ffn_g", [NFF, 128, 16, 128])
        inp("ffn_u", [NFF, 128, 16, 128])
        inp("ffn_d", [4, 16, 128, FB, 128])
        inp("router", [128, 16, 8])
        inp("moe_g", [NEXP, NFF, 128, 16, 128])
        inp("moe_u", [NEXP, NFF, 128, 16, 128])
        inp("moe_d", [NEXP, 4, 16, 128, FB, 128])
        inp("Umat", [128, 128])
        inp("tokid", [128, NT // 128])
        inp("svals", [128, NS])
        inp("wbase", [128, NFF])
        inp("wdbase", [128, 64])
        inp("Tab0", [NS * 512, 16])
        self.o_yT = outp("o_yT", [16, 128, NT])
        self.o_kT = outp("o_kT", [DEPTH, 2, 64, 512])
        self.o_v = outp("o_v", [DEPTH, 4, 128, 128])
        self.o_ckvT = outp("o_ckvT", [DEPTH, 2, 128, 512])
        self.o_krT = outp("o_krT", [DEPTH, 64, 512])
        def mk(name, shape):
            if name in scratch_in:
                return nc.dram_tensor(name, list(shape), F32, kind="ExternalInput").ap()
            return (outp if stop_after is not None else scr)(name, shape)
        self.xT = mk("s_xT", [16, 128, NT])
        self.qaT = mk("s_qaT", [8, 128, NT])
        self.kaT2 = mk("s_kaT2", [2, 128, NT])
        self.vaTok = mk("s_vaTok", [NT // 128, 128, 128])
        self.qnT = mk("s_qnT", [8, 128, NT])
        self.qrT = mk("s_qrT", [4, 128, NT])
        self.ckvT = mk("s_ckvT", [2, 128, NT])
        self.krT2 = mk("s_krT2", [128, NT])
        self.uT = mk("s_uT", [8, 128, NT])
        self.oaT = mk("s_oaT", [8, 128, NT])
        self.obT = mk("s_obT", [8, 128, NT])
        self.ocT = mk("s_ocT", [8, 128, NT])
        if moe_sparse:
            self.hTok = scr("s_hTok", [NT + 128, D])
            self.Ybuf = scr("s_Y", [2 * NT + 128, D])
            self.Tab = nc.dram_tensor("s_Tab", [NS * 512, 16], mybir.dt.int32, kind="Internal").ap()
        self.b_x = [[Buf() for _ in range(16)] for _ in range(NGRP)]
        self.b_scr = {k: Buf() for k in ("qaT", "kaT2", "vaTok", "qnT", "qrT", "ckvT", "krT2", "uT",
                                         "oaT", "obT", "ocT")}
        self.ps = [es.enter_context(nc.psum_tensor("ps%d" % i, [128, 512], F32)) for i in range(8)]
        self.psb = [Buf() for _ in range(8)]

    def I(self, name, dt=F32):
        if name not in self.din:
            self.din[name] = self.nc.dram_tensor(name, self.ishapes[name], dt, kind="ExternalInput").ap()
        return self.din[name]

    def mm(self, out, lhsT, rhs, start, stop, reads, writes, sync=None):
        nc = self.nc
        if sync is None:
            sync = stop
        self.fw.op("pe", lambda: nc.tensor.matmul(out, lhsT=lhsT, rhs=rhs, start=start, stop=stop),
                   reads=reads, writes=writes, sync=sync)

    def tr(self, out, in_, reads, writes, sync=True):
        nc = self.nc
        ident = self.ident[:]
        self.fw.op("pe", lambda: nc.tensor.transpose(out=out, in_=in_, identity=ident),
                   reads=reads + self.ident.b(), writes=writes, sync=sync)

    def copy(self, en, out, in_, reads, writes):
        nc = self.nc
        if en == "act":
            self.fw.op("act", lambda: nc.scalar.copy(out=out, in_=in_), reads=reads, writes=writes)
        elif en == "dve":
            self.fw.op("dve", lambda: nc.vector.tensor_copy(out=out, in_=in_), reads=reads, writes=writes)
        else:
            self.fw.op("pool", lambda: nc.gpsimd.tensor_copy(out=out, in_=in_), reads=reads, writes=writes)

    def evac_eng(self):
        self.evac_i += 1
        return "act" if self.evac_i % 2 else "dve"

    def act(self, out, in_, func, reads, writes, bias=None, scale=None, accum_out=None):
        nc = self.nc
        kw = {}
        if bias is not None:
            kw["bias"] = bias
        if scale is not None:
            kw["scale"] = scale
        if accum_out is not None:
            kw["accum_out"] = accum_out
        self.fw.op("act", lambda: nc.scalar.activation(out=out, in_=in_, func=func, **kw),
                   reads=reads, writes=writes)

    def tt(self, out, in0, in1, op, reads, writes, en="dve"):
        nc = self.nc
        eng = nc.vector if en == "dve" else nc.gpsimd
        self.fw.op(en, lambda: eng.tensor_tensor(out=out, in0=in0, in1=in1, op=op), reads=reads, writes=writes)

    def ts(self, out, in0, s1, s2, op0, op1, reads, writes):
        nc = self.nc
        if op1 is None:
            self.fw.op("dve", lambda: nc.vector.tensor_scalar(out=out, in0=in0, scalar1=s1, scalar2=None, op0=op0),
                       reads=reads, writes=writes)
        else:
            self.fw.op("dve", lambda: nc.vector.tensor_scalar(out=out, in0=in0, scalar1=s1, scalar2=s2,
                                                              op0=op0, op1=op1), reads=reads, writes=writes)

    def stt(self, out, in0, scalar, in1, op0, op1, reads, writes):
        nc = self.nc
        self.fw.op("dve", lambda: nc.vector.scalar_tensor_tensor(out=out, in0=in0, scalar=scalar, in1=in1,
                                                                 op0=op0, op1=op1), reads=reads, writes=writes)

    def wload(self, ring, dram_tile, nk, ncol=128):
        slot = ring.next()
        dst = slot.t[:, 0:nk * ncol].rearrange("p (k m) -> p k m", k=nk)
        self.fw.dma("pool", dst.bitcast(F32R), dram_tile, writes=slot.b())
        return dst.bitcast(F32R), slot

    def alloc_persistent(self):
        nc, es = self.nc, self.es
        self.ident = Tile(nc, es, "ident", [128, 128])
        self.ones = Tile(nc, es, "ones", [128, 128])
        self.identr = Tile(nc, es, "identr", [128, 128])
        self.vT = Tile(nc, es, "vT", [128, NROWS])
        self.modp = [Tile(nc, es, "modp%d" % l, [128, 6, 16, 2]) for l in range(DEPTH)]
        self.sinkb = Tile(nc, es, "sinkb", [128, DEPTH * 16])

    def phase_prologue(self):
        nc, fw = self.nc, self.fw
        with contextlib.ExitStack() as es:
            vrows = Tile(nc, es, "vrows", [128, 3, 128])
            scT = Tile(nc, es, "scT", [128, 32])
            modT = Tile(nc, es, "modT", [128, 96, 2])
            wring = Ring(nc, es, "wada", [128, 2048], 4)
            fw.dma("sp", self.ident[:], self.I("ident"), writes=self.ident.b())
            fw.dma("sp", vrows[:], self.I("vecs").rearrange("(a p) f -> p a f", p=128), writes=vrows.b())
            fw.dma("sp", self.sinkb[:],
                   self.I("sink").rearrange("l h -> (l h)").partition_broadcast(128), writes=self.sinkb.b())
            ones32 = Tile(nc, es, "ones32", [128, 128])
            fw.op("dve", lambda: nc.vector.memset(ones32[:], 1.0), writes=ones32.b())
            self.copy("act", self.ones.r(slice(None)), ones32[:], ones32.b(), self.ones.b())
            self.copy("act", self.identr.r(slice(None)), self.ident[:], self.ident.b(), self.identr.b())
            for a in range(3):
                self.tr(self.ps[0][:, a * 128:(a + 1) * 128], vrows[:, a, :], vrows.b(), [self.psb[0]])
            self.copy("dve", self.vT[:], self.ps[0][:, 0:384], [self.psb[0]], self.vT.b())
            self.act(scT.r(slice(None)), self.vT[:, 0:32], AF.Silu, self.vT.b(), scT.b())
            sc3 = scT.r(slice(None)).rearrange("p (c k) -> p k c", c=2)
            for l in range(DEPTH):
                for j in range(96):
                    w, slot = self.wload(wring, self.I("w_ada")[l, j], 16)
                    bank = self.ps[1 + (j % 2)]
                    bb = self.psb[1 + (j % 2)]
                    for k in range(16):
                        self.mm(bank[:, 0:2], w[:, k, :], sc3[:, k, :], k == 0, k == 15,
                                slot.b() + scT.b(), [bb])
                    col = R_BADA(l) + j
                    self.act(modT[:, j, :], bank[:, 0:2], AF.Identity, [bb] + self.vT.b(), modT.b(),
                             bias=self.vT[:, col:col + 1])
                mp = self.modp[l]
                for s in range(2):
                    lnr = R_LN1(l) if s == 0 else R_LN2(l)
                    sh, sc, gt = 3 * s, 3 * s + 1, 3 * s + 2
                    for cd in range(2):
                        self.stt(mp[:, 3 * s + 0, :, cd], modT[:, sc * 16:(sc + 1) * 16, cd], 1.0,
                                 self.vT[:, lnr:lnr + 16], ALU.add, ALU.mult,
                                 modT.b() + self.vT.b(), mp.b())
                        self.copy("dve", mp[:, 3 * s + 1, :, cd], modT[:, sh * 16:(sh + 1) * 16, cd], modT.b(), mp.b())
                        self.copy("dve", mp[:, 3 * s + 2, :, cd], modT[:, gt * 16:(gt + 1) * 16, cd], modT.b(), mp.b())
            xt = Ring(nc, es, "xcp", [128, 16 * 512], 2)
            for g in range(NGRP):
                t = xt.next()
                v = t.t[:].rearrange("p (c n) -> p c n", c=16)
                fw.dma("sp", v, self.I("xinT")[:, :, g * G:(g + 1) * G].rearrange("c p n -> p c n"), writes=t.b())
                fw.dma("sp", self.xT[:, :, g * G:(g + 1) * G].rearrange("c p n -> p c n"), v,
                       reads=t.b(), writes=self.b_x[g])
            fw.emit()

    def norm_stats(self, x3, xb, nchunk, nfeat, sqring, rstd, bank, bb):
        nc = self.nc
        for c in range(nchunk):
            sq = sqring.next()
            self.act(sq.r(slice(None)), x3[:, c, :], AF.Square, xb, sq.b())
            self.mm(bank[:, :], self.ones.r(slice(None)), sq.r(slice(None)), c == 0, c == nchunk - 1,
                    self.ones.b() + sq.b(), [bb], sync=True)
        self.act(rstd[:], bank[:, :], AF.Sqrt, [bb] + self.epsb.b(), rstd.b(), bias=self.epsb[:, 0:1], scale=1.0 / nfeat)
        self.fw.op("dve", lambda: nc.vector.reciprocal(out=rstd[:], in_=rstd[:]), reads=rstd.b(), writes=rstd.b())

    def norm_mod(self, g, l, s, xg, hT, sqring, tmpring, rstd, bank, bb):
        cd = 0 if g < 4 else 1
        mp = self.modp[l]
        x3 = xg.t[:].rearrange("p (c n) -> p c n", c=16)
        self.norm_stats(x3, xg.b(), 16, D, sqring, rstd, bank, bb)
        for c in range(16):
            tmp = tmpring.next()
            self.tt(tmp[:], x3[:, c, :], rstd[:], ALU.mult, xg.b() + rstd.b(), tmp.b())
            self.act(hT.r((slice(None), c, slice(None))), tmp[:], AF.Identity, tmp.b() + mp.b(), hT.b(c),
                     bias=mp[:, 3 * s + 1, c:c + 1, cd], scale=mp[:, 3 * s + 0, c:c + 1, cd])

    def load_x(self, g, xg):
        v = xg.t[:].rearrange("p (c n) -> p c n", c=16)
        self.fw.dma("sp", v, self.xT[:, :, g * G:(g + 1) * G].rearrange("c p n -> p c n"),
                    reads=self.b_x[g], writes=xg.b())

    def phase_proj(self, l):
        nc, fw = self.nc, self.fw
        with contextlib.ExitStack() as es:
            xg = Tile(nc, es, "xg", [128, 16 * 512])
            hT = Tile(nc, es, "hT", [128, 16, 512], split=True)
            wring = Ring(nc, es, "w1", [128, 2048], 4)
            wuq = Tile(nc, es, "wuq", [128, 12, 512])
            ropeC = Tile(nc, es, "ropeC", [128, 2048])
            ropeS = Tile(nc, es, "ropeS", [128, 2048])
            permM = Tile(nc, es, "permM", [128, 128])
            cqs = Tile(nc, es, "cqs", [128, 4, 512])
            cqn = Tile(nc, es, "cqn", [128, 4, 512])
            ckvs = Tile(nc, es, "ckvs", [128, 2, 512])
            stg = Ring(nc, es, "stg", [128, 512], 4)
            sqring = Ring(nc, es, "sq", [128, 512], 3)
            tmpring = Ring(nc, es, "tmp", [128, 512], 3)
            xsring = Ring(nc, es, "xs", [128, 512], 2)
            t1ring = Ring(nc, es, "t1", [128, 512], 2)
            rstd = Tile(nc, es, "rstd", [128, 512])
            rstd2 = Tile(nc, es, "rstd2", [128, 512])
            vtok = Ring(nc, es, "vtok", [128, 512], 2)
            self.epsb = Tile(nc, es, "epsb", [128, 1])
            fw.op("dve", lambda: nc.vector.memset(self.epsb[:], EPS), writes=self.epsb.b())
            fw.dma("sp", ropeC[:], self.I("ropeC"), writes=ropeC.b())
            fw.dma("sp", ropeS[:], self.I("ropeS"), writes=ropeS.b())
            fw.dma("pool", permM.r(slice(None)), self.I("permM"), writes=permM.b())
            fw.dma("pool", wuq.t[:].rearrange("p a (k m) -> p a k m", k=4).bitcast(F32R),
                   self.I("w_uq")[l].rearrange("a p k m -> p a k m"), writes=wuq.b())
            wuq4 = wuq.t[:].rearrange("p a (k m) -> p a k m", k=4).bitcast(F32R)
            bi = [0]

            def nextbank():
                bi[0] += 1
                i = bi[0] % 4
                return self.ps[i], self.psb[i]

            def finish_chunk(bank, bb, g, rope, dst, dbuf, P=128, dst2=None):
                st = stg.next()
                if rope and g < 4:
                    xs = xsring.next()
                    self.copy("act", xs.r(slice(None))[0:P], bank[0:P, :], [bb], xs.b())
                    pb, pbb = self.ps[4 + (bi[0] % 2)], self.psb[4 + (bi[0] % 2)]
                    self.mm(pb[0:P, :], permM.r(slice(None))[0:P, 0:P], xs.r(slice(None))[0:P], True, True,
                            permM.b() + xs.b(), [pbb])
                    t1 = t1ring.next()
                    self.tt(t1[0:P], xs[0:P], ropeC[0:P, g * G:(g + 1) * G], ALU.mult, xs.b() + ropeC.b(), t1.b())
                    self.tt(st[0:P], pb[0:P, :], ropeS[0:P, g * G:(g + 1) * G], ALU.mult, [pbb] + ropeS.b(), st.b())
                    self.tt(st[0:P], st[0:P], t1[0:P], ALU.add, st.b() + t1.b(), st.b())
                else:
                    self.copy(self.evac_eng(), st[0:P], bank[0:P, :], [bb], st.b())
                fw.dma("sp", dst, st[0:P], reads=st.b())
                if dst2 is not None:
                    fw.dma("sp", dst2, st[0:dst2.shape[0]], reads=st.b())
                return st

            for g in (DBG_GROUPS or range(NGRP)):
                gs = slice(g * G, (g + 1) * G)
                self.load_x(g, xg)
                self.norm_mod(g, l, 0, xg, hT, sqring, tmpring, rstd, self.ps[7], self.psb[7])
                for ch in range(26):
                    w, slot = self.wload(wring, self.I("w_inA")[l, ch], 16)
                    bank, bb = nextbank()
                    for k in range(16):
                        self.mm(bank[:, :], w[:, k, :], hT.r((slice(None), k, slice(None))), k == 0, k == 15,
                                slot.b() + hT.b(k), [bb])
                    if ch < 8:
                        finish_chunk(bank, bb, g, True, self.qaT[ch, :, gs], self.b_scr["qaT"])
                    elif ch < 10:
                        d2 = self.o_kT[l, ch - 8, :, :] if g == 4 else None
                        finish_chunk(bank, bb, g, True, self.kaT2[ch - 8, :, gs], self.b_scr["kaT2"], dst2=d2)
                    elif ch == 10:
                        st = stg.next()
                        self.copy(self.evac_eng(), st[:], bank[:, :], [bb], st.b())
                        tb, tbb = self.ps[6], self.psb[6]
                        for t in range(4):
                            self.tr(tb[:, t * 128:(t + 1) * 128], st[:, t * 128:(t + 1) * 128], st.b(), [tbb])
                        vt = vtok.next()
                        self.copy(self.evac_eng(), vt[:], tb[:, :], [tbb], vt.b())
                        fw.dma("sp", self.vaTok[g * 4:(g + 1) * 4].rearrange("t p d -> p t d"),
                               vt.t[:].rearrange("p (t d) -> p t d", t=4), reads=vt.b())
                        if g == 4:
                            fw.dma("sp", self.o_v[l].rearrange("t p d -> p t d"),
                                   vt.t[:].rearrange("p (t d) -> p t d", t=4), reads=vt.b())
                    elif ch < 15:
                        self.copy(self.evac_eng(), cqs[:, ch - 11, :], bank[:, :], [bb], cqs.b())
                        if ch == 14:
                            self.norm_stats(cqs.t[:], cqs.b(), 4, 512, sqring, rstd2, self.ps[7], self.psb[7])
                            for c in range(4):
                                col = R_QG(l) + c
                                self.stt(cqn.r((slice(None), c, slice(None))), cqs[:, c, :], self.vT[:, col:col + 1],
                                         rstd2[:], ALU.mult, ALU.mult, cqs.b() + rstd2.b() + self.vT.b(), cqn.b())
                            for a in range(12):
                                bank2, bb2 = nextbank()
                                for k in range(4):
                                    self.mm(bank2[:, :], wuq4[:, a, k, :], cqn.r((slice(None), k, slice(None))),
                                            k == 0, k == 3, wuq.b() + cqn.b(), [bb2])
                                if a < 8:
                                    finish_chunk(bank2, bb2, g, False, self.qnT[a, :, gs], self.b_scr["qnT"])
                                else:
                                    finish_chunk(bank2, bb2, g, True, self.qrT[a - 8, :, gs], self.b_scr["qrT"])
                    elif ch < 17:
                        self.copy(self.evac_eng(), ckvs[:, ch - 15, :], bank[:, :], [bb], ckvs.b())
                        if ch == 16:
                            self.norm_stats(ckvs.t[:], ckvs.b(), 2, 256, sqring, rstd2, self.ps[7], self.psb[7])
                            for c in range(2):
                                col = R_KVG(l) + c
                                st = stg.next()
                                self.stt(st[:], ckvs[:, c, :], self.vT[:, col:col + 1], rstd2[:], ALU.mult, ALU.mult,
                                         ckvs.b() + rstd2.b() + self.vT.b(), st.b())
                                fw.dma("sp", self.ckvT[c, :, gs], st[:], reads=st.b())
                                if g == 4:
                                    fw.dma("sp", self.o_ckvT[l, c], st[:], reads=st.b())
                    elif ch == 17:
                        d2 = self.o_krT[l] if g == 4 else None
                        finish_chunk(bank, bb, g, True, self.krT2[:, gs], self.b_scr["krT2"], dst2=d2)
                    else:
                        finish_chunk(bank, bb, g, False, self.uT[ch - 18, :, gs], self.b_scr["uT"])
            fw.emit()

    def rmax(self, out, in_, reads, writes):
        nc = self.nc
        self.fw.op("dve", lambda: nc.vector.reduce_max(out=out, in_=in_, axis=AX.X), reads=reads, writes=writes)

    def rsum(self, out, in_, reads, writes):
        nc = self.nc
        self.fw.op("dve", lambda: nc.vector.reduce_sum(out=out, in_=in_, axis=AX.X), reads=reads, writes=writes)

    def recip(self, out, in_, reads, writes):
        nc = self.nc
        self.fw.op("dve", lambda: nc.vector.reciprocal(out=out, in_=in_), reads=reads, writes=writes)

    def phase_attnA(self, l, seqs=SEQS, qb_limit=None):
        nc, fw = self.nc, self.fw
        with contextlib.ExitStack() as es:
            kT2 = Tile(nc, es, "kT2", [128, 2, 2560])
            Vt = Tile(nc, es, "Vt", [128, 20, 128], dt=BF16)
            identbA = Tile(nc, es, "identbA", [128, 128], dt=BF16)
            PbringA = Ring(nc, es, "PbA", [128, 896], 5, dt=BF16)
            qring = Ring(nc, es, "qc", [128, 2048], 2)
            maskA = Tile(nc, es, "maskA", [128, 384])
            Pring = Ring(nc, es, "Pa", [128, 896], 5)
            PTring = Ring(nc, es, "PTa", [128, 896], 3, dt=BF16)
            slring = Ring(nc, es, "sl", [128, 384], 2)
            small = Ring(nc, es, "sma", [128, 16], 4)
            rdring = Ring(nc, es, "rda", [128, 2], 3)
            Oqring = Ring(nc, es, "Oqa", [128, 128], 2)
            ostg = Ring(nc, es, "ostga", [128, 512], 2)
            fw.dma("sp", maskA[:], self.I("maskA"), writes=maskA.b())
            self.copy("dve", identbA[:], self.ident[:], self.ident.b(), identbA.b())
            for (tok0, n, ctx) in seqs:
                nqb = n // 128
                for g2 in range(2):
                    fw.dma("pool", kT2.r((slice(None), g2, slice(0, n))), self.kaT2[g2, :, tok0:tok0 + n],
                           writes=kT2.b())
                    if ctx:
                        fw.dma("pool", kT2.r((slice(None), g2, slice(n, n + 512))), self.I("ck2T")[l, g2],
                               writes=kT2.b())
                fw.dma("pool", Vt[:, 0:nqb, :],
                       self.vaTok[tok0 // 128:tok0 // 128 + nqb].rearrange("t p d -> p t d"),
                       writes=Vt.b())
                if ctx:
                    fw.dma("pool", Vt[:, nqb:nqb + 4, :],
                           self.I("cv")[l].rearrange("(t p) d -> p t d", p=128), writes=Vt.b())
                for c in range(8):
                    g2 = c // 4
                    qc = qring.next()
                    fw.dma("pool", qc.r((slice(None), slice(0, n))), self.qaT[c, :, tok0:tok0 + n],
                           writes=qc.b())
                    qbs = list(range(nqb)) if qb_limit is None else list(range(min(nqb, qb_limit)))
                    stbox = [None]

                    def stageA(qb, c=c, g2=g2, qc=qc):
                        if ctx:
                            kb_lo, kb_hi = max(qb - 1, 0), min(qb + 1, nqb - 1)
                        else:
                            kb_lo, kb_hi = 0, nqb - 1
                        nl = (kb_hi - kb_lo + 1) * 128
                        blocks = list(range(kb_lo, kb_hi + 1)) + ([nqb + i for i in range(4)] if ctx else [])
                        rd = rdring.next()
                        sm = small.next()
                        pts = []
                        for hh in range(2):
                            pb = hh * 64
                            sbank, sbb = self.ps[hh * 2], self.psb[hh * 2]
                            cbank, cbb = self.ps[hh * 2 + 1], self.psb[hh * 2 + 1]
                            lq = qc.r((slice(pb, pb + 64), slice(qb * 128, (qb + 1) * 128)))
                            self.mm(sbank[:, 0:nl], lq, kT2.r((slice(pb, pb + 64), g2, slice(kb_lo * 128, kb_lo * 128 + nl))),
                                    True, True, qc.b() + kT2.b(), [sbb])
                            if ctx:
                                self.mm(cbank[:, :], lq, kT2.r((slice(pb, pb + 64), g2, slice(n, n + 512))),
                                        True, True, qc.b() + kT2.b(), [cbb])
                            Pt = Pring.next()
                            if ctx:
                                mlo = 128 if qb == 0 else 0
                                self.tt(Pt[:, 0:nl], sbank[:, 0:nl], maskA[:, mlo:mlo + nl], ALU.add,
                                        [sbb] + maskA.b(), Pt.b())
                                self.rmax(sm[:, hh:hh + 1], Pt[:, 0:nl], Pt.b(), sm.b())
                                fw.op("dve", lambda cbank=cbank, Pt=Pt, sm=sm, nl=nl, hh=hh: nc.vector.tensor_scalar(
                                    out=Pt[:, nl:nl + 512], in0=cbank[:, :], scalar1=1.0, scalar2=None, op0=ALU.mult,
                                    op1=ALU.max, accum_out=sm[:, 2 + hh:3 + hh]), reads=[cbb], writes=Pt.b() + sm.b())
                            else:
                                fw.op("dve", lambda sbank=sbank, Pt=Pt, sm=sm, nl=nl, hh=hh: nc.vector.tensor_scalar(
                                    out=Pt[:, 0:nl], in0=sbank[:, 0:nl], scalar1=1.0, scalar2=None, op0=ALU.mult,
                                    op1=ALU.max, accum_out=sm[:, 4 + hh:5 + hh]), reads=[sbb], writes=Pt.b() + sm.b())
                            pts.append(Pt)
                        scol = l * 16 + 2 * c
                        sk2 = self.sinkb[:, scol:scol + 2]
                        if ctx:
                            self.tt(sm[:, 4:6], sm[:, 0:2], sm[:, 2:4], ALU.max, sm.b(), sm.b())
                        self.stt(sm[:, 6:8], sm[:, 4:6], 0.125, sk2, ALU.mult, ALU.max, sm.b() + self.sinkb.b(), sm.b())
                        self.ts(sm[:, 8:10], sm[:, 6:8], -1.0, None, ALU.mult, None, sm.b(), sm.b())
                        pbs = []
                        for hh in range(2):
                            Pt = pts[hh]
                            Pb = PbringA.next()
                            self.act(Pb[:, 0:nl], Pt[:, 0:nl], AF.Exp, Pt.b() + sm.b(), Pb.b() + sm.b(),
                                     bias=sm[:, 8 + hh:9 + hh], scale=0.125, accum_out=sm[:, 10 + hh:11 + hh])
                            if ctx:
                                self.act(Pb[:, nl:nl + 512], Pt[:, nl:nl + 512], AF.Exp, Pt.b() + sm.b(), Pb.b() + sm.b(),
                                         bias=sm[:, 8 + hh:9 + hh], scale=0.125, accum_out=sm[:, 12 + hh:13 + hh])
                            pbs.append(Pb)
                        return blocks, rd, pbs, sm, sk2

                    def stageA2(blocks, rd, pbs, sm, sk2):
                        self.tt(sm[:, 14:16], sk2, sm[:, 8:10], ALU.add, self.sinkb.b() + sm.b(), sm.b())
                        self.act(sm[:, 14:16], sm[:, 14:16], AF.Exp, sm.b(), sm.b())
                        self.tt(sm[:, 10:12], sm[:, 10:12], sm[:, 14:16], ALU.add, sm.b(), sm.b())
                        if ctx:
                            self.tt(sm[:, 10:12], sm[:, 10:12], sm[:, 12:14], ALU.add, sm.b(), sm.b())
                        self.recip(rd[:, 0:2], sm[:, 10:12], sm.b(), rd.b())

                    def stageB(qb, blocks, rd, pts, c=c, g2=g2):
                        nb = len(blocks)
                        obank, obb = self.ps[6], self.psb[6]
                        for hh in range(2):
                            Pt = pts[hh]
                            PT = PTring.next()
                            for i0 in range(0, nb, 4):
                                cnt = min(4, nb - i0)
                                tbb = self.psb[4 + (i0 // 4) % 2]
                                tb = self.ps[4 + (i0 // 4) % 2][:, :].bitcast(BF16)
                                for i in range(i0, i0 + cnt):
                                    fw.op("pe", lambda tb=tb, i=i, i0=i0, Pt=Pt: nc.tensor.transpose(
                                        out=tb[:, (i - i0) * 128:(i - i0 + 1) * 128], in_=Pt[:, i * 128:(i + 1) * 128],
                                        identity=identbA[:]), reads=Pt.b() + identbA.b(), writes=[tbb])
                                self.copy(self.evac_eng(), PT[:, i0 * 128:(i0 + cnt) * 128],
                                          tb[:, 0:cnt * 128], [tbb], PT.b())
                            for i, blk in enumerate(blocks):
                                self.mm(obank[:, hh * 64:(hh + 1) * 64], PT[:, i * 128:(i + 1) * 128],
                                        Vt[:, blk, g2 * 64:(g2 + 1) * 64], i == 0, i == nb - 1,
                                        PT.b() + Vt.b(), [obb], sync=True)
                        Oq = Oqring.next()
                        self.ts(Oq[:, 0:64], obank[:, 0:64], rd[:, 0:1], None, ALU.mult, None, [obb] + rd.b(), Oq.b())
                        self.ts(Oq[:, 64:128], obank[:, 64:128], rd[:, 1:2], None, ALU.mult, None, [obb] + rd.b(), Oq.b())
                        tb, tbb = self.ps[7], self.psb[7]
                        self.tr(tb[:, 0:128], Oq[:, :], Oq.b(), [tbb])
                        if qb % 4 == 0:
                            stbox[0] = ostg.next()
                        st = stbox[0]
                        self.copy(self.evac_eng(), st[:, (qb % 4) * 128:(qb % 4 + 1) * 128], tb[:, 0:128], [tbb], st.b())
                        if qb % 4 == 3 or qb == qbs[-1]:
                            q0 = (qb // 4) * 512
                            wd = (qb % 4 + 1) * 128
                            fw.dma("sp", self.oaT[c, :, tok0 + q0:tok0 + q0 + wd], st[:, 0:wd], reads=st.b())

                    has_ctx = ctx
                    nxt = stageA(qbs[0])
                    stageA2(*nxt)
                    for ii, qb in enumerate(qbs):
                        cur = nxt
                        if ii + 1 < len(qbs):
                            nxt = stageA(qbs[ii + 1])
                        stageB(qb, *cur[0:3])
                        if ii + 1 < len(qbs):
                            stageA2(*nxt)
            fw.emit()

    def phase_mla(self, l, seqs=SEQS, qb_limit=None, heads=range(8)):
        nc, fw = self.nc, self.fw
        scale = float((128 + 64) ** -0.5)
        with contextlib.ExitStack() as es:
            ckvA = Tile(nc, es, "ckvA", [128, 2, 2560])
            krA = Tile(nc, es, "krA", [128, 2560])
            wukv = Tile(nc, es, "wukv", [128, 16, 256])
            knT = Tile(nc, es, "knT", [128, 2560])
            vh = Tile(nc, es, "vh", [128, 20, 128], dt=BF16)
            identb = Tile(nc, es, "identb", [128, 128], dt=BF16)
            Pbring = Ring(nc, es, "Pbm", [128, 2560], 3, dt=BF16)
            qnr = Ring(nc, es, "qnm", [128, 2048], 2)
            qrr = Ring(nc, es, "qrm", [128, 2048], 2)
            Pring = Ring(nc, es, "Pm", [128, 2560], 3)
            PTring = Ring(nc, es, "PTm", [128, 2560], 2, dt=BF16)
            small = Ring(nc, es, "smm", [128, 16], 4)
            Oqring = Ring(nc, es, "Oqm", [128, 128], 2)
            ostg = Ring(nc, es, "ostgm", [128, 512], 2)
            self.copy("dve", identb[:], self.ident[:], self.ident.b(), identb.b())
            wukv4 = wukv.t[:].rearrange("p a (k m) -> p a k m", k=2).bitcast(F32R)
            fw.dma("pool", wukv4, self.I("w_ukv")[l].rearrange("a p k m -> p a k m"), writes=wukv.b())
            for (tok0, n, ctx) in seqs:
                nqb = n // 128
                nk = n + (512 if ctx else 0)
                nkb = nk // 128
                kgs = [(s, min(512, nk - s)) for s in range(0, nk, 512)]
                ng = len(kgs)
                for k in range(2):
                    fw.dma("pool", ckvA.r((slice(None), k, slice(0, n))), self.ckvT[k, :, tok0:tok0 + n],
                           writes=ckvA.b())
                    if ctx:
                        fw.dma("pool", ckvA.r((slice(None), k, slice(n, n + 512))), self.I("cckvT")[l, k], writes=ckvA.b())
                fw.dma("pool", krA.r((slice(None), slice(0, n))), self.krT2[:, tok0:tok0 + n],
                       writes=krA.b())
                if ctx:
                    fw.dma("pool", krA.r((slice(None), slice(n, n + 512))), self.I("ckr2T")[l], writes=krA.b())
                qr, qr_pair = None, -1
                for h in heads:
                    pb = (h % 2) * 64
                    for gi, (s, w) in enumerate(kgs):
                        bank, bb = self.ps[gi % 4], self.psb[gi % 4]
                        for k in range(2):
                            self.mm(bank[:, 0:w], wukv4[:, h, k, :], ckvA.r((slice(None), k, slice(s, s + w))),
                                    k == 0, k == 1, wukv.b() + ckvA.b(), [bb])
                        self.copy(self.evac_eng(), knT.r((slice(None), slice(s, s + w))), bank[:, 0:w], [bb], knT.b())
                    for kb0 in range(0, nkb, 4):
                        cnt = min(4, nkb - kb0)
                        bank, bb = self.ps[4 + (kb0 // 4) % 2], self.psb[4 + (kb0 // 4) % 2]
                        for kb in range(kb0, kb0 + cnt):
                            for k in range(2):
                                self.mm(bank[:, (kb - kb0) * 128:(kb - kb0 + 1) * 128],
                                        ckvA.r((slice(None), k, slice(kb * 128, (kb + 1) * 128))), wukv4[:, 8 + h, k, :],
                                        k == 0, k == 1, wukv.b() + ckvA.b(), [bb], sync=(k == 1 and kb == kb0 + cnt - 1))
                        self.copy(self.evac_eng(), vh[:, kb0:kb0 + cnt, :],
                                  bank[:, 0:cnt * 128].rearrange("p (a d) -> p a d", a=cnt), [bb], vh.b())
                    qn = qnr.next()
                    fw.dma("pool", qn.r((slice(None), slice(0, n))), self.qnT[h, :, tok0:tok0 + n],
                           writes=qn.b())
                    if qr_pair != h // 2:
                        qr_pair = h // 2
                        qr = qrr.next()
                        fw.dma("pool", qr.r((slice(None), slice(0, n))), self.qrT[h // 2, :, tok0:tok0 + n],
                               writes=qr.b())
                    qbs = list(range(nqb)) if qb_limit is None else list(range(min(nqb, qb_limit)))
                    stbox = [None]

                    def stageA(qb, qn=qn, qr=qr, pb=pb):
                        qsl = slice(qb * 128, (qb + 1) * 128)
                        sm = small.next()
                        Pt = Pring.next()
                        for gi, (s, w) in enumerate(kgs):
                            bank, bb = self.ps[gi], self.psb[gi]
                            self.mm(bank[:, 0:w], qn.r((slice(None), qsl)), knT.r((slice(None), slice(s, s + w))),
                                    True, False, qn.b() + knT.b(), [bb], sync=False)
                            self.mm(bank[:, 0:w], qr.r((slice(pb, pb + 64), qsl)), krA.r((slice(pb, pb + 64), slice(s, s + w))),
                                    False, True, qr.b() + krA.b(), [bb], sync=True)
                            fw.op("dve", lambda bank=bank, w=w, s=s, gi=gi, Pt=Pt, sm=sm: nc.vector.tensor_scalar(
                                out=Pt[:, s:s + w], in0=bank[:, 0:w], scalar1=1.0, scalar2=None, op0=ALU.mult, op1=ALU.max,
                                accum_out=sm[:, gi:gi + 1]), reads=[bb], writes=Pt.b() + sm.b())
                        self.rmax(sm[:, 8:9], sm[:, 0:ng], sm.b(), sm.b())
                        self.ts(sm[:, 9:10], sm[:, 8:9], -scale, None, ALU.mult, None, sm.b(), sm.b())
                        Pb = Pbring.next()
                        for gi, (s, w) in enumerate(kgs):
                            self.act(Pb[:, s:s + w], Pt[:, s:s + w], AF.Exp, Pt.b() + sm.b(), Pb.b() + sm.b(),
                                     bias=sm[:, 9:10], scale=scale, accum_out=sm[:, 10 + gi:11 + gi])
                        return sm, Pb

                    def stageA2(sm, Pb):
                        self.rsum(sm[:, 15:16], sm[:, 10:10 + ng], sm.b(), sm.b())
                        self.recip(sm[:, 15:16], sm[:, 15:16], sm.b(), sm.b())

                    def stageB(qb, sm, Pt, h=h):
                        PT = PTring.next()
                        for kb0 in range(0, nkb, 4):
                            cnt = min(4, nkb - kb0)
                            tbb = self.psb[5 + (kb0 // 4) % 2]
                            tb = self.ps[5 + (kb0 // 4) % 2][:, :].bitcast(BF16)
                            for kb in range(kb0, kb0 + cnt):
                                fw.op("pe", lambda tb=tb, kb=kb, kb0=kb0, Pt=Pt: nc.tensor.transpose(
                                    out=tb[:, (kb - kb0) * 128:(kb - kb0 + 1) * 128], in_=Pt[:, kb * 128:(kb + 1) * 128],
                                    identity=identb[:]), reads=Pt.b() + identb.b(), writes=[tbb])
                            self.copy(self.evac_eng(), PT[:, kb0 * 128:(kb0 + cnt) * 128],
                                      tb[:, 0:cnt * 128], [tbb], PT.b())
                        obank, obb = self.ps[7], self.psb[7]
                        for kb in range(nkb):
                            self.mm(obank[:, 0:128], PT[:, kb * 128:(kb + 1) * 128],
                                    vh[:, kb, :], kb == 0, kb == nkb - 1, PT.b() + vh.b(), [obb])
                        Oq = Oqring.next()
                        self.ts(Oq[:, :], obank[:, 0:128], sm[:, 15:16], None, ALU.mult, None, [obb] + sm.b(), Oq.b())
                        tb, tbb = self.ps[5], self.psb[5]
                        self.tr(tb[:, 0:128], Oq[:, :], Oq.b(), [tbb])
                        if qb % 4 == 0:
                            stbox[0] = ostg.next()
                        st = stbox[0]
                        self.copy(self.evac_eng(), st[:, (qb % 4) * 128:(qb % 4 + 1) * 128], tb[:, 0:128], [tbb], st.b())
                        if qb % 4 == 3 or qb == qbs[-1]:
                            q0 = (qb // 4) * 512
                            wd = (qb % 4 + 1) * 128
                            fw.dma("sp", self.obT[h, :, tok0 + q0:tok0 + q0 + wd], st[:, 0:wd], reads=st.b())

                    nxt = stageA(qbs[0])
                    stageA2(*nxt)
                    for ii, qb in enumerate(qbs):
                        cur = nxt
                        if ii + 1 < len(qbs):
                            nxt = stageA(qbs[ii + 1])
                        stageB(qb, *cur)
                        if ii + 1 < len(qbs):
                            stageA2(*nxt)
            fw.emit()

    def phase_pool(self, l, seqs=SEQS):
        nc, fw = self.nc, self.fw
        with contextlib.ExitStack() as es:
            invs = Tile(nc, es, "invs", [128, 4 * 2048])
            invp = Tile(nc, es, "invp", [128, 4 * 256])
            pw = Tile(nc, es, "pw", [128, 8, 256])
            upr = Ring(nc, es, "up", [128, 2064], 2)
            Ar = Ring(nc, es, "Apool", [128, 2064], 3)
            dT = [Tile(nc, es, "dT%d" % i, [128, 2048]) for i in range(2)]
            stg = Ring(nc, es, "pstg", [128, 512], 3)
            fw.dma("sp", invs[:], self.I("invc_s").rearrange("a n -> (a n)").partition_broadcast(128), writes=invs.b())
            fw.dma("sp", invp[:], self.I("invc_p").rearrange("a n -> (a n)").partition_broadcast(128), writes=invp.b())
            pw4 = pw.t[:].rearrange("p a (k m) -> p a k m", k=2).bitcast(F32R)
            fw.dma("pool", pw4, self.I("poolw")[l].rearrange("a p k m -> p a k m"), writes=pw.b())
            bi = 0
            for (tok0, n, ctx) in seqs:
                inv = invs if n == 2048 else invp
                for pg in range(4):
                    win = POOL_WINDOWS[pg]
                    left = win // 2
                    for half in range(2):
                        cc = pg * 2 + half
                        u = upr.next()
                        fw.op("dve", lambda u=u: nc.vector.memset(u[:, 0:8], 0.0), writes=u.b())
                        fw.op("dve", lambda u=u, n=n: nc.vector.memset(u[:, 8 + n:16 + n], 0.0), writes=u.b())
                        fw.dma("sp", u[:, 8:8 + n], self.uT[cc, :, tok0:tok0 + n], writes=u.b())
                        cur, L, step = u, n + 16, 1
                        while step < win:
                            nxt = Ar.next()
                            self.tt(nxt[:, 0:L - step], cur[:, 0:L - step], cur[:, step:L], ALU.add, cur.b(), nxt.b())
                            cur, L, step = nxt, L - step, step * 2
                        tmp = Ar.next()
                        self.tt(tmp[:, 0:n], cur[:, 8 - left:8 - left + n], inv[:, pg * n:(pg + 1) * n], ALU.mult,
                                cur.b() + inv.b(), tmp.b())
                        self.tt(dT[half].r((slice(None), slice(0, n))), tmp[:, 0:n], u[:, 8:8 + n], ALU.subtract,
                                tmp.b() + u.b(), dT[half].b())
                    for mh in range(2):
                        for tg in range(0, n, 512):
                            w = min(512, n - tg)
                            bi += 1
                            bank, bb = self.ps[bi % 4], self.psb[bi % 4]
                            for k in range(2):
                                self.mm(bank[:, 0:w], pw4[:, pg * 2 + mh, k, :], dT[k].r((slice(None), slice(tg, tg + w))),
                                        k == 0, k == 1, pw.b() + dT[k].b(), [bb])
                            st = stg.next()
                            col = R_PSC(l) + pg * 2 + mh
                            self.act(st[:, 0:w], bank[:, 0:w], AF.Copy, [bb] + self.vT.b(), st.b(),
                                     scale=self.vT[:, col:col + 1])
                            fw.dma("sp", self.ocT[pg * 2 + mh, :, tok0 + tg:tok0 + tg + w], st[:, 0:w], reads=st.b())
            fw.emit()

    def phase_merge(self, l, groups=None):
        nc, fw = self.nc, self.fw
        with contextlib.ExitStack() as es:
            xg = Tile(nc, es, "xg3", [128, 16 * 512])
            hT = Tile(nc, es, "hT3", [128, 16, 512], split=True)
            o3 = [Tile(nc, es, "o3_%d" % i, [128, 8, 512]) for i in range(3)]
            wring = Ring(nc, es, "w3", [128, 2048], 6)
            sgr = Ring(nc, es, "sg3", [128, 512], 2)
            tmr = Ring(nc, es, "tm3", [128, 512], 2)
            sqring = Ring(nc, es, "sq3", [128, 512], 2)
            tmpring = Ring(nc, es, "tmp3", [128, 512], 2)
            rstd = Tile(nc, es, "rstd3", [128, 512])
            xring = Ring(nc, es, "xr3", [128, 512], 3)
            self.epsb = Tile(nc, es, "epsb3", [128, 1])
            fw.op("dve", lambda: nc.vector.memset(self.epsb[:], EPS), writes=self.epsb.b())
            y3 = xg.t[:].rearrange("p (c n) -> p c n", c=16)
            srcs = [(self.oaT, "oaT"), (self.obT, "obT"), (self.ocT, "ocT")]
            mp = self.modp[l]
            it = 0
            for g in (groups or range(NGRP)):
                cd = 0 if g < 4 else 1
                gs = slice(g * G, (g + 1) * G)
                self.load_x(g, xg)
                self.norm_mod(g, l, 0, xg, hT, sqring, tmpring, rstd, self.ps[7], self.psb[7])
                for br in range(3):
                    fw.dma("pool", o3[br].r(slice(None)), srcs[br][0][:, :, gs].rearrange("c p n -> p c n"),
                           writes=o3[br].b())
                for d in range(16):
                    for br in range(3):
                        it += 1
                        w, slot = self.wload(wring, self.I("w_inG")[l, br * 16 + d], 16)
                        ga, gab = self.ps[it % 2], self.psb[it % 2]
                        for k in range(16):
                            self.mm(ga[:, :], w[:, k, :], hT.r((slice(None), k, slice(None))), k == 0, k == 15,
                                    slot.b() + hT.b(k), [gab])
                        w2, slot2 = self.wload(wring, self.I("wbr")[l, br, d], 8)
                        pr, prb = self.ps[2 + it % 2], self.psb[2 + it % 2]
                        for k in range(8):
                            self.mm(pr[:, :], w2[:, k, :], o3[br].r((slice(None), k, slice(None))), k == 0, k == 7,
                                    slot2.b() + o3[br].b(), [prb])
                        sg = sgr.next()
                        self.act(sg[:], ga[:, :], AF.Sigmoid, [gab], sg.b())
                        if br == 0:
                            self.tt(y3[:, d, :].bitcast(F32R), sg[:], pr[:, :], ALU.mult, sg.b() + [prb], xg.b())
                        else:
                            tm = tmr.next()
                            self.tt(tm[:], sg[:], pr[:, :], ALU.mult, sg.b() + [prb], tm.b())
                            out = y3[:, d, :].bitcast(F32R)
                            self.tt(out, y3[:, d, :], tm[:], ALU.add, xg.b() + tm.b(), xg.b())
                for d2 in range(16):
                    it += 1
                    w, slot = self.wload(wring, self.I("wout")[l, d2], 16)
                    bank, bb = self.ps[4 + it % 2], self.psb[4 + it % 2]
                    for k in range(16):
                        self.mm(bank[:, :], w[:, k, :], y3[:, k, :].bitcast(F32R), k == 0, k == 15, slot.b() + xg.b(), [bb])
                    xr = xring.next()
                    fw.dma("sp", xr[:], self.xT[d2, :, gs], reads=[self.b_x[g][d2]], writes=xr.b())
                    self.stt(xr[:], bank[:, :], mp[:, 2, d2:d2 + 1, cd], xr[:], ALU.mult, ALU.add,
                             [bb] + mp.b() + xr.b(), xr.b())
                    fw.dma("sp", self.xT[d2, :, gs], xr[:], reads=xr.b(), writes=[self.b_x[g][d2]])
            fw.emit()

    def phase_ffn(self, l, groups=None, final=False):
        nc, fw = self.nc, self.fw
        moe = (l % 2 == 1)
        nexp = self.moe_experts if moe else 1
        with contextlib.ExitStack() as es:
            xa = Tile(nc, es, "xa", [128, 16 * 512])
            hT = Tile(nc, es, "hT4", [128, 16, 512], split=True)
            aT = [Tile(nc, es, "aT%d" % i, [128, FB, 512], split=True) for i in range(2)]
            wgu = Ring(nc, es, "wgu", [128, 2048], 6)
            wdr = Ring(nc, es, "wdr", [128, FB * 128], 3)
            sglr = Ring(nc, es, "sgl", [128, 512], 2)
            t4r = Ring(nc, es, "t4", [128, 512], 2)
            sqring = Ring(nc, es, "sq4", [128, 512], 2)
            tmpring = Ring(nc, es, "tmp4", [128, 512], 2)
            rstd = Tile(nc, es, "rstd4", [128, 512])
            xring = Ring(nc, es, "xr4", [128, 512], 3)
            self.epsb = Tile(nc, es, "epsb4", [128, 1])
            fw.op("dve", lambda: nc.vector.memset(self.epsb[:], EPS), writes=self.epsb.b())
            if moe:
                rt = Tile(nc, es, "rt", [128, 16, 8])
                fw.dma("pool", rt.r(slice(None)), self.I("router"), writes=rt.b())
                gate = Tile(nc, es, "gate", [128, 4, 8])
                gsm = Ring(nc, es, "gsm", [128, 32], 2)
                gbr = Ring(nc, es, "gb", [128, 128], 2)
                gbcr = Ring(nc, es, "gbc", [128, 512], 2)
            a3 = xa.t[:].rearrange("p (c n) -> p c n", c=16)
            mp = self.modp[l]
            it = 0
            for g in (groups or range(NGRP)):
                cd = 0 if g < 4 else 1
                gs = slice(g * G, (g + 1) * G)
                self.load_x(g, xa)
                self.norm_mod(g, l, 1, xa, hT, sqring, tmpring, rstd, self.ps[7], self.psb[7])
                if moe:
                    for t in range(4):
                        lb, lbb = self.ps[6], self.psb[6]
                        for k in range(16):
                            self.mm(lb[:, t * 8:(t + 1) * 8], hT.r((slice(None), k, slice(t * 128, (t + 1) * 128))),
                                    rt.r((slice(None), k, slice(None))), k == 0, k == 15, hT.b(k) + rt.b(), [lbb])
                        sm = gsm.next()
                        lg = sm[:, 0:8]
                        self.copy("dve", lg, lb[:, t * 8:(t + 1) * 8], [lbb], sm.b())
                        self.rmax(sm[:, 24:25], lg, sm.b(), sm.b())
                        self.ts(sm[:, 8:16], lg, sm[:, 24:25], None, ALU.is_equal, None, sm.b(), sm.b())
                        self.stt(sm[:, 8:16], sm[:, 8:16], -1e30, lg, ALU.mult, ALU.add, sm.b(), sm.b())
                        self.rmax(sm[:, 25:26], sm[:, 8:16], sm.b(), sm.b())
                        self.ts(sm[:, 8:16], lg, sm[:, 25:26], None, ALU.is_ge, None, sm.b(), sm.b())
                        self.ts(sm[:, 26:27], sm[:, 24:25], -1.0, None, ALU.mult, None, sm.b(), sm.b())
                        self.act(sm[:, 16:24], lg, AF.Exp, sm.b(), sm.b(), bias=sm[:, 26:27], scale=1.0)
                        self.tt(sm[:, 16:24], sm[:, 16:24], sm[:, 8:16], ALU.mult, sm.b(), sm.b())
                        self.rsum(sm[:, 27:28], sm[:, 16:24], sm.b(), sm.b())
                        self.recip(sm[:, 27:28], sm[:, 27:28], sm.b(), sm.b())
                        self.ts(gate[:, t, :], sm[:, 16:24], sm[:, 27:28], None, ALU.mult, None, sm.b(), gate.b())
                first = True
                for e in range(nexp):
                    if moe:
                        gbank, gbb = self.ps[6], self.psb[6]
                        for t in range(4):
                            gb = gbr.next()
                            self.copy("dve", gb.r(slice(None)), gate[:, t, e:e + 1].to_broadcast([128, 128]), gate.b(), gb.b())
                            self.mm(gbank[:, t * 128:(t + 1) * 128], gb.r(slice(None)), self.identr.r(slice(None)), True, True,
                                    gb.b() + self.identr.b(), [gbb], sync=True)
                        gbc = gbcr.next()
                        self.copy("act", gbc[:], gbank[:, :], [gbb], gbc.b())
                        wg_d, wu_d, wd_d = self.I("moe_g")[e], self.I("moe_u")[e], self.I("moe_d")[e]
                    else:
                        wg_d, wu_d, wd_d = self.I("ffn_g"), self.I("ffn_u"), self.I("ffn_d")
                    for blk in range(NFF // FB):
                        at = aT[blk % 2]
                        for jj in range(FB):
                            j = blk * FB + jj
                            it += 1
                            wg, sg_ = self.wload(wgu, wg_d[j], 16)
                            gbk, gbkb = self.ps[it % 2], self.psb[it % 2]
                            for k in range(16):
                                self.mm(gbk[:, :], wg[:, k, :], hT.r((slice(None), k, slice(None))), k == 0, k == 15,
                                        sg_.b() + hT.b(k), [gbkb])
                            wu, su_ = self.wload(wgu, wu_d[j], 16)
                            ubk, ubkb = self.ps[2 + it % 2], self.psb[2 + it % 2]
                            for k in range(16):
                                self.mm(ubk[:, :], wu[:, k, :], hT.r((slice(None), k, slice(None))), k == 0, k == 15,
                                        su_.b() + hT.b(k), [ubkb])
                            sgl = sglr.next()
                            self.act(sgl[:], gbk[:, :], AF.Silu, [gbkb], sgl.b())
                            if moe:
                                t4 = t4r.next()
                                self.tt(t4[:], ubk[:, :], gbc[:], ALU.mult, [ubkb] + gbc.b(), t4.b())
                                self.tt(at.r((slice(None), jj, slice(None))), sgl[:], t4[:], ALU.mult, sgl.b() + t4.b(), at.b(jj))
                            else:
                                self.tt(at.r((slice(None), jj, slice(None))), sgl[:], ubk[:, :], ALU.mult, sgl.b() + [ubkb], at.b(jj))
                        for d in range(16):
                            it += 1
                            wd, sd_ = self.wload(wdr, wd_d[blk, d], FB)
                            dbk, dbkb = self.ps[4 + it % 2], self.psb[4 + it % 2]
                            for jj in range(FB):
                                self.mm(dbk[:, :], wd[:, jj, :], at.r((slice(None), jj, slice(None))), jj == 0, jj == FB - 1,
                                        sd_.b() + at.b(jj), [dbkb])
                            if first:
                                self.copy("dve", a3[:, d, :], dbk[:, :], [dbkb], xa.b())
                            else:
                                self.tt(a3[:, d, :], a3[:, d, :], dbk[:, :], ALU.add, xa.b() + [dbkb], xa.b())
                        first = False
                for d2 in range(16):
                    xr = xring.next()
                    fw.dma("sp", xr[:], self.xT[d2, :, gs], reads=[self.b_x[g][d2]], writes=xr.b())
                    self.stt(a3[:, d2, :], a3[:, d2, :], mp[:, 5, d2:d2 + 1, cd], xr[:], ALU.mult, ALU.add,
                             xa.b() + mp.b() + xr.b(), xa.b())
                    if not final:
                        fw.dma("sp", self.xT[d2, :, gs], a3[:, d2, :], reads=xa.b(), writes=[self.b_x[g][d2]])
                if final:
                    self.norm_stats(a3, xa.b(), 16, D, sqring, rstd, self.ps[7], self.psb[7])
                    for d2 in range(16):
                        xr = xring.next()
                        col = R_FG + d2
                        self.stt(xr[:], a3[:, d2, :], self.vT[:, col:col + 1], rstd[:], ALU.mult, ALU.mult,
                                 xa.b() + self.vT.b() + rstd.b(), xr.b())
                        fw.dma("sp", self.o_yT[d2, :, gs], xr[:], reads=xr.b())
            fw.emit()

    def phase_moe_sparse(self, l, final=False, slots=None, groups=None):
        nc, fw = self.nc, self.fw
        I32 = mybir.dt.int32
        mp = self.modp[l]
        NTI = NT // 128
        hTok, Ybuf, Tab = self.hTok, self.Ybuf, self.Tab
        b_hTok, b_Y, b_Tab = Buf(), Buf(), Buf()
        B3 = [128, NTI, 8]

        def tred(out, in_, op, reads, writes):
            fw.op("dve", lambda: nc.vector.tensor_reduce(out=out, in_=in_, axis=AX.X, op=op), reads=reads, writes=writes)

        with contextlib.ExitStack() as es0:
            esc = Tile(nc, es0, "esc", [128, 2 * NS])
            self.epsb = Tile(nc, es0, "epsb5", [128, 1])
            fw.op("dve", lambda: nc.vector.memset(self.epsb[:], EPS), writes=self.epsb.b())
            with contextlib.ExitStack() as es:
                xa = Tile(nc, es, "xa5", [128, 16 * 512])
                hT = Tile(nc, es, "hT5", [128, 16, 512], split=True)
                sqring = Ring(nc, es, "sq5", [128, 512], 2)
                tmpring = Ring(nc, es, "tmp5", [128, 512], 2)
                rstd = Tile(nc, es, "rstd5", [128, 512])
                hst = Ring(nc, es, "hst5", [128, 2048], 2)
                rt = Tile(nc, es, "rt5", [128, 16, 8])
                Um = Tile(nc, es, "Um", [128, 128])
                Lg = Tile(nc, es, "Lg", B3)
                eq1 = Tile(nc, es, "eq1", B3)
                sel = Tile(nc, es, "sel", B3)
                wk = Tile(nc, es, "wk", B3)
                wk2 = Tile(nc, es, "wk2", B3)
                gt = Tile(nc, es, "gt", B3)
                pos = Tile(nc, es, "pos", B3)
                tot = Tile(nc, es, "tot", B3)
                offs = Tile(nc, es, "offs", B3)
                m1 = Tile(nc, es, "m1", [128, NTI, 1])
                m2 = Tile(nc, es, "m2", [128, NTI, 1])
                sm = Tile(nc, es, "sm5", [128, 96])
                tokid = Tile(nc, es, "tokid", [128, NTI])
                svals = Tile(nc, es, "svals", [128, NS])
                rr = Tile(nc, es, "rr", [128, 2 * NTI])
                zero = Tile(nc, es, "zero5", [128, 2048])
                recs_t = es.enter_context(nc.sbuf_tensor("sb_recs", [128, 2 * NTI, 16], I32))
                ridx_t = es.enter_context(nc.sbuf_tensor("sb_ridx", [128, 2 * NTI], I32))
                b_recs, b_ridx = Buf(), Buf()
                fw.dma("pool", rt.r(slice(None)), self.I("router"), writes=rt.b())
                fw.dma("pool", Um.r(slice(None)), self.I("Umat"), writes=Um.b())
                fw.dma("sp", tokid[:], self.I("tokid"), writes=tokid.b())
                fw.dma("sp", svals[:], self.I("svals"), writes=svals.b())
                fw.dma("sp", Tab[:, :], self.I("Tab0", I32), writes=[b_Tab])
                fw.op("dve", lambda: nc.vector.memset(zero[:], 0.0), writes=zero.b())
                fw.op("dve", lambda: nc.vector.memset(recs_t[:], 0), writes=[b_recs])
                fw.dma("sp", hTok[NT:NT + 128, :], zero[:], reads=zero.b(), writes=[b_hTok])
                for g in range(NGRP):
                    self.load_x(g, xa)
                    self.norm_mod(g, l, 1, xa, hT, sqring, tmpring, rstd, self.ps[7], self.psb[7])
                    for t in range(4):
                        lb, lbb = self.ps[6], self.psb[6]
                        for k in range(16):
                            self.mm(lb[:, t * 8:(t + 1) * 8], hT.r((slice(None), k, slice(t * 128, (t + 1) * 128))),
                                    rt.r((slice(None), k, slice(None))), k == 0, k == 15, hT.b(k) + rt.b(), [lbb])
                        self.copy("dve", Lg[:, g * 4 + t, :], lb[:, t * 8:(t + 1) * 8], [lbb], Lg.b())
                        hs = hst.next()
                        for c4 in range(4):
                            tb, tbb = self.ps[c4], self.psb[c4]
                            for ci in range(4):
                                c = c4 * 4 + ci
                                self.tr(tb[:, ci * 128:(ci + 1) * 128], hT[:, c, t * 128:(t + 1) * 128], hT.b(c), [tbb])
                            self.copy(self.evac_eng(), hs[:, c4 * 512:(c4 + 1) * 512], tb[:, :], [tbb], hs.b())
                        r0 = g * 512 + t * 128
                        fw.dma("sp", hTok[r0:r0 + 128, :], hs[:], reads=hs.b(), writes=[b_hTok])
                tred(m1[:], Lg[:], ALU.max, Lg.b(), m1.b())
                self.tt(eq1[:], Lg[:], m1[:].to_broadcast(B3), ALU.is_equal, Lg.b() + m1.b(), eq1.b())
                self.stt(wk[:], eq1[:], -1e30, Lg[:], ALU.mult, ALU.add, eq1.b() + Lg.b(), wk.b())
                tred(m2[:], wk[:], ALU.max, wk.b(), m2.b())
                self.tt(sel.r(slice(None)), Lg[:], m2[:].to_broadcast(B3), ALU.is_ge, Lg.b() + m2.b(), sel.b())
                self.tt(wk[:], Lg[:], m1[:].to_broadcast(B3), ALU.subtract, Lg.b() + m1.b(), wk.b())
                self.act(wk[:], wk[:], AF.Exp, wk.b(), wk.b())
                self.tt(wk[:], wk[:], sel[:], ALU.mult, wk.b() + sel.b(), wk.b())
                tred(m2[:], wk[:], ALU.add, wk.b(), m2.b())
                self.recip(m2[:], m2[:], m2.b(), m2.b())
                self.tt(gt[:], wk[:], m2[:].to_broadcast(B3), ALU.mult, wk.b() + m2.b(), gt.b())
                pb_, pbb_ = self.ps[0], self.psb[0]
                tb_, tbb_ = self.ps[1], self.psb[1]
                for t in range(NTI):
                    self.mm(pb_[:, t * 8:(t + 1) * 8], Um.r(slice(None)), sel.r((slice(None), t, slice(None))), True, True,
                            Um.b() + sel.b(), [pbb_], sync=(t == NTI - 1))
                for t in range(NTI):
                    self.mm(tb_[:, t * 8:(t + 1) * 8], self.ones.r(slice(None)), sel.r((slice(None), t, slice(None))), True, True,
                            self.ones.b() + sel.b(), [tbb_], sync=(t == NTI - 1))
                self.copy("dve", pos.t[:].rearrange("p a e -> p (a e)"), pb_[:, 0:NTI * 8], [pbb_], pos.b())
                self.copy("act", tot.t[:].rearrange("p a e -> p (a e)"), tb_[:, 0:NTI * 8], [tbb_], tot.b())
                fw.op("dve", lambda: nc.vector.memset(offs[:, 0, :], 0.0), writes=offs.b())
                for t in range(1, NTI):
                    self.tt(offs[:, t, :], offs[:, t - 1, :], tot[:, t - 1, :], ALU.add, offs.b() + tot.b(), offs.b())
                self.tt(pos[:], pos[:], offs[:], ALU.add, pos.b() + offs.b(), pos.b())
                cnt, nsl, tmp8 = sm[:, 0:8], sm[:, 8:16], sm[:, 24:32]
                self.tt(cnt, offs[:, NTI - 1, :], tot[:, NTI - 1, :], ALU.add, offs.b() + tot.b(), sm.b())
                self.ts(nsl, cnt, 0.0, None, ALU.is_gt, None, sm.b(), sm.b())
                for kk in range(1, 5):
                    self.ts(tmp8, cnt, float(512 * kk), None, ALU.is_gt, None, sm.b(), sm.b())
                    self.tt(nsl, nsl, tmp8, ALU.add, sm.b(), sm.b())
                fw.op("dve", lambda: nc.vector.memset(sm[:, 16:17], 0.0), writes=sm.b())
                for e in range(1, NEXP):
                    self.tt(sm[:, 16 + e:17 + e], sm[:, 15 + e:16 + e], sm[:, 7 + e:8 + e], ALU.add, sm.b(), sm.b())
                self.ts(sm[:, 32:40], sm[:, 16:24], 512.0, None, ALU.mult, None, sm.b(), sm.b())
                self.tt(pos[:], pos[:], sm[:, 32:40].rearrange("p (a e) -> p a e", a=1).to_broadcast(B3), ALU.add,
                        pos.b() + sm.b(), pos.b())
                self.tt(wk2[:], sel[:], eq1[:], ALU.subtract, sel.b() + eq1.b(), wk2.b())
                rv = rr.t[:].rearrange("p (k a) -> p k a", k=2)
                recs_f = recs_t[:].bitcast(F32)
                for k_, oh in ((0, eq1), (1, wk2)):
                    ks = slice(k_ * NTI, (k_ + 1) * NTI)
                    self.tt(wk[:], pos[:], oh[:], ALU.mult, pos.b() + oh.b(), wk.b())
                    tred(rv[:, k_, :], wk[:], ALU.add, wk.b(), rr.b())
                    fw.op("dve", lambda ks=ks: nc.vector.tensor_copy(out=recs_t[:, ks, 0], in_=tokid[:]),
                          reads=tokid.b(), writes=[b_recs])
                    fw.op("dve", lambda ks=ks, k_=k_: nc.vector.tensor_scalar(out=recs_t[:, ks, 1], in0=tokid[:],
                                                                             scalar1=float(k_ * NT), scalar2=None, op0=ALU.add),
                          reads=tokid.b(), writes=[b_recs])
                    self.tt(wk[:], gt[:], oh[:], ALU.mult, gt.b() + oh.b(), wk.b())
                    tred(recs_f[:, ks, 2], wk[:], ALU.add, wk.b(), [b_recs])
                fw.op("dve", lambda: nc.vector.tensor_copy(out=ridx_t[:], in_=rr[:]), reads=rr.b(), writes=[b_ridx])
                for i in range(2 * NTI):
                    fw.dma_custom("pool", lambda i=i: nc.gpsimd.indirect_dma_start(
                        out=Tab[:, :], out_offset=bass.IndirectOffsetOnAxis(ap=ridx_t[:, i:i + 1], axis=0),
                        in_=recs_t[:, i, :], in_offset=None), reads=[b_recs, b_ridx], writes=[b_Tab])
                ev = sm[:, 40:40 + NS]
                tmpS = sm[:, 60:60 + NS]
                fw.op("dve", lambda: nc.vector.memset(ev, -1.0), writes=sm.b())
                for e in range(NEXP):
                    self.ts(tmpS, svals[:], sm[:, 16 + e:17 + e], None, ALU.is_ge, None, svals.b() + sm.b(), sm.b())
                    self.tt(ev, ev, tmpS, ALU.add, sm.b(), sm.b())
                self.ts(esc[:, 0:NS], ev, float(NFF * 128), None, ALU.mult, None, sm.b(), esc.b())
                self.ts(esc[:, NS:2 * NS], ev, float(64 * 128), None, ALU.mult, None, sm.b(), esc.b())
                fw.emit()
            with contextlib.ExitStack() as es:
                hTs = Tile(nc, es, "hTs", [128, 16, 512], split=True)
                acc = Tile(nc, es, "acc5", [128, 16 * 512])
                aT = [Tile(nc, es, "aT5_%d" % i, [128, FB, 512], split=True) for i in range(2)]
                wgu = Ring(nc, es, "wgu5", [128, 2048], 4)
                wdr = Ring(nc, es, "wdr5", [128, FB * 128], 3)
                sglr = Ring(nc, es, "sgl5", [128, 512], 2)
                gth = Ring(nc, es, "gth", [128, 2048], 2)
                otl = Ring(nc, es, "otl", [128, 2048], 2)
                wbase = Tile(nc, es, "wbase", [128, NFF])
                wdbase = Tile(nc, es, "wdbase", [128, 64])
                widx_t = [es.enter_context(nc.sbuf_tensor("sb_widx%d" % i, [128, NFF + 64], I32)) for i in range(2)]
                b_widx = [Buf(), Buf()]
                rbs_t = [es.enter_context(nc.sbuf_tensor("sb_rbs%d" % i, [128, 4, 16], I32)) for i in range(2)]
                b_rbs = [Buf(), Buf()]
                fw.dma("sp", wbase[:], self.I("wbase"), writes=wbase.b())
                fw.dma("sp", wdbase[:], self.I("wdbase"), writes=wdbase.b())
                a3 = acc.t[:].rearrange("p (c n) -> p c n", c=16)
                wg_rows = self.I("moe_g").rearrange("e j p k m -> (e j p) (k m)")
                wu_rows = self.I("moe_u").rearrange("e j p k m -> (e j p) (k m)")
                wd_rows = self.I("moe_d").rearrange("e b d p j m -> (e b d p) (j m)")
                it = 0
                for s in (slots if slots is not None else range(NS)):
                    widx, bw = widx_t[s % 2], b_widx[s % 2]
                    rbs, brb = rbs_t[s % 2], b_rbs[s % 2]
                    fw.op("dve", lambda widx=widx, s=s: nc.vector.tensor_scalar(
                        out=widx[:, 0:NFF], in0=wbase[:], scalar1=esc[:, s:s + 1], scalar2=None, op0=ALU.add),
                        reads=wbase.b() + esc.b(), writes=[bw])
                    fw.op("dve", lambda widx=widx, s=s: nc.vector.tensor_scalar(
                        out=widx[:, NFF:NFF + 64], in0=wdbase[:], scalar1=esc[:, NS + s:NS + s + 1], scalar2=None, op0=ALU.add),
                        reads=wdbase.b() + esc.b(), writes=[bw])
                    fw.dma("sp", rbs[:], Tab[s * 512:(s + 1) * 512, :].rearrange("(q p) c -> p q c", p=128),
                           reads=[b_Tab], writes=[brb])
                    for q in range(4):
                        gtile = gth.next()
                        fw.dma_custom("pool", lambda gtile=gtile, rbs=rbs, q=q: nc.gpsimd.indirect_dma_start(
                            out=gtile[:], out_offset=None, in_=hTok[:, :],
                            in_offset=bass.IndirectOffsetOnAxis(ap=rbs[:, q, 0:1], axis=0)),
                            reads=[brb, b_hTok], writes=gtile.b())
                        for c4 in range(4):
                            tb, tbb = self.ps[6 + c4 % 2], self.psb[6 + c4 % 2]
                            for ci in range(4):
                                c = c4 * 4 + ci
                                self.tr(tb[:, ci * 128:(ci + 1) * 128], gtile[:, c * 128:(c + 1) * 128], gtile.b(), [tbb])
                            self.copy(self.evac_eng(), hTs.r((slice(None), slice(c4 * 4, c4 * 4 + 4), slice(q * 128, (q + 1) * 128))),
                                      tb[:, :].rearrange("p (a n) -> p a n", a=4), [tbb],
                                      hTs.b(c4 * 4) + hTs.b(c4 * 4 + 1) + hTs.b(c4 * 4 + 2) + hTs.b(c4 * 4 + 3))

                    def wgather(ring, rows, col, nelem, widx=widx, bw=bw):
                        slot = ring.next()
                        dst = slot.t[:, 0:nelem].bitcast(F32R)
                        fw.dma_custom("pool", lambda: nc.gpsimd.indirect_dma_start(
                            out=dst, out_offset=None, in_=rows,
                            in_offset=bass.IndirectOffsetOnAxis(ap=widx[:, col:col + 1], axis=0)),
                            reads=[bw], writes=slot.b())
                        return slot
                    for blk in range(NFF // FB):
                        at = aT[blk % 2]
                        for jj in range(FB):
                            j = blk * FB + jj
                            it += 1
                            sg_ = wgather(wgu, wg_rows, j, 2048)
                            wg = sg_.t[:].rearrange("p (k m) -> p k m", k=16).bitcast(F32R)
                            gbk, gbkb = self.ps[it % 2], self.psb[it % 2]
                            for k in range(16):
                                self.mm(gbk[:, :], wg[:, k, :], hTs.r((slice(None), k, slice(None))), k == 0, k == 15,
                                        sg_.b() + hTs.b(k), [gbkb])
                            su_ = wgather(wgu, wu_rows, j, 2048)
                            wu = su_.t[:].rearrange("p (k m) -> p k m", k=16).bitcast(F32R)
                            ubk, ubkb = self.ps[2 + it % 2], self.psb[2 + it % 2]
                            for k in range(16):
                                self.mm(ubk[:, :], wu[:, k, :], hTs.r((slice(None), k, slice(None))), k == 0, k == 15,
                                        su_.b() + hTs.b(k), [ubkb])
                            sgl = sglr.next()
                            self.act(sgl[:], gbk[:, :], AF.Silu, [gbkb], sgl.b())
                            self.tt(at.r((slice(None), jj, slice(None))), sgl[:], ubk[:, :], ALU.mult, sgl.b() + [ubkb], at.b(jj))
                        for d in range(16):
                            it += 1
                            sd_ = wgather(wdr, wd_rows, NFF + blk * 16 + d, FB * 128)
                            wd = sd_.t[:].rearrange("p (k m) -> p k m", k=FB).bitcast(F32R)
                            dbk, dbkb = self.ps[4 + it % 2], self.psb[4 + it % 2]
                            for jj in range(FB):
                                self.mm(dbk[:, :], wd[:, jj, :], at.r((slice(None), jj, slice(None))), jj == 0, jj == FB - 1,
                                        sd_.b() + at.b(jj), [dbkb])
                            if blk == 0:
                                self.copy("dve", a3[:, d, :], dbk[:, :], [dbkb], acc.b())
                            else:
                                self.tt(a3[:, d, :], a3[:, d, :], dbk[:, :], ALU.add, acc.b() + [dbkb], acc.b())
                    rbs_f = rbs[:].bitcast(F32)
                    for q in range(4):
                        ot = otl.next()
                        for c4 in range(4):
                            tb, tbb = self.ps[6 + c4 % 2], self.psb[6 + c4 % 2]
                            for ci in range(4):
                                c = c4 * 4 + ci
                                self.tr(tb[:, ci * 128:(ci + 1) * 128], a3[:, c, q * 128:(q + 1) * 128], acc.b(), [tbb])
                            self.ts(ot[:, c4 * 512:(c4 + 1) * 512], tb[:, :], rbs_f[:, q, 2:3], None, ALU.mult, None,
                                    [tbb, brb], ot.b())
                        fw.dma_custom("pool", lambda ot=ot, rbs=rbs, q=q: nc.gpsimd.indirect_dma_start(
                            out=Ybuf[:, :], out_offset=bass.IndirectOffsetOnAxis(ap=rbs[:, q, 1:2], axis=0),
                            in_=ot[:], in_offset=None), reads=ot.b() + [brb], writes=[b_Y])
                fw.emit()
            with contextlib.ExitStack() as es:
                ys = Ring(nc, es, "ys", [128, 2048], 4)
                y2 = Ring(nc, es, "y2", [128, 2048], 2)
                xg = Tile(nc, es, "xg6", [128, 16 * 512])
                sqring = Ring(nc, es, "sq6", [128, 512], 2)
                rstd = Tile(nc, es, "rstd6", [128, 512])
                xring = Ring(nc, es, "xr6", [128, 512], 3)
                x3 = xg.t[:].rearrange("p (c n) -> p c n", c=16)
                for g in (groups or range(NGRP)):
                    cd = 0 if g < 4 else 1
                    gs = slice(g * G, (g + 1) * G)
                    self.load_x(g, xg)
                    yt = []
                    for t in range(4):
                        r0 = g * 512 + t * 128
                        a, b2 = ys.next(), y2.next()
                        fw.dma("sp", a[:], Ybuf[r0:r0 + 128, :], reads=[b_Y], writes=a.b())
                        fw.dma("sp", b2[:], Ybuf[NT + r0:NT + r0 + 128, :], reads=[b_Y], writes=b2.b())
                        self.tt(a[:], a[:], b2[:], ALU.add, a.b() + b2.b(), a.b(), en="pool" if t % 2 else "dve")
                        yt.append(a)
                    for c in range(16):
                        tb, tbb = self.ps[c % 4], self.psb[c % 4]
                        for t in range(4):
                            self.tr(tb[:, t * 128:(t + 1) * 128], yt[t][:, c * 128:(c + 1) * 128], yt[t].b(), [tbb])
                        self.stt(x3[:, c, :], tb[:, :], mp[:, 5, c:c + 1, cd], x3[:, c, :], ALU.mult, ALU.add,
                                 [tbb] + mp.b() + xg.b(), xg.b())
                        if not final:
                            fw.dma("sp", self.xT[c, :, gs], x3[:, c, :], reads=xg.b(), writes=[self.b_x[g][c]])
                    if final:
                        self.norm_stats(x3, xg.b(), 16, D, sqring, rstd, self.ps[7], self.psb[7])
                        for d2 in range(16):
                            xr = xring.next()
                            col = R_FG + d2
                            self.stt(xr[:], x3[:, d2, :], self.vT[:, col:col + 1], rstd[:], ALU.mult, ALU.mult,
                                     xg.b() + self.vT.b() + rstd.b(), xr.b())
                            fw.dma("sp", self.o_yT[d2, :, gs], xr[:], reads=xr.b())
                fw.emit()


def _tiles(W):
    K, M = W.shape
    return np.ascontiguousarray(W.reshape(K // 128, 128, M // 128, 128).transpose(2, 1, 0, 3))


def host_consts():
    c = {}
    c["ident"] = np.eye(128, dtype=np.float32)
    p = np.arange(128)
    d = p % 64
    partner = np.where((d % 32) < 16, p + 16, p - 16)
    pm = np.zeros((128, 128), np.float32)
    pm[partner, p] = 1.0
    c["permM"] = pm
    quarter = 16
    inv = (np.float32(10000.0) ** (-np.arange(quarter, dtype=np.float32) / np.float32(quarter))).astype(np.float32)
    n = np.arange(2048)
    rr = (n // 64).astype(np.float32)
    cc = (n % 64).astype(np.float32)
    ang_r = (rr[:, None] * inv[None, :]).astype(np.float32)
    ang_c = (cc[:, None] * inv[None, :]).astype(np.float32)
    C = np.zeros((128, 2048), np.float32)
    S = np.zeros((128, 2048), np.float32)
    for pp in range(128):
        dd = pp % 64
        j = dd % 16
        ang = ang_r[:, j] if dd < 32 else ang_c[:, j]
        C[pp] = np.cos(ang)
        sgn = -1.0 if (dd % 32) < 16 else 1.0
        S[pp] = sgn * np.sin(ang)
    c["ropeC"] = C
    c["ropeS"] = S
    r = np.arange(128)[:, None]
    cidx = np.arange(128)[None, :]
    m = np.zeros((128, 384), np.float32)
    m[:, 0:128] = np.where(cidx >= r, 0.0, -1e30)
    m[:, 256:384] = np.where(cidx <= r, 0.0, -1e30)
    c["maskA"] = m
    c["Umat"] = np.triu(np.ones((128, 128), np.float32), k=1)
    pp = np.arange(128, dtype=np.float32)[:, None]
    c["tokid"] = (np.arange(NT // 128, dtype=np.float32)[None, :] * 128 + pp).astype(np.float32)
    c["svals"] = np.tile(np.arange(NS, dtype=np.float32)[None, :], (128, 1))
    c["wbase"] = (np.arange(NFF, dtype=np.float32)[None, :] * 128 + pp).astype(np.float32)
    c["wdbase"] = (np.arange(64, dtype=np.float32)[None, :] * 128 + pp).astype(np.float32)
    tab0 = np.zeros((NS * 512, 16), np.int32)
    tab0[:, 0] = NT
    tab0[:, 1] = 2 * NT + (np.arange(NS * 512) % 128)
    c["Tab0"] = tab0
    for nm, nseq in (("invc_s", 2048), ("invc_p", 256)):
        t = np.arange(nseq)
        tab = np.zeros((4, nseq), np.float32)
        for gi, win in enumerate(POOL_WINDOWS):
            left = win // 2
            right = win - left - 1
            lo = np.maximum(t - left, 0)
            hi = np.minimum(t + right, nseq - 1) + 1
            tab[gi] = (1.0 / (hi - lo).astype(np.float32)).astype(np.float32)
        c[nm] = tab
    return c


def prep_shared(inp):
    sh = dict(host_consts())
    w_in = inp["w_in"]
    colsA = np.concatenate([
        np.arange(0, 1024),
        np.arange(1024, 1088), np.arange(1024, 1088),
        np.arange(1088, 1152), np.arange(1088, 1152),
        np.arange(1152, 1280),
        np.arange(1280, 1792),
        np.arange(1792, 2048),
        np.arange(2048, 2112), np.arange(2048, 2112),
        np.arange(2112, 3136)])
    sh["w_ada"] = np.stack([_tiles(inp["w_ada"][l]) for l in range(DEPTH)])
    sh["w_inA"] = np.stack([_tiles(w_in[l][:, colsA]) for l in range(DEPTH)])
    sh["w_inG"] = np.stack([_tiles(w_in[l][:, 3136:]) for l in range(DEPTH)])
    cq = [np.arange(h * 192, h * 192 + 128) for h in range(8)]
    for j in range(4):
        cq.append(np.concatenate([np.arange((2 * j) * 192 + 128, (2 * j) * 192 + 192),
                                  np.arange((2 * j + 1) * 192 + 128, (2 * j + 1) * 192 + 192)]))
    cq = np.concatenate(cq)
    sh["w_uq"] = np.stack([_tiles(inp["w_uq"][l][:, cq]) for l in range(DEPTH)])
    ckv = np.concatenate([np.arange(h * 256, h * 256 + 128) for h in range(8)] +
                         [np.arange(h * 256 + 128, h * 256 + 256) for h in range(8)])
    sh["w_ukv"] = np.stack([_tiles(inp["w_ukv"][l][:, ckv]) for l in range(DEPTH)])
    sh["poolw"] = np.stack([np.concatenate([_tiles(inp["pool_w"][l][g]) for g in range(4)]) for l in range(DEPTH)])
    sh["wbr"] = np.stack([np.stack([_tiles(inp[k][l]) for k in ("w_branch_a", "w_branch_b", "w_branch_c")])
                          for l in range(DEPTH)])
    sh["wout"] = np.stack([_tiles(inp["w_out"][l]) for l in range(DEPTH)])
    sh["ffn_g"] = _tiles(inp["ffn_w_gate"][0])
    sh["ffn_u"] = _tiles(inp["ffn_w_up"][0])

    def dtiles(wd):
        return np.ascontiguousarray(wd.reshape(4, FB, 128, 16, 128).transpose(0, 3, 2, 1, 4))
    sh["ffn_d"] = dtiles(inp["ffn_w_down"][0])
    sh["router"] = np.ascontiguousarray(inp["router_w"][0].reshape(16, 128, 8).transpose(1, 0, 2))
    sh["moe_g"] = np.stack([_tiles(inp["moe_w_gate"][0][e]) for e in range(NEXP)])
    sh["moe_u"] = np.stack([_tiles(inp["moe_w_up"][0][e]) for e in range(NEXP)])
    sh["moe_d"] = np.stack([dtiles(inp["moe_w_down"][0][e]) for e in range(NEXP)])
    sh["sink"] = np.ascontiguousarray(inp["attn_sink"])
    return sh


def prep_core(inp, i):
    m = {}
    x = np.concatenate([inp["x_sample"][i], inp["x_prompt"][2 * i], inp["x_prompt"][2 * i + 1]], axis=0)
    m["xinT"] = np.ascontiguousarray(x.T.reshape(16, 128, NT))
    v = np.zeros((NROWS, 128), np.float32)
    v[R_C:R_C + 16] = inp["c"][i].reshape(16, 128)
    v[R_CCTX:R_CCTX + 16] = inp["c_ctx"].reshape(16, 128)
    for l in range(DEPTH):
        v[R_LN1(l):R_LN1(l) + 16] = inp["ln1_g"][l].reshape(16, 128)
        v[R_LN2(l):R_LN2(l) + 16] = inp["ln2_g"][l].reshape(16, 128)
        v[R_BADA(l):R_BADA(l) + 96] = inp["b_ada"][l].reshape(96, 128)
        v[R_QG(l):R_QG(l) + 4] = inp["mla_q_norm_g"][l].reshape(4, 128)
        v[R_KVG(l):R_KVG(l) + 2] = inp["mla_kv_norm_g"][l].reshape(2, 128)
        v[R_PSC(l):R_PSC(l) + 8] = inp["pool_scale"][l].reshape(8, 128)
    v[R_FG:R_FG + 16] = inp["final_g"].reshape(16, 128)
    m["vecs"] = v
    ck = inp["cache_attn_k"][i]
    ckT = ck.transpose(0, 2, 3, 1)
    m["ck2T"] = np.ascontiguousarray(np.concatenate([ckT, ckT], axis=2))
    m["cv"] = np.ascontiguousarray(inp["cache_attn_v"][i].reshape(DEPTH, 512, 128))
    m["cckvT"] = np.ascontiguousarray(inp["cache_mla_ckv"][i].transpose(0, 2, 1).reshape(DEPTH, 2, 128, 512))
    krT = inp["cache_mla_krope"][i].transpose(0, 2, 1)
    m["ckr2T"] = np.ascontiguousarray(np.concatenate([krT, krT], axis=1))
    return m


def build_program():
    P = Prog()
    P.alloc_persistent()
    P.phase_prologue()
    for l in range(DEPTH):
        P.phase_proj(l)
        P.phase_attnA(l)
        P.phase_mla(l)
        P.phase_pool(l)
        P.phase_merge(l)
        if l % 2 == 1:
            P.phase_moe_sparse(l, final=(l == DEPTH - 1))
        else:
            P.phase_ffn(l, final=(l == DEPTH - 1))
    return P


def kernel(**inputs):
    inp = {k: np.asarray(v, dtype=np.float32) for k, v in inputs.items()}
    P = build_program()
    sh = prep_shared(inp)
    in_maps = []
    for i in range(NCORES):
        m = prep_core(inp, i)
        m.update(sh)
        in_maps.append({k: m[k] for k in P.din})
    res = run_bass_kernel_spmd(P.nc, in_maps, core_ids=list(range(NCORES)))
    y_prompt = np.zeros((16, 256, D), np.float32)
    y_sample = np.zeros((8, 2048, D), np.float32)
    st_k = np.zeros((16, DEPTH, 256, 2, 64), np.float32)
    st_v = np.zeros((16, DEPTH, 256, 2, 64), np.float32)
    st_ckv = np.zeros((16, DEPTH, 256, 256), np.float32)
    st_kr = np.zeros((16, DEPTH, 256, 64), np.float32)
    for i in range(NCORES):
        r = res.results[i]
        y = np.asarray(r["o_yT"]).reshape(D, NT).T
        y_sample[i] = y[0:2048]
        okT = np.asarray(r["o_kT"])
        ov = np.asarray(r["o_v"]).reshape(DEPTH, 512, 128)
        ockv = np.asarray(r["o_ckvT"]).reshape(DEPTH, 256, 512)
        okr = np.asarray(r["o_krT"])
        for j in range(2):
            b = 2 * i + j
            ts = slice(j * 256, (j + 1) * 256)
            y_prompt[b] = y[2048 + j * 256:2048 + (j + 1) * 256]
            st_k[b] = okT[:, :, :, ts].transpose(0, 3, 1, 2)
            st_v[b] = ov[:, ts, :].reshape(DEPTH, 256, 2, 64)
            st_ckv[b] = ockv[:, :, ts].transpose(0, 2, 1)
            st_kr[b] = okr[:, :, ts].transpose(0, 2, 1)
    return (y_prompt, y_sample, st_k, st_v, st_ckv, st_kr)
```

```python
import contextlib
import numpy as np
import concourse.bass as bass
import concourse.mybir as mybir
from concourse.bass_utils import run_bass_kernel_spmd

F32 = mybir.dt.float32
F32R = mybir.dt.float32r
BF16 = mybir.dt.bfloat16
AF = mybir.ActivationFunctionType
ALU = mybir.AluOpType
AX = mybir.AxisListType

NCORES = 8
D = 2048
NT = 2560
G = 512
NGRP = 5
DEPTH = 2
EPS = 1e-6
NFF = 44
FB = 11
NEXP = 8
NS = 17
DBG_GROUPS = None
SEQS = ((0, 2048, True), (2048, 256, False), (2304, 256, False))
POOL_WINDOWS = (2, 4, 8, 16)

R_C, R_CCTX = 0, 16


def R_LN1(l): return 32 + l * 142
def R_LN2(l): return 32 + l * 142 + 16
def R_BADA(l): return 32 + l * 142 + 32
def R_QG(l): return 32 + l * 142 + 128
def R_KVG(l): return 32 + l * 142 + 132
def R_PSC(l): return 32 + l * 142 + 134


R_FG = 32 + 2 * 142
NROWS = 384


class Buf:
    __slots__ = ("w", "r")

    def __init__(self):
        self.w = None
        self.r = {}


class Eng:
    def __init__(self, name, eng, sem):
        self.name = name
        self.eng = eng
        self.sem = sem
        self.cnt = 0
        self.seen = {}
        self.dsems = []
        self.dvals = []
        self.dn = 0
        self.pend = []
        self.prog = []


class FW:
    def __init__(self, nc, es, ndma_sems=8):
        self.nc = nc
        self.engs = {}
        for name, eng in (("pe", nc.tensor), ("act", nc.scalar), ("dve", nc.vector),
                          ("pool", nc.gpsimd), ("sp", nc.sync)):
            sem = es.enter_context(nc.semaphore("s_" + name))
            self.engs[name] = Eng(name, eng, sem)
        for qn in ("sp", "pool"):
            e = self.engs[qn]
            for i in range(ndma_sems):
                e.dsems.append(es.enter_context(nc.semaphore("d_%s%d" % (qn, i))))
                e.dvals.append(0)
        self.ninst = 0

    def _wait(self, e, ticket):
        sem, val, owner = ticket
        if owner is e:
            if e.name == "pe" or val > e.cnt:
                return
        key = id(sem)
        if e.seen.get(key, 0) >= val:
            return
        e.pend.append((sem, val))
        e.seen[key] = val
        self.ninst += 1

    def _deps(self, e, reads, writes):
        for b in reads:
            if b.w is not None:
                self._wait(e, b.w)
        for b in writes:
            if b.w is not None:
                self._wait(e, b.w)
            for t in b.r.values():
                self._wait(e, t)

    def _mark(self, e, ticket, reads, writes, key=None):
        for b in reads:
            b.r[key or e.name] = ticket
        for b in writes:
            b.w = ticket
            b.r = {}

    def op(self, en, fn, reads=(), writes=(), sync=True):
        e = self.engs[en]
        self._deps(e, reads, writes)
        self.ninst += 1
        waits, e.pend = e.pend, []
        if sync:
            e.cnt += 1
            e.prog.append((waits, fn, e.sem, 1))
            t = (e.sem, e.cnt, e)
        else:
            e.prog.append((waits, fn, None, 0))
            t = (e.sem, e.cnt + 1, e)
        self._mark(e, t, reads, writes)

    def dma(self, qn, out, in_, reads=(), writes=(), **kw):
        e = self.engs[qn]
        slot = e.dn % len(e.dsems)
        e.dn += 1
        sem = e.dsems[slot]
        if e.dvals[slot] > 0:
            self._wait(e, (sem, e.dvals[slot], None))
        self._deps(e, reads, writes)
        e.dvals[slot] += 16
        eng = e.eng
        waits, e.pend = e.pend, []
        e.prog.append((waits, (lambda: eng.dma_start(out=out, in_=in_, **kw)), sem, 16))
        self.ninst += 1
        self._mark(e, (sem, e.dvals[slot], None), reads, writes, key=(e.name, slot))

    def dma_custom(self, qn, fn, reads=(), writes=()):
        e = self.engs[qn]
        slot = e.dn % len(e.dsems)
        e.dn += 1
        sem = e.dsems[slot]
        if e.dvals[slot] > 0:
            self._wait(e, (sem, e.dvals[slot], None))
        self._deps(e, reads, writes)
        e.dvals[slot] += 16
        waits, e.pend = e.pend, []
        e.prog.append((waits, fn, sem, 16))
        self.ninst += 1
        self._mark(e, (sem, e.dvals[slot], None), reads, writes, key=(e.name, slot))

    def emit(self):
        sp = self.engs["sp"]
        for qn in ("sp", "pool"):
            q = self.engs[qn]
            for s, v in zip(q.dsems, q.dvals):
                if v > 0:
                    self._wait(sp, (s, v, None))
        for en in ("pe", "act", "dve", "pool"):
            e = self.engs[en]
            if e.cnt > 0:
                self._wait(sp, (e.sem, e.cnt, None))

        def replay(e):
            eng = e.eng
            for waits, fn, sem, inc in e.prog:
                for s, v in waits:
                    eng.wait_ge(s, v)
                ins = fn()
                if sem is not None:
                    ins.then_inc(sem, inc)
            for s, v in e.pend:
                eng.wait_ge(s, v)
            e.prog = []
            e.pend = []

        with self.nc.Block() as block:
            @block.sync
            def _(x):
                replay(self.engs["sp"])

            @block.tensor
            def _(x):
                replay(self.engs["pe"])

            @block.scalar
            def _(x):
                replay(self.engs["act"])

            @block.vector
            def _(x):
                replay(self.engs["dve"])

            @block.gpsimd
            def _(x):
                replay(self.engs["pool"])


class Tile:
    _n = [0]

    def __init__(self, nc, es, name, shape, split=False, dt=None):
        Tile._n[0] += 1
        self.t = es.enter_context(nc.sbuf_tensor("sb%d_%s" % (Tile._n[0], name), list(shape), dt or F32))
        self.split = split
        if split:
            self.bufs = [Buf() for _ in range(shape[1])]
        else:
            self.bufs = [Buf()]

    def __getitem__(self, idx):
        return self.t[idx]

    def r(self, idx):
        return self.t[idx].bitcast(F32R)

    def b(self, i=None):
        if self.split and i is not None:
            return [self.bufs[i]]
        return list(self.bufs)


class Ring:
    def __init__(self, nc, es, name, shape, n, dt=None):
        self.tiles = [Tile(nc, es, "%s%d" % (name, i), shape, dt=dt) for i in range(n)]
        self.i = 0

    def next(self):
        t = self.tiles[self.i % len(self.tiles)]
        self.i += 1
        return t


class Prog:
    def __init__(self, stop_after=None, moe_experts=NEXP, scratch_in=(), moe_sparse=True):
        self.stop_after = stop_after
        self.moe_experts = moe_experts
        self.nc = nc = bass.Bass("TRN2", target_bir_lowering=False)
        self.es = es = contextlib.ExitStack()
        self.fw = FW(nc, es)
        self.din = {}
        self.evac_i = 0

        self.ishapes = {}

        def inp(name, shape):
            self.ishapes[name] = list(shape)

        def outp(name, shape):
            return nc.dram_tensor(name, list(shape), F32, kind="ExternalOutput").ap()

        def scr(name, shape):
            return nc.dram_tensor(name, list(shape), F32, kind="Internal").ap()

        inp("xinT", [16, 128, NT])
        inp("vecs", [NROWS, 128])
        inp("sink", [DEPTH, 16])
        inp("ident", [128, 128])
        inp("permM", [128, 128])
        inp("ropeC", [128, 2048])
        inp("ropeS", [128, 2048])
        inp("maskA", [128, 384])
        inp("invc_s", [4, 2048])
        inp("invc_p", [4, 256])
        inp("ck2T", [DEPTH, 2, 128, 512])
        inp("cv", [DEPTH, 512, 128])
        inp("cckvT", [DEPTH, 2, 128, 512])
        inp("ckr2T", [DEPTH, 128, 512])
        inp("w_ada", [DEPTH, 96, 128, 16, 128])
        inp("w_inA", [DEPTH, 26, 128, 16, 128])
        inp("w_inG", [DEPTH, 48, 128, 16, 128])
        inp("w_uq", [DEPTH, 12, 128, 4, 128])
        inp("w_ukv", [DEPTH, 16, 128, 2, 128])
        inp("poolw", [DEPTH, 8, 128, 2, 128])
        inp("wbr", [DEPTH, 3, 16, 128, 8, 128])
        inp("wout", [DEPTH, 16, 128, 16, 128])
        inp("ffn_g", [NFF, 128, 16, 128])
        inp("ffn_u", [NFF, 128, 16, 128])
        inp("ffn_d", [4, 16, 128, FB, 128])
        inp("router", [128, 16, 8])
        inp("moe_g", [NEXP, NFF, 128, 16, 128])
        inp("moe_u", [NEXP, NFF, 128, 16, 128])
        inp("moe_d", [NEXP, 4, 16, 128, FB, 128])
        inp("Umat", [128, 128])
        inp("tokid", [128, NT // 128])
        inp("svals", [128, NS])
        inp("wbase", [128, NFF])
        inp("wdbase", [128, 64])
        inp("Tab0", [NS * 512, 16])
        self.o_yT = outp("o_yT", [16, 128, NT])
        self.o_kT = outp("o_kT", [DEPTH, 2, 64, 512])
        self.o_v = outp("o_v", [DEPTH, 4, 128, 128])
        self.o_ckvT = outp("o_ckvT", [DEPTH, 2, 128, 512])
        self.o_krT = outp("o_krT", [DEPTH, 64, 512])
        def mk(name, shape):
            if name in scratch_in:
                return nc.dram_tensor(name, list(shape), F32, kind="ExternalInput").ap()
            return (outp if stop_after is not None else scr)(name, shape)
        self.xT = mk("s_xT", [16, 128, NT])
        self.qaT = mk("s_qaT", [8, 128, NT])
        self.kaT2 = mk("s_kaT2", [2, 128, NT])
        self.vaTok = mk("s_vaTok", [NT // 128, 128, 128])
        self.qnT = mk("s_qnT", [8, 128, NT])
        self.qrT = mk("s_qrT", [4, 128, NT])
        self.ckvT = mk("s_ckvT", [2, 128, NT])
        self.krT2 = mk("s_krT2", [128, NT])
        self.uT = mk("s_uT", [8, 128, NT])
        self.oaT = mk("s_oaT", [8, 128, NT])
        self.obT = mk("s_obT", [8, 128, NT])
        self.ocT = mk("s_ocT", [8, 128, NT])
        if moe_sparse:
            self.hTok = scr("s_hTok", [NT + 128, D])
            self.Ybuf = scr("s_Y", [2 * NT + 128, D])
            self.Tab = nc.dram_tensor("s_Tab", [NS * 512, 16], mybir.dt.int32, kind="Internal").ap()
        self.b_x = [[Buf() for _ in range(16)] for _ in range(NGRP)]
        self.b_scr = {k: Buf() for k in ("qaT", "kaT2", "vaTok", "qnT", "qrT", "ckvT", "krT2", "uT",
                                         "oaT", "obT", "ocT")}
        self.ps = [es.enter_context(nc.psum_tensor("ps%d" % i, [128, 512], F32)) for i in range(8)]
        self.psb = [Buf() for _ in range(8)]

    def I(self, name, dt=F32):
        if name not in self.din:
            self.din[name] = self.nc.dram_tensor(name, self.ishapes[name], dt, kind="ExternalInput").ap()
        return self.din[name]

    def mm(self, out, lhsT, rhs, start, stop, reads, writes, sync=None):
        nc = self.nc
        if sync is None:
            sync = stop
        self.fw.op("pe", lambda: nc.tensor.matmul(out, lhsT=lhsT, rhs=rhs, start=start, stop=stop),
                   reads=reads, writes=writes, sync=sync)

    def tr(self, out, in_, reads, writes, sync=True):
        nc = self.nc
        ident = self.ident[:]
        self.fw.op("pe", lambda: nc.tensor.transpose(out=out, in_=in_, identity=ident),
                   reads=reads + self.ident.b(), writes=writes, sync=sync)

    def copy(self, en, out, in_, reads, writes):
        nc = self.nc
        if en == "act":
            self.fw.op("act", lambda: nc.scalar.copy(out=out, in_=in_), reads=reads, writes=writes)
        elif en == "dve":
            self.fw.op("dve", lambda: nc.vector.tensor_copy(out=out, in_=in_), reads=reads, writes=writes)
        else:
            self.fw.op("pool", lambda: nc.gpsimd.tensor_copy(out=out, in_=in_), reads=reads, writes=writes)

    def evac_eng(self):
        self.evac_i += 1
        return "act" if self.evac_i % 2 else "dve"

    def act(self, out, in_, func, reads, writes, bias=None, scale=None, accum_out=None):
        nc = self.nc
        kw = {}
        if bias is not None:
            kw["bias"] = bias
        if scale is not None:
            kw["scale"] = scale
        if accum_out is not None:
            kw["accum_out"] = accum_out
        self.fw.op("act", lambda: nc.scalar.activation(out=out, in_=in_, func=func, **kw),
                   reads=reads, writes=writes)

    def tt(self, out, in0, in1, op, reads, writes, en="dve"):
        nc = self.nc
        eng = nc.vector if en == "dve" else nc.gpsimd
        self.fw.op(en, lambda: eng.tensor_tensor(out=out, in0=in0, in1=in1, op=op), reads=reads, writes=writes)

    def ts(self, out, in0, s1, s2, op0, op1, reads, writes):
        nc = self.nc
        if op1 is None:
            self.fw.op("dve", lambda: nc.vector.tensor_scalar(out=out, in0=in0, scalar1=s1, scalar2=None, op0=op0),
                       reads=reads, writes=writes)
        else:
            self.fw.op("dve", lambda: nc.vector.tensor_scalar(out=out, in0=in0, scalar1=s1, scalar2=s2,
                                                              op0=op0, op1=op1), reads=reads, writes=writes)

    def stt(self, out, in0, scalar, in1, op0, op1, reads, writes):
        nc = self.nc
        self.fw.op("dve", lambda: nc.vector.scalar_tensor_tensor(out=out, in0=in0, scalar=scalar, in1=in1,
                                                                 op0=op0, op1=op1), reads=reads, writes=writes)

    def wload(self, ring, dram_tile, nk, ncol=128):
        slot = ring.next()
        dst = slot.t[:, 0:nk * ncol].rearrange("p (k m) -> p k m", k=nk)
        self.fw.dma("pool", dst.bitcast(F32R), dram_tile, writes=slot.b())
        return dst.bitcast(F32R), slot

    def alloc_persistent(self):
        nc, es = self.nc, self.es
        self.ident = Tile(nc, es, "ident", [128, 128])
        self.ones = Tile(nc, es, "ones", [128, 128])
        self.identr = Tile(nc, es, "identr", [128, 128])
        self.vT = Tile(nc, es, "vT", [128, NROWS])
        self.modp = [Tile(nc, es, "modp%d" % l, [128, 6, 16, 2]) for l in range(DEPTH)]
        self.sinkb = Tile(nc, es, "sinkb", [128, DEPTH * 16])

    def phase_prologue(self):
        nc, fw = self.nc, self.fw
        with contextlib.ExitStack() as es:
            vrows = Tile(nc, es, "vrows", [128, 3, 128])
            scT = Tile(nc, es, "scT", [128, 32])
            modT = Tile(nc, es, "modT", [128, 96, 2])
            wring = Ring(nc, es, "wada", [128, 2048], 4)
            fw.dma("sp", self.ident[:], self.I("ident"), writes=self.ident.b())
            fw.dma("sp", vrows[:], self.I("vecs").rearrange("(a p) f -> p a f", p=128), writes=vrows.b())
            fw.dma("sp", self.sinkb[:],
                   self.I("sink").rearrange("l h -> (l h)").partition_broadcast(128), writes=self.sinkb.b())
            ones32 = Tile(nc, es, "ones32", [128, 128])
            fw.op("dve", lambda: nc.vector.memset(ones32[:], 1.0), writes=ones32.b())
            self.copy("act", self.ones.r(slice(None)), ones32[:], ones32.b(), self.ones.b())
            self.copy("act", self.identr.r(slice(None)), self.ident[:], self.ident.b(), self.identr.b())
            for a in range(3):
                self.tr(self.ps[0][:, a * 128:(a + 1) * 128], vrows[:, a, :], vrows.b(), [self.psb[0]])
            self.copy("dve", self.vT[:], self.ps[0][:, 0:384], [self.psb[0]], self.vT.b())
            self.act(scT.r(slice(None)), self.vT[:, 0:32], AF.Silu, self.vT.b(), scT.b())
            sc3 = scT.r(slice(None)).rearrange("p (c k) -> p k c", c=2)
            for l in range(DEPTH):
                for j in range(96):
                    w, slot = self.wload(wring, self.I("w_ada")[l, j], 16)
                    bank = self.ps[1 + (j % 2)]
                    bb = self.psb[1 + (j % 2)]
                    for k in range(16):
                        self.mm(bank[:, 0:2], w[:, k, :], sc3[:, k, :], k == 0, k == 15,
                                slot.b() + scT.b(), [bb])
                    col = R_BADA(l) + j
                    self.act(modT[:, j, :], bank[:, 0:2], AF.Identity, [bb] + self.vT.b(), modT.b(),
                             bias=self.vT[:, col:col + 1])
                mp = self.modp[l]
                for s in range(2):
                    lnr = R_LN1(l) if s == 0 else R_LN2(l)
                    sh, sc, gt = 3 * s, 3 * s + 1, 3 * s + 2
                    for cd in range(2):
                        self.stt(mp[:, 3 * s + 0, :, cd], modT[:, sc * 16:(sc + 1) * 16, cd], 1.0,
                                 self.vT[:, lnr:lnr + 16], ALU.add, ALU.mult,
                                 modT.b() + self.vT.b(), mp.b())
                        self.copy("dve", mp[:, 3 * s + 1, :, cd], modT[:, sh * 16:(sh + 1) * 16, cd], modT.b(), mp.b())
                        self.copy("dve", mp[:, 3 * s + 2, :, cd], modT[:, gt * 16:(gt + 1) * 16, cd], modT.b(), mp.b())
            xt = Ring(nc, es, "xcp", [128, 16 * 512], 2)
            for g in range(NGRP):
                t = xt.next()
                v = t.t[:].rearrange("p (c n) -> p c n", c=16)
                fw.dma("sp", v, self.I("xinT")[:, :, g * G:(g + 1) * G].rearrange("c p n -> p c n"), writes=t.b())
                fw.dma("sp", self.xT[:, :, g * G:(g + 1) * G].rearrange("c p n -> p c n"), v,
                       reads=t.b(), writes=self.b_x[g])
            fw.emit()

    def norm_stats(self, x3, xb, nchunk, nfeat, sqring, rstd, bank, bb):
        nc = self.nc
        for c in range(nchunk):
            sq = sqring.next()
            self.act(sq.r(slice(None)), x3[:, c, :], AF.Square, xb, sq.b())
            self.mm(bank[:, :], self.ones.r(slice(None)), sq.r(slice(None)), c == 0, c == nchunk - 1,
                    self.ones.b() + sq.b(), [bb], sync=True)
        self.act(rstd[:], bank[:, :], AF.Sqrt, [bb] + self.epsb.b(), rstd.b(), bias=self.epsb[:, 0:1], scale=1.0 / nfeat)
        self.fw.op("dve", lambda: nc.vector.reciprocal(out=rstd[:], in_=rstd[:]), reads=rstd.b(), writes=rstd.b())

    def norm_mod(self, g, l, s, xg, hT, sqring, tmpring, rstd, bank, bb):
        cd = 0 if g < 4 else 1
        mp = self.modp[l]
        x3 = xg.t[:].rearrange("p (c n) -> p c n", c=16)
        self.norm_stats(x3, xg.b(), 16, D, sqring, rstd, bank, bb)
        for c in range(16):
            tmp = tmpring.next()
            self.tt(tmp[:], x3[:, c, :], rstd[:], ALU.mult, xg.b() + rstd.b(), tmp.b())
            self.act(hT.r((slice(None), c, slice(None))), tmp[:], AF.Identity, tmp.b() + mp.b(), hT.b(c),
                     bias=mp[:, 3 * s + 1, c:c + 1, cd], scale=mp[:, 3 * s + 0, c:c + 1, cd])

    def load_x(self, g, xg):
        v = xg.t[:].rearrange("p (c n) -> p c n", c=16)
        self.fw.dma("sp", v, self.xT[:, :, g * G:(g + 1) * G].rearrange("c p n -> p c n"),
                    reads=self.b_x[g], writes=xg.b())

    def phase_proj(self, l):
        nc, fw = self.nc, self.fw
        with contextlib.ExitStack() as es:
            xg = Tile(nc, es, "xg", [128, 16 * 512])
            hT = Tile(nc, es, "hT", [128, 16, 512], split=True)
            wring = Ring(nc, es, "w1", [128, 2048], 4)
            wuq = Tile(nc, es, "wuq", [128, 12, 512])
            ropeC = Tile(nc, es, "ropeC", [128, 2048])
            ropeS = Tile(nc, es, "ropeS", [128, 2048])
            permM = Tile(nc, es, "permM", [128, 128])
            cqs = Tile(nc, es, "cqs", [128, 4, 512])
            cqn = Tile(nc, es, "cqn", [128, 4, 512])
            ckvs = Tile(nc, es, "ckvs", [128, 2, 512])
            stg = Ring(nc, es, "stg", [128, 512], 4)
            sqring = Ring(nc, es, "sq", [128, 512], 3)
            tmpring = Ring(nc, es, "tmp", [128, 512], 3)
            xsring = Ring(nc, es, "xs", [128, 512], 2)
            t1ring = Ring(nc, es, "t1", [128, 512], 2)
            rstd = Tile(nc, es, "rstd", [128, 512])
            rstd2 = Tile(nc, es, "rstd2", [128, 512])
            vtok = Ring(nc, es, "vtok", [128, 512], 2)
            self.epsb = Tile(nc, es, "epsb", [128, 1])
            fw.op("dve", lambda: nc.vector.memset(self.epsb[:], EPS), writes=self.epsb.b())
            fw.dma("sp", ropeC[:], self.I("ropeC"), writes=ropeC.b())
            fw.dma("sp", ropeS[:], self.I("ropeS"), writes=ropeS.b())
            fw.dma("pool", permM.r(slice(None)), self.I("permM"), writes=permM.b())
            fw.dma("pool", wuq.t[:].rearrange("p a (k m) -> p a k m", k=4).bitcast(F32R),
                   self.I("w_uq")[l].rearrange("a p k m -> p a k m"), writes=wuq.b())
            wuq4 = wuq.t[:].rearrange("p a (k m) -> p a k m", k=4).bitcast(F32R)
            bi = [0]

            def nextbank():
                bi[0] += 1
                i = bi[0] % 4
                return self.ps[i], self.psb[i]

            def finish_chunk(bank, bb, g, rope, dst, dbuf, P=128, dst2=None):
                st = stg.next()
                if rope and g < 4:
                    xs = xsring.next()
                    self.copy("act", xs.r(slice(None))[0:P], bank[0:P, :], [bb], xs.b())
                    pb, pbb = self.ps[4 + (bi[0] % 2)], self.psb[4 + (bi[0] % 2)]
                    self.mm(pb[0:P, :], permM.r(slice(None))[0:P, 0:P], xs.r(slice(None))[0:P], True, True,
                            permM.b() + xs.b(), [pbb])
                    t1 = t1ring.next()
                    self.tt(t1[0:P], xs[0:P], ropeC[0:P, g * G:(g + 1) * G], ALU.mult, xs.b() + ropeC.b(), t1.b())
                    self.tt(st[0:P], pb[0:P, :], ropeS[0:P, g * G:(g + 1) * G], ALU.mult, [pbb] + ropeS.b(), st.b())
                    self.tt(st[0:P], st[0:P], t1[0:P], ALU.add, st.b() + t1.b(), st.b())
                else:
                    self.copy(self.evac_eng(), st[0:P], bank[0:P, :], [bb], st.b())
                fw.dma("sp", dst, st[0:P], reads=st.b())
                if dst2 is not None:
                    fw.dma("sp", dst2, st[0:dst2.shape[0]], reads=st.b())
                return st

            for g in (DBG_GROUPS or range(NGRP)):
                gs = slice(g * G, (g + 1) * G)
                self.load_x(g, xg)
                self.norm_mod(g, l, 0, xg, hT, sqring, tmpring, rstd, self.ps[7], self.psb[7])
                for ch in range(26):
                    w, slot = self.wload(wring, self.I("w_inA")[l, ch], 16)
                    bank, bb = nextbank()
                    for k in range(16):
                        self.mm(bank[:, :], w[:, k, :], hT.r((slice(None), k, slice(None))), k == 0, k == 15,
                                slot.b() + hT.b(k), [bb])
                    if ch < 8:
                        finish_chunk(bank, bb, g, True, self.qaT[ch, :, gs], self.b_scr["qaT"])
                    elif ch < 10:
                        d2 = self.o_kT[l, ch - 8, :, :] if g == 4 else None
                        finish_chunk(bank, bb, g, True, self.kaT2[ch - 8, :, gs], self.b_scr["kaT2"], dst2=d2)
                    elif ch == 10:
                        st = stg.next()
                        self.copy(self.evac_eng(), st[:], bank[:, :], [bb], st.b())
                        tb, tbb = self.ps[6], self.psb[6]
                        for t in range(4):
                            self.tr(tb[:, t * 128:(t + 1) * 128], st[:, t * 128:(t + 1) * 128], st.b(), [tbb])
                        vt = vtok.next()
                        self.copy(self.evac_eng(), vt[:], tb[:, :], [tbb], vt.b())
                        fw.dma("sp", self.vaTok[g * 4:(g + 1) * 4].rearrange("t p d -> p t d"),
                               vt.t[:].rearrange("p (t d) -> p t d", t=4), reads=vt.b())
                        if g == 4:
                            fw.dma("sp", self.o_v[l].rearrange("t p d -> p t d"),
                                   vt.t[:].rearrange("p (t d) -> p t d", t=4), reads=vt.b())
                    elif ch < 15:
                        self.copy(self.evac_eng(), cqs[:, ch - 11, :], bank[:, :], [bb], cqs.b())
                        if ch == 14:
                            self.norm_stats(cqs.t[:], cqs.b(), 4, 512, sqring, rstd2, self.ps[7], self.psb[7])
                            for c in range(4):
                                col = R_QG(l) + c
                                self.stt(cqn.r((slice(None), c, slice(None))), cqs[:, c, :], self.vT[:, col:col + 1],
                                         rstd2[:], ALU.mult, ALU.mult, cqs.b() + rstd2.b() + self.vT.b(), cqn.b())
                            for a in range(12):
                                bank2, bb2 = nextbank()
                                for k in range(4):
                                    self.mm(bank2[:, :], wuq4[:, a, k, :], cqn.r((slice(None), k, slice(None))),
                                            k == 0, k == 3, wuq.b() + cqn.b(), [bb2])
                                if a < 8:
                                    finish_chunk(bank2, bb2, g, False, self.qnT[a, :, gs], self.b_scr["qnT"])
                                else:
                                    finish_chunk(bank2, bb2, g, True, self.qrT[a - 8, :, gs], self.b_scr["qrT"])
                    elif ch < 17:
                        self.copy(self.evac_eng(), ckvs[:, ch - 15, :], bank[:, :], [bb], ckvs.b())
                        if ch == 16:
                            self.norm_stats(ckvs.t[:], ckvs.b(), 2, 256, sqring, rstd2, self.ps[7], self.psb[7])
                            for c in range(2):
                                col = R_KVG(l) + c
                                st = stg.next()
                                self.stt(st[:], ckvs[:, c, :], self.vT[:, col:col + 1], rstd2[:], ALU.mult, ALU.mult,
                                         ckvs.b() + rstd2.b() + self.vT.b(), st.b())
                                fw.dma("sp", self.ckvT[c, :, gs], st[:], reads=st.b())
                                if g == 4:
                                    fw.dma("sp", self.o_ckvT[l, c], st[:], reads=st.b())
                    elif ch == 17:
                        d2 = self.o_krT[l] if g == 4 else None
                        finish_chunk(bank, bb, g, True, self.krT2[:, gs], self.b_scr["krT2"], dst2=d2)
                    else:
                        finish_chunk(bank, bb, g, False, self.uT[ch - 18, :, gs], self.b_scr["uT"])
            fw.emit()

    def rmax(self, out, in_, reads, writes):
        nc = self.nc
        self.fw.op("dve", lambda: nc.vector.reduce_max(out=out, in_=in_, axis=AX.X), reads=reads, writes=writes)

    def rsum(self, out, in_, reads, writes):
        nc = self.nc
        self.fw.op("dve", lambda: nc.vector.reduce_sum(out=out, in_=in_, axis=AX.X), reads=reads, writes=writes)

    def recip(self, out, in_, reads, writes):
        nc = self.nc
        self.fw.op("dve", lambda: nc.vector.reciprocal(out=out, in_=in_), reads=reads, writes=writes)

    def phase_attnA(self, l, seqs=SEQS, qb_limit=None):
        nc, fw = self.nc, self.fw
        with contextlib.ExitStack() as es:
            kT2 = Tile(nc, es, "kT2", [128, 2, 2560])
            Vt = Tile(nc, es, "Vt", [128, 20, 128], dt=BF16)
            identbA = Tile(nc, es, "identbA", [128, 128], dt=BF16)
            PbringA = Ring(nc, es, "PbA", [128, 896], 7, dt=BF16)
            qring = Ring(nc, es, "qc", [128, 2048], 2)
            maskA = Tile(nc, es, "maskA", [128, 384])
            Pring = Ring(nc, es, "Pa", [128, 896], 7)
            PTring = Ring(nc, es, "PTa", [128, 896], 3, dt=BF16)
            slring = Ring(nc, es, "sl", [128, 384], 2)
            small = Ring(nc, es, "sma", [128, 16], 5)
            rdring = Ring(nc, es, "rda", [128, 2], 4)
            Oqring = Ring(nc, es, "Oqa", [128, 128], 2)
            ostg = Ring(nc, es, "ostga", [128, 512], 2)
            fw.dma("sp", maskA[:], self.I("maskA"), writes=maskA.b())
            self.copy("dve", identbA[:], self.ident[:], self.ident.b(), identbA.b())
            for (tok0, n, ctx) in seqs:
                nqb = n // 128
                for g2 in range(2):
                    fw.dma("pool", kT2.r((slice(None), g2, slice(0, n))), self.kaT2[g2, :, tok0:tok0 + n],
                           writes=kT2.b())
                    if ctx:
                        fw.dma("pool", kT2.r((slice(None), g2, slice(n, n + 512))), self.I("ck2T")[l, g2],
                               writes=kT2.b())
                fw.dma("pool", Vt[:, 0:nqb, :],
                       self.vaTok[tok0 // 128:tok0 // 128 + nqb].rearrange("t p d -> p t d"),
                       writes=Vt.b())
                if ctx:
                    fw.dma("pool", Vt[:, nqb:nqb + 4, :],
                           self.I("cv")[l].rearrange("(t p) d -> p t d", p=128), writes=Vt.b())
                for c in range(8):
                    g2 = c // 4
                    qc = qring.next()
                    fw.dma("pool", qc.r((slice(None), slice(0, n))), self.qaT[c, :, tok0:tok0 + n],
                           writes=qc.b())
                    qbs = list(range(nqb)) if qb_limit is None else list(range(min(nqb, qb_limit)))
                    stbox = [None]

                    def stageA(qb, c=c, g2=g2, qc=qc):
                        if ctx:
                            kb_lo, kb_hi = max(qb - 1, 0), min(qb + 1, nqb - 1)
                        else:
                            kb_lo, kb_hi = 0, nqb - 1
                        nl = (kb_hi - kb_lo + 1) * 128
                        blocks = list(range(kb_lo, kb_hi + 1)) + ([nqb + i for i in range(4)] if ctx else [])
                        rd = rdring.next()
                        sm = small.next()
                        pts = []
                        for hh in range(2):
                            pb = hh * 64
                            sbank, sbb = self.ps[hh * 2], self.psb[hh * 2]
                            cbank, cbb = self.ps[hh * 2 + 1], self.psb[hh * 2 + 1]
                            lq = qc.r((slice(pb, pb + 64), slice(qb * 128, (qb + 1) * 128)))
                            self.mm(sbank[:, 0:nl], lq, kT2.r((slice(pb, pb + 64), g2, slice(kb_lo * 128, kb_lo * 128 + nl))),
                                    True, True, qc.b() + kT2.b(), [sbb])
                            if ctx:
                                self.mm(cbank[:, :], lq, kT2.r((slice(pb, pb + 64), g2, slice(n, n + 512))),
                                        True, True, qc.b() + kT2.b(), [cbb])
                            Pt = Pring.next()
                            if ctx:
                                mlo = 128 if qb == 0 else 0
                                self.tt(Pt[:, 0:nl], sbank[:, 0:nl], maskA[:, mlo:mlo + nl], ALU.add,
                                        [sbb] + maskA.b(), Pt.b())
                                self.rmax(sm[:, hh:hh + 1], Pt[:, 0:nl], Pt.b(), sm.b())
                                fw.op("dve", lambda cbank=cbank, Pt=Pt, sm=sm, nl=nl, hh=hh: nc.vector.tensor_scalar(
                                    out=Pt[:, nl:nl + 512], in0=cbank[:, :], scalar1=1.0, scalar2=None, op0=ALU.mult,
                                    op1=ALU.max, accum_out=sm[:, 2 + hh:3 + hh]), reads=[cbb], writes=Pt.b() + sm.b())
                            else:
                                fw.op("dve", lambda sbank=sbank, Pt=Pt, sm=sm, nl=nl, hh=hh: nc.vector.tensor_scalar(
                                    out=Pt[:, 0:nl], in0=sbank[:, 0:nl], scalar1=1.0, scalar2=None, op0=ALU.mult,
                                    op1=ALU.max, accum_out=sm[:, 4 + hh:5 + hh]), reads=[sbb], writes=Pt.b() + sm.b())
                            pts.append(Pt)
                        scol = l * 16 + 2 * c
                        sk2 = self.sinkb[:, scol:scol + 2]
                        if ctx:
                            self.tt(sm[:, 4:6], sm[:, 0:2], sm[:, 2:4], ALU.max, sm.b(), sm.b())
                        self.stt(sm[:, 6:8], sm[:, 4:6], 0.125, sk2, ALU.mult, ALU.max, sm.b() + self.sinkb.b(), sm.b())
                        self.ts(sm[:, 8:10], sm[:, 6:8], -1.0, None, ALU.mult, None, sm.b(), sm.b())
                        pbs = []
                        for hh in range(2):
                            Pt = pts[hh]
                            Pb = PbringA.next()
                            self.act(Pb[:, 0:nl], Pt[:, 0:nl], AF.Exp, Pt.b() + sm.b(), Pb.b() + sm.b(),
                                     bias=sm[:, 8 + hh:9 + hh], scale=0.125, accum_out=sm[:, 10 + hh:11 + hh])
                            if ctx:
                                self.act(Pb[:, nl:nl + 512], Pt[:, nl:nl + 512], AF.Exp, Pt.b() + sm.b(), Pb.b() + sm.b(),
                                         bias=sm[:, 8 + hh:9 + hh], scale=0.125, accum_out=sm[:, 12 + hh:13 + hh])
                            pbs.append(Pb)
                        return blocks, rd, pbs, sm, sk2

                    def stageA2(blocks, rd, pbs, sm, sk2):
                        self.tt(sm[:, 14:16], sk2, sm[:, 8:10], ALU.add, self.sinkb.b() + sm.b(), sm.b())
                        self.act(sm[:, 14:16], sm[:, 14:16], AF.Exp, sm.b(), sm.b())
                        self.tt(sm[:, 10:12], sm[:, 10:12], sm[:, 14:16], ALU.add, sm.b(), sm.b())
                        if ctx:
                            self.tt(sm[:, 10:12], sm[:, 10:12], sm[:, 12:14], ALU.add, sm.b(), sm.b())
                        self.recip(rd[:, 0:2], sm[:, 10:12], sm.b(), rd.b())

                    def stageB(qb, blocks, rd, pts, c=c, g2=g2):
                        nb = len(blocks)
                        obank, obb = self.ps[6], self.psb[6]
                        for hh in range(2):
                            Pt = pts[hh]
                            PT = PTring.next()
                            for i0 in range(0, nb, 4):
                                cnt = min(4, nb - i0)
                                tbb = self.psb[4 + (i0 // 4) % 2]
                                tb = self.ps[4 + (i0 // 4) % 2][:, :].bitcast(BF16)
                                for i in range(i0, i0 + cnt):
                                    fw.op("pe", lambda tb=tb, i=i, i0=i0, Pt=Pt: nc.tensor.transpose(
                                        out=tb[:, (i - i0) * 128:(i - i0 + 1) * 128], in_=Pt[:, i * 128:(i + 1) * 128],
                                        identity=identbA[:]), reads=Pt.b() + identbA.b(), writes=[tbb])
                                self.copy(self.evac_eng(), PT[:, i0 * 128:(i0 + cnt) * 128],
                                          tb[:, 0:cnt * 128], [tbb], PT.b())
                            for i, blk in enumerate(blocks):
                                self.mm(obank[:, hh * 64:(hh + 1) * 64], PT[:, i * 128:(i + 1) * 128],
                                        Vt[:, blk, g2 * 64:(g2 + 1) * 64], i == 0, i == nb - 1,
                                        PT.b() + Vt.b(), [obb], sync=True)
                        Oq = Oqring.next()
                        self.ts(Oq[:, 0:64], obank[:, 0:64], rd[:, 0:1], None, ALU.mult, None, [obb] + rd.b(), Oq.b())
                        self.ts(Oq[:, 64:128], obank[:, 64:128], rd[:, 1:2], None, ALU.mult, None, [obb] + rd.b(), Oq.b())
                        tb, tbb = self.ps[7], self.psb[7]
                        self.tr(tb[:, 0:128], Oq[:, :], Oq.b(), [tbb])
                        if qb % 4 == 0:
                            stbox[0] = ostg.next()
                        st = stbox[0]
                        self.copy(self.evac_eng(), st[:, (qb % 4) * 128:(qb % 4 + 1) * 128], tb[:, 0:128], [tbb], st.b())
                        if qb % 4 == 3 or qb == qbs[-1]:
                            q0 = (qb // 4) * 512
                            wd = (qb % 4 + 1) * 128
                            fw.dma("sp", self.oaT[c, :, tok0 + q0:tok0 + q0 + wd], st[:, 0:wd], reads=st.b())

                    pend = [stageA(qbs[0])]
                    stageA2(*pend[0])
                    if len(qbs) > 1:
                        pend.append(stageA(qbs[1]))
                    for ii, qb in enumerate(qbs):
                        cur = pend.pop(0)
                        if ii + 2 < len(qbs):
                            pend.append(stageA(qbs[ii + 2]))
                        stageB(qb, *cur[0:3])
                        if pend:
                            stageA2(*pend[0])
            fw.emit()

    def phase_mla(self, l, seqs=SEQS, qb_limit=None, heads=range(8)):
        nc, fw = self.nc, self.fw
        scale = float((128 + 64) ** -0.5)
        with contextlib.ExitStack() as es:
            ckvA = Tile(nc, es, "ckvA", [128, 2, 2560])
            krA = Tile(nc, es, "krA", [128, 2560])
            wukv = Tile(nc, es, "wukv", [128, 16, 256])
            knT = Tile(nc, es, "knT", [128, 2560])
            vh = Tile(nc, es, "vh", [128, 20, 128], dt=BF16)
            identb = Tile(nc, es, "identb", [128, 128], dt=BF16)
            Pbring = Ring(nc, es, "Pbm", [128, 2560], 4, dt=BF16)
            qnr = Ring(nc, es, "qnm", [128, 2048], 2)
            qrr = Ring(nc, es, "qrm", [128, 2048], 2)
            Pring = Ring(nc, es, "Pm", [128, 2560], 4)
            PTring = Ring(nc, es, "PTm", [128, 2560], 2, dt=BF16)
            small = Ring(nc, es, "smm", [128, 16], 5)
            Oqring = Ring(nc, es, "Oqm", [128, 128], 2)
            ostg = Ring(nc, es, "ostgm", [128, 512], 2)
            self.copy("dve", identb[:], self.ident[:], self.ident.b(), identb.b())
            wukv4 = wukv.t[:].rearrange("p a (k m) -> p a k m", k=2).bitcast(F32R)
            fw.dma("pool", wukv4, self.I("w_ukv")[l].rearrange("a p k m -> p a k m"), writes=wukv.b())
            for (tok0, n, ctx) in seqs:
                nqb = n // 128
                nk = n + (512 if ctx else 0)
                nkb = nk // 128
                kgs = [(s, min(512, nk - s)) for s in range(0, nk, 512)]
                ng = len(kgs)
                for k in range(2):
                    fw.dma("pool", ckvA.r((slice(None), k, slice(0, n))), self.ckvT[k, :, tok0:tok0 + n],
                           writes=ckvA.b())
                    if ctx:
                        fw.dma("pool", ckvA.r((slice(None), k, slice(n, n + 512))), self.I("cckvT")[l, k], writes=ckvA.b())
                fw.dma("pool", krA.r((slice(None), slice(0, n))), self.krT2[:, tok0:tok0 + n],
                       writes=krA.b())
                if ctx:
                    fw.dma("pool", krA.r((slice(None), slice(n, n + 512))), self.I("ckr2T")[l], writes=krA.b())
                qr, qr_pair = None, -1
                for h in heads:
                    pb = (h % 2) * 64
                    for gi, (s, w) in enumerate(kgs):
                        bank, bb = self.ps[gi % 4], self.psb[gi % 4]
                        for k in range(2):
                            self.mm(bank[:, 0:w], wukv4[:, h, k, :], ckvA.r((slice(None), k, slice(s, s + w))),
                                    k == 0, k == 1, wukv.b() + ckvA.b(), [bb])
                        self.copy(self.evac_eng(), knT.r((slice(None), slice(s, s + w))), bank[:, 0:w], [bb], knT.b())
                    for kb0 in range(0, nkb, 4):
                        cnt = min(4, nkb - kb0)
                        bank, bb = self.ps[4 + (kb0 // 4) % 2], self.psb[4 + (kb0 // 4) % 2]
                        for kb in range(kb0, kb0 + cnt):
                            for k in range(2):
                                self.mm(bank[:, (kb - kb0) * 128:(kb - kb0 + 1) * 128],
                                        ckvA.r((slice(None), k, slice(kb * 128, (kb + 1) * 128))), wukv4[:, 8 + h, k, :],
                                        k == 0, k == 1, wukv.b() + ckvA.b(), [bb], sync=(k == 1 and kb == kb0 + cnt - 1))
                        self.copy(self.evac_eng(), vh[:, kb0:kb0 + cnt, :],
                                  bank[:, 0:cnt * 128].rearrange("p (a d) -> p a d", a=cnt), [bb], vh.b())
                    qn = qnr.next()
                    fw.dma("pool", qn.r((slice(None), slice(0, n))), self.qnT[h, :, tok0:tok0 + n],
                           writes=qn.b())
                    if qr_pair != h // 2:
                        qr_pair = h // 2
                        qr = qrr.next()
                        fw.dma("pool", qr.r((slice(None), slice(0, n))), self.qrT[h // 2, :, tok0:tok0 + n],
                               writes=qr.b())
                    qbs = list(range(nqb)) if qb_limit is None else list(range(min(nqb, qb_limit)))
                    stbox = [None]

                    def stageA(qb, qn=qn, qr=qr, pb=pb):
                        qsl = slice(qb * 128, (qb + 1) * 128)
                        sm = small.next()
                        Pt = Pring.next()
                        for gi, (s, w) in enumerate(kgs):
                            bank, bb = self.ps[gi], self.psb[gi]
                            self.mm(bank[:, 0:w], qn.r((slice(None), qsl)), knT.r((slice(None), slice(s, s + w))),
                                    True, False, qn.b() + knT.b(), [bb], sync=False)
                            self.mm(bank[:, 0:w], qr.r((slice(pb, pb + 64), qsl)), krA.r((slice(pb, pb + 64), slice(s, s + w))),
                                    False, True, qr.b() + krA.b(), [bb], sync=True)
                            fw.op("dve", lambda bank=bank, w=w, s=s, gi=gi, Pt=Pt, sm=sm: nc.vector.tensor_scalar(
                                out=Pt[:, s:s + w], in0=bank[:, 0:w], scalar1=1.0, scalar2=None, op0=ALU.mult, op1=ALU.max,
                                accum_out=sm[:, gi:gi + 1]), reads=[bb], writes=Pt.b() + sm.b())
                        self.rmax(sm[:, 8:9], sm[:, 0:ng], sm.b(), sm.b())
                        self.ts(sm[:, 9:10], sm[:, 8:9], -scale, None, ALU.mult, None, sm.b(), sm.b())
                        Pb = Pbring.next()
                        for gi, (s, w) in enumerate(kgs):
                            self.act(Pb[:, s:s + w], Pt[:, s:s + w], AF.Exp, Pt.b() + sm.b(), Pb.b() + sm.b(),
                                     bias=sm[:, 9:10], scale=scale, accum_out=sm[:, 10 + gi:11 + gi])
                        return sm, Pb

                    def stageA2(sm, Pb):
                        self.rsum(sm[:, 15:16], sm[:, 10:10 + ng], sm.b(), sm.b())
                        self.recip(sm[:, 15:16], sm[:, 15:16], sm.b(), sm.b())

                    def stageB(qb, sm, Pt, h=h):
                        PT = PTring.next()
                        for kb0 in range(0, nkb, 4):
                            cnt = min(4, nkb - kb0)
                            tbb = self.psb[5 + (kb0 // 4) % 2]
                            tb = self.ps[5 + (kb0 // 4) % 2][:, :].bitcast(BF16)
                            for kb in range(kb0, kb0 + cnt):
                                fw.op("pe", lambda tb=tb, kb=kb, kb0=kb0, Pt=Pt: nc.tensor.transpose(
                                    out=tb[:, (kb - kb0) * 128:(kb - kb0 + 1) * 128], in_=Pt[:, kb * 128:(kb + 1) * 128],
                                    identity=identb[:]), reads=Pt.b() + identb.b(), writes=[tbb])
                            self.copy(self.evac_eng(), PT[:, kb0 * 128:(kb0 + cnt) * 128],
                                      tb[:, 0:cnt * 128], [tbb], PT.b())
                        obank, obb = self.ps[7], self.psb[7]
                        for kb in range(nkb):
                            self.mm(obank[:, 0:128], PT[:, kb * 128:(kb + 1) * 128],
                                    vh[:, kb, :], kb == 0, kb == nkb - 1, PT.b() + vh.b(), [obb])
                        Oq = Oqring.next()
                        self.ts(Oq[:, :], obank[:, 0:128], sm[:, 15:16], None, ALU.mult, None, [obb] + sm.b(), Oq.b())
                        tb, tbb = self.ps[5], self.psb[5]
                        self.tr(tb[:, 0:128], Oq[:, :], Oq.b(), [tbb])
                        if qb % 4 == 0:
                            stbox[0] = ostg.next()
                        st = stbox[0]
                        self.copy(self.evac_eng(), st[:, (qb % 4) * 128:(qb % 4 + 1) * 128], tb[:, 0:128], [tbb], st.b())
                        if qb % 4 == 3 or qb == qbs[-1]:
                            q0 = (qb // 4) * 512
                            wd = (qb % 4 + 1) * 128
                            fw.dma("sp", self.obT[h, :, tok0 + q0:tok0 + q0 + wd], st[:, 0:wd], reads=st.b())

                    pend = [stageA(qbs[0])]
                    stageA2(*pend[0])
                    if len(qbs) > 1:
                        pend.append(stageA(qbs[1]))
                    for ii, qb in enumerate(qbs):
                        cur = pend.pop(0)
                        if ii + 2 < len(qbs):
                            pend.append(stageA(qbs[ii + 2]))
                        stageB(qb, *cur)
                        if pend:
                            stageA2(*pend[0])
            fw.emit()

    def phase_pool(self, l, seqs=SEQS):
        nc, fw = self.nc, self.fw
        with contextlib.ExitStack() as es:
            invs = Tile(nc, es, "invs", [128, 4 * 2048])
            invp = Tile(nc, es, "invp", [128, 4 * 256])
            pw = Tile(nc, es, "pw", [128, 8, 256])
            upr = Ring(nc, es, "up", [128, 2064], 2)
            Ar = Ring(nc, es, "Apool", [128, 2064], 3)
            dT = [Tile(nc, es, "dT%d" % i, [128, 2048]) for i in range(2)]
            stg = Ring(nc, es, "pstg", [128, 512], 3)
            fw.dma("sp", invs[:], self.I("invc_s").rearrange("a n -> (a n)").partition_broadcast(128), writes=invs.b())
            fw.dma("sp", invp[:], self.I("invc_p").rearrange("a n -> (a n)").partition_broadcast(128), writes=invp.b())
            pw4 = pw.t[:].rearrange("p a (k m) -> p a k m", k=2).bitcast(F32R)
            fw.dma("pool", pw4, self.I("poolw")[l].rearrange("a p k m -> p a k m"), writes=pw.b())
            bi = 0
            for (tok0, n, ctx) in seqs:
                inv = invs if n == 2048 else invp
                for pg in range(4):
                    win = POOL_WINDOWS[pg]
                    left = win // 2
                    for half in range(2):
                        cc = pg * 2 + half
                        u = upr.next()
                        fw.op("dve", lambda u=u: nc.vector.memset(u[:, 0:8], 0.0), writes=u.b())
                        fw.op("dve", lambda u=u, n=n: nc.vector.memset(u[:, 8 + n:16 + n], 0.0), writes=u.b())
                        fw.dma("sp", u[:, 8:8 + n], self.uT[cc, :, tok0:tok0 + n], writes=u.b())
                        cur, L, step = u, n + 16, 1
                        while step < win:
                            nxt = Ar.next()
                            self.tt(nxt[:, 0:L - step], cur[:, 0:L - step], cur[:, step:L], ALU.add, cur.b(), nxt.b())
                            cur, L, step = nxt, L - step, step * 2
                        tmp = Ar.next()
                        self.tt(tmp[:, 0:n], cur[:, 8 - left:8 - left + n], inv[:, pg * n:(pg + 1) * n], ALU.mult,
                                cur.b() + inv.b(), tmp.b())
                        self.tt(dT[half].r((slice(None), slice(0, n))), tmp[:, 0:n], u[:, 8:8 + n], ALU.subtract,
                                tmp.b() + u.b(), dT[half].b())
                    for mh in range(2):
                        for tg in range(0, n, 512):
                            w = min(512, n - tg)
                            bi += 1
                            bank, bb = self.ps[bi % 4], self.psb[bi % 4]
                            for k in range(2):
                                self.mm(bank[:, 0:w], pw4[:, pg * 2 + mh, k, :], dT[k].r((slice(None), slice(tg, tg + w))),
                                        k == 0, k == 1, pw.b() + dT[k].b(), [bb])
                            st = stg.next()
                            col = R_PSC(l) + pg * 2 + mh
                            self.act(st[:, 0:w], bank[:, 0:w], AF.Copy, [bb] + self.vT.b(), st.b(),
                                     scale=self.vT[:, col:col + 1])
                            fw.dma("sp", self.ocT[pg * 2 + mh, :, tok0 + tg:tok0 + tg + w], st[:, 0:w], reads=st.b())
            fw.emit()

    def phase_merge(self, l, groups=None):
        nc, fw = self.nc, self.fw
        with contextlib.ExitStack() as es:
            xg = Tile(nc, es, "xg3", [128, 16 * 512])
            hT = Tile(nc, es, "hT3", [128, 16, 512], split=True)
            o3 = [Tile(nc, es, "o3_%d" % i, [128, 8, 512]) for i in range(3)]
            wring = Ring(nc, es, "w3", [128, 2048], 6)
            sgr = Ring(nc, es, "sg3", [128, 512], 2)
            tmr = Ring(nc, es, "tm3", [128, 512], 2)
            sqring = Ring(nc, es, "sq3", [128, 512], 2)
            tmpring = Ring(nc, es, "tmp3", [128, 512], 2)
            rstd = Tile(nc, es, "rstd3", [128, 512])
            xring = Ring(nc, es, "xr3", [128, 512], 3)
            self.epsb = Tile(nc, es, "epsb3", [128, 1])
            fw.op("dve", lambda: nc.vector.memset(self.epsb[:], EPS), writes=self.epsb.b())
            y3 = xg.t[:].rearrange("p (c n) -> p c n", c=16)
            srcs = [(self.oaT, "oaT"), (self.obT, "obT"), (self.ocT, "ocT")]
            mp = self.modp[l]
            it = 0
            for g in (groups or range(NGRP)):
                cd = 0 if g < 4 else 1
                gs = slice(g * G, (g + 1) * G)
                self.load_x(g, xg)
                self.norm_mod(g, l, 0, xg, hT, sqring, tmpring, rstd, self.ps[7], self.psb[7])
                for br in range(3):
                    fw.dma("pool", o3[br].r(slice(None)), srcs[br][0][:, :, gs].rearrange("c p n -> p c n"),
                           writes=o3[br].b())
                for d in range(16):
                    for br in range(3):
                        it += 1
                        w, slot = self.wload(wring, self.I("w_inG")[l, br * 16 + d], 16)
                        ga, gab = self.ps[it % 2], self.psb[it % 2]
                        for k in range(16):
                            self.mm(ga[:, :], w[:, k, :], hT.r((slice(None), k, slice(None))), k == 0, k == 15,
                                    slot.b() + hT.b(k), [gab])
                        w2, slot2 = self.wload(wring, self.I("wbr")[l, br, d], 8)
                        pr, prb = self.ps[2 + it % 2], self.psb[2 + it % 2]
                        for k in range(8):
                            self.mm(pr[:, :], w2[:, k, :], o3[br].r((slice(None), k, slice(None))), k == 0, k == 7,
                                    slot2.b() + o3[br].b(), [prb])
                        sg = sgr.next()
                        self.act(sg[:], ga[:, :], AF.Sigmoid, [gab], sg.b())
                        if br == 0:
                            self.tt(y3[:, d, :].bitcast(F32R), sg[:], pr[:, :], ALU.mult, sg.b() + [prb], xg.b())
                        else:
                            tm = tmr.next()
                            self.tt(tm[:], sg[:], pr[:, :], ALU.mult, sg.b() + [prb], tm.b())
                            out = y3[:, d, :].bitcast(F32R)
                            self.tt(out, y3[:, d, :], tm[:], ALU.add, xg.b() + tm.b(), xg.b())
                for d2 in range(16):
                    it += 1
                    w, slot = self.wload(wring, self.I("wout")[l, d2], 16)
                    bank, bb = self.ps[4 + it % 2], self.psb[4 + it % 2]
                    for k in range(16):
                        self.mm(bank[:, :], w[:, k, :], y3[:, k, :].bitcast(F32R), k == 0, k == 15, slot.b() + xg.b(), [bb])
                    xr = xring.next()
                    fw.dma("sp", xr[:], self.xT[d2, :, gs], reads=[self.b_x[g][d2]], writes=xr.b())
                    self.stt(xr[:], bank[:, :], mp[:, 2, d2:d2 + 1, cd], xr[:], ALU.mult, ALU.add,
                             [bb] + mp.b() + xr.b(), xr.b())
                    fw.dma("sp", self.xT[d2, :, gs], xr[:], reads=xr.b(), writes=[self.b_x[g][d2]])
            fw.emit()

    def phase_ffn(self, l, groups=None, final=False):
        nc, fw = self.nc, self.fw
        moe = (l % 2 == 1)
        nexp = self.moe_experts if moe else 1
        with contextlib.ExitStack() as es:
            xa = Tile(nc, es, "xa", [128, 16 * 512])
            hT = Tile(nc, es, "hT4", [128, 16, 512], split=True)
            aT = [Tile(nc, es, "aT%d" % i, [128, FB, 512], split=True) for i in range(2)]
            wgu = Ring(nc, es, "wgu", [128, 2048], 6)
            wdr = Ring(nc, es, "wdr", [128, FB * 128], 3)
            sglr = Ring(nc, es, "sgl", [128, 512], 2)
            t4r = Ring(nc, es, "t4", [128, 512], 2)
            sqring = Ring(nc, es, "sq4", [128, 512], 2)
            tmpring = Ring(nc, es, "tmp4", [128, 512], 2)
            rstd = Tile(nc, es, "rstd4", [128, 512])
            xring = Ring(nc, es, "xr4", [128, 512], 3)
            self.epsb = Tile(nc, es, "epsb4", [128, 1])
            fw.op("dve", lambda: nc.vector.memset(self.epsb[:], EPS), writes=self.epsb.b())
            if moe:
                rt = Tile(nc, es, "rt", [128, 16, 8])
                fw.dma("pool", rt.r(slice(None)), self.I("router"), writes=rt.b())
                gate = Tile(nc, es, "gate", [128, 4, 8])
                gsm = Ring(nc, es, "gsm", [128, 32], 2)
                gbr = Ring(nc, es, "gb", [128, 128], 2)
                gbcr = Ring(nc, es, "gbc", [128, 512], 2)
            a3 = xa.t[:].rearrange("p (c n) -> p c n", c=16)
            mp = self.modp[l]
            it = 0
            for g in (groups or range(NGRP)):
                cd = 0 if g < 4 else 1
                gs = slice(g * G, (g + 1) * G)
                self.load_x(g, xa)
                self.norm_mod(g, l, 1, xa, hT, sqring, tmpring, rstd, self.ps[7], self.psb[7])
                if moe:
                    for t in range(4):
                        lb, lbb = self.ps[6], self.psb[6]
                        for k in range(16):
                            self.mm(lb[:, t * 8:(t + 1) * 8], hT.r((slice(None), k, slice(t * 128, (t + 1) * 128))),
                                    rt.r((slice(None), k, slice(None))), k == 0, k == 15, hT.b(k) + rt.b(), [lbb])
                        sm = gsm.next()
                        lg = sm[:, 0:8]
                        self.copy("dve", lg, lb[:, t * 8:(t + 1) * 8], [lbb], sm.b())
                        self.rmax(sm[:, 24:25], lg, sm.b(), sm.b())
                        self.ts(sm[:, 8:16], lg, sm[:, 24:25], None, ALU.is_equal, None, sm.b(), sm.b())
                        self.stt(sm[:, 8:16], sm[:, 8:16], -1e30, lg, ALU.mult, ALU.add, sm.b(), sm.b())
                        self.rmax(sm[:, 25:26], sm[:, 8:16], sm.b(), sm.b())
                        self.ts(sm[:, 8:16], lg, sm[:, 25:26], None, ALU.is_ge, None, sm.b(), sm.b())
                        self.ts(sm[:, 26:27], sm[:, 24:25], -1.0, None, ALU.mult, None, sm.b(), sm.b())
                        self.act(sm[:, 16:24], lg, AF.Exp, sm.b(), sm.b(), bias=sm[:, 26:27], scale=1.0)
                        self.tt(sm[:, 16:24], sm[:, 16:24], sm[:, 8:16], ALU.mult, sm.b(), sm.b())
                        self.rsum(sm[:, 27:28], sm[:, 16:24], sm.b(), sm.b())
                        self.recip(sm[:, 27:28], sm[:, 27:28], sm.b(), sm.b())
                        self.ts(gate[:, t, :], sm[:, 16:24], sm[:, 27:28], None, ALU.mult, None, sm.b(), gate.b())
                first = True
                for e in range(nexp):
                    if moe:
                        gbank, gbb = self.ps[6], self.psb[6]
                        for t in range(4):
                            gb = gbr.next()
                            self.copy("dve", gb.r(slice(None)), gate[:, t, e:e + 1].to_broadcast([128, 128]), gate.b(), gb.b())
                            self.mm(gbank[:, t * 128:(t + 1) * 128], gb.r(slice(None)), self.identr.r(slice(None)), True, True,
                                    gb.b() + self.identr.b(), [gbb], sync=True)
                        gbc = gbcr.next()
                        self.copy("act", gbc[:], gbank[:, :], [gbb], gbc.b())
                        wg_d, wu_d, wd_d = self.I("moe_g")[e], self.I("moe_u")[e], self.I("moe_d")[e]
                    else:
                        wg_d, wu_d, wd_d = self.I("ffn_g"), self.I("ffn_u"), self.I("ffn_d")
                    for blk in range(NFF // FB):
                        at = aT[blk % 2]
                        for jj in range(FB):
                            j = blk * FB + jj
                            it += 1
                            wg, sg_ = self.wload(wgu, wg_d[j], 16)
                            gbk, gbkb = self.ps[it % 2], self.psb[it % 2]
                            for k in range(16):
                                self.mm(gbk[:, :], wg[:, k, :], hT.r((slice(None), k, slice(None))), k == 0, k == 15,
                                        sg_.b() + hT.b(k), [gbkb])
                            wu, su_ = self.wload(wgu, wu_d[j], 16)
                            ubk, ubkb = self.ps[2 + it % 2], self.psb[2 + it % 2]
                            for k in range(16):
                                self.mm(ubk[:, :], wu[:, k, :], hT.r((slice(None), k, slice(None))), k == 0, k == 15,
                                        su_.b() + hT.b(k), [ubkb])
                            sgl = sglr.next()
                            self.act(sgl[:], gbk[:, :], AF.Silu, [gbkb], sgl.b())
                            if moe:
                                t4 = t4r.next()
                                self.tt(t4[:], ubk[:, :], gbc[:], ALU.mult, [ubkb] + gbc.b(), t4.b())
                                self.tt(at.r((slice(None), jj, slice(None))), sgl[:], t4[:], ALU.mult, sgl.b() + t4.b(), at.b(jj))
                            else:
                                self.tt(at.r((slice(None), jj, slice(None))), sgl[:], ubk[:, :], ALU.mult, sgl.b() + [ubkb], at.b(jj))
                        for d in range(16):
                            it += 1
                            wd, sd_ = self.wload(wdr, wd_d[blk, d], FB)
                            dbk, dbkb = self.ps[4 + it % 2], self.psb[4 + it % 2]
                            for jj in range(FB):
                                self.mm(dbk[:, :], wd[:, jj, :], at.r((slice(None), jj, slice(None))), jj == 0, jj == FB - 1,
                                        sd_.b() + at.b(jj), [dbkb])
                            if first:
                                self.copy("dve", a3[:, d, :], dbk[:, :], [dbkb], xa.b())
                            else:
                                self.tt(a3[:, d, :], a3[:, d, :], dbk[:, :], ALU.add, xa.b() + [dbkb], xa.b())
                        first = False
                for d2 in range(16):
                    xr = xring.next()
                    fw.dma("sp", xr[:], self.xT[d2, :, gs], reads=[self.b_x[g][d2]], writes=xr.b())
                    self.stt(a3[:, d2, :], a3[:, d2, :], mp[:, 5, d2:d2 + 1, cd], xr[:], ALU.mult, ALU.add,
                             xa.b() + mp.b() + xr.b(), xa.b())
                    if not final:
                        fw.dma("sp", self.xT[d2, :, gs], a3[:, d2, :], reads=xa.b(), writes=[self.b_x[g][d2]])
                if final:
                    self.norm_stats(a3, xa.b(), 16, D, sqring, rstd, self.ps[7], self.psb[7])
                    for d2 in range(16):
                        xr = xring.next()
                        col = R_FG + d2
                        self.stt(xr[:], a3[:, d2, :], self.vT[:, col:col + 1], rstd[:], ALU.mult, ALU.mult,
                                 xa.b() + self.vT.b() + rstd.b(), xr.b())
                        fw.dma("sp", self.o_yT[d2, :, gs], xr[:], reads=xr.b())
            fw.emit()

    def phase_moe_sparse(self, l, final=False, slots=None, groups=None):
        nc, fw = self.nc, self.fw
        I32 = mybir.dt.int32
        mp = self.modp[l]
        NTI = NT // 128
        hTok, Ybuf, Tab = self.hTok, self.Ybuf, self.Tab
        b_hTok, b_Y, b_Tab = Buf(), Buf(), Buf()
        B3 = [128, NTI, 8]

        def tred(out, in_, op, reads, writes):
            fw.op("dve", lambda: nc.vector.tensor_reduce(out=out, in_=in_, axis=AX.X, op=op), reads=reads, writes=writes)

        with contextlib.ExitStack() as es0:
            esc = Tile(nc, es0, "esc", [128, 2 * NS])
            self.epsb = Tile(nc, es0, "epsb5", [128, 1])
            fw.op("dve", lambda: nc.vector.memset(self.epsb[:], EPS), writes=self.epsb.b())
            with contextlib.ExitStack() as es:
                xa = Tile(nc, es, "xa5", [128, 16 * 512])
                hT = Tile(nc, es, "hT5", [128, 16, 512], split=True)
                sqring = Ring(nc, es, "sq5", [128, 512], 2)
                tmpring = Ring(nc, es, "tmp5", [128, 512], 2)
                rstd = Tile(nc, es, "rstd5", [128, 512])
                hst = Ring(nc, es, "hst5", [128, 2048], 2)
                rt = Tile(nc, es, "rt5", [128, 16, 8])
                Um = Tile(nc, es, "Um", [128, 128])
                Lg = Tile(nc, es, "Lg", B3)
                eq1 = Tile(nc, es, "eq1", B3)
                sel = Tile(nc, es, "sel", B3)
                wk = Tile(nc, es, "wk", B3)
                wk2 = Tile(nc, es, "wk2", B3)
                gt = Tile(nc, es, "gt", B3)
                pos = Tile(nc, es, "pos", B3)
                tot = Tile(nc, es, "tot", B3)
                offs = Tile(nc, es, "offs", B3)
                m1 = Tile(nc, es, "m1", [128, NTI, 1])
                m2 = Tile(nc, es, "m2", [128, NTI, 1])
                sm = Tile(nc, es, "sm5", [128, 96])
                tokid = Tile(nc, es, "tokid", [128, NTI])
                svals = Tile(nc, es, "svals", [128, NS])
                rr = Tile(nc, es, "rr", [128, 2 * NTI])
                zero = Tile(nc, es, "zero5", [128, 2048])
                recs_t = es.enter_context(nc.sbuf_tensor("sb_recs", [128, 2 * NTI, 16], I32))
                ridx_t = es.enter_context(nc.sbuf_tensor("sb_ridx", [128, 2 * NTI], I32))
                b_recs, b_ridx = Buf(), Buf()
                fw.dma("pool", rt.r(slice(None)), self.I("router"), writes=rt.b())
                fw.dma("pool", Um.r(slice(None)), self.I("Umat"), writes=Um.b())
                fw.dma("sp", tokid[:], self.I("tokid"), writes=tokid.b())
                fw.dma("sp", svals[:], self.I("svals"), writes=svals.b())
                fw.dma("sp", Tab[:, :], self.I("Tab0", I32), writes=[b_Tab])
                fw.op("dve", lambda: nc.vector.memset(zero[:], 0.0), writes=zero.b())
                fw.op("dve", lambda: nc.vector.memset(recs_t[:], 0), writes=[b_recs])
                fw.dma("sp", hTok[NT:NT + 128, :], zero[:], reads=zero.b(), writes=[b_hTok])
                for g in range(NGRP):
                    self.load_x(g, xa)
                    self.norm_mod(g, l, 1, xa, hT, sqring, tmpring, rstd, self.ps[7], self.psb[7])
                    for t in range(4):
                        lb, lbb = self.ps[6], self.psb[6]
                        for k in range(16):
                            self.mm(lb[:, t * 8:(t + 1) * 8], hT.r((slice(None), k, slice(t * 128, (t + 1) * 128))),
                                    rt.r((slice(None), k, slice(None))), k == 0, k == 15, hT.b(k) + rt.b(), [lbb])
                        self.copy("dve", Lg[:, g * 4 + t, :], lb[:, t * 8:(t + 1) * 8], [lbb], Lg.b())
                        hs = hst.next()
                        for c4 in range(4):
                            tb, tbb = self.ps[c4], self.psb[c4]
                            for ci in range(4):
                                c = c4 * 4 + ci
                                self.tr(tb[:, ci * 128:(ci + 1) * 128], hT[:, c, t * 128:(t + 1) * 128], hT.b(c), [tbb])
                            self.copy(self.evac_eng(), hs[:, c4 * 512:(c4 + 1) * 512], tb[:, :], [tbb], hs.b())
                        r0 = g * 512 + t * 128
                        fw.dma("sp", hTok[r0:r0 + 128, :], hs[:], reads=hs.b(), writes=[b_hTok])
                tred(m1[:], Lg[:], ALU.max, Lg.b(), m1.b())
                self.tt(eq1[:], Lg[:], m1[:].to_broadcast(B3), ALU.is_equal, Lg.b() + m1.b(), eq1.b())
                self.stt(wk[:], eq1[:], -1e30, Lg[:], ALU.mult, ALU.add, eq1.b() + Lg.b(), wk.b())
                tred(m2[:], wk[:], ALU.max, wk.b(), m2.b())
                self.tt(sel.r(slice(None)), Lg[:], m2[:].to_broadcast(B3), ALU.is_ge, Lg.b() + m2.b(), sel.b())
                self.tt(wk[:], Lg[:], m1[:].to_broadcast(B3), ALU.subtract, Lg.b() + m1.b(), wk.b())
                self.act(wk[:], wk[:], AF.Exp, wk.b(), wk.b())
                self.tt(wk[:], wk[:], sel[:], ALU.mult, wk.b() + sel.b(), wk.b())
                tred(m2[:], wk[:], ALU.add, wk.b(), m2.b())
                self.recip(m2[:], m2[:], m2.b(), m2.b())
                self.tt(gt[:], wk[:], m2[:].to_broadcast(B3), ALU.mult, wk.b() + m2.b(), gt.b())
                pb_, pbb_ = self.ps[0], self.psb[0]
                tb_, tbb_ = self.ps[1], self.psb[1]
                for t in range(NTI):
                    self.mm(pb_[:, t * 8:(t + 1) * 8], Um.r(slice(None)), sel.r((slice(None), t, slice(None))), True, True,
                            Um.b() + sel.b(), [pbb_], sync=(t == NTI - 1))
                for t in range(NTI):
                    self.mm(tb_[:, t * 8:(t + 1) * 8], self.ones.r(slice(None)), sel.r((slice(None), t, slice(None))), True, True,
                            self.ones.b() + sel.b(), [tbb_], sync=(t == NTI - 1))
                self.copy("dve", pos.t[:].rearrange("p a e -> p (a e)"), pb_[:, 0:NTI * 8], [pbb_], pos.b())
                self.copy("act", tot.t[:].rearrange("p a e -> p (a e)"), tb_[:, 0:NTI * 8], [tbb_], tot.b())
                fw.op("dve", lambda: nc.vector.memset(offs[:, 0, :], 0.0), writes=offs.b())
                for t in range(1, NTI):
                    self.tt(offs[:, t, :], offs[:, t - 1, :], tot[:, t - 1, :], ALU.add, offs.b() + tot.b(), offs.b())
                self.tt(pos[:], pos[:], offs[:], ALU.add, pos.b() + offs.b(), pos.b())
                cnt, nsl, tmp8 = sm[:, 0:8], sm[:, 8:16], sm[:, 24:32]
                self.tt(cnt, offs[:, NTI - 1, :], tot[:, NTI - 1, :], ALU.add, offs.b() + tot.b(), sm.b())
                self.ts(nsl, cnt, 0.0, None, ALU.is_gt, None, sm.b(), sm.b())
                for kk in range(1, 5):
                    self.ts(tmp8, cnt, float(512 * kk), None, ALU.is_gt, None, sm.b(), sm.b())
                    self.tt(nsl, nsl, tmp8, ALU.add, sm.b(), sm.b())
                fw.op("dve", lambda: nc.vector.memset(sm[:, 16:17], 0.0), writes=sm.b())
                for e in range(1, NEXP):
                    self.tt(sm[:, 16 + e:17 + e], sm[:, 15 + e:16 + e], sm[:, 7 + e:8 + e], ALU.add, sm.b(), sm.b())
                self.ts(sm[:, 32:40], sm[:, 16:24], 512.0, None, ALU.mult, None, sm.b(), sm.b())
                self.tt(pos[:], pos[:], sm[:, 32:40].rearrange("p (a e) -> p a e", a=1).to_broadcast(B3), ALU.add,
                        pos.b() + sm.b(), pos.b())
                self.tt(wk2[:], sel[:], eq1[:], ALU.subtract, sel.b() + eq1.b(), wk2.b())
                rv = rr.t[:].rearrange("p (k a) -> p k a", k=2)
                recs_f = recs_t[:].bitcast(F32)
                for k_, oh in ((0, eq1), (1, wk2)):
                    ks = slice(k_ * NTI, (k_ + 1) * NTI)
                    self.tt(wk[:], pos[:], oh[:], ALU.mult, pos.b() + oh.b(), wk.b())
                    tred(rv[:, k_, :], wk[:], ALU.add, wk.b(), rr.b())
                    fw.op("dve", lambda ks=ks: nc.vector.tensor_copy(out=recs_t[:, ks, 0], in_=tokid[:]),
                          reads=tokid.b(), writes=[b_recs])
                    fw.op("dve", lambda ks=ks, k_=k_: nc.vector.tensor_scalar(out=recs_t[:, ks, 1], in0=tokid[:],
                                                                             scalar1=float(k_ * NT), scalar2=None, op0=ALU.add),
                          reads=tokid.b(), writes=[b_recs])
                    self.tt(wk[:], gt[:], oh[:], ALU.mult, gt.b() + oh.b(), wk.b())
                    tred(recs_f[:, ks, 2], wk[:], ALU.add, wk.b(), [b_recs])
                fw.op("dve", lambda: nc.vector.tensor_copy(out=ridx_t[:], in_=rr[:]), reads=rr.b(), writes=[b_ridx])
                for i in range(2 * NTI):
                    fw.dma_custom("pool", lambda i=i: nc.gpsimd.indirect_dma_start(
                        out=Tab[:, :], out_offset=bass.IndirectOffsetOnAxis(ap=ridx_t[:, i:i + 1], axis=0),
                        in_=recs_t[:, i, :], in_offset=None), reads=[b_recs, b_ridx], writes=[b_Tab])
                ev = sm[:, 40:40 + NS]
                tmpS = sm[:, 60:60 + NS]
                fw.op("dve", lambda: nc.vector.memset(ev, -1.0), writes=sm.b())
                for e in range(NEXP):
                    self.ts(tmpS, svals[:], sm[:, 16 + e:17 + e], None, ALU.is_ge, None, svals.b() + sm.b(), sm.b())
                    self.tt(ev, ev, tmpS, ALU.add, sm.b(), sm.b())
                self.ts(esc[:, 0:NS], ev, float(NFF * 128), None, ALU.mult, None, sm.b(), esc.b())
                self.ts(esc[:, NS:2 * NS], ev, float(64 * 128), None, ALU.mult, None, sm.b(), esc.b())
                fw.emit()
            with contextlib.ExitStack() as es:
                hTs = Tile(nc, es, "hTs", [128, 16, 512], split=True)
                acc = Tile(nc, es, "acc5", [128, 16 * 512])
                aT = [Tile(nc, es, "aT5_%d" % i, [128, FB, 512], split=True) for i in range(2)]
                wgu = Ring(nc, es, "wgu5", [128, 2048], 4)
                wdr = Ring(nc, es, "wdr5", [128, FB * 128], 3)
                sglr = Ring(nc, es, "sgl5", [128, 512], 2)
                gth = Ring(nc, es, "gth", [128, 2048], 2)
                otl = Ring(nc, es, "otl", [128, 2048], 2)
                wbase = Tile(nc, es, "wbase", [128, NFF])
                wdbase = Tile(nc, es, "wdbase", [128, 64])
                widx_t = [es.enter_context(nc.sbuf_tensor("sb_widx%d" % i, [128, NFF + 64], I32)) for i in range(2)]
                b_widx = [Buf(), Buf()]
                rbs_t = [es.enter_context(nc.sbuf_tensor("sb_rbs%d" % i, [128, 4, 16], I32)) for i in range(2)]
                b_rbs = [Buf(), Buf()]
                fw.dma("sp", wbase[:], self.I("wbase"), writes=wbase.b())
                fw.dma("sp", wdbase[:], self.I("wdbase"), writes=wdbase.b())
                a3 = acc.t[:].rearrange("p (c n) -> p c n", c=16)
                wg_rows = self.I("moe_g").rearrange("e j p k m -> (e j p) (k m)")
                wu_rows = self.I("moe_u").rearrange("e j p k m -> (e j p) (k m)")
                wd_rows = self.I("moe_d").rearrange("e b d p j m -> (e b d p) (j m)")
                it = 0
                for s in (slots if slots is not None else range(NS)):
                    widx, bw = widx_t[s % 2], b_widx[s % 2]
                    rbs, brb = rbs_t[s % 2], b_rbs[s % 2]
                    fw.op("dve", lambda widx=widx, s=s: nc.vector.tensor_scalar(
                        out=widx[:, 0:NFF], in0=wbase[:], scalar1=esc[:, s:s + 1], scalar2=None, op0=ALU.add),
                        reads=wbase.b() + esc.b(), writes=[bw])
                    fw.op("dve", lambda widx=widx, s=s: nc.vector.tensor_scalar(
                        out=widx[:, NFF:NFF + 64], in0=wdbase[:], scalar1=esc[:, NS + s:NS + s + 1], scalar2=None, op0=ALU.add),
                        reads=wdbase.b() + esc.b(), writes=[bw])
                    fw.dma("sp", rbs[:], Tab[s * 512:(s + 1) * 512, :].rearrange("(q p) c -> p q c", p=128),
                           reads=[b_Tab], writes=[brb])
                    for q in range(4):
                        gtile = gth.next()
                        fw.dma_custom("pool", lambda gtile=gtile, rbs=rbs, q=q: nc.gpsimd.indirect_dma_start(
                            out=gtile[:], out_offset=None, in_=hTok[:, :],
                            in_offset=bass.IndirectOffsetOnAxis(ap=rbs[:, q, 0:1], axis=0)),
                            reads=[brb, b_hTok], writes=gtile.b())
                        for c4 in range(4):
                            tb, tbb = self.ps[6 + c4 % 2], self.psb[6 + c4 % 2]
                            for ci in range(4):
                                c = c4 * 4 + ci
                                self.tr(tb[:, ci * 128:(ci + 1) * 128], gtile[:, c * 128:(c + 1) * 128], gtile.b(), [tbb])
                            self.copy(self.evac_eng(), hTs.r((slice(None), slice(c4 * 4, c4 * 4 + 4), slice(q * 128, (q + 1) * 128))),
                                      tb[:, :].rearrange("p (a n) -> p a n", a=4), [tbb],
                                      hTs.b(c4 * 4) + hTs.b(c4 * 4 + 1) + hTs.b(c4 * 4 + 2) + hTs.b(c4 * 4 + 3))

                    def wgather(ring, rows, col, nelem, widx=widx, bw=bw):
                        slot = ring.next()
                        dst = slot.t[:, 0:nelem].bitcast(F32R)
                        fw.dma_custom("pool", lambda: nc.gpsimd.indirect_dma_start(
                            out=dst, out_offset=None, in_=rows,
                            in_offset=bass.IndirectOffsetOnAxis(ap=widx[:, col:col + 1], axis=0)),
                            reads=[bw], writes=slot.b())
                        return slot
                    for blk in range(NFF // FB):
                        at = aT[blk % 2]
                        for jj in range(FB):
                            j = blk * FB + jj
                            it += 1
                            sg_ = wgather(wgu, wg_rows, j, 2048)
                            wg = sg_.t[:].rearrange("p (k m) -> p k m", k=16).bitcast(F32R)
                            gbk, gbkb = self.ps[it % 2], self.psb[it % 2]
                            for k in range(16):
                                self.mm(gbk[:, :], wg[:, k, :], hTs.r((slice(None), k, slice(None))), k == 0, k == 15,
                                        sg_.b() + hTs.b(k), [gbkb])
                            su_ = wgather(wgu, wu_rows, j, 2048)
                            wu = su_.t[:].rearrange("p (k m) -> p k m", k=16).bitcast(F32R)
                            ubk, ubkb = self.ps[2 + it % 2], self.psb[2 + it % 2]
                            for k in range(16):
                                self.mm(ubk[:, :], wu[:, k, :], hTs.r((slice(None), k, slice(None))), k == 0, k == 15,
                                        su_.b() + hTs.b(k), [ubkb])
                            sgl = sglr.next()
                            self.act(sgl[:], gbk[:, :], AF.Silu, [gbkb], sgl.b())
                            self.tt(at.r((slice(None), jj, slice(None))), sgl[:], ubk[:, :], ALU.mult, sgl.b() + [ubkb], at.b(jj))
                        for d in range(16):
                            it += 1
                            sd_ = wgather(wdr, wd_rows, NFF + blk * 16 + d, FB * 128)
                            wd = sd_.t[:].rearrange("p (k m) -> p k m", k=FB).bitcast(F32R)
                            dbk, dbkb = self.ps[4 + it % 2], self.psb[4 + it % 2]
                            for jj in range(FB):
                                self.mm(dbk[:, :], wd[:, jj, :], at.r((slice(None), jj, slice(None))), jj == 0, jj == FB - 1,
                                        sd_.b() + at.b(jj), [dbkb])
                            if blk == 0:
                                self.copy("dve", a3[:, d, :], dbk[:, :], [dbkb], acc.b())
                            else:
                                self.tt(a3[:, d, :], a3[:, d, :], dbk[:, :], ALU.add, acc.b() + [dbkb], acc.b())
                    rbs_f = rbs[:].bitcast(F32)
                    for q in range(4):
                        ot = otl.next()
                        for c4 in range(4):
                            tb, tbb = self.ps[6 + c4 % 2], self.psb[6 + c4 % 2]
                            for ci in range(4):
                                c = c4 * 4 + ci
                                self.tr(tb[:, ci * 128:(ci + 1) * 128], a3[:, c, q * 128:(q + 1) * 128], acc.b(), [tbb])
                            self.ts(ot[:, c4 * 512:(c4 + 1) * 512], tb[:, :], rbs_f[:, q, 2:3], None, ALU.mult, None,
                                    [tbb, brb], ot.b())
                        fw.dma_custom("pool", lambda ot=ot, rbs=rbs, q=q: nc.gpsimd.indirect_dma_start(
                            out=Ybuf[:, :], out_offset=bass.IndirectOffsetOnAxis(ap=rbs[:, q, 1:2], axis=0),
                            in_=ot[:], in_offset=None), reads=ot.b() + [brb], writes=[b_Y])
                fw.emit()
            with contextlib.ExitStack() as es:
                ys = Ring(nc, es, "ys", [128, 2048], 4)
                y2 = Ring(nc, es, "y2", [128, 2048], 2)
                xg = Tile(nc, es, "xg6", [128, 16 * 512])
                sqring = Ring(nc, es, "sq6", [128, 512], 2)
                rstd = Tile(nc, es, "rstd6", [128, 512])
                xring = Ring(nc, es, "xr6", [128, 512], 3)
                x3 = xg.t[:].rearrange("p (c n) -> p c n", c=16)
                for g in (groups or range(NGRP)):
                    cd = 0 if g < 4 else 1
                    gs = slice(g * G, (g + 1) * G)
                    self.load_x(g, xg)
                    yt = []
                    for t in range(4):
                        r0 = g * 512 + t * 128
                        a, b2 = ys.next(), y2.next()
                        fw.dma("sp", a[:], Ybuf[r0:r0 + 128, :], reads=[b_Y], writes=a.b())
                        fw.dma("sp", b2[:], Ybuf[NT + r0:NT + r0 + 128, :], reads=[b_Y], writes=b2.b())
                        self.tt(a[:], a[:], b2[:], ALU.add, a.b() + b2.b(), a.b(), en="pool" if t % 2 else "dve")
                        yt.append(a)
                    for c in range(16):
                        tb, tbb = self.ps[c % 4], self.psb[c % 4]
                        for t in range(4):
                            self.tr(tb[:, t * 128:(t + 1) * 128], yt[t][:, c * 128:(c + 1) * 128], yt[t].b(), [tbb])
                        self.stt(x3[:, c, :], tb[:, :], mp[:, 5, c:c + 1, cd], x3[:, c, :], ALU.mult, ALU.add,
                                 [tbb] + mp.b() + xg.b(), xg.b())
                        if not final:
                            fw.dma("sp", self.xT[c, :, gs], x3[:, c, :], reads=xg.b(), writes=[self.b_x[g][c]])
                    if final:
                        self.norm_stats(x3, xg.b(), 16, D, sqring, rstd, self.ps[7], self.psb[7])
                        for d2 in range(16):
                            xr = xring.next()
                            col = R_FG + d2
                            self.stt(xr[:], x3[:, d2, :], self.vT[:, col:col + 1], rstd[:], ALU.mult, ALU.mult,
                                     xg.b() + self.vT.b() + rstd.b(), xr.b())
                            fw.dma("sp", self.o_yT[d2, :, gs], xr[:], reads=xr.b())
                fw.emit()


def _tiles(W):
    K, M = W.shape
    return np.ascontiguousarray(W.reshape(K // 128, 128, M // 128, 128).transpose(2, 1, 0, 3))


def host_consts():
    c = {}
    c["ident"] = np.eye(128, dtype=np.float32)
    p = np.arange(128)
    d = p % 64
    partner = np.where((d % 32) < 16, p + 16, p - 16)
    pm = np.zeros((128, 128), np.float32)
    pm[partner, p] = 1.0
    c["permM"] = pm
    quarter = 16
    inv = (np.float32(10000.0) ** (-np.arange(quarter, dtype=np.float32) / np.float32(quarter))).astype(np.float32)
    n = np.arange(2048)
    rr = (n // 64).astype(np.float32)
    cc = (n % 64).astype(np.float32)
    ang_r = (rr[:, None] * inv[None, :]).astype(np.float32)
    ang_c = (cc[:, None] * inv[None, :]).astype(np.float32)
    C = np.zeros((128, 2048), np.float32)
    S = np.zeros((128, 2048), np.float32)
    for pp in range(128):
        dd = pp % 64
        j = dd % 16
        ang = ang_r[:, j] if dd < 32 else ang_c[:, j]
        C[pp] = np.cos(ang)
        sgn = -1.0 if (dd % 32) < 16 else 1.0
        S[pp] = sgn * np.sin(ang)
    c["ropeC"] = C
    c["ropeS"] = S
    r = np.arange(128)[:, None]
    cidx = np.arange(128)[None, :]
    m = np.zeros((128, 384), np.float32)
    m[:, 0:128] = np.where(cidx >= r, 0.0, -1e30)
    m[:, 256:384] = np.where(cidx <= r, 0.0, -1e30)
    c["maskA"] = m
    c["Umat"] = np.triu(np.ones((128, 128), np.float32), k=1)
    pp = np.arange(128, dtype=np.float32)[:, None]
    c["tokid"] = (np.arange(NT // 128, dtype=np.float32)[None, :] * 128 + pp).astype(np.float32)
    c["svals"] = np.tile(np.arange(NS, dtype=np.float32)[None, :], (128, 1))
    c["wbase"] = (np.arange(NFF, dtype=np.float32)[None, :] * 128 + pp).astype(np.float32)
    c["wdbase"] = (np.arange(64, dtype=np.float32)[None, :] * 128 + pp).astype(np.float32)
    tab0 = np.zeros((NS * 512, 16), np.int32)
    tab0[:, 0] = NT
    tab0[:, 1] = 2 * NT + (np.arange(NS * 512) % 128)
    c["Tab0"] = tab0
    for nm, nseq in (("invc_s", 2048), ("invc_p", 256)):
        t = np.arange(nseq)
        tab = np.zeros((4, nseq), np.float32)
        for gi, win in enumerate(POOL_WINDOWS):
            left = win // 2
            right = win - left - 1
            lo = np.maximum(t - left, 0)
            hi = np.minimum(t + right, nseq - 1) + 1
            tab[gi] = (1.0 / (hi - lo).astype(np.float32)).astype(np.float32)
        c[nm] = tab
    return c


def prep_shared(inp):
    sh = dict(host_consts())
    w_in = inp["w_in"]
    colsA = np.concatenate([
        np.arange(0, 1024),
        np.arange(1024, 1088), np.arange(1024, 1088),
        np.arange(1088, 1152), np.arange(1088, 1152),
        np.arange(1152, 1280),
        np.arange(1280, 1792),
        np.arange(1792, 2048),
        np.arange(2048, 2112), np.arange(2048, 2112),
        np.arange(2112, 3136)])
    sh["w_ada"] = np.stack([_tiles(inp["w_ada"][l]) for l in range(DEPTH)])
    sh["w_inA"] = np.stack([_tiles(w_in[l][:, colsA]) for l in range(DEPTH)])
    sh["w_inG"] = np.stack([_tiles(w_in[l][:, 3136:]) for l in range(DEPTH)])
    cq = [np.arange(h * 192, h * 192 + 128) for h in range(8)]
    for j in range(4):
        cq.append(np.concatenate([np.arange((2 * j) * 192 + 128, (2 * j) * 192 + 192),
                                  np.arange((2 * j + 1) * 192 + 128, (2 * j + 1) * 192 + 192)]))
    cq = np.concatenate(cq)
    sh["w_uq"] = np.stack([_tiles(inp["w_uq"][l][:, cq]) for l in range(DEPTH)])
    ckv = np.concatenate([np.arange(h * 256, h * 256 + 128) for h in range(8)] +
                         [np.arange(h * 256 + 128, h * 256 + 256) for h in range(8)])
    sh["w_ukv"] = np.stack([_tiles(inp["w_ukv"][l][:, ckv]) for l in range(DEPTH)])
    sh["poolw"] = np.stack([np.concatenate([_tiles(inp["pool_w"][l][g]) for g in range(4)]) for l in range(DEPTH)])
    sh["wbr"] = np.stack([np.stack([_tiles(inp[k][l]) for k in ("w_branch_a", "w_branch_b", "w_branch_c")])
                          for l in range(DEPTH)])
    sh["wout"] = np.stack([_tiles(inp["w_out"][l]) for l in range(DEPTH)])
    sh["ffn_g"] = _tiles(inp["ffn_w_gate"][0])
    sh["ffn_u"] = _tiles(inp["ffn_w_up"][0])

    def dtiles(wd):
        return np.ascontiguousarray(wd.reshape(4, FB, 128, 16, 128).transpose(0, 3, 2, 1, 4))
    sh["ffn_d"] = dtiles(inp["ffn_w_down"][0])
    sh["router"] = np.ascontiguousarray(inp["router_w"][0].reshape(16, 128, 8).transpose(1, 0, 2))
    sh["moe_g"] = np.stack([_tiles(inp["moe_w_gate"][0][e]) for e in range(NEXP)])
    sh["moe_u"] = np.stack([_tiles(inp["moe_w_up"][0][e]) for e in range(NEXP)])
    sh["moe_d"] = np.stack([dtiles(inp["moe_w_down"][0][e]) for e in range(NEXP)])
    sh["sink"] = np.ascontiguousarray(inp["attn_sink"])
    return sh


def prep_core(inp, i):
    m = {}
    x = np.concatenate([inp["x_sample"][i], inp["x_prompt"][2 * i], inp["x_prompt"][2 * i + 1]], axis=0)
    m["xinT"] = np.ascontiguousarray(x.T.reshape(16, 128, NT))
    v = np.zeros((NROWS, 128), np.float32)
    v[R_C:R_C + 16] = inp["c"][i].reshape(16, 128)
    v[R_CCTX:R_CCTX + 16] = inp["c_ctx"].reshape(16, 128)
    for l in range(DEPTH):
        v[R_LN1(l):R_LN1(l) + 16] = inp["ln1_g"][l].reshape(16, 128)
        v[R_LN2(l):R_LN2(l) + 16] = inp["ln2_g"][l].reshape(16, 128)
        v[R_BADA(l):R_BADA(l) + 96] = inp["b_ada"][l].reshape(96, 128)
        v[R_QG(l):R_QG(l) + 4] = inp["mla_q_norm_g"][l].reshape(4, 128)
        v[R_KVG(l):R_KVG(l) + 2] = inp["mla_kv_norm_g"][l].reshape(2, 128)
        v[R_PSC(l):R_PSC(l) + 8] = inp["pool_scale"][l].reshape(8, 128)
    v[R_FG:R_FG + 16] = inp["final_g"].reshape(16, 128)
    m["vecs"] = v
    ck = inp["cache_attn_k"][i]
    ckT = ck.transpose(0, 2, 3, 1)
    m["ck2T"] = np.ascontiguousarray(np.concatenate([ckT, ckT], axis=2))
    m["cv"] = np.ascontiguousarray(inp["cache_attn_v"][i].reshape(DEPTH, 512, 128))
    m["cckvT"] = np.ascontiguousarray(inp["cache_mla_ckv"][i].transpose(0, 2, 1).reshape(DEPTH, 2, 128, 512))
    krT = inp["cache_mla_krope"][i].transpose(0, 2, 1)
    m["ckr2T"] = np.ascontiguousarray(np.concatenate([krT, krT], axis=1))
    return m


def build_program():
    P = Prog()
    P.alloc_persistent()
    P.phase_prologue()
    for l in range(DEPTH):
        P.phase_proj(l)
        P.phase_attnA(l)
        P.phase_mla(l)
        P.phase_pool(l)
        P.phase_merge(l)
        if l % 2 == 1:
            P.phase_moe_sparse(l, final=(l == DEPTH - 1))
        else:
            P.phase_ffn(l, final=(l == DEPTH - 1))
    return P


def kernel(**inputs):
    inp = {k: np.asarray(v, dtype=np.float32) for k, v in inputs.items()}
    P = build_program()
    sh = prep_shared(inp)
    in_maps = []
    for i in range(NCORES):
        m = prep_core(inp, i)
        m.update(sh)
        in_maps.append({k: m[k] for k in P.din})
    res = run_bass_kernel_spmd(P.nc, in_maps, core_ids=list(range(NCORES)))
    y_prompt = np.zeros((16, 256, D), np.float32)
    y_sample = np.zeros((8, 2048, D), np.float32)
    st_k = np.zeros((16, DEPTH, 256, 2, 64), np.float32)
    st_v = np.zeros((16, DEPTH, 256, 2, 64), np.float32)
    st_ckv = np.zeros((16, DEPTH, 256, 256), np.float32)
    st_kr = np.zeros((16, DEPTH, 256, 64), np.float32)
    for i in range(NCORES):
        r = res.results[i]
        y = np.asarray(r["o_yT"]).reshape(D, NT).T
        y_sample[i] = y[0:2048]
        okT = np.asarray(r["o_kT"])
        ov = np.asarray(r["o_v"]).reshape(DEPTH, 512, 128)
        ockv = np.asarray(r["o_ckvT"]).reshape(DEPTH, 256, 512)
        okr = np.asarray(r["o_krT"])
        for j in range(2):
            b = 2 * i + j
            ts = slice(j * 256, (j + 1) * 256)
            y_prompt[b] = y[2048 + j * 256:2048 + (j + 1) * 256]
            st_k[b] = okT[:, :, :, ts].transpose(0, 3, 1, 2)
            st_v[b] = ov[:, ts, :].reshape(DEPTH, 256, 2, 64)
            st_ckv[b] = ockv[:, :, ts].transpose(0, 2, 1)
            st_kr[b] = okr[:, :, ts].transpose(0, 2, 1)
    return (y_prompt, y_sample, st_k, st_v, st_ckv, st_kr)
```
